# Optimizing a Trainium2 kernel written in Bass

```python
import math
import jax, jax.numpy as jnp
from jax import lax
import numpy as np

D_MODEL = 1024
BATCH = 4
SEQ = 8192
DEPTH = 2

GRID_W = 64
HEAD_DIM = 64
NA_HEADS = 6
NA_WIN_H = 8
NA_WIN_W = 16
NA_W = NA_HEADS * HEAD_DIM
GQA_HEADS = 6
GQA_KV_HEADS = 2
GQA_QW = GQA_HEADS * HEAD_DIM
GQA_KVW = GQA_KV_HEADS * HEAD_DIM
MLA_HEADS = 4
MLA_Q_RANK = 384
MLA_KV_RANK = 256
MLA_NOPE = 64
MLA_ROPE = 32
MLA_V = 64
MLA_QK = MLA_NOPE + MLA_ROPE
MLA_W = MLA_HEADS * MLA_V
ROPE_THETA = 10000.0
Q_BLOCK = 128
N_BRANCH = 3
N_EXPERTS = 32
TOP_K = 4
D_EXPERT = 1024
SWIGLU_LIMIT = 7.0
SWIGLU_ALPHA = 1.702
MOE_BLOCK = 512
LN_EPS = 1e-5
RMS_EPS = 1e-6
DN_ALPHA = (2.0 * DEPTH) ** 0.25
DN_BETA = (8.0 * DEPTH) ** -0.25
NEG_INF = -1e30

SPLIT_WIDTHS = [NA_W, NA_W, NA_W, GQA_QW, GQA_KVW, GQA_KVW,
                MLA_Q_RANK, MLA_KV_RANK, MLA_ROPE, N_BRANCH * D_MODEL]
SPLIT_POINTS = [int(v) for v in np.cumsum(SPLIT_WIDTHS)[:-1]]
D_IN = int(sum(SPLIT_WIDTHS))

kernel_name = "hybrid_na_gqa_mla_moe_encoder"


def layer_norm(x, g, b):
    xf = x.astype(jnp.float32)
    mu = jnp.mean(xf, -1, keepdims=True)
    var = jnp.mean(jnp.square(xf - mu), -1, keepdims=True)
    y = (xf - mu) * lax.rsqrt(var + LN_EPS)
    return (y * g.astype(jnp.float32) + b.astype(jnp.float32)).astype(x.dtype)


def rms_norm(x, g):
    xf = x.astype(jnp.float32)
    y = xf * lax.rsqrt(jnp.mean(xf * xf, -1, keepdims=True) + RMS_EPS)
    return (y * g.astype(jnp.float32)).astype(x.dtype)


def axial_rope(seq_len, dim):
    quarter = dim // 4
    inv = ROPE_THETA ** (-jnp.arange(quarter, dtype=jnp.float32) / quarter)
    t = jnp.arange(seq_len)
    row = (t // GRID_W).astype(jnp.float32)
    col = (t % GRID_W).astype(jnp.float32)
    ang = jnp.concatenate([row[:, None] * inv, col[:, None] * inv], -1)
    return jnp.cos(ang), jnp.sin(ang)


def apply_rope(x, cos, sin):
    half = x.shape[-1] // 2
    xf = x.astype(jnp.float32)
    x1, x2 = xf[..., :half], xf[..., half:]
    c, s = cos[:, None, :], sin[:, None, :]
    return jnp.concatenate([x1 * c - x2 * s, x1 * s + x2 * c], -1).astype(x.dtype)


def blocked_attention(q, k, v, scale):
    B, S, H, dq = q.shape
    G = k.shape[2]
    rep = H // G
    nb = S // Q_BLOCK
    qb = jnp.moveaxis(q.reshape(B, nb, Q_BLOCK, G, rep, dq), 1, 0)

    def one_block(qblk):
        s = jnp.einsum('bqgrd,bkgd->bgrqk', qblk, k,
                       preferred_element_type=jnp.float32) * scale
        p = jax.nn.softmax(s, axis=-1)
        return jnp.einsum('bgrqk,bkgd->bqgrd', p.astype(v.dtype), v)

    out = lax.map(one_block, qb)
    return jnp.moveaxis(out, 0, 1).reshape(B, S, H, v.shape[-1])


def neighbourhood_attention(q, k, v, rpb):
    B, S, H, d = q.shape
    R = S // GRID_W
    kh = min(NA_WIN_H, R)
    kw = NA_WIN_W
    rows = jnp.arange(R)
    cols = jnp.arange(GRID_W)
    r0 = jnp.clip(rows - kh // 2, 0, R - kh)
    c0 = jnp.clip(cols - kw // 2, 0, GRID_W - kw)
    row_idx = r0[:, None] + jnp.arange(kh)[None, :]
    qg = q.reshape(B, R, GRID_W, H, d)
    kg = k.reshape(B, R, GRID_W, H, d)[:, row_idx]
    vg = v.reshape(B, R, GRID_W, H, d)[:, row_idx]
    s = jnp.einsum('brqhd,brikhd->brhqik', qg, kg,
                   preferred_element_type=jnp.float32) * (d ** -0.5)
    idx_r = row_idx - rows[:, None] + (NA_WIN_H - 1)
    dc = cols[None, :] - cols[:, None]
    idx_c = jnp.clip(dc + (NA_WIN_W - 1), 0, 2 * NA_WIN_W - 2)
    bias = rpb[:, idx_r][..., idx_c]
    bias = bias.transpose(1, 0, 3, 2, 4).astype(jnp.float32)
    in_win = (cols[None, :] >= c0[:, None]) & (cols[None, :] < c0[:, None] + kw)
    s = jnp.where(in_win[:, None, :], s + bias[None], NEG_INF)
    p = jax.nn.softmax(s.reshape(B, R, H, GRID_W, kh * GRID_W), axis=-1).reshape(s.shape)
    out = jnp.einsum('brhqik,brikhd->brqhd', p.astype(v.dtype), vg)
    return out.reshape(B, S, H, d)


def moe_ffn(x, w_router, b_router, w_gate_up, b_gate_up, w_down, b_down):
    B, S, D = x.shape
    T = B * S
    TK = T * TOP_K
    xt = x.reshape(T, D)
    logits = jnp.dot(xt, w_router, preferred_element_type=jnp.float32) + b_router.astype(jnp.float32)
    top_val, top_idx = lax.top_k(logits, TOP_K)
    gate = jax.nn.softmax(top_val, axis=-1)
    flat_e = top_idx.reshape(-1).astype(jnp.int32)
    order = jnp.argsort(flat_e)
    e_sorted = flat_e[order]
    counts = jnp.bincount(flat_e, length=N_EXPERTS).astype(jnp.int32)
    padded = ((counts + MOE_BLOCK - 1) // MOE_BLOCK) * MOE_BLOCK
    cum_padded = jnp.cumsum(padded)
    starts_sorted = jnp.cumsum(counts) - counts
    starts_padded = cum_padded - padded
    rank = jnp.arange(TK, dtype=jnp.int32) - starts_sorted[e_sorted]
    dest = starts_padded[e_sorted] + rank
    n_blocks = TK // MOE_BLOCK + N_EXPERTS
    P = n_blocks * MOE_BLOCK
    slot_token = jnp.full((P,), T, jnp.int32).at[dest].set((order // TOP_K).astype(jnp.int32))
    slot_gate = jnp.zeros((P,), jnp.float32).at[dest].set(gate.reshape(-1)[order])
    block_start = jnp.arange(n_blocks, dtype=jnp.int32) * MOE_BLOCK
    block_expert = jnp.minimum(jnp.searchsorted(cum_padded, block_start, side='right'),
                               N_EXPERTS - 1).astype(jnp.int32)
    xpad = jnp.concatenate([xt, jnp.zeros((1, D), xt.dtype)], 0)
    xb = xpad[slot_token].reshape(n_blocks, MOE_BLOCK, D)

    def expert_block(args):
        xblk, e = args
        h = jnp.dot(xblk, w_gate_up[e]) + b_gate_up[e]
        g, u = h[:, :D_EXPERT], h[:, D_EXPERT:]
        g = jnp.minimum(g, SWIGLU_LIMIT)
        u = jnp.clip(u, -SWIGLU_LIMIT, SWIGLU_LIMIT)
        a = g * jax.nn.sigmoid(SWIGLU_ALPHA * g) * (u + 1.0)
        return jnp.dot(a, w_down[e]) + b_down[e]

    yb = lax.map(expert_block, (xb, block_expert))
    y = yb.reshape(P, D).astype(jnp.float32) * slot_gate[:, None]
    out = jnp.zeros((T + 1, D), jnp.float32).at[slot_token].add(y)[:T]
    return out.reshape(B, S, D).astype(x.dtype)


def hybrid_layer(x, cos64, sin64, cos32, sin32, w_in, na_rpb, gqa_q_norm, gqa_k_norm,
                 mla_q_norm, mla_kv_norm, w_uq, w_ukv, w_branch_a, w_branch_b, w_branch_c,
                 w_out, ln1_g, ln1_b, w_router, b_router, w_gate_up, b_gate_up,
                 w_down, b_down, ln2_g, ln2_b):
    B, S, D = x.shape
    h = x @ w_in
    (na_q, na_k, na_v, g_q, g_k, g_v, c_q, c_kv, k_rope, gates) = jnp.split(h, SPLIT_POINTS, axis=-1)

    hd = (B, S, NA_HEADS, HEAD_DIM)
    y_a = neighbourhood_attention(na_q.reshape(hd), na_k.reshape(hd), na_v.reshape(hd), na_rpb)
    y_a = y_a.reshape(B, S, NA_W) @ w_branch_a

    q_b = rms_norm(g_q.reshape(B, S, GQA_HEADS, HEAD_DIM), gqa_q_norm)
    k_b = rms_norm(g_k.reshape(B, S, GQA_KV_HEADS, HEAD_DIM), gqa_k_norm)
    q_b = apply_rope(q_b, cos64, sin64)
    k_b = apply_rope(k_b, cos64, sin64)
    v_b = g_v.reshape(B, S, GQA_KV_HEADS, HEAD_DIM)
    y_b = blocked_attention(q_b, k_b, v_b, HEAD_DIM ** -0.5)
    y_b = y_b.reshape(B, S, GQA_QW) @ w_branch_b

    q_c = (rms_norm(c_q, mla_q_norm) @ w_uq).reshape(B, S, MLA_HEADS, MLA_QK)
    q_nope, q_rope = q_c[..., :MLA_NOPE], apply_rope(q_c[..., MLA_NOPE:], cos32, sin32)
    kv_c = (rms_norm(c_kv, mla_kv_norm) @ w_ukv).reshape(B, S, MLA_HEADS, MLA_NOPE + MLA_V)
    k_nope, v_c = kv_c[..., :MLA_NOPE], kv_c[..., MLA_NOPE:]
    k_r = apply_rope(k_rope.reshape(B, S, 1, MLA_ROPE), cos32, sin32)
    q_full = jnp.concatenate([q_nope, q_rope], -1)
    k_full = jnp.concatenate([k_nope, jnp.broadcast_to(k_r, (B, S, MLA_HEADS, MLA_ROPE))], -1)
    y_c = blocked_attention(q_full, k_full, v_c, MLA_QK ** -0.5)
    y_c = y_c.reshape(B, S, MLA_W) @ w_branch_c

    g = jax.nn.sigmoid(gates.astype(jnp.float32)).reshape(B, S, N_BRANCH, D).astype(x.dtype)
    mixed = g[:, :, 0] * y_a + g[:, :, 1] * y_b + g[:, :, 2] * y_c
    x = layer_norm(DN_ALPHA * x + mixed @ w_out, ln1_g, ln1_b)

    f = moe_ffn(x, w_router, b_router, w_gate_up, b_gate_up, w_down, b_down)
    return layer_norm(DN_ALPHA * x + f, ln2_g, ln2_b)


def setup_inputs(seed: int = 0) -> dict:
    key = jax.random.key(seed)
    ks = jax.random.split(key, 28)
    L, D, E, F = DEPTH, D_MODEL, N_EXPERTS, D_EXPERT

    def nrm(k, shape, scale):
        return jax.random.normal(k, shape, jnp.float32) * scale

    return {
        "x": nrm(ks[0], (BATCH, SEQ, D), 1.0),
        "w_in": nrm(ks[1], (L, D, D_IN), D ** -0.5),
        "na_rpb": nrm(ks[2], (L, NA_HEADS, 2 * NA_WIN_H - 1, 2 * NA_WIN_W - 1), 0.1),
        "gqa_q_norm": 1.0 + nrm(ks[3], (L, HEAD_DIM), 0.02),
        "gqa_k_norm": 1.0 + nrm(ks[4], (L, HEAD_DIM), 0.02),
        "mla_q_norm": 1.0 + nrm(ks[5], (L, MLA_Q_RANK), 0.02),
        "mla_kv_norm": 1.0 + nrm(ks[6], (L, MLA_KV_RANK), 0.02),
        "w_uq": nrm(ks[7], (L, MLA_Q_RANK, MLA_HEADS * MLA_QK), MLA_Q_RANK ** -0.5),
        "w_ukv": nrm(ks[8], (L, MLA_KV_RANK, MLA_HEADS * (MLA_NOPE + MLA_V)), MLA_KV_RANK ** -0.5),
        "w_branch_a": nrm(ks[9], (L, NA_W, D), NA_W ** -0.5),
        "w_branch_b": nrm(ks[10], (L, GQA_QW, D), GQA_QW ** -0.5),
        "w_branch_c": nrm(ks[11], (L, MLA_W, D), MLA_W ** -0.5),
        "w_out": nrm(ks[12], (L, D, D), DN_BETA * D ** -0.5),
        "ln1_g": 1.0 + nrm(ks[13], (L, D), 0.02),
        "ln1_b": nrm(ks[14], (L, D), 0.02),
        "w_router": nrm(ks[15], (L, D, E), D ** -0.5),
        "b_router": nrm(ks[16], (L, E), 0.01),
        "w_gate_up": nrm(ks[17], (L, E, D, 2 * F), D ** -0.5),
        "b_gate_up": nrm(ks[18], (L, E, 2 * F), 0.01),
        "w_down": nrm(ks[19], (L, E, F, D), DN_BETA * F ** -0.5),
        "b_down": nrm(ks[20], (L, E, D), 0.01),
        "ln2_g": 1.0 + nrm(ks[21], (L, D), 0.02),
        "ln2_b": nrm(ks[22], (L, D), 0.02),
    }


def reference(x, w_in, na_rpb, gqa_q_norm, gqa_k_norm, mla_q_norm, mla_kv_norm, w_uq, w_ukv,
              w_branch_a, w_branch_b, w_branch_c, w_out, ln1_g, ln1_b, w_router, b_router,
              w_gate_up, b_gate_up, w_down, b_down, ln2_g, ln2_b):
    S = x.shape[1]
    cos64, sin64 = axial_rope(S, HEAD_DIM)
    cos32, sin32 = axial_rope(S, MLA_ROPE)
    for l in range(DEPTH):
        x = hybrid_layer(x, cos64, sin64, cos32, sin32, w_in[l], na_rpb[l], gqa_q_norm[l],
                         gqa_k_norm[l], mla_q_norm[l], mla_kv_norm[l], w_uq[l], w_ukv[l],
                         w_branch_a[l], w_branch_b[l], w_branch_c[l], w_out[l], ln1_g[l],
                         ln1_b[l], w_router[l], b_router[l], w_gate_up[l], b_gate_up[l],
                         w_down[l], b_down[l], ln2_g[l], ln2_b[l])
    return x
```

```python
from contextlib import ExitStack
import numpy as np
import ml_dtypes
import concourse.bass as bass
import concourse.mybir as mybir
from concourse.bass_utils import run_bass_kernel_spmd

F32 = mybir.dt.float32
BF16 = mybir.dt.bfloat16
I32 = mybir.dt.int32
U32 = mybir.dt.uint32
AF = mybir.ActivationFunctionType
ALU = mybir.AluOpType
AX = mybir.AxisListType

D = 1024
S = 8192
HALF = 4096
DIN = 5536
NEXP = 32
DEXP = 1024
LN_EPS = 1e-5
RMS_EPS = 1e-6
DN_ALPHA = 4.0 ** 0.25
NEG = -30000.0
NA_ROWS = 72

COMPUTE = ("pe", "act", "dve", "pool")


class Prog:
    def __init__(self, nc, stack, ring=8):
        self.nc = nc
        self.e = {"pe": nc.tensor, "act": nc.scalar, "dve": nc.vector,
                  "pool": nc.gpsimd, "sp": nc.sync}
        self.ops = []
        self.done = 0
        self.sem = {k: stack.enter_context(nc.semaphore("s_" + k)) for k in COMPUTE}
        self.ticket = {k: 0 for k in COMPUTE}
        self.ring = {q: [stack.enter_context(nc.semaphore("d_%s%d" % (q, i))) for i in range(ring)]
                     for q in ("sp", "pool")}
        self.ring_cnt = {q: [0] * ring for q in ("sp", "pool")}
        self.ring_last = {q: [None] * ring for q in ("sp", "pool")}
        self.ring_pos = {q: 0 for q in ("sp", "pool")}
        self.waited = {k: {} for k in self.e}
        self.last_w = {}
        self.readers = {}
        self.sig = {}
        self.eidx = {}
        self.ecount = {k: 0 for k in self.e}
        self.last_op = {k: None for k in self.e}
        self.dma_open = []

    def op(self, eng, fn, r=(), w=(), dma=False):
        self.ops.append((eng, fn, tuple(r), tuple(w), dma))

    def barrier(self):
        self.ops.append(("BAR", None, (), (), False))

    def _wait(self, eng, sem, val):
        cur = self.waited[eng].get(sem, 0)
        if cur < val:
            self.e[eng].wait_ge(sem, val)
            self.waited[eng][sem] = val

    def emit(self):
        ops = self.ops
        n = len(ops)
        start = self.done
        deps = {}
        needed = set()
        last_w, readers = self.last_w, self.readers
        eidx, ecount = self.eidx, self.ecount
        openg = {}
        for i in range(start, n):
            eng, fn, r, w, dma = ops[i]
            if eng == "BAR":
                continue
            eidx[i] = ecount[eng]
            ecount[eng] += 1
            openg[i] = (eng, dma)
            d = set()
            raw = set()
            for k in r:
                if k in last_w:
                    d.add(last_w[k])
                    raw.add(last_w[k])
                if isinstance(k, str) and k.startswith("ps"):
                    rd = readers.get(k)
                    if rd:
                        d.update(rd[0].values())
                        d.update(rd[1])
            for k in w:
                if k in last_w:
                    d.add(last_w[k])
                rd = readers.get(k)
                if rd:
                    d.update(rd[0].values())
                    d.update(rd[1])
            for k in r:
                rd = readers.setdefault(k, ({}, []))
                if dma:
                    rd[1].append(i)
                else:
                    rd[0][eng] = i
            for k in w:
                last_w[k] = i
                readers[k] = ({}, [])
            d.discard(i)
            keep = set()
            for j in d:
                if j < start and j not in self.sig and j not in openg:
                    continue
                jeng, jdma = self._opinfo(j, openg)
                if (not dma) and (not jdma) and jeng == eng:
                    if eng == "pe":
                        continue
                    if j in raw and eidx[i] - eidx[j] <= 2:
                        keep.add(j)
                    continue
                keep.add(j)
            deps[i] = keep
            needed.update(keep)
        self._openg_all = getattr(self, "_openg_all", {})
        self._openg_all.update(openg)
        for i in range(start, n):
            eng, fn, r, w, dma = ops[i]
            if eng == "BAR":
                self._emit_barrier()
                continue
            if dma:
                q = eng
                pos = self.ring_pos[q]
                self.ring_pos[q] = (pos + 1) % len(self.ring[q])
                sem = self.ring[q][pos]
                prev = self.ring_last[q][pos]
                if prev is not None:
                    self._wait(eng, sem, prev)
            for j in sorted(deps[i]):
                if j in self.sig:
                    s, v = self.sig[j]
                    self._wait(eng, s, v)
            ins = fn(self.e[eng])
            if dma:
                self.ring_cnt[q][pos] += 16
                val = self.ring_cnt[q][pos]
                ins.then_inc(sem, 16)
                self.ring_last[q][pos] = val
                self.sig[i] = (sem, val)
                self.dma_open.append(i)
            else:
                self.last_op[eng] = i
                if i in needed:
                    self.ticket[eng] += 1
                    ins.then_inc(self.sem[eng], 1)
                    self.sig[i] = (self.sem[eng], self.ticket[eng])
        self.done = n

    def _opinfo(self, j, openg):
        if j in openg:
            return openg[j]
        return self._openg_all[j]

    def _emit_barrier(self):
        marks = []
        for eng in COMPUTE:
            self.ticket[eng] += 1
            self.e[eng].drain().then_inc(self.sem[eng], 1)
            marks.append((self.sem[eng], self.ticket[eng]))
        dmas = [self.sig[i] for i in self.dma_open]
        self.dma_open = []
        for eng in self.e:
            for s, v in marks:
                self._wait(eng, s, v)
            for s, v in dmas:
                self._wait(eng, s, v)
        self.last_w.clear()
        self.readers.clear()


class Ctx:
    pass


_UID = [0]


def _sb(c, name, shape, dt):
    _UID[0] += 1
    return c.ph.enter_context(c.nc.sbuf_tensor("sb%d_%s" % (_UID[0], name), list(shape), dt))


def _ps(c, name, shape, dt):
    _UID[0] += 1
    return c.ph.enter_context(c.nc.psum_tensor("pp%d_%s" % (_UID[0], name), list(shape), dt))


def mm_group(p, out_ap, pairs, r, w):
    n = len(pairs)

    def fn(e):
        ins = None
        for i, (l, rr) in enumerate(pairs):
            ins = e.matmul(out_ap, l, rr, start=(i == 0), stop=(i == n - 1))
        return ins
    p.op("pe", fn, r, w)


def tr_group(p, outs_ins, ident, r, w):
    def fn(e):
        ins = None
        for o, i_ in outs_ins:
            ins = e.transpose(o, i_, ident)
        return ins
    p.op("pe", fn, r, w)


def dma(p, q, out_ap, in_ap, r, w):
    p.op(q, lambda e: e.dma_start(out=out_ap, in_=in_ap), r, w, dma=True)


def load_cast_cols(p, dst, src, ncols, r, w, step=2048):
    for c0 in range(0, ncols, step):
        c1 = min(ncols, c0 + step)
        dma(p, "pool", dst[:, c0:c1], src[:, c0:c1], r, w)


def phase1(c, L):
    nc, p = c.nc, c.p
    with ExitStack() as ph:
        c.ph = ph
        winb = _sb(c, "winb", [128, 8, DIN], BF16)
        wuqb = _sb(c, "wuqb", [128, 3, 384], BF16)
        wukvb = _sb(c, "wukvb", [128, 2, 512], BF16)
        identb = _sb(c, "identb", [128, 128], BF16)
        gq = _sb(c, "gq", [128, 384], F32)
        gk = _sb(c, "gk", [128, 128], F32)
        mq = _sb(c, "mq", [128, 384], F32)
        mkv = _sb(c, "mkv", [128, 256], F32)
        xf = [_sb(c, "xf%d" % i, [128, D], F32) for i in range(2)]
        xb = [_sb(c, "xb%d" % i, [128, D], BF16) for i in range(2)]
        xT = [_sb(c, "xT%d" % i, [128, 8, 512], BF16) for i in range(2)]
        rp = [_sb(c, "rp%d" % i, [128, 192], F32) for i in range(2)]
        wk = [_sb(c, "wk%d" % i, [128, 384], F32) for i in range(6)]
        sm = [_sb(c, "sm%d" % i, [128, 8], F32) for i in range(4)]
        ob = [_sb(c, "ob%d" % i, [128, 384], BF16) for i in range(4)]
        tT = [_sb(c, "tT%d" % i, [128, 512], BF16) for i in range(2)]
        blk = {k: [_sb(c, "blk_%s%d" % (k, i), [128, 6 * 512], BF16) for i in range(2)]
               for k in ("a", "b", "c", "d")}
        vna = [_sb(c, "vna%d" % i, [128, 6, 65], BF16) for i in range(2)]
        vg = [_sb(c, "vg%d" % i, [128, 2, 65], BF16) for i in range(2)]
        vm = [_sb(c, "vm%d" % i, [128, 4, 65], BF16) for i in range(2)]
        gout = [_sb(c, "gout%d" % i, [128, 512], BF16) for i in range(3)]
        psT = _ps(c, "psT", [128, 8, 128], BF16)
        psR = _ps(c, "psR", [128, 8, 128], BF16)
        psM = [_ps(c, "psM%d" % i, [128, 512], F32) for i in range(4)]
        psG = [_ps(c, "psG%d" % i, [128, 512], F32) for i in range(2)]

        w_in = c.w["w_in"][L]
        w_in_v = w_in.rearrange("(c p) n -> p c n", p=128)
        for ch in range(8):
            load_cast_cols(p, winb[:, ch, :], w_in_v[:, ch, :], DIN, [], ["winb"])
        wuq_v = c.w["w_uq"][L].rearrange("(c p) n -> p c n", p=128)
        for ch in range(3):
            load_cast_cols(p, wuqb[:, ch, :], wuq_v[:, ch, :], 384, [], ["wuqb"])
        wukv_v = c.w["w_ukv"][L].rearrange("(c p) n -> p c n", p=128)
        for ch in range(2):
            load_cast_cols(p, wukvb[:, ch, :], wukv_v[:, ch, :], 512, [], ["wukvb"])
        dma(p, "sp", identb[:], c.w["identb"][:, :], [], ["identb"])
        dma(p, "sp", gq[:], c.w["gq_rep"][L], [], ["gq"])
        dma(p, "sp", gk[:], c.w["gk_rep"][L], [], ["gk"])
        dma(p, "sp", mq[:], c.w["mq_rep"][L], [], ["mq"])
        dma(p, "sp", mkv[:], c.w["mkv_rep"][L], [], ["mkv"])
        for i in range(2):
            p.op("pool", lambda e, t=vna[i]: e.memset(t[:], 1.0), [], [("vna", i)])
            p.op("pool", lambda e, t=vg[i]: e.memset(t[:], 1.0), [], [("vg", i)])
            p.op("pool", lambda e, t=vm[i]: e.memset(t[:], 1.0), [], [("vm", i)])

        cnt = {"tile": 0, "psM": 0, "psG": 0, "wk": 0, "sm": 0, "ob": 0, "tT": 0, "gout": 0}
        pending = []

        def st(out_ap, in_ap, r, w):
            pending.append((out_ap, in_ap, r, w))

        def flush():
            for o_, i_, r_, w_ in pending:
                dma(p, "sp", o_, i_, r_, w_)
            del pending[:]

        def nxt(k, n):
            v = cnt[k] % n
            cnt[k] += 1
            return v

        def load_tile(src_ap, rope_ap, to_xT=None, mask_col=None):
            s = nxt("tile", 2)
            dma(p, "sp", xf[s][:], src_ap, [], [("xf", s)])
            if rope_ap is not None:
                dma(p, "sp", rp[s][:], rope_ap, [], [("rp", s)])
            if mask_col is not None:
                p.op("dve", lambda e: e.tensor_scalar(out=xf[s][:], in0=xf[s][:],
                                                      scalar1=c.hm[:, mask_col:mask_col + 1], scalar2=None,
                                                      op0=ALU.mult), [("xf", s), "hm"], [("xf", s)])
            p.op("pool", lambda e: e.tensor_copy(xb[s][:], xf[s][:]), [("xf", s)], [("xb", s)])
            tr_group(p, [(psT[:, ch, :], xb[s][:, ch * 128:(ch + 1) * 128]) for ch in range(8)],
                     identb[:], [("xb", s), "identb"], ["psT"])
            if to_xT is None:
                dst, key = tT_full[s][:], ("tTf", s)
            else:
                dst, key = to_xT
            p.op("act", lambda e: e.activation(out=dst, in_=psT[:], func=AF.Copy), ["psT"], [key])
            return dst, key, rp[s], ("rp", s)

        tT_full = [_sb(c, "tTf%d" % i, [128, 8, 128], BF16) for i in range(2)]

        def tok_mm(xTa, xTkey, c0, c1):
            b = nxt("psM", 4)
            mm_group(p, psM[b][:, 0:c1 - c0],
                     [(xTa[:, ch, :], winb[:, ch, c0:c1]) for ch in range(8)],
                     [xTkey, "winb"], ["psM%d" % b])
            return psM[b], "psM%d" % b

        def transp_out(src_bf, src_key, nh, dh, dst_ap, dst_key):
            tr_group(p, [(psR[0:dh, h, :], src_bf[:, h * dh:(h + 1) * dh]) for h in range(nh)],
                     identb[:], [src_key, "identb"], ["psR"])
            p.op("dve", lambda e: e.tensor_copy(dst_ap, psR[0:dh, 0:nh, :]), ["psR"], [dst_key])

        def rms_rope(ps_ap, pskey, nh, dh, g_ap, gkey, rope_t, rpkey, coff, out_bf, outkey):
            n = nh * dh
            hd = dh // 2
            a = nxt("wk", 6); b2 = nxt("wk", 6); c2 = nxt("wk", 6); d2 = nxt("wk", 6); e2 = nxt("wk", 6)
            s1 = nxt("sm", 4)
            A, B, C, Dd, E = wk[a], wk[b2], wk[c2], wk[d2], wk[e2]
            p.op("act", lambda e: e.activation(out=A[:, 0:n], in_=ps_ap, func=AF.Square),
                 [pskey], [("wk", a)])
            p.op("dve", lambda e: e.tensor_reduce(
                out=sm[s1][:, 0:nh], in_=A[:, 0:n].rearrange("p (h d) -> p h d", h=nh),
                axis=AX.X, op=ALU.add), [("wk", a)], [("sm", s1)])
            p.op("act", lambda e: e.activation(
                out=sm[s1][:, 0:nh], in_=sm[s1][:, 0:nh], func=AF.Sqrt, scale=1.0 / dh, bias=c.eps_rms[:, 0:1]),
                [("sm", s1)], [("sm", s1)])
            p.op("dve", lambda e: e.reciprocal(out=sm[s1][:, 0:nh], in_=sm[s1][:, 0:nh]),
                 [("sm", s1)], [("sm", s1)])
            p.op("dve", lambda e: e.tensor_tensor(
                out=B[:, 0:n].rearrange("p (h d) -> p h d", h=nh),
                in0=ps_ap.rearrange("p (h d) -> p h d", h=nh),
                in1=sm[s1][:, 0:nh].unsqueeze(2).to_broadcast([128, nh, dh]),
                op=ALU.mult), [pskey, ("sm", s1)], [("wk", b2)])
            p.op("pool", lambda e: e.tensor_tensor(out=C[:, 0:n], in0=B[:, 0:n], in1=g_ap, op=ALU.mult),
                 [("wk", b2), gkey], [("wk", c2)])
            rope(C, ("wk", c2), nh, dh, rope_t, rpkey, coff, Dd, ("wk", d2), E, ("wk", e2),
                 out_bf[:, 0:n].rearrange("p (h d) -> p h d", h=nh), outkey)

        def rope(X, xkey, nh, dh, rope_t, rpkey, coff, Dd, dkey, E, ekey, out3, outkey, xview=None):
            n = nh * dh
            hd = dh // 2
            x3 = xview if xview is not None else X[:, 0:n].rearrange("p (h d) -> p h d", h=nh)
            cs = rope_t[:, coff:coff + dh].unsqueeze(1).to_broadcast([128, nh, dh])
            sslo = rope_t[:, coff + dh:coff + dh + hd].unsqueeze(1).to_broadcast([128, nh, hd])
            sshi = rope_t[:, coff + dh + hd:coff + 2 * dh].unsqueeze(1).to_broadcast([128, nh, hd])
            d3 = Dd[:, 0:n].rearrange("p (h d) -> p h d", h=nh)
            e3 = E[:, 0:n].rearrange("p (h d) -> p h d", h=nh)
            p.op("pool", lambda e: e.tensor_tensor(out=d3, in0=x3, in1=cs, op=ALU.mult),
                 [xkey, rpkey], [dkey])
            p.op("dve", lambda e: e.tensor_tensor(out=e3[:, :, 0:hd], in0=x3[:, :, hd:dh], in1=sslo,
                                                  op=ALU.mult), [xkey, rpkey], [ekey])
            p.op("dve", lambda e: e.tensor_tensor(out=e3[:, :, hd:dh], in0=x3[:, :, 0:hd], in1=sshi,
                                                  op=ALU.mult), [xkey, rpkey, ekey], [ekey])
            p.op("pool", lambda e: e.tensor_tensor(out=out3, in0=d3, in1=e3, op=ALU.add),
                 [dkey, ekey], [outkey])

        def rms_full(ps_ap, pskey, n, g_ap, gkey, out_bf, outkey):
            a = nxt("wk", 6)
            s1 = nxt("sm", 4)
            p.op("act", lambda e: e.activation(out=wk[a][:, 0:n], in_=ps_ap, func=AF.Square,
                                               accum_out=sm[s1][:, 0:1]),
                 [pskey], [("wk", a), ("sm", s1)])
            p.op("act", lambda e: e.activation(
                out=sm[s1][:, 1:2], in_=sm[s1][:, 0:1], func=AF.Sqrt, scale=1.0 / n, bias=c.eps_rms[:, 0:1]),
                [("sm", s1)], [("sm", s1)])
            p.op("dve", lambda e: e.reciprocal(out=sm[s1][:, 2:3], in_=sm[s1][:, 1:2]),
                 [("sm", s1)], [("sm", s1)])
            p.op("dve", lambda e: e.scalar_tensor_tensor(
                out=out_bf, in0=ps_ap, scalar=sm[s1][:, 2:3], in1=g_ap,
                op0=ALU.mult, op1=ALU.mult), [pskey, ("sm", s1), gkey], [outkey])

        def na_kv(xTa, xTkey, tok_off, sub, bs):
            ps, pk = tok_mm(xTa, xTkey, 384, 768)
            o = nxt("ob", 4)
            p.op("act", lambda e, ps=ps, o=o: e.activation(out=ob[o][:, 0:384], in_=ps[:, 0:384],
                                                           func=AF.Copy), [pk], [("ob", o)])
            kb = blk["b"][bs][0:64, :].rearrange("p (h t) -> p h t", h=6)
            transp_out(ob[o], ("ob", o), 6, 64, kb[:, :, sub * 128:(sub + 1) * 128], ("blk_b", bs))
            ps, pk = tok_mm(xTa, xTkey, 768, 1152)
            s = nxt("tile", 2) if False else (cnt["tile"] - 1) % 2
            p.op("act", lambda e, ps=ps, s=s: e.activation(
                out=vna[s][:, :, 0:64], in_=ps[:, 0:384].rearrange("p (h d) -> p h d", h=6),
                func=AF.Copy), [pk], [("vna", s)])
            st(c.s["v_na"][tok_off:tok_off + 128, :], vna[s][:].rearrange("p h d -> p (h d)"),
                [("vna", s)], [("d_v_na", tok_off)])

        def flush_blk(name, bs, dh, nh, dst, t0, nt):
            src = blk[name][bs][0:dh, 0:nh * 512].rearrange("p (h t) -> p h t", h=nh)[:, :, 0:nt]
            st(dst[:, :, t0:t0 + nt].rearrange("h d t -> d h t"), src,
               [("blk_" + name, bs)], [("d_" + name, t0)])

        def own_load(t):
            bi_, sub_ = divmod(t, 4)
            return load_tile(c.q_src[t * 128:(t + 1) * 128, :], c.rope_q[t * 128:(t + 1) * 128, :],
                             to_xT=(xT[bi_ % 2][:, :, sub_ * 128:(sub_ + 1) * 128], ("xT", bi_ % 2)))
        nxt_h = own_load(0)
        for bi in range(8):
            bs = bi % 2
            for sub in range(4):
                t = bi * 4 + sub
                xTa, xTkey, rpt, rpk = nxt_h
                if t + 1 < 32:
                    nxt_h = own_load(t + 1)
                flush()
                ps, pk = tok_mm(xTa, xTkey, 0, 384)
                o = nxt("ob", 4)
                p.op("act", lambda e, o=o, ps=ps: e.activation(out=ob[o][:, 0:384], in_=ps[:, 0:384],
                                                               func=AF.Copy), [pk], [("ob", o)])
                qa = blk["a"][bs][0:64, :].rearrange("p (h t) -> p h t", h=6)
                transp_out(ob[o], ("ob", o), 6, 64, qa[:, :, sub * 128:(sub + 1) * 128], ("blk_a", bs))
                na_kv(xTa, xTkey, 256 + t * 128, sub, bs)
                ps, pk = tok_mm(xTa, xTkey, 1152, 1536)
                o = nxt("ob", 4)
                rms_rope(ps[:, 0:384], pk, 6, 64, gq[:], "gq", rpt, rpk, 0, ob[o], ("ob", o))
                qg = blk["c"][bs][0:64, :].rearrange("p (h t) -> p h t", h=6)
                transp_out(ob[o], ("ob", o), 6, 64, qg[:, :, sub * 128:(sub + 1) * 128], ("blk_c", bs))
                ps, pk = tok_mm(xTa, xTkey, 1792, 2176)
                o = nxt("ob", 4)
                rms_full(ps[:, 0:384], pk, 384, mq[:], "mq", ob[o][:, 0:384], ("ob", o))
                tt = nxt("tT", 2)
                tr_group(p, [(psR[:, ch, :], ob[o][:, ch * 128:(ch + 1) * 128]) for ch in range(3)],
                         identb[:], [("ob", o), "identb"], ["psR"])
                p.op("act", lambda e, tt=tt: e.activation(
                    out=tT[tt][:, 0:384].rearrange("p (c t) -> p c t", c=3), in_=psR[:, 0:3, :],
                    func=AF.Copy), ["psR"], [("tT", tt)])
                g = nxt("psG", 2)
                tT3 = tT[tt][:, 0:384].rearrange("p (c t) -> p c t", c=3)
                mm_group(p, psG[g][:, 0:384], [(tT3[:, ch, :], wuqb[:, ch, :]) for ch in range(3)],
                         [("tT", tt), "wuqb"], ["psG%d" % g])
                o2 = nxt("ob", 4)
                qc3 = psG[g][:, 0:384].rearrange("p (h d) -> p h d", h=4)
                out3 = ob[o2][:, 0:384].rearrange("p (h d) -> p h d", h=4)
                p.op("act", lambda e, qc3=qc3, out3=out3: e.activation(
                    out=out3[:, :, 0:64], in_=qc3[:, :, 0:64], func=AF.Copy),
                    ["psG%d" % g], [("ob", o2)])
                a = nxt("wk", 6); d2 = nxt("wk", 6); e2 = nxt("wk", 6)
                xr3 = wk[a][:, 0:128].rearrange("p (h d) -> p h d", h=4)
                p.op("act", lambda e, qc3=qc3, xr3=xr3: e.activation(out=xr3, in_=qc3[:, :, 64:96],
                                                                     func=AF.Copy),
                     ["psG%d" % g], [("wk", a)])
                rope(wk[a], ("wk", a), 4, 32, rpt, rpk, 128, wk[d2], ("wk", d2), wk[e2], ("wk", e2),
                     out3[:, :, 64:96], ("ob", o2))
                qm = blk["d"][bs][0:96, 0:2048].rearrange("p (h t) -> p h t", h=4)
                transp_out(ob[o2], ("ob", o2), 4, 96, qm[:, :, sub * 128:(sub + 1) * 128], ("blk_d", bs))
            flush_blk("a", bs, 64, 6, c.s["qT_na"], bi * 512, 512)
            flush_blk("b", bs, 64, 6, c.s["kT_na"], 256 + bi * 512, 512)
            flush_blk("c", bs, 64, 6, c.s["qT_gqa"], bi * 512, 512)
            flush_blk("d", bs, 96, 4, c.s["qT_mla"], bi * 512, 512)
            for cc in range(24):
                g = nxt("psG", 2)
                c0 = 2464 + cc * 128
                mm_group(p, psG[g][:, :], [(winb[:, ch, c0:c0 + 128], xT[bs][:, ch, :]) for ch in range(8)],
                         [("xT", bs), "winb"], ["psG%d" % g])
                go = nxt("gout", 3)
                p.op("act", lambda e, g=g, go=go: e.activation(out=gout[go][:], in_=psG[g][:, :],
                                                               func=AF.Sigmoid),
                     ["psG%d" % g], [("gout", go)])
                dma(p, "sp", c.s["gT"][cc * 128:(cc + 1) * 128, bi * 512:(bi + 1) * 512], gout[go][:],
                    [("gout", go)], [("d_gT", cc, bi)])

        for hi in range(4):
            hsrc = (c.c_src[HALF - 256 + hi * 128:HALF - 256 + (hi + 1) * 128, :] if hi < 2 else
                    c.c_src[(hi - 2) * 128:(hi - 1) * 128, :])
            xTa, xTkey, rpt, rpk = load_tile(hsrc, None, mask_col=c.hm_cols[0 if hi < 2 else 1])
            flush()
            tok_off = hi * 128 if hi < 2 else 4352 + (hi - 2) * 128
            na_kv(xTa, xTkey, tok_off, hi % 2, 0)
            if hi % 2 == 1:
                flush_blk("b", 0, 64, 6, c.s["kT_na"], 0 if hi == 1 else 4352, 256)

        def full_load(t):
            return load_tile(c.full_src[t * 128:(t + 1) * 128, :], c.rope_full[t * 128:(t + 1) * 128, :])
        flush()
        if c.do_full:
            nxt_h = full_load(0)
        for bi in range(16 if c.do_full else 0):
            bs = bi % 2
            for sub in range(4):
                t = bi * 4 + sub
                xTa, xTkey, rpt, rpk = nxt_h
                if t + 1 < 64:
                    nxt_h = full_load(t + 1)
                flush()
                s = (cnt["tile"] - 1) % 2
                ps, pk = tok_mm(xTa, xTkey, 1536, 1792)
                o = nxt("ob", 4)
                rms_rope(ps[:, 0:128], pk, 2, 64, gk[:], "gk", rpt, rpk, 0, ob[o], ("ob", o))
                kg = blk["a"][bs][0:64, 0:1024].rearrange("p (h t) -> p h t", h=2)
                transp_out(ob[o], ("ob", o), 2, 64, kg[:, :, sub * 128:(sub + 1) * 128], ("blk_a", bs))
                p.op("act", lambda e, ps=ps, s=s: e.activation(
                    out=vg[s][:, :, 0:64], in_=ps[:, 128:256].rearrange("p (h d) -> p h d", h=2),
                    func=AF.Copy), [pk], [("vg", s)])
                st(c.s["v_gqa"][t * 128:(t + 1) * 128, :], vg[s][:].rearrange("p h d -> p (h d)"),
                    [("vg", s)], [("d_v_gqa", t)])
                ps, pk = tok_mm(xTa, xTkey, 2176, 2464)
                o = nxt("ob", 4)
                rms_full(ps[:, 0:256], pk, 256, mkv[:], "mkv", ob[o][:, 0:256], ("ob", o))
                a = nxt("wk", 6)
                p.op("act", lambda e, ps=ps, a=a: e.activation(out=wk[a][:, 0:32], in_=ps[:, 256:288],
                                                               func=AF.Copy), [pk], [("wk", a)])
                tt = nxt("tT", 2)
                tr_group(p, [(psR[:, ch, :], ob[o][:, ch * 128:(ch + 1) * 128]) for ch in range(2)],
                         identb[:], [("ob", o), "identb"], ["psR"])
                tT2 = tT[tt][:, 0:256].rearrange("p (c t) -> p c t", c=2)
                p.op("act", lambda e, tT2=tT2: e.activation(out=tT2, in_=psR[:, 0:2, :], func=AF.Copy),
                     ["psR"], [("tT", tt)])
                g = nxt("psG", 2)
                mm_group(p, psG[g][:, :], [(tT2[:, ch, :], wukvb[:, ch, :]) for ch in range(2)],
                         [("tT", tt), "wukvb"], ["psG%d" % g])
                kv3 = psG[g][:, :].rearrange("p (h d) -> p h d", h=4)
                o2 = nxt("ob", 4)
                out3 = ob[o2][:, 0:384].rearrange("p (h d) -> p h d", h=4)
                p.op("act", lambda e, kv3=kv3, out3=out3: e.activation(
                    out=out3[:, :, 0:64], in_=kv3[:, :, 0:64], func=AF.Copy),
                    ["psG%d" % g], [("ob", o2)])
                p.op("dve", lambda e, kv3=kv3, s=s: e.tensor_copy(vm[s][:, :, 0:64], kv3[:, :, 64:128]),
                     ["psG%d" % g], [("vm", s)])
                st(c.s["v_mla"][t * 128:(t + 1) * 128, :], vm[s][:].rearrange("p h d -> p (h d)"),
                    [("vm", s)], [("d_v_mla", t)])
                d2 = nxt("wk", 6); e2 = nxt("wk", 6); f2 = nxt("wk", 6)
                kr3 = wk[f2][:, 0:32].rearrange("p (h d) -> p h d", h=1)
                rope(wk[a], ("wk", a), 1, 32, rpt, rpk, 128, wk[d2], ("wk", d2), wk[e2], ("wk", e2),
                     kr3, ("wk", f2))
                p.op("pool", lambda e, out3=out3, f2=f2: e.tensor_copy(
                    out3[:, :, 64:96], wk[f2][:, 0:32].unsqueeze(1).to_broadcast([128, 4, 32])),
                    [("wk", f2), ("ob", o2)], [("ob", o2)])
                km = blk["d"][bs][0:96, 0:2048].rearrange("p (h t) -> p h t", h=4)
                transp_out(ob[o2], ("ob", o2), 4, 96, km[:, :, sub * 128:(sub + 1) * 128], ("blk_d", bs))
            flush_blk("a", bs, 64, 2, c.s["kT_gqa"], bi * 512, 512)
            flush_blk("d", bs, 96, 4, c.s["kT_mla"], bi * 512, 512)
        flush()
        p.barrier()
        p.emit()


W_SHAPES = {
    "w_in": ([D, DIN], F32), "w_uq": ([384, 384], F32), "w_ukv": ([256, 512], F32),
    "w_ba": ([384, D], F32), "w_bb": ([384, D], F32), "w_bc": ([256, D], F32),
    "w_out": ([D, D], F32), "w_router": ([D, NEXP], F32),
    "w_gu": ([NEXP, D, 2 * DEXP], F32), "w_dn": ([NEXP, DEXP, D], F32),
    "gq_rep": ([128, 384], F32), "gk_rep": ([128, 128], F32),
    "mq_rep": ([128, 384], F32), "mkv_rep": ([128, 256], F32),
    "ln1g": ([128, D], F32), "ln1b": ([128, D], F32), "ln2g": ([128, D], F32), "ln2b": ([128, D], F32),
    "brouter": ([128, NEXP], F32), "bgu_t": ([128, NEXP, 16], F32), "bdn": ([NEXP, D], F32),
    "na_bint": ([64, 6, 8, 64], F32), "na_bbnd": ([7, 64, 6, 12, 64], F32),
    "na_bbnd_o": ([7, 64, 6, 12, 64], F32),
}
C_SHAPES = {
    "rope_loc": ([S, 192], F32), "hmask": ([128, 4], F32),
    "identb": ([128, 128], BF16), "identf": ([128, 128], F32),
    "triu": ([128, 128], F32), "ones128": ([128, 128], F32),
    "iota_e": ([128, NEXP], F32), "iota_cap": ([128, NEXP], F32),
}
CAP = 768
NSLOT = NEXP * CAP
S_SHAPES = {
    "qT_na": ([6, 64, HALF], BF16), "kT_na": ([6, 64, NA_ROWS * 64], BF16),
    "v_na": ([NA_ROWS * 64, 390], BF16),
    "qT_gqa": ([6, 64, HALF], BF16), "kT_gqa": ([2, 64, S], BF16), "v_gqa": ([S, 130], BF16),
    "qT_mla": ([4, 96, HALF], BF16), "kT_mla": ([4, 96, S], BF16), "v_mla": ([S, 260], BF16),
    "gT": ([3 * D, HALF], BF16),
    "oT": ([16, 64, HALF], BF16),
    "x1": ([HALF, D], F32),
    "x1T": ([D, HALF], BF16), "gateT": ([NEXP, HALF], F32), "ymoe": ([HALF, D], F32),
    "xg": ([NSLOT, D], BF16), "yg": ([NSLOT, D], F32),
    "slots": ([HALF, 4], I32), "gks": ([HALF, 4], F32),
}


def build(taps=(), passes=("A", "B", "C"), phases=None, dst_override=None):
    nc = bass.Bass("TRN2", target_bir_lowering=False)
    phases = phases or ALL_PHASES
    c = Ctx()
    c.nc = nc
    c.w = {}
    for k, (shp, dt) in W_SHAPES.items():
        c.w[k] = nc.dram_tensor(k, [2] + shp, dt, kind="ExternalInput").ap()
    for k, (shp, dt) in C_SHAPES.items():
        c.w[k] = nc.dram_tensor(k, shp, dt, kind="ExternalInput").ap()
    x_loc = nc.dram_tensor("x_loc", [S, D], F32, kind="ExternalInput").ap()
    c.s = {}
    for k, (shp, dt) in S_SHAPES.items():
        kind = "ExternalOutput" if k in taps else "Internal"
        c.s[k] = nc.dram_tensor("s_" + k, shp, dt, kind=kind).ap()
    y01 = nc.dram_tensor("y01", [S, D], F32, kind="Internal").ap()
    c.y = nc.dram_tensor("y", [HALF, D], F32, kind="ExternalOutput").ap()
    with ExitStack() as stack:
        c.p = Prog(nc, stack)
        c.breg = nc.gpsimd.to_reg(NSLOT - 1)
        c.eps_rms = stack.enter_context(nc.sbuf_tensor("eps_rms", [128, 1], F32))
        c.eps_ln = stack.enter_context(nc.sbuf_tensor("eps_ln", [128, 1], F32))
        c.hm = stack.enter_context(nc.sbuf_tensor("hmask_sb", [128, 4], F32))
        c.p.op("pool", lambda e: e.memset(c.eps_rms[:], RMS_EPS), [], ["eps"])
        c.p.op("pool", lambda e: e.memset(c.eps_ln[:], LN_EPS), [], ["eps"])
        dma(c.p, "sp", c.hm[:], c.w["hmask"][:, :], [], ["hm"])
        c.p.barrier()
        c.p.emit()
        rope = c.w["rope_loc"]
        c.rope_full = rope
        with ExitStack() as ph:
            zt = ph.enter_context(nc.sbuf_tensor("zt", [128, 8, D], BF16))
            c.p.op("pool", lambda e: e.memset(zt[:], 0.0), [], ["zt"])
            xg_v = c.s["xg"].rearrange("(n p) d -> p n d", p=128)
            for i in range(NSLOT // 1024):
                dma(c.p, "sp", xg_v[:, i * 8:(i + 1) * 8, :], zt[:], ["zt"], [("d_xg0", i)])
            c.p.barrier()
            c.p.emit()
        for ps_ in passes:
            if ps_ == "A":
                L, c.q_src, c.c_src, c.full_src = 0, x_loc[0:HALF, :], x_loc[HALF:S, :], x_loc
                c.rope_q, c.bbnd, c.hm_cols, c.do_full, dst = rope[0:HALF, :], c.w["na_bbnd"][0], (0, 1), True, y01[0:HALF, :]
            elif ps_ == "B":
                L, c.q_src, c.c_src, c.full_src = 0, x_loc[HALF:S, :], x_loc[0:HALF, :], x_loc
                c.rope_q, c.bbnd, c.hm_cols, c.do_full, dst = rope[HALF:S, :], c.w["na_bbnd_o"][0], (2, 3), False, y01[HALF:S, :]
            else:
                L, c.q_src, c.c_src, c.full_src = 1, y01[0:HALF, :], y01[HALF:S, :], y01
                c.rope_q, c.bbnd, c.hm_cols, c.do_full, dst = rope[0:HALF, :], c.w["na_bbnd"][1], (0, 1), True, c.y
            if "p1" in phases:
                phase1(c, L)
            if "na" in phases:
                phase_na(c, L)
            if "gqa" in phases:
                phase_dense_attn(c, L, "gqa")
            if "mla" in phases:
                phase_dense_attn(c, L, "mla")
            if "merge" in phases:
                phase_merge(c, L)
            if "moe" in phases:
                phase_moe(c, L)
            if "moes" in phases:
                phase_moe_sparse(c, L)
            if "ln2" in phases:
                phase_ln2(c, L, dst if dst_override is None else dst_override(c), sparse=("moes" in phases))
        c.p.barrier()
        c.p.emit()
    return nc


def rope_tables():
    t = np.arange(S)
    row = (t // 64).astype(np.float32)
    col = (t % 64).astype(np.float32)
    out = []
    for dim in (64, 32):
        quarter = dim // 4
        inv = (10000.0 ** (-np.arange(quarter, dtype=np.float32) / quarter)).astype(np.float32)
        ang = np.concatenate([row[:, None] * inv, col[:, None] * inv], -1).astype(np.float32)
        cs, sn = np.cos(ang).astype(np.float32), np.sin(ang).astype(np.float32)
        out.append(np.concatenate([cs, cs], -1))
        out.append(np.concatenate([-sn, sn], -1))
    return np.ascontiguousarray(np.concatenate(out, -1).astype(np.float32))


def na_bias_tables(rpb, hf):
    cols = np.arange(64)
    c0 = np.clip(cols - 8, 0, 48)
    in_win = (cols[None, :] >= c0[:, None]) & (cols[None, :] < c0[:, None] + 16)
    idx_c = np.clip(cols[None, :] - cols[:, None] + 15, 0, 30)

    def tab(j, rows):
        r = hf * 64 + j
        r0 = int(np.clip(r - 4, 0, 120))
        out = np.full((64, 6, len(rows), 64), NEG, np.float32)
        for ii, lr in enumerate(rows):
            gr = hf * 64 + lr - 4
            i = gr - r0
            if i < 0 or i >= 8 or gr < 0 or gr >= 128:
                continue
            ir = gr - r + 7
            b = rpb[:, ir][:, idx_c]
            b = np.where(in_win[None], b, NEG)
            out[:, :, ii, :] = b.transpose(2, 0, 1)
        return out
    interior = tab(10, list(range(10, 18)))
    bnd = []
    for j in (0, 1, 2, 3):
        bnd.append(tab(j, list(range(0, 12))))
    for j in (61, 62, 63):
        bnd.append(tab(j, list(range(60, 72))))
    return interior, np.stack(bnd, 0)


def prep_weights(inp, layers, hf):
    w = {}
    ls = list(layers)
    st = lambda f: np.ascontiguousarray(np.stack([f(l) for l in ls], 0))
    w["w_in"] = st(lambda l: inp["w_in"][l])
    w["w_uq"] = st(lambda l: inp["w_uq"][l])
    w["w_ukv"] = st(lambda l: inp["w_ukv"][l])
    w["w_ba"] = st(lambda l: inp["w_branch_a"][l])
    w["w_bb"] = st(lambda l: inp["w_branch_b"][l])
    w["w_bc"] = st(lambda l: inp["w_branch_c"][l])
    w["w_out"] = st(lambda l: inp["w_out"][l])
    w["w_router"] = st(lambda l: inp["w_router"][l])
    w["w_gu"] = st(lambda l: inp["w_gate_up"][l])
    w["w_dn"] = st(lambda l: inp["w_down"][l])
    w["gq_rep"] = st(lambda l: np.tile(inp["gqa_q_norm"][l][None, :], (128, 6)))
    w["gk_rep"] = st(lambda l: np.tile(inp["gqa_k_norm"][l][None, :], (128, 2)))
    w["mq_rep"] = st(lambda l: np.tile(inp["mla_q_norm"][l][None, :], (128, 1)))
    w["mkv_rep"] = st(lambda l: np.tile(inp["mla_kv_norm"][l][None, :], (128, 1)))
    for k, src in (("ln1g", "ln1_g"), ("ln1b", "ln1_b"), ("ln2g", "ln2_g"), ("ln2b", "ln2_b")):
        w[k] = st(lambda l: np.tile(inp[src][l][None, :], (128, 1)))
    w["brouter"] = st(lambda l: np.tile(inp["b_router"][l][None, :], (128, 1)))
    w["bgu_t"] = st(lambda l: inp["b_gate_up"][l].reshape(NEXP, 16, 128).transpose(2, 0, 1))
    w["bdn"] = st(lambda l: inp["b_down"][l])
    bi, bb = zip(*[na_bias_tables(inp["na_rpb"][l], hf) for l in ls])
    w["na_bint"] = np.ascontiguousarray(np.stack(bi, 0))
    w["na_bbnd"] = np.ascontiguousarray(np.stack(bb, 0))
    w["na_bbnd_o"] = np.ascontiguousarray(np.stack([na_bias_tables(inp["na_rpb"][l], 1 - hf)[1] for l in ls], 0))
    return {k: np.ascontiguousarray(v.astype(np.float32)) for k, v in w.items()}


def prep_consts(hf):
    rt = rope_tables()
    own, oth = rt[hf * HALF:(hf + 1) * HALF], rt[(1 - hf) * HALF:(2 - hf) * HALF]
    hm = np.zeros((128, 4), np.float32)
    hm[:, 0], hm[:, 1], hm[:, 2], hm[:, 3] = hf, 1 - hf, 1 - hf, hf
    return {
        "rope_loc": np.ascontiguousarray(np.concatenate([own, oth], 0)),
        "hmask": hm,
        "identb": np.eye(128, dtype=np.float32).astype(ml_dtypes.bfloat16),
        "identf": np.eye(128, dtype=np.float32),
        "triu": np.triu(np.ones((128, 128), np.float32), 1),
        "ones128": np.ones((128, 128), np.float32),
        "iota_e": np.tile(np.arange(NEXP, dtype=np.float32)[None, :], (128, 1)),
        "iota_cap": np.tile((np.arange(NEXP, dtype=np.float32) * CAP)[None, :], (128, 1)),
    }


def prep_acts(xb, hf):
    own, oth = xb[hf * HALF:(hf + 1) * HALF], xb[(1 - hf) * HALF:(2 - hf) * HALF]
    return {"x_loc": np.ascontiguousarray(np.concatenate([own, oth], 0))}


def _normalize(c, psO, okey, ncol, rsb, rkey, psB, ou, oukey, onesf, dst_ap, dst_key, nh=1):
    p = c.p
    p.op("dve", lambda e: e.reciprocal(out=rsb[64:65, 0:ncol], in_=psO[64:65, 0:ncol]), [okey], [rkey])
    p.op("pe", lambda e: e.matmul(psB[0:64, 0:ncol], onesf[64:65, 0:64], rsb[64:65, 0:ncol],
                                  start=True, stop=True), [rkey, "onesf"], ["psB"])
    p.op("act", lambda e: e.activation(out=ou[0:64, 0:ncol], in_=psO[0:64, 0:ncol], func=AF.Copy),
         [okey], [oukey])
    a0 = ou[0:64, 0:ncol]
    a1 = psB[0:64, 0:ncol]
    if nh > 1:
        a0 = a0.rearrange("p (h q) -> p h q", h=nh)
        a1 = a1.rearrange("p (h q) -> p h q", h=nh)
    p.op("dve", lambda e: e.tensor_tensor(out=dst_ap, in0=a0, in1=a1, op=ALU.mult),
         [oukey, "psB"], [dst_key])


def phase_dense_attn(c, L, kind):
    nc, p = c.nc, c.p
    if kind == "gqa":
        nH, dh, nK, nV, scale, obase = 6, 64, 2, 2, 64 ** -0.5, 6
        qT, kT, vS = c.s["qT_gqa"], c.s["kT_gqa"], c.s["v_gqa"]
        kmap = [0, 0, 0, 1, 1, 1]
    else:
        nH, dh, nK, nV, scale, obase = 4, 96, 4, 4, 96 ** -0.5, 12
        qT, kT, vS = c.s["qT_mla"], c.s["kT_mla"], c.s["v_mla"]
        kmap = [0, 1, 2, 3]
    with ExitStack() as ph:
        c.ph = ph
        KT = _sb(c, "KT", [128, nK, S], BF16)
        V = _sb(c, "V", [128, 64, nV * 65], BF16)
        Q = [_sb(c, "Q%d" % i, [128, nH, 512], BF16) for i in range(2)]
        pT = [_sb(c, "pT%d" % i, [128, 512], BF16) for i in range(4)]
        rsb = _sb(c, "rsb", [128, 512], F32)
        ou = _sb(c, "ou", [128, 512], F32)
        ot = [_sb(c, "ot%d" % i, [128, 512], BF16) for i in range(2)]
        onesf = _sb(c, "onesf", [128, 64], F32)
        psS = [_ps(c, "psS%d" % i, [128, 512], F32) for i in range(3)]
        psO = [_ps(c, "psO%d" % i, [128, 512], F32) for i in range(2)]
        psB = _ps(c, "psB", [128, 512], F32)
        p.op("pool", lambda e: e.memset(onesf[:], 1.0), [], ["onesf"])
        pack = (kind == "gqa")
        if pack:
            kT2 = kT.rearrange("g d t -> (g d) t")
            for half in range(2):
                dma(p, "sp", KT[:, 0, half * HALF:(half + 1) * HALF], kT2[:, half * HALF:(half + 1) * HALF],
                    [], ["KT"])
            for i in range(2):
                p.op("pool", lambda e, i=i: e.memset(Q[i][:], 0.0), [], [("Q", i)])
        else:
            for k in range(nK):
                for half in range(2):
                    dma(p, "sp", KT[0:dh, k, half * HALF:(half + 1) * HALF],
                        kT[k, :, half * HALF:(half + 1) * HALF], [], ["KT"])
        vv = vS.rearrange("(t p) c -> p t c", p=128)
        for q4 in range(16):
            dma(p, "sp", V[:, q4 * 4:(q4 + 1) * 4, :], vv[:, q4 * 4:(q4 + 1) * 4, :], [], ["V"])
        it = 0
        for qb in range(8):
            qs = qb % 2
            if pack:
                dma(p, "sp", Q[qs][0:64, 0:3, :], qT[0:3, :, qb * 512:(qb + 1) * 512].rearrange("h d t -> d h t"),
                    [], [("Q", qs)])
                dma(p, "sp", Q[qs][64:128, 3:6, :], qT[3:6, :, qb * 512:(qb + 1) * 512].rearrange("h d t -> d h t"),
                    [], [("Q", qs)])
            else:
                dma(p, "sp", Q[qs][0:dh, :, :], qT[:, :, qb * 512:(qb + 1) * 512].rearrange("h d t -> d h t"),
                    [], [("Q", qs)])
            for h in range(nH):
                ob_ = it % 2
                it += 1
                okey = "psO%d" % ob_
                ki = kmap[h]

                def qk(kt, h=h, ki=ki, qs=qs):
                    b = kt % 3
                    if pack:
                        p.op("pe", lambda e: e.matmul(psS[b][:, :], KT[:, 0, kt * 128:(kt + 1) * 128],
                                                      Q[qs][:, h, :], start=True, stop=True),
                             ["KT", ("Q", qs)], ["psS%d" % b])
                    else:
                        p.op("pe", lambda e: e.matmul(psS[b][:, :], KT[0:dh, ki, kt * 128:(kt + 1) * 128],
                                                      Q[qs][0:dh, h, :], start=True, stop=True),
                             ["KT", ("Q", qs)], ["psS%d" % b])
                qk(0)
                qk(1)
                for kt in range(64):
                    b = kt % 3
                    pb = kt % 4
                    p.op("act", lambda e, b=b, pb=pb: e.activation(out=pT[pb][:], in_=psS[b][:, :],
                                                                   func=AF.Exp, scale=scale),
                         ["psS%d" % b], [("pT", pb)])
                    if kt + 2 < 64:
                        qk(kt + 2)
                    p.op("pe", lambda e, kt=kt, pb=pb, ob_=ob_, ki=ki: e.matmul(
                        psO[ob_][0:65, :], V[:, kt, ki * 65:(ki + 1) * 65], pT[pb][:],
                        start=(kt == 0), stop=(kt == 63)), ["V", ("pT", pb)], [okey])
                os_ = it % 2
                _normalize(c, psO[ob_], okey, 512, rsb, "rsb", psB, ou, "ou", onesf,
                           ot[os_][0:64, :], ("ot", os_))
                dma(p, "sp", c.s["oT"][obase + h, :, qb * 512:(qb + 1) * 512], ot[os_][0:64, :],
                    [("ot", os_)], [("d_oT", obase + h, qb)])
        p.barrier()
        p.emit()


def phase_na(c, L):
    nc, p = c.nc, c.p
    with ExitStack() as ph:
        c.ph = ph
        Qb = [_sb(c, "Qb%d" % i, [128, 6, 512], BF16) for i in range(2)]
        Kb = [_sb(c, "Kb%d" % i, [128, 6, 1024], BF16) for i in range(2)]
        Vb = [_sb(c, "Vb%d" % i, [128, 16, 390], BF16) for i in range(2)]
        bint = _sb(c, "bint", [128, 6, 8, 64], F32)
        bbnd = _sb(c, "bbnd", [128, 6, 12, 64], F32)
        sc = [_sb(c, "sc%d" % i, [128, 768], F32) for i in range(2)]
        pp = [_sb(c, "pp%d" % i, [128, 768], BF16) for i in range(2)]
        rsb = _sb(c, "rsb", [128, 512], F32)
        ou = _sb(c, "ou", [128, 512], F32)
        ot = [_sb(c, "ot%d" % i, [128, 6, 512], BF16) for i in range(2)]
        onesf = _sb(c, "onesf", [128, 64], F32)
        psS = [_ps(c, "psS%d" % i, [128, 1024], F32) for i in range(2)]
        psO = [_ps(c, "psO%d" % i, [128, 512], F32) for i in range(2)]
        psB = _ps(c, "psB", [128, 512], F32)
        p.op("pool", lambda e: e.memset(onesf[:], 1.0), [], ["onesf"])
        dma(p, "sp", bint[0:64], c.w["na_bint"][L], [], ["bint"])
        vrow = c.s["v_na"].rearrange("(r k) c -> k r c", k=64)
        it = 0
        for b8 in range(8):
            s = b8 % 2
            dma(p, "sp", Qb[s][0:64, :, :], c.s["qT_na"][:, :, b8 * 512:(b8 + 1) * 512].rearrange("h d t -> d h t"),
                [], [("Qb", s)])
            dma(p, "sp", Kb[s][0:64, :, :], c.s["kT_na"][:, :, b8 * 512:b8 * 512 + 1024].rearrange("h d t -> d h t"),
                [], [("Kb", s)])
            for hv in range(2):
                dma(p, "sp", Vb[s][0:64, hv * 8:(hv + 1) * 8, :], vrow[:, b8 * 8 + hv * 8:b8 * 8 + (hv + 1) * 8, :],
                    [], [("Vb", s)])
            for jj in range(8):
                j = b8 * 8 + jj
                if j < 4:
                    rows = list(range(0, 12)); bidx = j
                elif j > 60:
                    rows = list(range(60, 72)); bidx = 4 + (j - 61)
                else:
                    rows = list(range(j, j + 8)); bidx = None
                nr = len(rows)
                if bidx is not None:
                    dma(p, "sp", bbnd[0:64], c.bbnd[bidx], [], ["bbnd"])
                    btile, bkey = bbnd, "bbnd"
                else:
                    btile, bkey = bint, "bint"
                ob_ = j % 2
                okey = "psO%d" % ob_
                for h in range(6):
                    sb_ = it % 2
                    it += 1
                    skey = "psS%d" % sb_

                    def fqk(e, h=h, sb_=sb_, rows=rows, jj=jj, s=s, b8=b8):
                        ins = None
                        for i, lr in enumerate(rows):
                            ins = e.matmul(psS[sb_][0:64, i * 64:(i + 1) * 64],
                                           Kb[s][0:64, h, (lr - b8 * 8) * 64:(lr - b8 * 8 + 1) * 64],
                                           Qb[s][0:64, h, jj * 64:(jj + 1) * 64], start=True, stop=True)
                        return ins
                    p.op("pe", fqk, [("Qb", s), ("Kb", s)], [skey])
                    p.op("dve", lambda e, h=h, sb_=sb_, nr=nr, btile=btile: e.scalar_tensor_tensor(
                        out=sc[sb_][0:64, 0:nr * 64], in0=psS[sb_][0:64, 0:nr * 64], scalar=0.125,
                        in1=btile[0:64, h, 0:nr, :].rearrange("p r q -> p (r q)"),
                        op0=ALU.mult, op1=ALU.add), [skey, bkey], [("sc", sb_)])
                    p.op("act", lambda e, sb_=sb_, nr=nr: e.activation(
                        out=pp[sb_][0:64, 0:nr * 64], in_=sc[sb_][0:64, 0:nr * 64], func=AF.Exp),
                        [("sc", sb_)], [("pp", sb_)])

                    def fpv(e, h=h, sb_=sb_, rows=rows, ob_=ob_, s=s, b8=b8):
                        ins = None
                        n = len(rows)
                        for i, lr in enumerate(rows):
                            ins = e.matmul(psO[ob_][0:65, h * 64:(h + 1) * 64],
                                           Vb[s][0:64, lr - b8 * 8, h * 65:(h + 1) * 65],
                                           pp[sb_][0:64, i * 64:(i + 1) * 64],
                                           start=(i == 0), stop=(i == n - 1))
                        return ins
                    p.op("pe", fpv, [("Vb", s), ("pp", sb_)], [okey])
                _normalize(c, psO[ob_], okey, 384, rsb, "rsb", psB, ou, "ou", onesf,
                           ot[s][0:64, :, jj * 64:(jj + 1) * 64],
                           ("ot", s), nh=6)
            dma(p, "sp", c.s["oT"][0:6, :, b8 * 512:(b8 + 1) * 512].rearrange("h d t -> d h t"),
                ot[s][0:64, :, :], [("ot", s)], [("d_oT_na", b8)])
        p.barrier()
        p.emit()


def layer_norm_tile(c, tsb, tkey, junk, jkey, sm, smkey, g_t, b_t, gbkey, out_t, outkey):
    p = c.p
    p.op("act", lambda e: e.activation(out=junk[:], in_=tsb[:], func=AF.Copy, accum_out=sm[:, 0:1]),
         [tkey], [jkey, smkey])
    p.op("act", lambda e: e.activation(out=junk[:], in_=tsb[:], func=AF.Square, accum_out=sm[:, 1:2]),
         [tkey], [jkey, smkey])
    p.op("dve", lambda e: e.tensor_scalar(out=sm[:, 2:3], in0=sm[:, 0:1], scalar1=1.0 / D, scalar2=None,
                                          op0=ALU.mult), [smkey], [smkey])
    p.op("dve", lambda e: e.tensor_tensor(out=sm[:, 3:4], in0=sm[:, 2:3], in1=sm[:, 2:3], op=ALU.mult),
         [smkey], [smkey])
    p.op("dve", lambda e: e.scalar_tensor_tensor(out=sm[:, 4:5], in0=sm[:, 1:2], scalar=1.0 / D,
                                                 in1=sm[:, 3:4], op0=ALU.mult, op1=ALU.subtract),
         [smkey], [smkey])
    p.op("act", lambda e: e.activation(out=sm[:, 5:6], in_=sm[:, 4:5], func=AF.Sqrt,
                                       bias=c.eps_ln[:, 0:1]), [smkey], [smkey])
    p.op("dve", lambda e: e.reciprocal(out=sm[:, 6:7], in_=sm[:, 5:6]), [smkey], [smkey])
    p.op("dve", lambda e: e.tensor_scalar(out=junk[:], in0=tsb[:], scalar1=sm[:, 2:3], scalar2=sm[:, 6:7],
                                          op0=ALU.subtract, op1=ALU.mult), [tkey, smkey], [jkey])
    p.op("pool", lambda e: e.tensor_tensor(out=junk[:], in0=junk[:], in1=g_t[:], op=ALU.mult),
         [jkey, gbkey], [jkey])
    p.op("pool", lambda e: e.tensor_tensor(out=out_t[:], in0=junk[:], in1=b_t[:], op=ALU.add),
         [jkey, gbkey], [outkey])


def phase_merge(c, L):
    nc, p = c.nc, c.p
    with ExitStack() as ph:
        c.ph = ph
        wb = _sb(c, "wb", [128, 16, D], BF16)
        woutb = _sb(c, "woutb", [128, 8, D], BF16)
        lng = _sb(c, "lng", [128, D], F32)
        lnb = _sb(c, "lnb", [128, D], F32)
        wr = _sb(c, "wr", [128, 8, NEXP], F32)
        br = _sb(c, "br", [128, NEXP], F32)
        identb = _sb(c, "identb", [128, 128], BF16)
        identf = _sb(c, "identf", [128, 128], F32)
        oTb = [_sb(c, "oTb%d" % i, [128, 16, 512], BF16) for i in range(2)]
        gTc = [_sb(c, "gTc%d" % i, [128, 3, 512], BF16) for i in range(2)]
        mixT = [_sb(c, "mixT%d" % i, [128, 8, 512], BF16) for i in range(2)]
        mt = [_sb(c, "mt%d" % i, [128, 512], F32) for i in range(6)]
        xo = [_sb(c, "xo%d" % i, [128, D], F32) for i in range(2)]
        tsb = [_sb(c, "tsb%d" % i, [128, D], F32) for i in range(2)]
        junk = _sb(c, "junk", [128, D], F32)
        x1t = [_sb(c, "x1t%d" % i, [128, D], F32) for i in range(2)]
        x1b = [_sb(c, "x1b%d" % i, [128, D], BF16) for i in range(2)]
        x1Tb = [_sb(c, "x1Tb%d" % i, [128, 8, 128], BF16) for i in range(2)]
        x1Tf = [_sb(c, "x1Tf%d" % i, [128, 8, 128], F32) for i in range(2)]
        sm = [_sb(c, "sm%d" % i, [128, 8], F32) for i in range(2)]
        rt_ = [_sb(c, "rt%d" % i, [128, 160], F32) for i in range(2)]
        gTt = [_sb(c, "gTt%d" % i, [128, 128], F32) for i in range(2)]
        psY = [_ps(c, "psY%d" % i, [128, 512], F32) for i in range(3)]
        psOut = [_ps(c, "psOut%d" % i, [128, 512], F32) for i in range(2)]
        psT = _ps(c, "psT", [128, 8, 128], BF16)
        psTf = _ps(c, "psTf", [128, 8, 128], F32)
        triu = _sb(c, "triu", [128, 128], F32)
        ones128 = _sb(c, "ones128", [128, 128], F32)
        iota_e = _sb(c, "iota_e", [128, NEXP], F32)
        iota_cap = _sb(c, "iota_cap", [128, NEXP], F32)
        msum = _sb(c, "msum", [128, NEXP], F32)
        rx = [_sb(c, "rx%d" % i, [128, 160], F32) for i in range(2)]
        idxu = [_sb(c, "idxu%d" % i, [128, 8], U32) for i in range(2)]
        slotu = [_sb(c, "slotu%d" % i, [128, 4], I32) for i in range(2)]
        dma(p, "sp", triu[:], c.w["triu"][:, :], [], ["triu"])
        dma(p, "sp", ones128[:], c.w["ones128"][:, :], [], ["ones128"])
        dma(p, "sp", iota_e[:], c.w["iota_e"][:, :], [], ["iota_e"])
        dma(p, "sp", iota_cap[:], c.w["iota_cap"][:, :], [], ["iota_cap"])
        p.op("pool", lambda e: e.memset(msum[:], 0.0), [], ["msum"])

        for h in range(16):
            src = (c.w["w_ba"][L][h * 64:(h + 1) * 64, :] if h < 6 else
                   c.w["w_bb"][L][(h - 6) * 64:(h - 5) * 64, :] if h < 12 else
                   c.w["w_bc"][L][(h - 12) * 64:(h - 11) * 64, :])
            dma(p, "pool", wb[0:64, h, :], src, [], ["wb"])
        wo_v = c.w["w_out"][L].rearrange("(c p) n -> p c n", p=128)
        for ch in range(8):
            dma(p, "pool", woutb[:, ch, :], wo_v[:, ch, :], [], ["woutb"])
        dma(p, "sp", lng[:], c.w["ln1g"][L], [], ["lngb"])
        dma(p, "sp", lnb[:], c.w["ln1b"][L], [], ["lngb"])
        dma(p, "sp", wr[:], c.w["w_router"][L].rearrange("(c p) e -> p c e", p=128), [], ["wr"])
        dma(p, "sp", br[:], c.w["brouter"][L], [], ["br"])
        dma(p, "sp", identb[:], c.w["identb"][:, :], [], ["identb"])
        dma(p, "sp", identf[:], c.w["identf"][:, :], [], ["identf"])
        gT_v = c.s["gT"].rearrange("(i c p) t -> p i c t", i=3, c=8, p=128)
        x1T_v = c.s["x1T"].rearrange("(c p) t -> p c t", p=128)
        gcnt = 0
        for blk in range(8):
            s = blk % 2
            for hv in range(2):
                dma(p, "sp", oTb[s][0:64, hv * 8:(hv + 1) * 8, :],
                    c.s["oT"][hv * 8:(hv + 1) * 8, :, blk * 512:(blk + 1) * 512].rearrange("h d t -> d h t"),
                    [], [("oTb", s)])
            for dc in range(8):
                gs = gcnt % 2
                gcnt += 1
                dma(p, "sp", gTc[gs][:], gT_v[:, :, dc, blk * 512:(blk + 1) * 512], [], [("gTc", gs)])
                for i, (h0, nh) in enumerate(((0, 6), (6, 6), (12, 4))):
                    mm_group(p, psY[i][:, :],
                             [(wb[0:64, h0 + k, dc * 128:(dc + 1) * 128], oTb[s][0:64, h0 + k, :])
                              for k in range(nh)], ["wb", ("oTb", s)], ["psY%d" % i])
                m3 = [(gcnt * 3 + i) % 6 for i in range(3)]
                for i in range(3):
                    p.op("dve", lambda e, i=i, gs=gs, m=m3[i]: e.tensor_tensor(
                        out=mt[m][:], in0=psY[i][:, :], in1=gTc[gs][:, i, :], op=ALU.mult),
                        ["psY%d" % i, ("gTc", gs)], [("mt", m3[i])])
                p.op("pool", lambda e, m3=m3: e.tensor_tensor(out=mt[m3[0]][:], in0=mt[m3[0]][:],
                                                              in1=mt[m3[1]][:], op=ALU.add),
                     [("mt", m3[0]), ("mt", m3[1])], [("mt", m3[0])])
                p.op("pool", lambda e, m3=m3, s=s, dc=dc: e.tensor_tensor(
                    out=mixT[s][:, dc, :], in0=mt[m3[0]][:], in1=mt[m3[2]][:], op=ALU.add),
                    [("mt", m3[0]), ("mt", m3[2])], [("mixT", s)])
            for tt in range(4):
                t = blk * 4 + tt
                ts_ = t % 2
                dma(p, "sp", xo[ts_][:], c.q_src[t * 128:(t + 1) * 128, :], [], [("xo", ts_)])
                for half in range(2):
                    mm_group(p, psOut[half][:, :],
                             [(mixT[s][:, dc, tt * 128:(tt + 1) * 128], woutb[:, dc, half * 512:(half + 1) * 512])
                              for dc in range(8)], [("mixT", s), "woutb"], ["psOut%d" % half])
                    p.op("dve", lambda e, half=half, ts_=ts_: e.scalar_tensor_tensor(
                        out=tsb[ts_][:, half * 512:(half + 1) * 512], in0=xo[ts_][:, half * 512:(half + 1) * 512],
                        scalar=DN_ALPHA, in1=psOut[half][:, :], op0=ALU.mult, op1=ALU.add),
                        [("xo", ts_), "psOut%d" % half], [("tsb", ts_)])
                layer_norm_tile(c, tsb[ts_], ("tsb", ts_), junk, "junk", sm[ts_], ("sm", ts_),
                                lng, lnb, "lngb", x1t[ts_], ("x1t", ts_))
                dma(p, "sp", c.s["x1"][t * 128:(t + 1) * 128, :], x1t[ts_][:], [("x1t", ts_)], [("d_x1", t)])
                p.op("act", lambda e, ts_=ts_: e.activation(out=x1b[ts_][:], in_=x1t[ts_][:], func=AF.Copy),
                     [("x1t", ts_)], [("x1b", ts_)])
                tr_group(p, [(psT[:, ch, :], x1b[ts_][:, ch * 128:(ch + 1) * 128]) for ch in range(8)],
                         identb[:], [("x1b", ts_), "identb"], ["psT"])
                p.op("dve", lambda e, ts_=ts_: e.tensor_copy(x1Tb[ts_][:], psT[:]), ["psT"], [("x1Tb", ts_)])
                for hv in range(2):
                    dma(p, "sp", x1T_v[:, hv * 4:(hv + 1) * 4, t * 128:(t + 1) * 128], x1Tb[ts_][:, hv * 4:(hv + 1) * 4, :],
                        [("x1Tb", ts_)], [("d_x1T", t, hv)])
                tr_group(p, [(psTf[:, ch, :], x1t[ts_][:, ch * 128:(ch + 1) * 128]) for ch in range(8)],
                         identf[:], [("x1t", ts_), "identf"], ["psTf"])
                p.op("act", lambda e, ts_=ts_: e.activation(out=x1Tf[ts_][:], in_=psTf[:], func=AF.Copy),
                     ["psTf"], [("x1Tf", ts_)])
                mm_group(p, psY[0][:, 0:NEXP], [(x1Tf[ts_][:, ch, :], wr[:, ch, :]) for ch in range(8)],
                         [("x1Tf", ts_), "wr"], ["psY0"])
                R = rt_[ts_]
                rk = ("rt", ts_)
                lg, mx, msk, ex, exm = R[:, 0:32], R[:, 32:40], R[:, 40:72], R[:, 72:104], R[:, 104:136]
                nm, ssum, rs = R[:, 136:137], R[:, 137:138], R[:, 138:139]
                p.op("dve", lambda e, lg=lg: e.tensor_tensor(out=lg, in0=psY[0][:, 0:NEXP], in1=br[:], op=ALU.add),
                     ["psY0", "br"], [rk])
                p.op("dve", lambda e, lg=lg, mx=mx: e.max(out=mx, in_=lg), [rk], [rk])
                p.op("dve", lambda e, lg=lg, mx=mx, msk=msk: e.tensor_scalar(
                    out=msk, in0=lg, scalar1=mx[:, 3:4], scalar2=None, op0=ALU.is_ge), [rk], [rk])
                p.op("dve", lambda e, mx=mx, nm=nm: e.tensor_scalar(
                    out=nm, in0=mx[:, 0:1], scalar1=-1.0, scalar2=None, op0=ALU.mult), [rk], [rk])
                p.op("act", lambda e, lg=lg, ex=ex, nm=nm: e.activation(out=ex, in_=lg, func=AF.Exp, bias=nm),
                     [rk], [rk])
                p.op("dve", lambda e, ex=ex, msk=msk, exm=exm: e.tensor_tensor(out=exm, in0=ex, in1=msk,
                                                                               op=ALU.mult), [rk], [rk])
                p.op("dve", lambda e, exm=exm, ssum=ssum: e.reduce_sum(out=ssum, in_=exm, axis=AX.X), [rk], [rk])
                p.op("dve", lambda e, ssum=ssum, rs=rs: e.reciprocal(out=rs, in_=ssum), [rk], [rk])
                p.op("dve", lambda e, exm=exm, rs=rs: e.tensor_scalar(
                    out=exm, in0=exm, scalar1=rs, scalar2=None, op0=ALU.mult), [rk], [rk])
                p.op("pe", lambda e, exm=exm: e.transpose(psY[1][0:32, 0:128], exm, identf[:]),
                     [rk, "identf"], ["psY1"])
                p.op("act", lambda e, ts_=ts_: e.activation(out=gTt[ts_][0:32, :], in_=psY[1][0:32, 0:128],
                                                            func=AF.Copy), ["psY1"], [("gTt", ts_)])
                dma(p, "sp", c.s["gateT"][:, t * 128:(t + 1) * 128], gTt[ts_][0:32, :],
                    [("gTt", ts_)], [("d_gateT", t)])
                X = rx[ts_]
                xk = ("rx", ts_)
                idxf, sv, ov, eq, tmp = X[:, 0:8], X[:, 8:40], X[:, 40:72], X[:, 72:104], X[:, 104:136]
                slotf, gkf = X[:, 136:140], X[:, 140:144]
                p.op("dve", lambda e, ts_=ts_, lg=lg, mx=mx: e.max_index(out=idxu[ts_][:], in_max=mx, in_values=lg),
                     [rk], [("idxu", ts_)])
                p.op("dve", lambda e, ts_=ts_, idxf=idxf: e.tensor_copy(idxf, idxu[ts_][:]),
                     [("idxu", ts_)], [xk])
                p.op("pe", lambda e, msk=msk: e.matmul(psY[2][:, 0:NEXP], triu[:], msk, start=True, stop=False),
                     [rk, "triu"], ["psY2"])
                p.op("pe", lambda e: e.matmul(psY[2][:, 0:NEXP], ones128[:], msum[:], start=False, stop=True),
                     ["msum", "ones128"], ["psY2"])
                p.op("dve", lambda e, ov=ov: e.tensor_scalar(out=ov, in0=psY[2][:, 0:NEXP], scalar1=float(CAP),
                                                             scalar2=1.0e6, op0=ALU.is_ge, op1=ALU.mult),
                     ["psY2"], [xk])
                p.op("dve", lambda e, sv=sv: e.tensor_tensor(out=sv, in0=psY[2][:, 0:NEXP], in1=iota_cap[:],
                                                             op=ALU.add), ["psY2", "iota_cap"], [xk])
                p.op("dve", lambda e, sv=sv, ov=ov: e.tensor_tensor(out=sv, in0=sv, in1=ov, op=ALU.add), [xk], [xk])
                p.op("pool", lambda e, msk=msk: e.tensor_tensor(out=msum[:], in0=msum[:], in1=msk, op=ALU.add),
                     [rk, "msum"], ["msum"])
                for k4 in range(4):
                    p.op("dve", lambda e, eq=eq, idxf=idxf, k4=k4: e.tensor_scalar(
                        out=eq, in0=iota_e[:], scalar1=idxf[:, k4:k4 + 1], scalar2=None, op0=ALU.is_equal),
                        [xk, "iota_e"], [xk])
                    p.op("dve", lambda e, eq=eq, sv=sv, tmp=tmp: e.tensor_tensor(out=tmp, in0=eq, in1=sv, op=ALU.mult),
                         [xk], [xk])
                    p.op("dve", lambda e, tmp=tmp, slotf=slotf, k4=k4: e.reduce_sum(
                        out=slotf[:, k4:k4 + 1], in_=tmp, axis=AX.X), [xk], [xk])
                    p.op("dve", lambda e, eq=eq, exm=exm, tmp=tmp: e.tensor_tensor(out=tmp, in0=eq, in1=exm,
                                                                                   op=ALU.mult), [xk, rk], [xk])
                    p.op("dve", lambda e, tmp=tmp, gkf=gkf, k4=k4: e.reduce_sum(
                        out=gkf[:, k4:k4 + 1], in_=tmp, axis=AX.X), [xk], [xk])
                p.op("dve", lambda e, ts_=ts_, slotf=slotf: e.tensor_copy(slotu[ts_][:], slotf),
                     [xk], [("slotu", ts_)])
                for k4 in range(4):
                    p.op("pool", lambda e, ts_=ts_, k4=k4: e.indirect_dma_start(
                        out=c.s["xg"][:, :], out_offset=bass.IndirectOffsetOnAxis(ap=slotu[ts_][:, k4:k4 + 1], axis=0),
                        in_=x1b[ts_][:], in_offset=None, bounds_check=c.breg, oob_is_err=False),
                        [("slotu", ts_), ("x1b", ts_)], [("d_xg", t, k4)], dma=True)
                dma(p, "sp", c.s["slots"][t * 128:(t + 1) * 128, :], slotu[ts_][:], [("slotu", ts_)], [("d_slots", t)])
                dma(p, "sp", c.s["gks"][t * 128:(t + 1) * 128, :], gkf, [xk], [("d_gks", t)])
        p.barrier()
        p.emit()


SIG_MAX = float(1.0 / (1.0 + np.exp(-1.702 * 7.0)))


def phase_moe(c, L):
    nc, p = c.nc, c.p
    with ExitStack() as ph:
        c.ph = ph
        wgu = [_sb(c, "wgu%d" % i, [128, 8, 2 * DEXP], BF16) for i in range(2)]
        wdn = [_sb(c, "wdn%d" % i, [128, 8, D], BF16) for i in range(2)]
        xT = _sb(c, "xTsb", [128, 8, 1024], BF16)
        gT = _sb(c, "gTsb", [128, 1024], F32)
        yacc = _sb(c, "yacc", [128, 8, D], F32)
        bgu = _sb(c, "bgu", [128, NEXP, 16], F32)
        bgs = _sb(c, "bgs", [128, NEXP, 8], F32)
        bdn = _sb(c, "bdn", [128, D], F32)
        identf = _sb(c, "identf", [128, 128], F32)
        sg = [_sb(c, "sg%d" % i, [128, 512], F32) for i in range(2)]
        gc = [_sb(c, "gc%d" % i, [128, 512], F32) for i in range(2)]
        uc = [_sb(c, "uc%d" % i, [128, 512], F32) for i in range(2)]
        t1 = [_sb(c, "t1%d" % i, [128, 512], F32) for i in range(2)]
        t2 = [_sb(c, "t2%d" % i, [128, 512], F32) for i in range(2)]
        aT = [_sb(c, "aT%d" % i, [128, 8, 512], BF16) for i in range(2)]
        yev = [_sb(c, "yev%d" % i, [128, 512], F32) for i in range(2)]
        psg = [_ps(c, "psg%d" % i, [128, 512], F32) for i in range(2)]
        psu = [_ps(c, "psu%d" % i, [128, 512], F32) for i in range(2)]
        psGb = _ps(c, "psGb", [128, 512], F32)
        psy = [_ps(c, "psy%d" % i, [128, 512], F32) for i in range(2)]

        dma(p, "sp", bgu[:], c.w["bgu_t"][L], [], ["bgu"])
        dma(p, "sp", bdn[0:32, :], c.w["bdn"][L], [], ["bdn"])
        dma(p, "sp", identf[:], c.w["identf"][:, :], [], ["identf"])
        p.op("dve", lambda e: e.tensor_scalar(out=bgs[:], in0=bgu[:, :, 0:8], scalar1=1.702, scalar2=None,
                                              op0=ALU.mult), ["bgu"], ["bgs"])
        x1T_v = c.s["x1T"].rearrange("(c p) t -> p c t", p=128)
        ym_v = c.s["ymoe"].rearrange("(t p) d -> p t d", p=128)
        cnt = 0
        ycnt = 0
        for sb in range(4):
            dma(p, "sp", xT[:], x1T_v[:, :, sb * 1024:(sb + 1) * 1024], [], ["xTsb"])
            dma(p, "sp", gT[0:32, :], c.s["gateT"][:, sb * 1024:(sb + 1) * 1024], [], ["gTsb"])
            for ex in range(NEXP):
                ws = ex % 2
                gu_v = c.w["w_gu"][L, ex].rearrange("(c p) n -> p c n", p=128)
                dn_v = c.w["w_dn"][L, ex].rearrange("(c p) n -> p c n", p=128)
                for ch in range(8):
                    dma(p, "pool", wgu[ws][:, ch, :], gu_v[:, ch, :], [], [("wgu", ws)])
                for ch in range(8):
                    dma(p, "pool", wdn[ws][:, ch, :], dn_v[:, ch, :], [], [("wdn", ws)])
                for tb in range(2):
                    as_ = (ex * 2 + tb) % 2
                    p.op("pe", lambda e, ex=ex, tb=tb: e.matmul(
                        psGb[:, :], identf[0:32, ex:ex + 1].to_broadcast([32, 128]),
                        gT[0:32, tb * 512:(tb + 1) * 512], start=True, stop=True),
                        ["identf", "gTsb"], ["psGb"])
                    for j in range(8):
                        k = cnt % 2
                        cnt += 1
                        mm_group(p, psg[k][:, :],
                                 [(wgu[ws][:, ch, j * 128:(j + 1) * 128], xT[:, ch, tb * 512:(tb + 1) * 512])
                                  for ch in range(8)], [("wgu", ws), "xTsb"], ["psg%d" % k])
                        mm_group(p, psu[k][:, :],
                                 [(wgu[ws][:, ch, DEXP + j * 128:DEXP + (j + 1) * 128],
                                   xT[:, ch, tb * 512:(tb + 1) * 512]) for ch in range(8)],
                                 [("wgu", ws), "xTsb"], ["psu%d" % k])
                        p.op("dve", lambda e, k=k, ex=ex, j=j: e.tensor_scalar(
                            out=gc[k][:], in0=psg[k][:, :], scalar1=bgu[:, ex, j:j + 1], scalar2=7.0,
                            op0=ALU.add, op1=ALU.min), ["psg%d" % k, "bgu"], [("gc", k)])
                        p.op("act", lambda e, k=k: e.activation(
                            out=sg[k][:], in_=gc[k][:], func=AF.Sigmoid, scale=1.702),
                            [("gc", k)], [("sg", k)])
                        p.op("dve", lambda e, k=k, ex=ex, j=j: e.tensor_scalar(
                            out=uc[k][:], in0=psu[k][:, :], scalar1=bgu[:, ex, 8 + j:9 + j], scalar2=7.0,
                            op0=ALU.add, op1=ALU.min), ["psu%d" % k, "bgu"], [("uc", k)])
                        p.op("dve", lambda e, k=k: e.tensor_scalar(
                            out=uc[k][:], in0=uc[k][:], scalar1=-7.0, scalar2=1.0,
                            op0=ALU.max, op1=ALU.add), [("uc", k)], [("uc", k)])
                        p.op("pool", lambda e, k=k: e.tensor_tensor(
                            out=t1[k][:], in0=sg[k][:], in1=gc[k][:], op=ALU.mult),
                            [("sg", k), ("gc", k)], [("t1", k)])
                        p.op("dve", lambda e, k=k: e.tensor_tensor(out=t2[k][:], in0=uc[k][:], in1=psGb[:, :],
                                                                   op=ALU.mult), [("uc", k), "psGb"], [("t2", k)])
                        p.op("pool", lambda e, k=k, j=j, as_=as_: e.tensor_tensor(
                            out=aT[as_][:, j, :], in0=t1[k][:], in1=t2[k][:], op=ALU.mult),
                            [("t1", k), ("t2", k)], [("aT", as_)])
                    for tt in range(4):
                        tile = tb * 4 + tt
                        for half in range(2):
                            yk = ycnt % 2
                            ycnt += 1
                            pairs = [(aT[as_][:, j, tt * 128:(tt + 1) * 128],
                                      wdn[ws][:, j, half * 512:(half + 1) * 512]) for j in range(8)]
                            rds = [("aT", as_), ("wdn", ws)]
                            if ex == 0:
                                pairs.append((gT[0:32, tile * 128:(tile + 1) * 128],
                                              bdn[0:32, half * 512:(half + 1) * 512]))
                                rds += ["gTsb", "bdn"]
                            mm_group(p, psy[yk][:, :], pairs, rds, ["psy%d" % yk])
                            ya = yacc[:, tile, half * 512:(half + 1) * 512]
                            if ex == 0:
                                p.op("act", lambda e, ya=ya, yk=yk: e.activation(out=ya, in_=psy[yk][:, :],
                                                                                 func=AF.Copy),
                                     ["psy%d" % yk], [("yacc", tile, half)])
                            else:
                                p.op("act", lambda e, yk=yk: e.activation(out=yev[yk][:], in_=psy[yk][:, :],
                                                                          func=AF.Copy),
                                     ["psy%d" % yk], [("yev", yk)])
                                p.op("pool", lambda e, ya=ya, yk=yk: e.tensor_tensor(out=ya, in0=ya, in1=yev[yk][:],
                                                                                     op=ALU.add),
                                     [("yev", yk), ("yacc", tile, half)], [("yacc", tile, half)])
            dma(p, "sp", ym_v[:, sb * 8:(sb + 1) * 8, :], yacc[:],
                [("yacc", t_, h_) for t_ in range(8) for h_ in range(2)], [("d_ymoe", sb)])
        p.barrier()
        p.emit()


def load_w_cast(c, dst_ap, src_ap, stg, skey, dkey, eng="pool"):
    p = c.p
    dma(p, "sp", stg, src_ap, [], [skey])
    if eng == "act":
        p.op("act", lambda e: e.activation(out=dst_ap, in_=stg, func=AF.Copy), [skey], [dkey])
    else:
        p.op(eng, lambda e: e.tensor_copy(dst_ap, stg), [skey], [dkey])


def phase_moe_sparse(c, L):
    nc, p = c.nc, c.p
    NT = CAP // 128
    groups = [(0, 512), (512, CAP)] if CAP > 512 else [(0, CAP)]
    with ExitStack() as ph:
        c.ph = ph
        wgu = [_sb(c, "wgu%d" % i, [128, 8, 2 * DEXP], BF16) for i in range(2)]
        wdn = [_sb(c, "wdn%d" % i, [128, 8, D], BF16) for i in range(2)]
        stg = [_sb(c, "stg%d" % i, [128, 2 * DEXP], F32) for i in range(3)]
        xgt = [_sb(c, "xgt%d" % i, [128, D], BF16) for i in range(2)]
        xgT = [_sb(c, "xgT%d" % i, [128, 8, CAP], BF16) for i in range(2)]
        bgu = _sb(c, "bgu", [128, NEXP, 16], F32)
        bdr = [_sb(c, "bdr%d" % i, [128, D], F32) for i in range(2)]
        ones1 = _sb(c, "ones1", [128, 128], F32)
        identb = _sb(c, "identb", [128, 128], BF16)
        sg = [_sb(c, "sg%d" % i, [128, 512], F32) for i in range(2)]
        gc = [_sb(c, "gc%d" % i, [128, 512], F32) for i in range(2)]
        uc = [_sb(c, "uc%d" % i, [128, 512], F32) for i in range(2)]
        t1 = [_sb(c, "t1%d" % i, [128, 512], F32) for i in range(2)]
        aT = _sb(c, "aT", [128, 8, CAP], BF16)
        yout = [_sb(c, "yout%d" % i, [128, D], F32) for i in range(2)]
        psT = _ps(c, "psT", [128, 8, 128], BF16)
        psg = [_ps(c, "psg%d" % i, [128, 512], F32) for i in range(2)]
        psu = [_ps(c, "psu%d" % i, [128, 512], F32) for i in range(2)]
        psy = [_ps(c, "psy%d" % i, [128, 512], F32) for i in range(2)]
        dma(p, "sp", bgu[:], c.w["bgu_t"][L], [], ["bgu"])
        dma(p, "sp", identb[:], c.w["identb"][:, :], [], ["identb"])
        p.op("pool", lambda e: e.memset(ones1[:], 1.0), [], ["ones1"])
        st8 = {"scnt": 0, "cnt": 0, "ycnt": 0, "xcnt": 0}
        cast_rr = ("dve", "act", "pool")

        def wload_steps(ex):
            ws = ex % 2
            gu_v = c.w["w_gu"][L, ex].rearrange("(c p) n -> p c n", p=128)
            dn_v = c.w["w_dn"][L, ex].rearrange("(c p) n -> p c n", p=128)
            steps = []
            for ch in range(12):
                si = st8["scnt"] % 3
                eng = cast_rr[st8["scnt"] % 3]
                st8["scnt"] += 1
                if ch < 8:
                    src, stv, dstv, dkey = gu_v[:, ch, :], stg[si][:], wgu[ws][:, ch, :], ("wgu", ws)
                else:
                    c2 = ch - 8
                    src = dn_v[:, 2 * c2:2 * c2 + 2, :]
                    stv = stg[si][:].rearrange("p (c n) -> p c n", c=2)
                    dstv, dkey = wdn[ws][:, 2 * c2:2 * c2 + 2, :], ("wdn", ws)

                def f_dma(src=src, stv=stv, si=si):
                    dma(p, "sp", stv, src, [], [("stg", si)])

                def f_cast(stv=stv, dstv=dstv, si=si, dkey=dkey, eng=eng):
                    if eng == "act":
                        p.op("act", lambda e: e.activation(out=dstv, in_=stv, func=AF.Copy), [("stg", si)], [dkey])
                    else:
                        p.op(eng, lambda e: e.tensor_copy(dstv, stv), [("stg", si)], [dkey])
                steps.append((f_dma, f_cast))
            steps.append((lambda ws=ws, ex=ex: dma(p, "sp", bdr[ws][0:1, :], c.w["bdn"][L, ex:ex + 1, :], [],
                                                   [("bdr", ws)]), lambda: None))
            return steps

        def xg_load(ex):
            xb_ = ex % 2
            for st in range(NT):
                xs = st8["xcnt"] % 2
                st8["xcnt"] += 1
                r0 = ex * CAP + st * 128
                dma(p, "sp", xgt[xs][:], c.s["xg"][r0:r0 + 128, :], [], [("xgt", xs)])
                tr_group(p, [(psT[:, ch, :], xgt[xs][:, ch * 128:(ch + 1) * 128]) for ch in range(8)],
                         identb[:], [("xgt", xs), "identb"], ["psT"])
                p.op("act", lambda e, st=st, xb_=xb_: e.activation(
                    out=xgT[xb_][:, :, st * 128:(st + 1) * 128], in_=psT[:], func=AF.Copy), ["psT"], [("xgT", xb_)])

        pend_cast = None
        for f_dma, f_cast in wload_steps(0):
            f_dma()
            if pend_cast is not None:
                pend_cast()
            pend_cast = f_cast
        pend_cast()
        xg_load(0)
        for ex in range(NEXP):
            ws = ex % 2
            xb_ = ex % 2
            nxt_steps = wload_steps(ex + 1) if ex + 1 < NEXP else []
            pend_cast = None
            for (n0, n1) in groups:
                w = n1 - n0
                for j in range(8):
                    k = st8["cnt"] % 2
                    st8["cnt"] += 1
                    if nxt_steps:
                        f_dma, f_cast = nxt_steps.pop(0)
                        f_dma()
                        if pend_cast is not None:
                            pend_cast()
                        pend_cast = f_cast
                    mm_group(p, psg[k][:, 0:w], [(wgu[ws][:, ch, j * 128:(j + 1) * 128], xgT[xb_][:, ch, n0:n1])
                                                 for ch in range(8)], [("wgu", ws), ("xgT", xb_)], ["psg%d" % k])
                    mm_group(p, psu[k][:, 0:w], [(wgu[ws][:, ch, DEXP + j * 128:DEXP + (j + 1) * 128],
                                                  xgT[xb_][:, ch, n0:n1]) for ch in range(8)],
                             [("wgu", ws), ("xgT", xb_)], ["psu%d" % k])
                    p.op("dve", lambda e, k=k, ex=ex, j=j, w=w: e.tensor_scalar(
                        out=gc[k][:, 0:w], in0=psg[k][:, 0:w], scalar1=bgu[:, ex, j:j + 1], scalar2=7.0,
                        op0=ALU.add, op1=ALU.min), ["psg%d" % k, "bgu"], [("gc", k)])
                    p.op("act", lambda e, k=k, w=w: e.activation(out=sg[k][:, 0:w], in_=gc[k][:, 0:w],
                                                                 func=AF.Sigmoid, scale=1.702),
                         [("gc", k)], [("sg", k)])
                    p.op("dve", lambda e, k=k, ex=ex, j=j, w=w: e.tensor_scalar(
                        out=uc[k][:, 0:w], in0=psu[k][:, 0:w], scalar1=bgu[:, ex, 8 + j:9 + j], scalar2=7.0,
                        op0=ALU.add, op1=ALU.min), ["psu%d" % k, "bgu"], [("uc", k)])
                    p.op("dve", lambda e, k=k, w=w: e.tensor_scalar(
                        out=uc[k][:, 0:w], in0=uc[k][:, 0:w], scalar1=-7.0, scalar2=1.0,
                        op0=ALU.max, op1=ALU.add), [("uc", k)], [("uc", k)])
                    p.op("pool", lambda e, k=k, w=w: e.tensor_tensor(out=t1[k][:, 0:w], in0=sg[k][:, 0:w],
                                                                     in1=gc[k][:, 0:w], op=ALU.mult),
                         [("sg", k), ("gc", k)], [("t1", k)])
                    p.op("pool", lambda e, k=k, j=j, n0=n0, n1=n1, w=w: e.tensor_tensor(
                        out=aT[:, j, n0:n1], in0=t1[k][:, 0:w], in1=uc[k][:, 0:w], op=ALU.mult),
                        [("t1", k), ("uc", k)], ["aT"])
            while nxt_steps:
                f_dma, f_cast = nxt_steps.pop(0)
                f_dma()
                if pend_cast is not None:
                    pend_cast()
                pend_cast = f_cast
            if pend_cast is not None:
                pend_cast()
            if ex + 1 < NEXP:
                xg_load(ex + 1)
            for st in range(NT):
                ys = st8["ycnt"] % 2
                st8["ycnt"] += 1
                for half in range(2):
                    yk = (st8["ycnt"] + half) % 2
                    pairs = [(aT[:, j, st * 128:(st + 1) * 128], wdn[ws][:, j, half * 512:(half + 1) * 512])
                             for j in range(8)]
                    pairs.append((ones1[0:1, 0:128], bdr[ws][0:1, half * 512:(half + 1) * 512]))
                    mm_group(p, psy[yk][:, :], pairs, ["aT", ("wdn", ws), ("bdr", ws), "ones1"], ["psy%d" % yk])
                    p.op("act", lambda e, ys=ys, yk=yk, half=half: e.activation(
                        out=yout[ys][:, half * 512:(half + 1) * 512], in_=psy[yk][:, :], func=AF.Copy),
                        ["psy%d" % yk], [("yout", ys)])
                r0 = ex * CAP + st * 128
                dma(p, "sp", c.s["yg"][r0:r0 + 128, :], yout[ys][:], [("yout", ys)], [("d_yg", ex, st)])
        p.barrier()
        p.emit()


def phase_ln2(c, L, dst, sparse=True):
    nc, p = c.nc, c.p
    with ExitStack() as ph:
        c.ph = ph
        lng = _sb(c, "lng2", [128, D], F32)
        lnb = _sb(c, "lnb2", [128, D], F32)
        xa = [_sb(c, "xa%d" % i, [128, D], F32) for i in range(2)]
        ya = [_sb(c, "ya%d" % i, [128, D], F32) for i in range(2)]
        yk_ = [[_sb(c, "yk%d_%d" % (i, k), [128, D], F32) for k in range(4)] for i in range(2)]
        sl = [_sb(c, "sl%d" % i, [128, 4], I32) for i in range(2)]
        gk = [_sb(c, "gk%d" % i, [128, 4], F32) for i in range(2)]
        tsb = [_sb(c, "tsb%d" % i, [128, D], F32) for i in range(2)]
        out = [_sb(c, "out%d" % i, [128, D], F32) for i in range(2)]
        junk = _sb(c, "junk2", [128, D], F32)
        sm = [_sb(c, "sm%d" % i, [128, 8], F32) for i in range(2)]
        dma(p, "sp", lng[:], c.w["ln2g"][L], [], ["lngb"])
        dma(p, "sp", lnb[:], c.w["ln2b"][L], [], ["lngb"])
        for t in range(32):
            s = t % 2
            dma(p, "sp", xa[s][:], c.s["x1"][t * 128:(t + 1) * 128, :], [], [("xa", s)])
            if sparse:
                dma(p, "sp", sl[s][:], c.s["slots"][t * 128:(t + 1) * 128, :], [], [("sl", s)])
                dma(p, "sp", gk[s][:], c.s["gks"][t * 128:(t + 1) * 128, :], [], [("gk", s)])
                for k in range(4):
                    p.op("pool", lambda e, s=s, k=k: e.indirect_dma_start(
                        out=yk_[s][k][:], out_offset=None, in_=c.s["yg"][:, :],
                        in_offset=bass.IndirectOffsetOnAxis(ap=sl[s][:, k:k + 1], axis=0),
                        bounds_check=c.breg, oob_is_err=False), [("sl", s)], [("yk", s, k)], dma=True)
                p.op("dve", lambda e, s=s: e.tensor_scalar(out=ya[s][:], in0=yk_[s][0][:], scalar1=gk[s][:, 0:1],
                                                           scalar2=None, op0=ALU.mult),
                     [("yk", s, 0), ("gk", s)], [("ya", s)])
                for k in range(1, 4):
                    p.op("dve", lambda e, s=s, k=k: e.scalar_tensor_tensor(
                        out=ya[s][:], in0=yk_[s][k][:], scalar=gk[s][:, k:k + 1], in1=ya[s][:],
                        op0=ALU.mult, op1=ALU.add), [("yk", s, k), ("gk", s), ("ya", s)], [("ya", s)])
            else:
                dma(p, "sp", ya[s][:], c.s["ymoe"][t * 128:(t + 1) * 128, :], [], [("ya", s)])
            p.op("dve", lambda e, s=s: e.scalar_tensor_tensor(
                out=tsb[s][:], in0=xa[s][:], scalar=DN_ALPHA, in1=ya[s][:], op0=ALU.mult, op1=ALU.add),
                [("xa", s), ("ya", s)], [("tsb", s)])
            layer_norm_tile(c, tsb[s], ("tsb", s), junk, "junk", sm[s], ("sm", s), lng, lnb, "lngb",
                            out[s], ("out", s))
            dma(p, "sp", dst[t * 128:(t + 1) * 128, :], out[s][:], [("out", s)], [("d_out", t)])
        p.barrier()
        p.emit()


ALL_PHASES = ("p1", "na", "gqa", "mla", "merge", "moes", "ln2")
_NC_CACHE = {}


def kernel(**inputs):
    inp = {k: np.asarray(v) for k, v in inputs.items()}
    x = np.ascontiguousarray(inp["x"].astype(np.float32))
    nb = x.shape[0]
    if "nc" not in _NC_CACHE:
        _NC_CACHE["nc"] = build()
    consts = [prep_consts(hf) for hf in range(2)]
    wts = [prep_weights(inp, [0, 1], hf) for hf in range(2)]
    in_maps = []
    for cid in range(2 * nb):
        b, hf = cid // 2, cid % 2
        m = {}
        m.update(wts[hf])
        m.update(consts[hf])
        m.update(prep_acts(x[b], hf))
        in_maps.append(m)
    res = run_bass_kernel_spmd(_NC_CACHE["nc"], in_maps, core_ids=list(range(2 * nb)))
    return np.stack([np.concatenate([np.asarray(res.results[2 * b]["y"]),
                                     np.asarray(res.results[2 * b + 1]["y"])], 0)
                     for b in range(nb)], 0).astype(np.float32)
```

```python
from contextlib import ExitStack
import numpy as np
import ml_dtypes
import concourse.bass as bass
import concourse.mybir as mybir
from concourse.bass_utils import run_bass_kernel_spmd

F32 = mybir.dt.float32
BF16 = mybir.dt.bfloat16
I32 = mybir.dt.int32
U32 = mybir.dt.uint32
AF = mybir.ActivationFunctionType
ALU = mybir.AluOpType
AX = mybir.AxisListType

D = 1024
S = 8192
HALF = 4096
DIN = 5536
NEXP = 32
DEXP = 1024
LN_EPS = 1e-5
RMS_EPS = 1e-6
DN_ALPHA = 4.0 ** 0.25
NEG = -30000.0
NA_ROWS = 72

COMPUTE = ("pe", "act", "dve", "pool")


class Prog:
    def __init__(self, nc, stack, ring=8):
        self.nc = nc
        self.e = {"pe": nc.tensor, "act": nc.scalar, "dve": nc.vector,
                  "pool": nc.gpsimd, "sp": nc.sync}
        self.ops = []
        self.done = 0
        self.sem = {k: stack.enter_context(nc.semaphore("s_" + k)) for k in COMPUTE}
        self.ticket = {k: 0 for k in COMPUTE}
        self.ring = {q: [stack.enter_context(nc.semaphore("d_%s%d" % (q, i))) for i in range(ring)]
                     for q in ("sp", "pool")}
        self.ring_cnt = {q: [0] * ring for q in ("sp", "pool")}
        self.ring_last = {q: [None] * ring for q in ("sp", "pool")}
        self.ring_pos = {q: 0 for q in ("sp", "pool")}
        self.waited = {k: {} for k in self.e}
        self.last_w = {}
        self.readers = {}
        self.sig = {}
        self.eidx = {}
        self.ecount = {k: 0 for k in self.e}
        self.last_op = {k: None for k in self.e}
        self.dma_open = []

    def op(self, eng, fn, r=(), w=(), dma=False):
        self.ops.append((eng, fn, tuple(r), tuple(w), dma))

    def barrier(self):
        self.ops.append(("BAR", None, (), (), False))

    def _wait(self, eng, sem, val):
        cur = self.waited[eng].get(sem, 0)
        if cur < val:
            self.e[eng].wait_ge(sem, val)
            self.waited[eng][sem] = val

    def emit(self):
        ops = self.ops
        n = len(ops)
        start = self.done
        deps = {}
        needed = set()
        last_w, readers = self.last_w, self.readers
        eidx, ecount = self.eidx, self.ecount
        openg = {}
        for i in range(start, n):
            eng, fn, r, w, dma = ops[i]
            if eng == "BAR":
                continue
            eidx[i] = ecount[eng]
            ecount[eng] += 1
            openg[i] = (eng, dma)
            d = set()
            raw = set()
            for k in r:
                if k in last_w:
                    d.add(last_w[k])
                    raw.add(last_w[k])
                if isinstance(k, str) and k.startswith("ps"):
                    rd = readers.get(k)
                    if rd:
                        d.update(rd[0].values())
                        d.update(rd[1])
            for k in w:
                if k in last_w:
                    d.add(last_w[k])
                rd = readers.get(k)
                if rd:
                    d.update(rd[0].values())
                    d.update(rd[1])
            for k in r:
                rd = readers.setdefault(k, ({}, []))
                if dma:
                    rd[1].append(i)
                else:
                    rd[0][eng] = i
            for k in w:
                last_w[k] = i
                readers[k] = ({}, [])
            d.discard(i)
            keep = set()
            for j in d:
                if j < start and j not in self.sig and j not in openg:
                    continue
                jeng, jdma = self._opinfo(j, openg)
                if (not dma) and (not jdma) and jeng == eng:
                    if eng == "pe":
                        continue
                    if j in raw and eidx[i] - eidx[j] <= 2:
                        keep.add(j)
                    continue
                keep.add(j)
            deps[i] = keep
            needed.update(keep)
        self._openg_all = getattr(self, "_openg_all", {})
        self._openg_all.update(openg)
        for i in range(start, n):
            eng, fn, r, w, dma = ops[i]
            if eng == "BAR":
                self._emit_barrier()
                continue
            if dma:
                q = eng
                pos = self.ring_pos[q]
                self.ring_pos[q] = (pos + 1) % len(self.ring[q])
                sem = self.ring[q][pos]
                prev = self.ring_last[q][pos]
                if prev is not None:
                    self._wait(eng, sem, prev)
            for j in sorted(deps[i]):
                if j in self.sig:
                    s, v = self.sig[j]
                    self._wait(eng, s, v)
            ins = fn(self.e[eng])
            if dma:
                self.ring_cnt[q][pos] += 16
                val = self.ring_cnt[q][pos]
                ins.then_inc(sem, 16)
                self.ring_last[q][pos] = val
                self.sig[i] = (sem, val)
                self.dma_open.append(i)
            else:
                self.last_op[eng] = i
                if i in needed:
                    self.ticket[eng] += 1
                    ins.then_inc(self.sem[eng], 1)
                    self.sig[i] = (self.sem[eng], self.ticket[eng])
        self.done = n

    def _opinfo(self, j, openg):
        if j in openg:
            return openg[j]
        return self._openg_all[j]

    def _emit_barrier(self):
        marks = []
        for eng in COMPUTE:
            self.ticket[eng] += 1
            self.e[eng].drain().then_inc(self.sem[eng], 1)
            marks.append((self.sem[eng], self.ticket[eng]))
        dmas = [self.sig[i] for i in self.dma_open]
        self.dma_open = []
        for eng in self.e:
            for s, v in marks:
                self._wait(eng, s, v)
            for s, v in dmas:
                self._wait(eng, s, v)
        self.last_w.clear()
        self.readers.clear()


class Ctx:
    pass


_UID = [0]


def _sb(c, name, shape, dt):
    _UID[0] += 1
    return c.ph.enter_context(c.nc.sbuf_tensor("sb%d_%s" % (_UID[0], name), list(shape), dt))


def _ps(c, name, shape, dt):
    _UID[0] += 1
    return c.ph.enter_context(c.nc.psum_tensor("pp%d_%s" % (_UID[0], name), list(shape), dt))


def mm_group(p, out_ap, pairs, r, w):
    n = len(pairs)

    def fn(e):
        ins = None
        for i, (l, rr) in enumerate(pairs):
            ins = e.matmul(out_ap, l, rr, start=(i == 0), stop=(i == n - 1))
        return ins
    p.op("pe", fn, r, w)


def tr_group(p, outs_ins, ident, r, w):
    def fn(e):
        ins = None
        for o, i_ in outs_ins:
            ins = e.transpose(o, i_, ident)
        return ins
    p.op("pe", fn, r, w)


def dma(p, q, out_ap, in_ap, r, w):
    p.op(q, lambda e: e.dma_start(out=out_ap, in_=in_ap), r, w, dma=True)


def load_cast_cols(p, dst, src, ncols, r, w, step=2048):
    for c0 in range(0, ncols, step):
        c1 = min(ncols, c0 + step)
        dma(p, "pool", dst[:, c0:c1], src[:, c0:c1], r, w)


def phase1(c, L):
    nc, p = c.nc, c.p
    with ExitStack() as ph:
        c.ph = ph
        winb = _sb(c, "winb", [128, 8, DIN], BF16)
        wuqb = _sb(c, "wuqb", [128, 3, 384], BF16)
        wukvb = _sb(c, "wukvb", [128, 2, 512], BF16)
        identb = _sb(c, "identb", [128, 128], BF16)
        gq = _sb(c, "gq", [128, 384], F32)
        gk = _sb(c, "gk", [128, 128], F32)
        mq = _sb(c, "mq", [128, 384], F32)
        mkv = _sb(c, "mkv", [128, 256], F32)
        xf = [_sb(c, "xf%d" % i, [128, D], F32) for i in range(2)]
        xb = [_sb(c, "xb%d" % i, [128, D], BF16) for i in range(2)]
        xT = [_sb(c, "xT%d" % i, [128, 8, 512], BF16) for i in range(2)]
        rp = [_sb(c, "rp%d" % i, [128, 192], F32) for i in range(2)]
        wk = [_sb(c, "wk%d" % i, [128, 384], F32) for i in range(6)]
        sm = [_sb(c, "sm%d" % i, [128, 8], F32) for i in range(4)]
        ob = [_sb(c, "ob%d" % i, [128, 384], BF16) for i in range(4)]
        tT = [_sb(c, "tT%d" % i, [128, 512], BF16) for i in range(2)]
        blk = {k: [_sb(c, "blk_%s%d" % (k, i), [128, 6 * 512], BF16) for i in range(2)]
               for k in ("a", "b", "c", "d")}
        vna = [_sb(c, "vna%d" % i, [128, 6, 65], BF16) for i in range(2)]
        vg = [_sb(c, "vg%d" % i, [128, 2, 65], BF16) for i in range(2)]
        vm = [_sb(c, "vm%d" % i, [128, 4, 65], BF16) for i in range(2)]
        gout = [_sb(c, "gout%d" % i, [128, 512], BF16) for i in range(3)]
        psT = _ps(c, "psT", [128, 8, 128], BF16)
        psR = _ps(c, "psR", [128, 8, 128], BF16)
        psM = [_ps(c, "psM%d" % i, [128, 512], F32) for i in range(4)]
        psG = [_ps(c, "psG%d" % i, [128, 512], F32) for i in range(2)]

        w_in = c.w["w_in"][L]
        w_in_v = w_in.rearrange("(c p) n -> p c n", p=128)
        for ch in range(8):
            load_cast_cols(p, winb[:, ch, :], w_in_v[:, ch, :], DIN, [], ["winb"])
        wuq_v = c.w["w_uq"][L].rearrange("(c p) n -> p c n", p=128)
        for ch in range(3):
            load_cast_cols(p, wuqb[:, ch, :], wuq_v[:, ch, :], 384, [], ["wuqb"])
        wukv_v = c.w["w_ukv"][L].rearrange("(c p) n -> p c n", p=128)
        for ch in range(2):
            load_cast_cols(p, wukvb[:, ch, :], wukv_v[:, ch, :], 512, [], ["wukvb"])
        dma(p, "sp", identb[:], c.w["identb"][:, :], [], ["identb"])
        dma(p, "sp", gq[:], c.w["gq_rep"][L], [], ["gq"])
        dma(p, "sp", gk[:], c.w["gk_rep"][L], [], ["gk"])
        dma(p, "sp", mq[:], c.w["mq_rep"][L], [], ["mq"])
        dma(p, "sp", mkv[:], c.w["mkv_rep"][L], [], ["mkv"])
        for i in range(2):
            p.op("pool", lambda e, t=vna[i]: e.memset(t[:], 1.0), [], [("vna", i)])
            p.op("pool", lambda e, t=vg[i]: e.memset(t[:], 1.0), [], [("vg", i)])
            p.op("pool", lambda e, t=vm[i]: e.memset(t[:], 1.0), [], [("vm", i)])

        cnt = {"tile": 0, "psM": 0, "psG": 0, "wk": 0, "sm": 0, "ob": 0, "tT": 0, "gout": 0}
        pending = []

        def st(out_ap, in_ap, r, w):
            pending.append((out_ap, in_ap, r, w))

        def flush():
            for o_, i_, r_, w_ in pending:
                dma(p, "sp", o_, i_, r_, w_)
            del pending[:]

        def nxt(k, n):
            v = cnt[k] % n
            cnt[k] += 1
            return v

        def load_tile(src_ap, rope_ap, to_xT=None, mask_col=None):
            s = nxt("tile", 2)
            dma(p, "sp", xf[s][:], src_ap, [], [("xf", s)])
            if rope_ap is not None:
                dma(p, "sp", rp[s][:], rope_ap, [], [("rp", s)])
            if mask_col is not None:
                p.op("dve", lambda e: e.tensor_scalar(out=xf[s][:], in0=xf[s][:],
                                                      scalar1=c.hm[:, mask_col:mask_col + 1], scalar2=None,
                                                      op0=ALU.mult), [("xf", s), "hm"], [("xf", s)])
            p.op("pool", lambda e: e.tensor_copy(xb[s][:], xf[s][:]), [("xf", s)], [("xb", s)])
            tr_group(p, [(psT[:, ch, :], xb[s][:, ch * 128:(ch + 1) * 128]) for ch in range(8)],
                     identb[:], [("xb", s), "identb"], ["psT"])
            if to_xT is None:
                dst, key = tT_full[s][:], ("tTf", s)
            else:
                dst, key = to_xT
            p.op("act", lambda e: e.activation(out=dst, in_=psT[:], func=AF.Copy), ["psT"], [key])
            return dst, key, rp[s], ("rp", s)

        tT_full = [_sb(c, "tTf%d" % i, [128, 8, 128], BF16) for i in range(2)]

        def tok_mm(xTa, xTkey, c0, c1):
            b = nxt("psM", 4)
            mm_group(p, psM[b][:, 0:c1 - c0],
                     [(xTa[:, ch, :], winb[:, ch, c0:c1]) for ch in range(8)],
                     [xTkey, "winb"], ["psM%d" % b])
            return psM[b], "psM%d" % b

        def transp_out(src_bf, src_key, nh, dh, dst_ap, dst_key):
            tr_group(p, [(psR[0:dh, h, :], src_bf[:, h * dh:(h + 1) * dh]) for h in range(nh)],
                     identb[:], [src_key, "identb"], ["psR"])
            p.op("dve", lambda e: e.tensor_copy(dst_ap, psR[0:dh, 0:nh, :]), ["psR"], [dst_key])

        def rms_rope(ps_ap, pskey, nh, dh, g_ap, gkey, rope_t, rpkey, coff, out_bf, outkey):
            n = nh * dh
            hd = dh // 2
            a = nxt("wk", 6); b2 = nxt("wk", 6); c2 = nxt("wk", 6); d2 = nxt("wk", 6); e2 = nxt("wk", 6)
            s1 = nxt("sm", 4)
            A, B, C, Dd, E = wk[a], wk[b2], wk[c2], wk[d2], wk[e2]
            p.op("act", lambda e: e.activation(out=A[:, 0:n], in_=ps_ap, func=AF.Square),
                 [pskey], [("wk", a)])
            p.op("dve", lambda e: e.tensor_reduce(
                out=sm[s1][:, 0:nh], in_=A[:, 0:n].rearrange("p (h d) -> p h d", h=nh),
                axis=AX.X, op=ALU.add), [("wk", a)], [("sm", s1)])
            p.op("act", lambda e: e.activation(
                out=sm[s1][:, 0:nh], in_=sm[s1][:, 0:nh], func=AF.Sqrt, scale=1.0 / dh, bias=c.eps_rms[:, 0:1]),
                [("sm", s1)], [("sm", s1)])
            p.op("dve", lambda e: e.reciprocal(out=sm[s1][:, 0:nh], in_=sm[s1][:, 0:nh]),
                 [("sm", s1)], [("sm", s1)])
            p.op("dve", lambda e: e.tensor_tensor(
                out=B[:, 0:n].rearrange("p (h d) -> p h d", h=nh),
                in0=ps_ap.rearrange("p (h d) -> p h d", h=nh),
                in1=sm[s1][:, 0:nh].unsqueeze(2).to_broadcast([128, nh, dh]),
                op=ALU.mult), [pskey, ("sm", s1)], [("wk", b2)])
            p.op("pool", lambda e: e.tensor_tensor(out=C[:, 0:n], in0=B[:, 0:n], in1=g_ap, op=ALU.mult),
                 [("wk", b2), gkey], [("wk", c2)])
            rope(C, ("wk", c2), nh, dh, rope_t, rpkey, coff, Dd, ("wk", d2), E, ("wk", e2),
                 out_bf[:, 0:n].rearrange("p (h d) -> p h d", h=nh), outkey)

        def rope(X, xkey, nh, dh, rope_t, rpkey, coff, Dd, dkey, E, ekey, out3, outkey, xview=None):
            n = nh * dh
            hd = dh // 2
            x3 = xview if xview is not None else X[:, 0:n].rearrange("p (h d) -> p h d", h=nh)
            cs = rope_t[:, coff:coff + dh].unsqueeze(1).to_broadcast([128, nh, dh])
            sslo = rope_t[:, coff + dh:coff + dh + hd].unsqueeze(1).to_broadcast([128, nh, hd])
            sshi = rope_t[:, coff + dh + hd:coff + 2 * dh].unsqueeze(1).to_broadcast([128, nh, hd])
            d3 = Dd[:, 0:n].rearrange("p (h d) -> p h d", h=nh)
            e3 = E[:, 0:n].rearrange("p (h d) -> p h d", h=nh)
            p.op("pool", lambda e: e.tensor_tensor(out=d3, in0=x3, in1=cs, op=ALU.mult),
                 [xkey, rpkey], [dkey])
            p.op("dve", lambda e: e.tensor_tensor(out=e3[:, :, 0:hd], in0=x3[:, :, hd:dh], in1=sslo,
                                                  op=ALU.mult), [xkey, rpkey], [ekey])
            p.op("dve", lambda e: e.tensor_tensor(out=e3[:, :, hd:dh], in0=x3[:, :, 0:hd], in1=sshi,
                                                  op=ALU.mult), [xkey, rpkey, ekey], [ekey])
            p.op("pool", lambda e: e.tensor_tensor(out=out3, in0=d3, in1=e3, op=ALU.add),
                 [dkey, ekey], [outkey])

        def rms_full(ps_ap, pskey, n, g_ap, gkey, out_bf, outkey):
            a = nxt("wk", 6)
            s1 = nxt("sm", 4)
            p.op("act", lambda e: e.activation(out=wk[a][:, 0:n], in_=ps_ap, func=AF.Square,
                                               accum_out=sm[s1][:, 0:1]),
                 [pskey], [("wk", a), ("sm", s1)])
            p.op("act", lambda e: e.activation(
                out=sm[s1][:, 1:2], in_=sm[s1][:, 0:1], func=AF.Sqrt, scale=1.0 / n, bias=c.eps_rms[:, 0:1]),
                [("sm", s1)], [("sm", s1)])
            p.op("dve", lambda e: e.reciprocal(out=sm[s1][:, 2:3], in_=sm[s1][:, 1:2]),
                 [("sm", s1)], [("sm", s1)])
            p.op("dve", lambda e: e.scalar_tensor_tensor(
                out=out_bf, in0=ps_ap, scalar=sm[s1][:, 2:3], in1=g_ap,
                op0=ALU.mult, op1=ALU.mult), [pskey, ("sm", s1), gkey], [outkey])

        def na_kv(xTa, xTkey, tok_off, sub, bs):
            ps, pk = tok_mm(xTa, xTkey, 384, 768)
            o = nxt("ob", 4)
            p.op("act", lambda e, ps=ps, o=o: e.activation(out=ob[o][:, 0:384], in_=ps[:, 0:384],
                                                           func=AF.Copy), [pk], [("ob", o)])
            kb = blk["b"][bs][0:64, :].rearrange("p (h t) -> p h t", h=6)
            transp_out(ob[o], ("ob", o), 6, 64, kb[:, :, sub * 128:(sub + 1) * 128], ("blk_b", bs))
            ps, pk = tok_mm(xTa, xTkey, 768, 1152)
            s = nxt("tile", 2) if False else (cnt["tile"] - 1) % 2
            p.op("act", lambda e, ps=ps, s=s: e.activation(
                out=vna[s][:, :, 0:64], in_=ps[:, 0:384].rearrange("p (h d) -> p h d", h=6),
                func=AF.Copy), [pk], [("vna", s)])
            st(c.s["v_na"][tok_off:tok_off + 128, :], vna[s][:].rearrange("p h d -> p (h d)"),
                [("vna", s)], [("d_v_na", tok_off)])

        def flush_blk(name, bs, dh, nh, dst, t0, nt):
            src = blk[name][bs][0:dh, 0:nh * 512].rearrange("p (h t) -> p h t", h=nh)[:, :, 0:nt]
            st(dst[:, :, t0:t0 + nt].rearrange("h d t -> d h t"), src,
               [("blk_" + name, bs)], [("d_" + name, t0)])

        def own_load(t):
            bi_, sub_ = divmod(t, 4)
            return load_tile(c.q_src[t * 128:(t + 1) * 128, :], c.rope_q[t * 128:(t + 1) * 128, :],
                             to_xT=(xT[bi_ % 2][:, :, sub_ * 128:(sub_ + 1) * 128], ("xT", bi_ % 2)))
        nxt_h = own_load(0)
        for bi in range(8):
            bs = bi % 2
            for sub in range(4):
                t = bi * 4 + sub
                xTa, xTkey, rpt, rpk = nxt_h
                if t + 1 < 32:
                    nxt_h = own_load(t + 1)
                flush()
                ps, pk = tok_mm(xTa, xTkey, 0, 384)
                o = nxt("ob", 4)
                p.op("act", lambda e, o=o, ps=ps: e.activation(out=ob[o][:, 0:384], in_=ps[:, 0:384],
                                                               func=AF.Copy), [pk], [("ob", o)])
                qa = blk["a"][bs][0:64, :].rearrange("p (h t) -> p h t", h=6)
                transp_out(ob[o], ("ob", o), 6, 64, qa[:, :, sub * 128:(sub + 1) * 128], ("blk_a", bs))
                na_kv(xTa, xTkey, 256 + t * 128, sub, bs)
                ps, pk = tok_mm(xTa, xTkey, 1152, 1536)
                o = nxt("ob", 4)
                rms_rope(ps[:, 0:384], pk, 6, 64, gq[:], "gq", rpt, rpk, 0, ob[o], ("ob", o))
                qg = blk["c"][bs][0:64, :].rearrange("p (h t) -> p h t", h=6)
                transp_out(ob[o], ("ob", o), 6, 64, qg[:, :, sub * 128:(sub + 1) * 128], ("blk_c", bs))
                ps, pk = tok_mm(xTa, xTkey, 1792, 2176)
                o = nxt("ob", 4)
                rms_full(ps[:, 0:384], pk, 384, mq[:], "mq", ob[o][:, 0:384], ("ob", o))
                tt = nxt("tT", 2)
                tr_group(p, [(psR[:, ch, :], ob[o][:, ch * 128:(ch + 1) * 128]) for ch in range(3)],
                         identb[:], [("ob", o), "identb"], ["psR"])
                p.op("act", lambda e, tt=tt: e.activation(
                    out=tT[tt][:, 0:384].rearrange("p (c t) -> p c t", c=3), in_=psR[:, 0:3, :],
                    func=AF.Copy), ["psR"], [("tT", tt)])
                g = nxt("psG", 2)
                tT3 = tT[tt][:, 0:384].rearrange("p (c t) -> p c t", c=3)
                mm_group(p, psG[g][:, 0:384], [(tT3[:, ch, :], wuqb[:, ch, :]) for ch in range(3)],
                         [("tT", tt), "wuqb"], ["psG%d" % g])
                o2 = nxt("ob", 4)
                qc3 = psG[g][:, 0:384].rearrange("p (h d) -> p h d", h=4)
                out3 = ob[o2][:, 0:384].rearrange("p (h d) -> p h d", h=4)
                p.op("act", lambda e, qc3=qc3, out3=out3: e.activation(
                    out=out3[:, :, 0:64], in_=qc3[:, :, 0:64], func=AF.Copy),
                    ["psG%d" % g], [("ob", o2)])
                a = nxt("wk", 6); d2 = nxt("wk", 6); e2 = nxt("wk", 6)
                xr3 = wk[a][:, 0:128].rearrange("p (h d) -> p h d", h=4)
                p.op("act", lambda e, qc3=qc3, xr3=xr3: e.activation(out=xr3, in_=qc3[:, :, 64:96],
                                                                     func=AF.Copy),
                     ["psG%d" % g], [("wk", a)])
                rope(wk[a], ("wk", a), 4, 32, rpt, rpk, 128, wk[d2], ("wk", d2), wk[e2], ("wk", e2),
                     out3[:, :, 64:96], ("ob", o2))
                qm = blk["d"][bs][0:96, 0:2048].rearrange("p (h t) -> p h t", h=4)
                transp_out(ob[o2], ("ob", o2), 4, 96, qm[:, :, sub * 128:(sub + 1) * 128], ("blk_d", bs))
            flush_blk("a", bs, 64, 6, c.s["qT_na"], bi * 512, 512)
            flush_blk("b", bs, 64, 6, c.s["kT_na"], 256 + bi * 512, 512)
            flush_blk("c", bs, 64, 6, c.s["qT_gqa"], bi * 512, 512)
            flush_blk("d", bs, 96, 4, c.s["qT_mla"], bi * 512, 512)
            for cc in range(24):
                g = nxt("psG", 2)
                c0 = 2464 + cc * 128
                mm_group(p, psG[g][:, :], [(winb[:, ch, c0:c0 + 128], xT[bs][:, ch, :]) for ch in range(8)],
                         [("xT", bs), "winb"], ["psG%d" % g])
                go = nxt("gout", 3)
                p.op("act", lambda e, g=g, go=go: e.activation(out=gout[go][:], in_=psG[g][:, :],
                                                               func=AF.Sigmoid),
                     ["psG%d" % g], [("gout", go)])
                dma(p, "sp", c.s["gT"][cc * 128:(cc + 1) * 128, bi * 512:(bi + 1) * 512], gout[go][:],
                    [("gout", go)], [("d_gT", cc, bi)])

        for hi in range(4):
            hsrc = (c.c_src[HALF - 256 + hi * 128:HALF - 256 + (hi + 1) * 128, :] if hi < 2 else
                    c.c_src[(hi - 2) * 128:(hi - 1) * 128, :])
            xTa, xTkey, rpt, rpk = load_tile(hsrc, None, mask_col=c.hm_cols[0 if hi < 2 else 1])
            flush()
            tok_off = hi * 128 if hi < 2 else 4352 + (hi - 2) * 128
            na_kv(xTa, xTkey, tok_off, hi % 2, 0)
            if hi % 2 == 1:
                flush_blk("b", 0, 64, 6, c.s["kT_na"], 0 if hi == 1 else 4352, 256)

        def full_load(t):
            return load_tile(c.full_src[t * 128:(t + 1) * 128, :], c.rope_full[t * 128:(t + 1) * 128, :])
        flush()
        if c.do_full:
            nxt_h = full_load(0)
        for bi in range(16 if c.do_full else 0):
            bs = bi % 2
            for sub in range(4):
                t = bi * 4 + sub
                xTa, xTkey, rpt, rpk = nxt_h
                if t + 1 < 64:
                    nxt_h = full_load(t + 1)
                flush()
                s = (cnt["tile"] - 1) % 2
                ps, pk = tok_mm(xTa, xTkey, 1536, 1792)
                o = nxt("ob", 4)
                rms_rope(ps[:, 0:128], pk, 2, 64, gk[:], "gk", rpt, rpk, 0, ob[o], ("ob", o))
                kg = blk["a"][bs][0:64, 0:1024].rearrange("p (h t) -> p h t", h=2)
                transp_out(ob[o], ("ob", o), 2, 64, kg[:, :, sub * 128:(sub + 1) * 128], ("blk_a", bs))
                p.op("act", lambda e, ps=ps, s=s: e.activation(
                    out=vg[s][:, :, 0:64], in_=ps[:, 128:256].rearrange("p (h d) -> p h d", h=2),
                    func=AF.Copy), [pk], [("vg", s)])
                st(c.s["v_gqa"][t * 128:(t + 1) * 128, :], vg[s][:].rearrange("p h d -> p (h d)"),
                    [("vg", s)], [("d_v_gqa", t)])
                ps, pk = tok_mm(xTa, xTkey, 2176, 2464)
                o = nxt("ob", 4)
                rms_full(ps[:, 0:256], pk, 256, mkv[:], "mkv", ob[o][:, 0:256], ("ob", o))
                a = nxt("wk", 6)
                p.op("act", lambda e, ps=ps, a=a: e.activation(out=wk[a][:, 0:32], in_=ps[:, 256:288],
                                                               func=AF.Copy), [pk], [("wk", a)])
                tt = nxt("tT", 2)
                tr_group(p, [(psR[:, ch, :], ob[o][:, ch * 128:(ch + 1) * 128]) for ch in range(2)],
                         identb[:], [("ob", o), "identb"], ["psR"])
                tT2 = tT[tt][:, 0:256].rearrange("p (c t) -> p c t", c=2)
                p.op("act", lambda e, tT2=tT2: e.activation(out=tT2, in_=psR[:, 0:2, :], func=AF.Copy),
                     ["psR"], [("tT", tt)])
                g = nxt("psG", 2)
                mm_group(p, psG[g][:, :], [(tT2[:, ch, :], wukvb[:, ch, :]) for ch in range(2)],
                         [("tT", tt), "wukvb"], ["psG%d" % g])
                kv3 = psG[g][:, :].rearrange("p (h d) -> p h d", h=4)
                o2 = nxt("ob", 4)
                out3 = ob[o2][:, 0:384].rearrange("p (h d) -> p h d", h=4)
                p.op("act", lambda e, kv3=kv3, out3=out3: e.activation(
                    out=out3[:, :, 0:64], in_=kv3[:, :, 0:64], func=AF.Copy),
                    ["psG%d" % g], [("ob", o2)])
                p.op("dve", lambda e, kv3=kv3, s=s: e.tensor_copy(vm[s][:, :, 0:64], kv3[:, :, 64:128]),
                     ["psG%d" % g], [("vm", s)])
                st(c.s["v_mla"][t * 128:(t + 1) * 128, :], vm[s][:].rearrange("p h d -> p (h d)"),
                    [("vm", s)], [("d_v_mla", t)])
                d2 = nxt("wk", 6); e2 = nxt("wk", 6); f2 = nxt("wk", 6)
                kr3 = wk[f2][:, 0:32].rearrange("p (h d) -> p h d", h=1)
                rope(wk[a], ("wk", a), 1, 32, rpt, rpk, 128, wk[d2], ("wk", d2), wk[e2], ("wk", e2),
                     kr3, ("wk", f2))
                p.op("pool", lambda e, out3=out3, f2=f2: e.tensor_copy(
                    out3[:, :, 64:96], wk[f2][:, 0:32].unsqueeze(1).to_broadcast([128, 4, 32])),
                    [("wk", f2), ("ob", o2)], [("ob", o2)])
                km = blk["d"][bs][0:96, 0:2048].rearrange("p (h t) -> p h t", h=4)
                transp_out(ob[o2], ("ob", o2), 4, 96, km[:, :, sub * 128:(sub + 1) * 128], ("blk_d", bs))
            flush_blk("a", bs, 64, 2, c.s["kT_gqa"], bi * 512, 512)
            flush_blk("d", bs, 96, 4, c.s["kT_mla"], bi * 512, 512)
        flush()
        p.barrier()
        p.emit()


W_SHAPES = {
    "w_in": ([D, DIN], F32), "w_uq": ([384, 384], F32), "w_ukv": ([256, 512], F32),
    "w_ba": ([384, D], F32), "w_bb": ([384, D], F32), "w_bc": ([256, D], F32),
    "w_out": ([D, D], F32), "w_router": ([D, NEXP], F32),
    "w_gu": ([NEXP, D, 2 * DEXP], F32), "w_dn": ([NEXP, DEXP, D], F32),
    "gq_rep": ([128, 384], F32), "gk_rep": ([128, 128], F32),
    "mq_rep": ([128, 384], F32), "mkv_rep": ([128, 256], F32),
    "ln1g": ([128, D], F32), "ln1b": ([128, D], F32), "ln2g": ([128, D], F32), "ln2b": ([128, D], F32),
    "brouter": ([128, NEXP], F32), "bgu_t": ([128, NEXP, 16], F32), "bdn": ([NEXP, D], F32),
    "na_bint": ([64, 6, 8, 64], F32), "na_bbnd": ([7, 64, 6, 12, 64], F32),
    "na_bbnd_o": ([7, 64, 6, 12, 64], F32),
}
C_SHAPES = {
    "rope_loc": ([S, 192], F32), "hmask": ([128, 4], F32),
    "identb": ([128, 128], BF16), "identf": ([128, 128], F32),
    "triu": ([128, 128], F32), "ones128": ([128, 128], F32),
    "iota_e": ([128, NEXP], F32), "iota_cap": ([128, NEXP], F32),
}
CAP = 768
NSLOT = NEXP * CAP
S_SHAPES = {
    "qT_na": ([6, 64, HALF], BF16), "kT_na": ([6, 64, NA_ROWS * 64], BF16),
    "v_na": ([NA_ROWS * 64, 390], BF16),
    "qT_gqa": ([6, 64, HALF], BF16), "kT_gqa": ([2, 64, S], BF16), "v_gqa": ([S, 130], BF16),
    "qT_mla": ([4, 96, HALF], BF16), "kT_mla": ([4, 96, S], BF16), "v_mla": ([S, 260], BF16),
    "gT": ([3 * D, HALF], BF16),
    "oT": ([16, 64, HALF], BF16),
    "x1": ([HALF, D], F32),
    "x1T": ([D, HALF], BF16), "gateT": ([NEXP, HALF], F32), "ymoe": ([HALF, D], F32),
    "xg": ([NSLOT, D], BF16), "yg": ([NSLOT, D], F32),
    "slots": ([HALF, 4], I32), "gks": ([HALF, 4], F32),
}


def build(taps=(), passes=("A", "B", "C"), phases=None, dst_override=None):
    nc = bass.Bass("TRN2", target_bir_lowering=False)
    phases = phases or ALL_PHASES
    c = Ctx()
    c.nc = nc
    c.w = {}
    for k, (shp, dt) in W_SHAPES.items():
        c.w[k] = nc.dram_tensor(k, [2] + shp, dt, kind="ExternalInput").ap()
    for k, (shp, dt) in C_SHAPES.items():
        c.w[k] = nc.dram_tensor(k, shp, dt, kind="ExternalInput").ap()
    x_loc = nc.dram_tensor("x_loc", [S, D], F32, kind="ExternalInput").ap()
    c.s = {}
    for k, (shp, dt) in S_SHAPES.items():
        kind = "ExternalOutput" if k in taps else "Internal"
        c.s[k] = nc.dram_tensor("s_" + k, shp, dt, kind=kind).ap()
    y01 = nc.dram_tensor("y01", [S, D], F32, kind="Internal").ap()
    c.y = nc.dram_tensor("y", [HALF, D], F32, kind="ExternalOutput").ap()
    with ExitStack() as stack:
        c.p = Prog(nc, stack)
        c.breg = nc.gpsimd.to_reg(NSLOT - 1)
        c.eps_rms = stack.enter_context(nc.sbuf_tensor("eps_rms", [128, 1], F32))
        c.eps_ln = stack.enter_context(nc.sbuf_tensor("eps_ln", [128, 1], F32))
        c.hm = stack.enter_context(nc.sbuf_tensor("hmask_sb", [128, 4], F32))
        c.p.op("pool", lambda e: e.memset(c.eps_rms[:], RMS_EPS), [], ["eps"])
        c.p.op("pool", lambda e: e.memset(c.eps_ln[:], LN_EPS), [], ["eps"])
        dma(c.p, "sp", c.hm[:], c.w["hmask"][:, :], [], ["hm"])
        c.p.barrier()
        c.p.emit()
        rope = c.w["rope_loc"]
        c.rope_full = rope
        with ExitStack() as ph:
            zt = ph.enter_context(nc.sbuf_tensor("zt", [128, 8, D], BF16))
            c.p.op("pool", lambda e: e.memset(zt[:], 0.0), [], ["zt"])
            xg_v = c.s["xg"].rearrange("(n p) d -> p n d", p=128)
            for i in range(NSLOT // 1024):
                dma(c.p, "sp", xg_v[:, i * 8:(i + 1) * 8, :], zt[:], ["zt"], [("d_xg0", i)])
            c.p.barrier()
            c.p.emit()
        for ps_ in passes:
            if ps_ == "A":
                L, c.q_src, c.c_src, c.full_src = 0, x_loc[0:HALF, :], x_loc[HALF:S, :], x_loc
                c.rope_q, c.bbnd, c.hm_cols, c.do_full, dst = rope[0:HALF, :], c.w["na_bbnd"][0], (0, 1), True, y01[0:HALF, :]
            elif ps_ == "B":
                L, c.q_src, c.c_src, c.full_src = 0, x_loc[HALF:S, :], x_loc[0:HALF, :], x_loc
                c.rope_q, c.bbnd, c.hm_cols, c.do_full, dst = rope[HALF:S, :], c.w["na_bbnd_o"][0], (2, 3), False, y01[HALF:S, :]
            else:
                L, c.q_src, c.c_src, c.full_src = 1, y01[0:HALF, :], y01[HALF:S, :], y01
                c.rope_q, c.bbnd, c.hm_cols, c.do_full, dst = rope[0:HALF, :], c.w["na_bbnd"][1], (0, 1), True, c.y
            if "p1" in phases:
                phase1(c, L)
            if "na" in phases:
                phase_na(c, L)
            if "gqa" in phases:
                phase_dense_attn(c, L, "gqa")
            if "mla" in phases:
                phase_dense_attn(c, L, "mla")
            if "merge" in phases:
                phase_merge(c, L)
            if "moe" in phases:
                phase_moe(c, L)
            if "moes" in phases:
                phase_moe_sparse(c, L)
            if "ln2" in phases:
                phase_ln2(c, L, dst if dst_override is None else dst_override(c), sparse=("moes" in phases))
        c.p.barrier()
        c.p.emit()
    return nc


def rope_tables():
    t = np.arange(S)
    row = (t // 64).astype(np.float32)
    col = (t % 64).astype(np.float32)
    out = []
    for dim in (64, 32):
        quarter = dim // 4
        inv = (10000.0 ** (-np.arange(quarter, dtype=np.float32) / quarter)).astype(np.float32)
        ang = np.concatenate([row[:, None] * inv, col[:, None] * inv], -1).astype(np.float32)
        cs, sn = np.cos(ang).astype(np.float32), np.sin(ang).astype(np.float32)
        out.append(np.concatenate([cs, cs], -1))
        out.append(np.concatenate([-sn, sn], -1))
    return np.ascontiguousarray(np.concatenate(out, -1).astype(np.float32))


def na_bias_tables(rpb, hf):
    cols = np.arange(64)
    c0 = np.clip(cols - 8, 0, 48)
    in_win = (cols[None, :] >= c0[:, None]) & (cols[None, :] < c0[:, None] + 16)
    idx_c = np.clip(cols[None, :] - cols[:, None] + 15, 0, 30)

    def tab(j, rows):
        r = hf * 64 + j
        r0 = int(np.clip(r - 4, 0, 120))
        out = np.full((64, 6, len(rows), 64), NEG, np.float32)
        for ii, lr in enumerate(rows):
            gr = hf * 64 + lr - 4
            i = gr - r0
            if i < 0 or i >= 8 or gr < 0 or gr >= 128:
                continue
            ir = gr - r + 7
            b = rpb[:, ir][:, idx_c]
            b = np.where(in_win[None], b, NEG)
            out[:, :, ii, :] = b.transpose(2, 0, 1)
        return out
    interior = tab(10, list(range(10, 18)))
    bnd = []
    for j in (0, 1, 2, 3):
        bnd.append(tab(j, list(range(0, 12))))
    for j in (61, 62, 63):
        bnd.append(tab(j, list(range(60, 72))))
    return interior, np.stack(bnd, 0)


def prep_weights(inp, layers, hf):
    w = {}
    ls = list(layers)
    st = lambda f: np.ascontiguousarray(np.stack([f(l) for l in ls], 0))
    w["w_in"] = st(lambda l: inp["w_in"][l])
    w["w_uq"] = st(lambda l: inp["w_uq"][l])
    w["w_ukv"] = st(lambda l: inp["w_ukv"][l])
    w["w_ba"] = st(lambda l: inp["w_branch_a"][l])
    w["w_bb"] = st(lambda l: inp["w_branch_b"][l])
    w["w_bc"] = st(lambda l: inp["w_branch_c"][l])
    w["w_out"] = st(lambda l: inp["w_out"][l])
    w["w_router"] = st(lambda l: inp["w_router"][l])
    w["w_gu"] = st(lambda l: inp["w_gate_up"][l])
    w["w_dn"] = st(lambda l: inp["w_down"][l])
    w["gq_rep"] = st(lambda l: np.tile(inp["gqa_q_norm"][l][None, :], (128, 6)))
    w["gk_rep"] = st(lambda l: np.tile(inp["gqa_k_norm"][l][None, :], (128, 2)))
    w["mq_rep"] = st(lambda l: np.tile(inp["mla_q_norm"][l][None, :], (128, 1)))
    w["mkv_rep"] = st(lambda l: np.tile(inp["mla_kv_norm"][l][None, :], (128, 1)))
    for k, src in (("ln1g", "ln1_g"), ("ln1b", "ln1_b"), ("ln2g", "ln2_g"), ("ln2b", "ln2_b")):
        w[k] = st(lambda l: np.tile(inp[src][l][None, :], (128, 1)))
    w["brouter"] = st(lambda l: np.tile(inp["b_router"][l][None, :], (128, 1)))
    w["bgu_t"] = st(lambda l: inp["b_gate_up"][l].reshape(NEXP, 16, 128).transpose(2, 0, 1))
    w["bdn"] = st(lambda l: inp["b_down"][l])
    bi, bb = zip(*[na_bias_tables(inp["na_rpb"][l], hf) for l in ls])
    w["na_bint"] = np.ascontiguousarray(np.stack(bi, 0))
    w["na_bbnd"] = np.ascontiguousarray(np.stack(bb, 0))
    w["na_bbnd_o"] = np.ascontiguousarray(np.stack([na_bias_tables(inp["na_rpb"][l], 1 - hf)[1] for l in ls], 0))
    return {k: np.ascontiguousarray(v.astype(np.float32)) for k, v in w.items()}


def prep_consts(hf):
    rt = rope_tables()
    own, oth = rt[hf * HALF:(hf + 1) * HALF], rt[(1 - hf) * HALF:(2 - hf) * HALF]
    hm = np.zeros((128, 4), np.float32)
    hm[:, 0], hm[:, 1], hm[:, 2], hm[:, 3] = hf, 1 - hf, 1 - hf, hf
    return {
        "rope_loc": np.ascontiguousarray(np.concatenate([own, oth], 0)),
        "hmask": hm,
        "identb": np.eye(128, dtype=np.float32).astype(ml_dtypes.bfloat16),
        "identf": np.eye(128, dtype=np.float32),
        "triu": np.triu(np.ones((128, 128), np.float32), 1),
        "ones128": np.ones((128, 128), np.float32),
        "iota_e": np.tile(np.arange(NEXP, dtype=np.float32)[None, :], (128, 1)),
        "iota_cap": np.tile((np.arange(NEXP, dtype=np.float32) * CAP)[None, :], (128, 1)),
    }


def prep_acts(xb, hf):
    own, oth = xb[hf * HALF:(hf + 1) * HALF], xb[(1 - hf) * HALF:(2 - hf) * HALF]
    return {"x_loc": np.ascontiguousarray(np.concatenate([own, oth], 0))}


def _normalize(c, psO, okey, ncol, rsb, rkey, psB, ou, oukey, onesf, dst_ap, dst_key, nh=1):
    p = c.p
    p.op("dve", lambda e: e.reciprocal(out=rsb[64:65, 0:ncol], in_=psO[64:65, 0:ncol]), [okey], [rkey])
    p.op("pe", lambda e: e.matmul(psB[0:64, 0:ncol], onesf[64:65, 0:64], rsb[64:65, 0:ncol],
                                  start=True, stop=True), [rkey, "onesf"], ["psB"])
    p.op("act", lambda e: e.activation(out=ou[0:64, 0:ncol], in_=psO[0:64, 0:ncol], func=AF.Copy),
         [okey], [oukey])
    a0 = ou[0:64, 0:ncol]
    a1 = psB[0:64, 0:ncol]
    if nh > 1:
        a0 = a0.rearrange("p (h q) -> p h q", h=nh)
        a1 = a1.rearrange("p (h q) -> p h q", h=nh)
    p.op("dve", lambda e: e.tensor_tensor(out=dst_ap, in0=a0, in1=a1, op=ALU.mult),
         [oukey, "psB"], [dst_key])


def phase_dense_attn(c, L, kind):
    nc, p = c.nc, c.p
    if kind == "gqa":
        nH, dh, nK, nV, scale, obase = 6, 64, 2, 2, 64 ** -0.5, 6
        qT, kT, vS = c.s["qT_gqa"], c.s["kT_gqa"], c.s["v_gqa"]
        kmap = [0, 0, 0, 1, 1, 1]
    else:
        nH, dh, nK, nV, scale, obase = 4, 96, 4, 4, 96 ** -0.5, 12
        qT, kT, vS = c.s["qT_mla"], c.s["kT_mla"], c.s["v_mla"]
        kmap = [0, 1, 2, 3]
    with ExitStack() as ph:
        c.ph = ph
        KT = _sb(c, "KT", [128, nK, S], BF16)
        V = _sb(c, "V", [128, 64, nV * 65], BF16)
        Q = [_sb(c, "Q%d" % i, [128, nH, 512], BF16) for i in range(2)]
        pT = [_sb(c, "pT%d" % i, [128, 1024], BF16) for i in range(3)]
        rsb = _sb(c, "rsb", [128, 512], F32)
        ou = _sb(c, "ou", [128, 512], F32)
        ot = [_sb(c, "ot%d" % i, [128, 512], BF16) for i in range(2)]
        onesf = _sb(c, "onesf", [128, 64], F32)
        psS = [_ps(c, "psS%d" % i, [128, 1024], F32) for i in range(2)]
        psO = [_ps(c, "psO%d" % i, [128, 512], F32) for i in range(2)]
        psB = _ps(c, "psB", [128, 512], F32)
        p.op("pool", lambda e: e.memset(onesf[:], 1.0), [], ["onesf"])
        pack = (kind == "gqa")
        if pack:
            kT2 = kT.rearrange("g d t -> (g d) t")
            for half in range(2):
                dma(p, "sp", KT[:, 0, half * HALF:(half + 1) * HALF], kT2[:, half * HALF:(half + 1) * HALF],
                    [], ["KT"])
            for i in range(2):
                p.op("pool", lambda e, i=i: e.memset(Q[i][:], 0.0), [], [("Q", i)])
        else:
            for k in range(nK):
                for half in range(2):
                    dma(p, "sp", KT[0:dh, k, half * HALF:(half + 1) * HALF],
                        kT[k, :, half * HALF:(half + 1) * HALF], [], ["KT"])
        vv = vS.rearrange("(t p) c -> p t c", p=128)
        for q4 in range(16):
            dma(p, "sp", V[:, q4 * 4:(q4 + 1) * 4, :], vv[:, q4 * 4:(q4 + 1) * 4, :], [], ["V"])
        it = 0
        for qb in range(8):
            qs = qb % 2
            if pack:
                dma(p, "sp", Q[qs][0:64, 0:3, :], qT[0:3, :, qb * 512:(qb + 1) * 512].rearrange("h d t -> d h t"),
                    [], [("Q", qs)])
                dma(p, "sp", Q[qs][64:128, 3:6, :], qT[3:6, :, qb * 512:(qb + 1) * 512].rearrange("h d t -> d h t"),
                    [], [("Q", qs)])
            else:
                dma(p, "sp", Q[qs][0:dh, :, :], qT[:, :, qb * 512:(qb + 1) * 512].rearrange("h d t -> d h t"),
                    [], [("Q", qs)])
            for h in range(nH):
                ob_ = it % 2
                it += 1
                okey = "psO%d" % ob_
                ki = kmap[h]

                def qk2(k2, h=h, ki=ki, qs=qs):
                    b = k2 % 2

                    def fn(e):
                        ins = None
                        for u in range(2):
                            kt = 2 * k2 + u
                            if pack:
                                ins = e.matmul(psS[b][:, u * 512:(u + 1) * 512], KT[:, 0, kt * 128:(kt + 1) * 128],
                                               Q[qs][:, h, :], start=True, stop=True)
                            else:
                                ins = e.matmul(psS[b][:, u * 512:(u + 1) * 512],
                                               KT[0:dh, ki, kt * 128:(kt + 1) * 128], Q[qs][0:dh, h, :],
                                               start=True, stop=True)
                        return ins
                    p.op("pe", fn, ["KT", ("Q", qs)], ["psS%d" % b])
                qk2(0)
                for k2 in range(32):
                    b = k2 % 2
                    pb = k2 % 3
                    if k2 + 1 < 32:
                        qk2(k2 + 1)
                    p.op("act", lambda e, b=b, pb=pb: e.activation(out=pT[pb][:], in_=psS[b][:, :],
                                                                   func=AF.Exp, scale=scale),
                         ["psS%d" % b], [("pT", pb)])

                    def fpv(e, k2=k2, pb=pb, ob_=ob_, ki=ki):
                        ins = None
                        for u in range(2):
                            kt = 2 * k2 + u
                            ins = e.matmul(psO[ob_][0:65, :], V[:, kt, ki * 65:(ki + 1) * 65],
                                           pT[pb][:, u * 512:(u + 1) * 512],
                                           start=(kt == 0), stop=(kt == 63))
                        return ins
                    p.op("pe", fpv, ["V", ("pT", pb)], [okey])
                os_ = it % 2
                _normalize(c, psO[ob_], okey, 512, rsb, "rsb", psB, ou, "ou", onesf,
                           ot[os_][0:64, :], ("ot", os_))
                dma(p, "sp", c.s["oT"][obase + h, :, qb * 512:(qb + 1) * 512], ot[os_][0:64, :],
                    [("ot", os_)], [("d_oT", obase + h, qb)])
        p.barrier()
        p.emit()


def phase_na(c, L):
    nc, p = c.nc, c.p
    with ExitStack() as ph:
        c.ph = ph
        Qb = [_sb(c, "Qb%d" % i, [128, 6, 512], BF16) for i in range(2)]
        Kb = [_sb(c, "Kb%d" % i, [128, 6, 1024], BF16) for i in range(2)]
        Vb = [_sb(c, "Vb%d" % i, [128, 16, 390], BF16) for i in range(2)]
        bint = _sb(c, "bint", [128, 6, 8, 64], F32)
        bbnd = _sb(c, "bbnd", [128, 6, 12, 64], F32)
        sc = [_sb(c, "sc%d" % i, [128, 768], F32) for i in range(2)]
        pp = [_sb(c, "pp%d" % i, [128, 768], BF16) for i in range(2)]
        rsb = _sb(c, "rsb", [128, 512], F32)
        ou = _sb(c, "ou", [128, 512], F32)
        ot = [_sb(c, "ot%d" % i, [128, 6, 512], BF16) for i in range(2)]
        onesf = _sb(c, "onesf", [128, 64], F32)
        psS = [_ps(c, "psS%d" % i, [128, 1024], F32) for i in range(2)]
        psO = [_ps(c, "psO%d" % i, [128, 512], F32) for i in range(2)]
        psB = _ps(c, "psB", [128, 512], F32)
        p.op("pool", lambda e: e.memset(onesf[:], 1.0), [], ["onesf"])
        dma(p, "sp", bint[0:64], c.w["na_bint"][L], [], ["bint"])
        vrow = c.s["v_na"].rearrange("(r k) c -> k r c", k=64)
        it = 0
        for b8 in range(8):
            s = b8 % 2
            dma(p, "sp", Qb[s][0:64, :, :], c.s["qT_na"][:, :, b8 * 512:(b8 + 1) * 512].rearrange("h d t -> d h t"),
                [], [("Qb", s)])
            dma(p, "sp", Kb[s][0:64, :, :], c.s["kT_na"][:, :, b8 * 512:b8 * 512 + 1024].rearrange("h d t -> d h t"),
                [], [("Kb", s)])
            for hv in range(2):
                dma(p, "sp", Vb[s][0:64, hv * 8:(hv + 1) * 8, :], vrow[:, b8 * 8 + hv * 8:b8 * 8 + (hv + 1) * 8, :],
                    [], [("Vb", s)])
            for jj in range(8):
                j = b8 * 8 + jj
                if j < 4:
                    rows = list(range(0, 12)); bidx = j
                elif j > 60:
                    rows = list(range(60, 72)); bidx = 4 + (j - 61)
                else:
                    rows = list(range(j, j + 8)); bidx = None
                nr = len(rows)
                if bidx is not None:
                    dma(p, "sp", bbnd[0:64], c.bbnd[bidx], [], ["bbnd"])
                    btile, bkey = bbnd, "bbnd"
                else:
                    btile, bkey = bint, "bint"
                ob_ = j % 2
                okey = "psO%d" % ob_
                for h in range(6):
                    sb_ = it % 2
                    it += 1
                    skey = "psS%d" % sb_

                    def fqk(e, h=h, sb_=sb_, rows=rows, jj=jj, s=s, b8=b8):
                        ins = None
                        for i, lr in enumerate(rows):
                            ins = e.matmul(psS[sb_][0:64, i * 64:(i + 1) * 64],
                                           Kb[s][0:64, h, (lr - b8 * 8) * 64:(lr - b8 * 8 + 1) * 64],
                                           Qb[s][0:64, h, jj * 64:(jj + 1) * 64], start=True, stop=True)
                        return ins
                    p.op("pe", fqk, [("Qb", s), ("Kb", s)], [skey])
                    p.op("dve", lambda e, h=h, sb_=sb_, nr=nr, btile=btile: e.scalar_tensor_tensor(
                        out=sc[sb_][0:64, 0:nr * 64], in0=psS[sb_][0:64, 0:nr * 64], scalar=0.125,
                        in1=btile[0:64, h, 0:nr, :].rearrange("p r q -> p (r q)"),
                        op0=ALU.mult, op1=ALU.add), [skey, bkey], [("sc", sb_)])
                    p.op("act", lambda e, sb_=sb_, nr=nr: e.activation(
                        out=pp[sb_][0:64, 0:nr * 64], in_=sc[sb_][0:64, 0:nr * 64], func=AF.Exp),
                        [("sc", sb_)], [("pp", sb_)])

                    def fpv(e, h=h, sb_=sb_, rows=rows, ob_=ob_, s=s, b8=b8):
                        ins = None
                        n = len(rows)
                        for i, lr in enumerate(rows):
                            ins = e.matmul(psO[ob_][0:65, h * 64:(h + 1) * 64],
                                           Vb[s][0:64, lr - b8 * 8, h * 65:(h + 1) * 65],
                                           pp[sb_][0:64, i * 64:(i + 1) * 64],
                                           start=(i == 0), stop=(i == n - 1))
                        return ins
                    p.op("pe", fpv, [("Vb", s), ("pp", sb_)], [okey])
                _normalize(c, psO[ob_], okey, 384, rsb, "rsb", psB, ou, "ou", onesf,
                           ot[s][0:64, :, jj * 64:(jj + 1) * 64],
                           ("ot", s), nh=6)
            dma(p, "sp", c.s["oT"][0:6, :, b8 * 512:(b8 + 1) * 512].rearrange("h d t -> d h t"),
                ot[s][0:64, :, :], [("ot", s)], [("d_oT_na", b8)])
        p.barrier()
        p.emit()


def layer_norm_tile(c, tsb, tkey, junk, jkey, sm, smkey, g_t, b_t, gbkey, out_t, outkey):
    p = c.p
    p.op("act", lambda e: e.activation(out=junk[:], in_=tsb[:], func=AF.Copy, accum_out=sm[:, 0:1]),
         [tkey], [jkey, smkey])
    p.op("act", lambda e: e.activation(out=junk[:], in_=tsb[:], func=AF.Square, accum_out=sm[:, 1:2]),
         [tkey], [jkey, smkey])
    p.op("dve", lambda e: e.tensor_scalar(out=sm[:, 2:3], in0=sm[:, 0:1], scalar1=1.0 / D, scalar2=None,
                                          op0=ALU.mult), [smkey], [smkey])
    p.op("dve", lambda e: e.tensor_tensor(out=sm[:, 3:4], in0=sm[:, 2:3], in1=sm[:, 2:3], op=ALU.mult),
         [smkey], [smkey])
    p.op("dve", lambda e: e.scalar_tensor_tensor(out=sm[:, 4:5], in0=sm[:, 1:2], scalar=1.0 / D,
                                                 in1=sm[:, 3:4], op0=ALU.mult, op1=ALU.subtract),
         [smkey], [smkey])
    p.op("act", lambda e: e.activation(out=sm[:, 5:6], in_=sm[:, 4:5], func=AF.Sqrt,
                                       bias=c.eps_ln[:, 0:1]), [smkey], [smkey])
    p.op("dve", lambda e: e.reciprocal(out=sm[:, 6:7], in_=sm[:, 5:6]), [smkey], [smkey])
    p.op("dve", lambda e: e.tensor_scalar(out=junk[:], in0=tsb[:], scalar1=sm[:, 2:3], scalar2=sm[:, 6:7],
                                          op0=ALU.subtract, op1=ALU.mult), [tkey, smkey], [jkey])
    p.op("pool", lambda e: e.tensor_tensor(out=junk[:], in0=junk[:], in1=g_t[:], op=ALU.mult),
         [jkey, gbkey], [jkey])
    p.op("pool", lambda e: e.tensor_tensor(out=out_t[:], in0=junk[:], in1=b_t[:], op=ALU.add),
         [jkey, gbkey], [outkey])


def phase_merge(c, L):
    nc, p = c.nc, c.p
    with ExitStack() as ph:
        c.ph = ph
        wb = _sb(c, "wb", [128, 16, D], BF16)
        woutb = _sb(c, "woutb", [128, 8, D], BF16)
        lng = _sb(c, "lng", [128, D], F32)
        lnb = _sb(c, "lnb", [128, D], F32)
        wr = _sb(c, "wr", [128, 8, NEXP], F32)
        br = _sb(c, "br", [128, NEXP], F32)
        identb = _sb(c, "identb", [128, 128], BF16)
        identf = _sb(c, "identf", [128, 128], F32)
        oTb = [_sb(c, "oTb%d" % i, [128, 16, 512], BF16) for i in range(2)]
        gTc = [_sb(c, "gTc%d" % i, [128, 3, 512], BF16) for i in range(2)]
        mixT = [_sb(c, "mixT%d" % i, [128, 8, 512], BF16) for i in range(2)]
        mt = [_sb(c, "mt%d" % i, [128, 512], F32) for i in range(6)]
        xo = [_sb(c, "xo%d" % i, [128, D], F32) for i in range(2)]
        tsb = [_sb(c, "tsb%d" % i, [128, D], F32) for i in range(2)]
        junk = _sb(c, "junk", [128, D], F32)
        x1t = [_sb(c, "x1t%d" % i, [128, D], F32) for i in range(2)]
        x1b = [_sb(c, "x1b%d" % i, [128, D], BF16) for i in range(2)]
        x1Tb = [_sb(c, "x1Tb%d" % i, [128, 8, 128], BF16) for i in range(2)]
        x1Tf = [_sb(c, "x1Tf%d" % i, [128, 8, 128], F32) for i in range(2)]
        sm = [_sb(c, "sm%d" % i, [128, 8], F32) for i in range(2)]
        rt_ = [_sb(c, "rt%d" % i, [128, 160], F32) for i in range(2)]
        gTt = [_sb(c, "gTt%d" % i, [128, 128], F32) for i in range(2)]
        psY = [_ps(c, "psY%d" % i, [128, 512], F32) for i in range(3)]
        psOut = [_ps(c, "psOut%d" % i, [128, 512], F32) for i in range(2)]
        psT = _ps(c, "psT", [128, 8, 128], BF16)
        psTf = _ps(c, "psTf", [128, 8, 128], F32)
        triu = _sb(c, "triu", [128, 128], F32)
        ones128 = _sb(c, "ones128", [128, 128], F32)
        iota_e = _sb(c, "iota_e", [128, NEXP], F32)
        iota_cap = _sb(c, "iota_cap", [128, NEXP], F32)
        msum = _sb(c, "msum", [128, NEXP], F32)
        rx = [_sb(c, "rx%d" % i, [128, 160], F32) for i in range(2)]
        idxu = [_sb(c, "idxu%d" % i, [128, 8], U32) for i in range(2)]
        slotu = [_sb(c, "slotu%d" % i, [128, 4], I32) for i in range(2)]
        dma(p, "sp", triu[:], c.w["triu"][:, :], [], ["triu"])
        dma(p, "sp", ones128[:], c.w["ones128"][:, :], [], ["ones128"])
        dma(p, "sp", iota_e[:], c.w["iota_e"][:, :], [], ["iota_e"])
        dma(p, "sp", iota_cap[:], c.w["iota_cap"][:, :], [], ["iota_cap"])
        p.op("pool", lambda e: e.memset(msum[:], 0.0), [], ["msum"])

        for h in range(16):
            src = (c.w["w_ba"][L][h * 64:(h + 1) * 64, :] if h < 6 else
                   c.w["w_bb"][L][(h - 6) * 64:(h - 5) * 64, :] if h < 12 else
                   c.w["w_bc"][L][(h - 12) * 64:(h - 11) * 64, :])
            dma(p, "pool", wb[0:64, h, :], src, [], ["wb"])
        wo_v = c.w["w_out"][L].rearrange("(c p) n -> p c n", p=128)
        for ch in range(8):
            dma(p, "pool", woutb[:, ch, :], wo_v[:, ch, :], [], ["woutb"])
        dma(p, "sp", lng[:], c.w["ln1g"][L], [], ["lngb"])
        dma(p, "sp", lnb[:], c.w["ln1b"][L], [], ["lngb"])
        dma(p, "sp", wr[:], c.w["w_router"][L].rearrange("(c p) e -> p c e", p=128), [], ["wr"])
        dma(p, "sp", br[:], c.w["brouter"][L], [], ["br"])
        dma(p, "sp", identb[:], c.w["identb"][:, :], [], ["identb"])
        dma(p, "sp", identf[:], c.w["identf"][:, :], [], ["identf"])
        gT_v = c.s["gT"].rearrange("(i c p) t -> p i c t", i=3, c=8, p=128)
        x1T_v = c.s["x1T"].rearrange("(c p) t -> p c t", p=128)
        gcnt = 0
        for blk in range(8):
            s = blk % 2
            for hv in range(2):
                dma(p, "sp", oTb[s][0:64, hv * 8:(hv + 1) * 8, :],
                    c.s["oT"][hv * 8:(hv + 1) * 8, :, blk * 512:(blk + 1) * 512].rearrange("h d t -> d h t"),
                    [], [("oTb", s)])
            for dc in range(8):
                gs = gcnt % 2
                gcnt += 1
                dma(p, "sp", gTc[gs][:], gT_v[:, :, dc, blk * 512:(blk + 1) * 512], [], [("gTc", gs)])
                for i, (h0, nh) in enumerate(((0, 6), (6, 6), (12, 4))):
                    mm_group(p, psY[i][:, :],
                             [(wb[0:64, h0 + k, dc * 128:(dc + 1) * 128], oTb[s][0:64, h0 + k, :])
                              for k in range(nh)], ["wb", ("oTb", s)], ["psY%d" % i])
                m3 = [(gcnt * 3 + i) % 6 for i in range(3)]
                for i in range(3):
                    p.op("dve", lambda e, i=i, gs=gs, m=m3[i]: e.tensor_tensor(
                        out=mt[m][:], in0=psY[i][:, :], in1=gTc[gs][:, i, :], op=ALU.mult),
                        ["psY%d" % i, ("gTc", gs)], [("mt", m3[i])])
                p.op("pool", lambda e, m3=m3: e.tensor_tensor(out=mt[m3[0]][:], in0=mt[m3[0]][:],
                                                              in1=mt[m3[1]][:], op=ALU.add),
                     [("mt", m3[0]), ("mt", m3[1])], [("mt", m3[0])])
                p.op("pool", lambda e, m3=m3, s=s, dc=dc: e.tensor_tensor(
                    out=mixT[s][:, dc, :], in0=mt[m3[0]][:], in1=mt[m3[2]][:], op=ALU.add),
                    [("mt", m3[0]), ("mt", m3[2])], [("mixT", s)])
            for tt in range(4):
                t = blk * 4 + tt
                ts_ = t % 2
                dma(p, "sp", xo[ts_][:], c.q_src[t * 128:(t + 1) * 128, :], [], [("xo", ts_)])
                for half in range(2):
                    mm_group(p, psOut[half][:, :],
                             [(mixT[s][:, dc, tt * 128:(tt + 1) * 128], woutb[:, dc, half * 512:(half + 1) * 512])
                              for dc in range(8)], [("mixT", s), "woutb"], ["psOut%d" % half])
                    p.op("dve", lambda e, half=half, ts_=ts_: e.scalar_tensor_tensor(
                        out=tsb[ts_][:, half * 512:(half + 1) * 512], in0=xo[ts_][:, half * 512:(half + 1) * 512],
                        scalar=DN_ALPHA, in1=psOut[half][:, :], op0=ALU.mult, op1=ALU.add),
                        [("xo", ts_), "psOut%d" % half], [("tsb", ts_)])
                layer_norm_tile(c, tsb[ts_], ("tsb", ts_), junk, "junk", sm[ts_], ("sm", ts_),
                                lng, lnb, "lngb", x1t[ts_], ("x1t", ts_))
                dma(p, "sp", c.s["x1"][t * 128:(t + 1) * 128, :], x1t[ts_][:], [("x1t", ts_)], [("d_x1", t)])
                p.op("act", lambda e, ts_=ts_: e.activation(out=x1b[ts_][:], in_=x1t[ts_][:], func=AF.Copy),
                     [("x1t", ts_)], [("x1b", ts_)])
                tr_group(p, [(psT[:, ch, :], x1b[ts_][:, ch * 128:(ch + 1) * 128]) for ch in range(8)],
                         identb[:], [("x1b", ts_), "identb"], ["psT"])
                p.op("dve", lambda e, ts_=ts_: e.tensor_copy(x1Tb[ts_][:], psT[:]), ["psT"], [("x1Tb", ts_)])
                for hv in range(2):
                    dma(p, "sp", x1T_v[:, hv * 4:(hv + 1) * 4, t * 128:(t + 1) * 128], x1Tb[ts_][:, hv * 4:(hv + 1) * 4, :],
                        [("x1Tb", ts_)], [("d_x1T", t, hv)])
                tr_group(p, [(psTf[:, ch, :], x1t[ts_][:, ch * 128:(ch + 1) * 128]) for ch in range(8)],
                         identf[:], [("x1t", ts_), "identf"], ["psTf"])
                p.op("act", lambda e, ts_=ts_: e.activation(out=x1Tf[ts_][:], in_=psTf[:], func=AF.Copy),
                     ["psTf"], [("x1Tf", ts_)])
                mm_group(p, psY[0][:, 0:NEXP], [(x1Tf[ts_][:, ch, :], wr[:, ch, :]) for ch in range(8)],
                         [("x1Tf", ts_), "wr"], ["psY0"])
                R = rt_[ts_]
                rk = ("rt", ts_)
                lg, mx, msk, ex, exm = R[:, 0:32], R[:, 32:40], R[:, 40:72], R[:, 72:104], R[:, 104:136]
                nm, ssum, rs = R[:, 136:137], R[:, 137:138], R[:, 138:139]
                p.op("dve", lambda e, lg=lg: e.tensor_tensor(out=lg, in0=psY[0][:, 0:NEXP], in1=br[:], op=ALU.add),
                     ["psY0", "br"], [rk])
                p.op("dve", lambda e, lg=lg, mx=mx: e.max(out=mx, in_=lg), [rk], [rk])
                p.op("dve", lambda e, lg=lg, mx=mx, msk=msk: e.tensor_scalar(
                    out=msk, in0=lg, scalar1=mx[:, 3:4], scalar2=None, op0=ALU.is_ge), [rk], [rk])
                p.op("dve", lambda e, mx=mx, nm=nm: e.tensor_scalar(
                    out=nm, in0=mx[:, 0:1], scalar1=-1.0, scalar2=None, op0=ALU.mult), [rk], [rk])
                p.op("act", lambda e, lg=lg, ex=ex, nm=nm: e.activation(out=ex, in_=lg, func=AF.Exp, bias=nm),
                     [rk], [rk])
                p.op("dve", lambda e, ex=ex, msk=msk, exm=exm: e.tensor_tensor(out=exm, in0=ex, in1=msk,
                                                                               op=ALU.mult), [rk], [rk])
                p.op("dve", lambda e, exm=exm, ssum=ssum: e.reduce_sum(out=ssum, in_=exm, axis=AX.X), [rk], [rk])
                p.op("dve", lambda e, ssum=ssum, rs=rs: e.reciprocal(out=rs, in_=ssum), [rk], [rk])
                p.op("dve", lambda e, exm=exm, rs=rs: e.tensor_scalar(
                    out=exm, in0=exm, scalar1=rs, scalar2=None, op0=ALU.mult), [rk], [rk])
                p.op("pe", lambda e, exm=exm: e.transpose(psY[1][0:32, 0:128], exm, identf[:]),
                     [rk, "identf"], ["psY1"])
                p.op("act", lambda e, ts_=ts_: e.activation(out=gTt[ts_][0:32, :], in_=psY[1][0:32, 0:128],
                                                            func=AF.Copy), ["psY1"], [("gTt", ts_)])
                dma(p, "sp", c.s["gateT"][:, t * 128:(t + 1) * 128], gTt[ts_][0:32, :],
                    [("gTt", ts_)], [("d_gateT", t)])
                X = rx[ts_]
                xk = ("rx", ts_)
                idxf, sv, ov, eq, tmp = X[:, 0:8], X[:, 8:40], X[:, 40:72], X[:, 72:104], X[:, 104:136]
                slotf, gkf = X[:, 136:140], X[:, 140:144]
                p.op("dve", lambda e, ts_=ts_, lg=lg, mx=mx: e.max_index(out=idxu[ts_][:], in_max=mx, in_values=lg),
                     [rk], [("idxu", ts_)])
                p.op("dve", lambda e, ts_=ts_, idxf=idxf: e.tensor_copy(idxf, idxu[ts_][:]),
                     [("idxu", ts_)], [xk])
                p.op("pe", lambda e, msk=msk: e.matmul(psY[2][:, 0:NEXP], triu[:], msk, start=True, stop=False),
                     [rk, "triu"], ["psY2"])
                p.op("pe", lambda e: e.matmul(psY[2][:, 0:NEXP], ones128[:], msum[:], start=False, stop=True),
                     ["msum", "ones128"], ["psY2"])
                p.op("dve", lambda e, ov=ov: e.tensor_scalar(out=ov, in0=psY[2][:, 0:NEXP], scalar1=float(CAP),
                                                             scalar2=1.0e6, op0=ALU.is_ge, op1=ALU.mult),
                     ["psY2"], [xk])
                p.op("dve", lambda e, sv=sv: e.tensor_tensor(out=sv, in0=psY[2][:, 0:NEXP], in1=iota_cap[:],
                                                             op=ALU.add), ["psY2", "iota_cap"], [xk])
                p.op("dve", lambda e, sv=sv, ov=ov: e.tensor_tensor(out=sv, in0=sv, in1=ov, op=ALU.add), [xk], [xk])
                p.op("pool", lambda e, msk=msk: e.tensor_tensor(out=msum[:], in0=msum[:], in1=msk, op=ALU.add),
                     [rk, "msum"], ["msum"])
                for k4 in range(4):
                    p.op("dve", lambda e, eq=eq, idxf=idxf, k4=k4: e.tensor_scalar(
                        out=eq, in0=iota_e[:], scalar1=idxf[:, k4:k4 + 1], scalar2=None, op0=ALU.is_equal),
                        [xk, "iota_e"], [xk])
                    p.op("dve", lambda e, eq=eq, sv=sv, tmp=tmp: e.tensor_tensor(out=tmp, in0=eq, in1=sv, op=ALU.mult),
                         [xk], [xk])
                    p.op("dve", lambda e, tmp=tmp, slotf=slotf, k4=k4: e.reduce_sum(
                        out=slotf[:, k4:k4 + 1], in_=tmp, axis=AX.X), [xk], [xk])
                    p.op("dve", lambda e, eq=eq, exm=exm, tmp=tmp: e.tensor_tensor(out=tmp, in0=eq, in1=exm,
                                                                                   op=ALU.mult), [xk, rk], [xk])
                    p.op("dve", lambda e, tmp=tmp, gkf=gkf, k4=k4: e.reduce_sum(
                        out=gkf[:, k4:k4 + 1], in_=tmp, axis=AX.X), [xk], [xk])
                p.op("dve", lambda e, ts_=ts_, slotf=slotf: e.tensor_copy(slotu[ts_][:], slotf),
                     [xk], [("slotu", ts_)])
                for k4 in range(4):
                    p.op("pool", lambda e, ts_=ts_, k4=k4: e.indirect_dma_start(
                        out=c.s["xg"][:, :], out_offset=bass.IndirectOffsetOnAxis(ap=slotu[ts_][:, k4:k4 + 1], axis=0),
                        in_=x1b[ts_][:], in_offset=None, bounds_check=c.breg, oob_is_err=False),
                        [("slotu", ts_), ("x1b", ts_)], [("d_xg", t, k4)], dma=True)
                dma(p, "sp", c.s["slots"][t * 128:(t + 1) * 128, :], slotu[ts_][:], [("slotu", ts_)], [("d_slots", t)])
                dma(p, "sp", c.s["gks"][t * 128:(t + 1) * 128, :], gkf, [xk], [("d_gks", t)])
        p.barrier()
        p.emit()


SIG_MAX = float(1.0 / (1.0 + np.exp(-1.702 * 7.0)))


def phase_moe(c, L):
    nc, p = c.nc, c.p
    with ExitStack() as ph:
        c.ph = ph
        wgu = [_sb(c, "wgu%d" % i, [128, 8, 2 * DEXP], BF16) for i in range(2)]
        wdn = [_sb(c, "wdn%d" % i, [128, 8, D], BF16) for i in range(2)]
        xT = _sb(c, "xTsb", [128, 8, 1024], BF16)
        gT = _sb(c, "gTsb", [128, 1024], F32)
        yacc = _sb(c, "yacc", [128, 8, D], F32)
        bgu = _sb(c, "bgu", [128, NEXP, 16], F32)
        bgs = _sb(c, "bgs", [128, NEXP, 8], F32)
        bdn = _sb(c, "bdn", [128, D], F32)
        identf = _sb(c, "identf", [128, 128], F32)
        sg = [_sb(c, "sg%d" % i, [128, 512], F32) for i in range(2)]
        gc = [_sb(c, "gc%d" % i, [128, 512], F32) for i in range(2)]
        uc = [_sb(c, "uc%d" % i, [128, 512], F32) for i in range(2)]
        t1 = [_sb(c, "t1%d" % i, [128, 512], F32) for i in range(2)]
        t2 = [_sb(c, "t2%d" % i, [128, 512], F32) for i in range(2)]
        aT = [_sb(c, "aT%d" % i, [128, 8, 512], BF16) for i in range(2)]
        yev = [_sb(c, "yev%d" % i, [128, 512], F32) for i in range(2)]
        psg = [_ps(c, "psg%d" % i, [128, 512], F32) for i in range(2)]
        psu = [_ps(c, "psu%d" % i, [128, 512], F32) for i in range(2)]
        psGb = _ps(c, "psGb", [128, 512], F32)
        psy = [_ps(c, "psy%d" % i, [128, 512], F32) for i in range(2)]

        dma(p, "sp", bgu[:], c.w["bgu_t"][L], [], ["bgu"])
        dma(p, "sp", bdn[0:32, :], c.w["bdn"][L], [], ["bdn"])
        dma(p, "sp", identf[:], c.w["identf"][:, :], [], ["identf"])
        p.op("dve", lambda e: e.tensor_scalar(out=bgs[:], in0=bgu[:, :, 0:8], scalar1=1.702, scalar2=None,
                                              op0=ALU.mult), ["bgu"], ["bgs"])
        x1T_v = c.s["x1T"].rearrange("(c p) t -> p c t", p=128)
        ym_v = c.s["ymoe"].rearrange("(t p) d -> p t d", p=128)
        cnt = 0
        ycnt = 0
        for sb in range(4):
            dma(p, "sp", xT[:], x1T_v[:, :, sb * 1024:(sb + 1) * 1024], [], ["xTsb"])
            dma(p, "sp", gT[0:32, :], c.s["gateT"][:, sb * 1024:(sb + 1) * 1024], [], ["gTsb"])
            for ex in range(NEXP):
                ws = ex % 2
                gu_v = c.w["w_gu"][L, ex].rearrange("(c p) n -> p c n", p=128)
                dn_v = c.w["w_dn"][L, ex].rearrange("(c p) n -> p c n", p=128)
                for ch in range(8):
                    dma(p, "pool", wgu[ws][:, ch, :], gu_v[:, ch, :], [], [("wgu", ws)])
                for ch in range(8):
                    dma(p, "pool", wdn[ws][:, ch, :], dn_v[:, ch, :], [], [("wdn", ws)])
                for tb in range(2):
                    as_ = (ex * 2 + tb) % 2
                    p.op("pe", lambda e, ex=ex, tb=tb: e.matmul(
                        psGb[:, :], identf[0:32, ex:ex + 1].to_broadcast([32, 128]),
                        gT[0:32, tb * 512:(tb + 1) * 512], start=True, stop=True),
                        ["identf", "gTsb"], ["psGb"])
                    for j in range(8):
                        k = cnt % 2
                        cnt += 1
                        mm_group(p, psg[k][:, :],
                                 [(wgu[ws][:, ch, j * 128:(j + 1) * 128], xT[:, ch, tb * 512:(tb + 1) * 512])
                                  for ch in range(8)], [("wgu", ws), "xTsb"], ["psg%d" % k])
                        mm_group(p, psu[k][:, :],
                                 [(wgu[ws][:, ch, DEXP + j * 128:DEXP + (j + 1) * 128],
                                   xT[:, ch, tb * 512:(tb + 1) * 512]) for ch in range(8)],
                                 [("wgu", ws), "xTsb"], ["psu%d" % k])
                        p.op("dve", lambda e, k=k, ex=ex, j=j: e.tensor_scalar(
                            out=gc[k][:], in0=psg[k][:, :], scalar1=bgu[:, ex, j:j + 1], scalar2=7.0,
                            op0=ALU.add, op1=ALU.min), ["psg%d" % k, "bgu"], [("gc", k)])
                        p.op("act", lambda e, k=k: e.activation(
                            out=sg[k][:], in_=gc[k][:], func=AF.Sigmoid, scale=1.702),
                            [("gc", k)], [("sg", k)])
                        p.op("dve", lambda e, k=k, ex=ex, j=j: e.tensor_scalar(
                            out=uc[k][:], in0=psu[k][:, :], scalar1=bgu[:, ex, 8 + j:9 + j], scalar2=7.0,
                            op0=ALU.add, op1=ALU.min), ["psu%d" % k, "bgu"], [("uc", k)])
                        p.op("dve", lambda e, k=k: e.tensor_scalar(
                            out=uc[k][:], in0=uc[k][:], scalar1=-7.0, scalar2=1.0,
                            op0=ALU.max, op1=ALU.add), [("uc", k)], [("uc", k)])
                        p.op("pool", lambda e, k=k: e.tensor_tensor(
                            out=t1[k][:], in0=sg[k][:], in1=gc[k][:], op=ALU.mult),
                            [("sg", k), ("gc", k)], [("t1", k)])
                        p.op("dve", lambda e, k=k: e.tensor_tensor(out=t2[k][:], in0=uc[k][:], in1=psGb[:, :],
                                                                   op=ALU.mult), [("uc", k), "psGb"], [("t2", k)])
                        p.op("pool", lambda e, k=k, j=j, as_=as_: e.tensor_tensor(
                            out=aT[as_][:, j, :], in0=t1[k][:], in1=t2[k][:], op=ALU.mult),
                            [("t1", k), ("t2", k)], [("aT", as_)])
                    for tt in range(4):
                        tile = tb * 4 + tt
                        for half in range(2):
                            yk = ycnt % 2
                            ycnt += 1
                            pairs = [(aT[as_][:, j, tt * 128:(tt + 1) * 128],
                                      wdn[ws][:, j, half * 512:(half + 1) * 512]) for j in range(8)]
                            rds = [("aT", as_), ("wdn", ws)]
                            if ex == 0:
                                pairs.append((gT[0:32, tile * 128:(tile + 1) * 128],
                                              bdn[0:32, half * 512:(half + 1) * 512]))
                                rds += ["gTsb", "bdn"]
                            mm_group(p, psy[yk][:, :], pairs, rds, ["psy%d" % yk])
                            ya = yacc[:, tile, half * 512:(half + 1) * 512]
                            if ex == 0:
                                p.op("act", lambda e, ya=ya, yk=yk: e.activation(out=ya, in_=psy[yk][:, :],
                                                                                 func=AF.Copy),
                                     ["psy%d" % yk], [("yacc", tile, half)])
                            else:
                                p.op("act", lambda e, yk=yk: e.activation(out=yev[yk][:], in_=psy[yk][:, :],
                                                                          func=AF.Copy),
                                     ["psy%d" % yk], [("yev", yk)])
                                p.op("pool", lambda e, ya=ya, yk=yk: e.tensor_tensor(out=ya, in0=ya, in1=yev[yk][:],
                                                                                     op=ALU.add),
                                     [("yev", yk), ("yacc", tile, half)], [("yacc", tile, half)])
            dma(p, "sp", ym_v[:, sb * 8:(sb + 1) * 8, :], yacc[:],
                [("yacc", t_, h_) for t_ in range(8) for h_ in range(2)], [("d_ymoe", sb)])
        p.barrier()
        p.emit()


def load_w_cast(c, dst_ap, src_ap, stg, skey, dkey, eng="pool"):
    p = c.p
    dma(p, "sp", stg, src_ap, [], [skey])
    if eng == "act":
        p.op("act", lambda e: e.activation(out=dst_ap, in_=stg, func=AF.Copy), [skey], [dkey])
    else:
        p.op(eng, lambda e: e.tensor_copy(dst_ap, stg), [skey], [dkey])


def phase_moe_sparse(c, L):
    nc, p = c.nc, c.p
    NT = CAP // 128
    groups = [(0, 512), (512, CAP)] if CAP > 512 else [(0, CAP)]
    with ExitStack() as ph:
        c.ph = ph
        wgu = [_sb(c, "wgu%d" % i, [128, 8, 2 * DEXP], BF16) for i in range(2)]
        wdn = [_sb(c, "wdn%d" % i, [128, 8, D], BF16) for i in range(2)]
        stg = [_sb(c, "stg%d" % i, [128, 2 * DEXP], F32) for i in range(3)]
        xgt = [_sb(c, "xgt%d" % i, [128, D], BF16) for i in range(NT)]
        xgT = [_sb(c, "xgT%d" % i, [128, 8, CAP], BF16) for i in range(2)]
        bgu = _sb(c, "bgu", [128, NEXP, 16], F32)
        bdr1 = _sb(c, "bdr", [128, D], F32)
        bdr = [bdr1, bdr1]
        bdb = [_sb(c, "bdb%d" % i, [128, D], BF16) for i in range(2)]
        ones1 = _sb(c, "ones1", [128, 128], BF16)
        identb = _sb(c, "identb", [128, 128], BF16)
        sg = [_sb(c, "sg%d" % i, [128, 512], F32) for i in range(2)]
        gc = [_sb(c, "gc%d" % i, [128, 512], F32) for i in range(2)]
        uc = [_sb(c, "uc%d" % i, [128, 512], F32) for i in range(2)]
        t1 = [_sb(c, "t1%d" % i, [128, 512], F32) for i in range(2)]
        aT = _sb(c, "aT", [128, 8, CAP], BF16)
        yout = [_sb(c, "yout%d" % i, [128, D], F32) for i in range(2)]
        psT2 = [_ps(c, "psT%d" % i, [128, 8, 128], BF16) for i in range(2)]
        psg = [_ps(c, "psg%d" % i, [128, 512], F32) for i in range(2)]
        psu = [_ps(c, "psu%d" % i, [128, 512], F32) for i in range(2)]
        psy = [_ps(c, "psy%d" % i, [128, 512], F32) for i in range(2)]
        dma(p, "sp", bgu[:], c.w["bgu_t"][L], [], ["bgu"])
        dma(p, "sp", identb[:], c.w["identb"][:, :], [], ["identb"])
        p.op("pool", lambda e: e.memset(ones1[:], 1.0), [], ["ones1"])
        st8 = {"scnt": 0, "cnt": 0, "ycnt": 0, "xcnt": 0}
        cast_rr = ("dve", "act", "pool")

        def wload_steps(ex):
            ws = ex % 2
            gu_v = c.w["w_gu"][L, ex].rearrange("(c p) n -> p c n", p=128)
            dn_v = c.w["w_dn"][L, ex].rearrange("(c p) n -> p c n", p=128)
            steps = []
            for ch in range(12):
                si = st8["scnt"] % 3
                eng = cast_rr[st8["scnt"] % 3]
                st8["scnt"] += 1
                if ch < 8:
                    src, stv, dstv, dkey = gu_v[:, ch, :], stg[si][:], wgu[ws][:, ch, :], ("wgu", ws)
                else:
                    c2 = ch - 8
                    src = dn_v[:, 2 * c2:2 * c2 + 2, :]
                    stv = stg[si][:].rearrange("p (c n) -> p c n", c=2)
                    dstv, dkey = wdn[ws][:, 2 * c2:2 * c2 + 2, :], ("wdn", ws)

                def f_dma(src=src, stv=stv, si=si):
                    dma(p, "sp", stv, src, [], [("stg", si)])

                def f_cast(stv=stv, dstv=dstv, si=si, dkey=dkey, eng=eng):
                    if eng == "act":
                        p.op("act", lambda e: e.activation(out=dstv, in_=stv, func=AF.Copy), [("stg", si)], [dkey])
                    else:
                        p.op(eng, lambda e: e.tensor_copy(dstv, stv), [("stg", si)], [dkey])
                steps.append((f_dma, f_cast))
            steps.append((lambda ws=ws, ex=ex: dma(p, "sp", bdr[ws][0:1, :], c.w["bdn"][L, ex:ex + 1, :], [],
                                                   ["bdr"]),
                          lambda ws=ws: p.op("pool", lambda e: e.tensor_copy(bdb[ws][0:1, :], bdr[ws][0:1, :]),
                                             ["bdr"], [("bdb", ws)])))
            return steps

        def xg_dma(ex):
            for st in range(NT):
                r0 = ex * CAP + st * 128
                dma(p, "sp", xgt[st][:], c.s["xg"][r0:r0 + 128, :], [], [("xgt", st)])

        def xg_load(ex):
            xb_ = ex % 2
            for st in range(NT):
                pt = st8["xcnt"] % 2
                st8["xcnt"] += 1
                tr_group(p, [(psT2[pt][:, ch, :], xgt[st][:, ch * 128:(ch + 1) * 128]) for ch in range(8)],
                         identb[:], [("xgt", st), "identb"], ["psT%d" % pt])
                eng = "act" if st % 2 == 0 else "dve"
                if eng == "act":
                    p.op("act", lambda e, st=st, xb_=xb_, pt=pt: e.activation(
                        out=xgT[xb_][:, :, st * 128:(st + 1) * 128], in_=psT2[pt][:], func=AF.Copy),
                        ["psT%d" % pt], [("xgT", xb_)])
                else:
                    p.op("dve", lambda e, st=st, xb_=xb_, pt=pt: e.tensor_copy(
                        xgT[xb_][:, :, st * 128:(st + 1) * 128], psT2[pt][:]), ["psT%d" % pt], [("xgT", xb_)])

        pend_cast = None
        for f_dma, f_cast in wload_steps(0):
            f_dma()
            if pend_cast is not None:
                pend_cast()
            pend_cast = f_cast
        pend_cast()
        xg_dma(0)
        xg_load(0)
        for ex in range(NEXP):
            ws = ex % 2
            xb_ = ex % 2
            nxt_steps = wload_steps(ex + 1) if ex + 1 < NEXP else []
            pend_cast = None
            if ex + 1 < NEXP:
                xg_dma(ex + 1)
            for (n0, n1) in groups:
                w = n1 - n0
                for j in range(8):
                    k = st8["cnt"] % 2
                    st8["cnt"] += 1
                    if nxt_steps:
                        f_dma, f_cast = nxt_steps.pop(0)
                        f_dma()
                        if pend_cast is not None:
                            pend_cast()
                        pend_cast = f_cast
                    mm_group(p, psg[k][:, 0:w], [(wgu[ws][:, ch, j * 128:(j + 1) * 128], xgT[xb_][:, ch, n0:n1])
                                                 for ch in range(8)], [("wgu", ws), ("xgT", xb_)], ["psg%d" % k])
                    mm_group(p, psu[k][:, 0:w], [(wgu[ws][:, ch, DEXP + j * 128:DEXP + (j + 1) * 128],
                                                  xgT[xb_][:, ch, n0:n1]) for ch in range(8)],
                             [("wgu", ws), ("xgT", xb_)], ["psu%d" % k])
                    p.op("dve", lambda e, k=k, ex=ex, j=j, w=w: e.tensor_scalar(
                        out=gc[k][:, 0:w], in0=psg[k][:, 0:w], scalar1=bgu[:, ex, j:j + 1], scalar2=7.0,
                        op0=ALU.add, op1=ALU.min), ["psg%d" % k, "bgu"], [("gc", k)])
                    p.op("act", lambda e, k=k, w=w: e.activation(out=sg[k][:, 0:w], in_=gc[k][:, 0:w],
                                                                 func=AF.Sigmoid, scale=1.702),
                         [("gc", k)], [("sg", k)])
                    p.op("dve", lambda e, k=k, ex=ex, j=j, w=w: e.tensor_scalar(
                        out=uc[k][:, 0:w], in0=psu[k][:, 0:w], scalar1=bgu[:, ex, 8 + j:9 + j], scalar2=7.0,
                        op0=ALU.add, op1=ALU.min), ["psu%d" % k, "bgu"], [("uc", k)])
                    p.op("dve", lambda e, k=k, w=w: e.tensor_scalar(
                        out=uc[k][:, 0:w], in0=uc[k][:, 0:w], scalar1=-7.0, scalar2=1.0,
                        op0=ALU.max, op1=ALU.add), [("uc", k)], [("uc", k)])
                    p.op("pool", lambda e, k=k, w=w: e.tensor_tensor(out=t1[k][:, 0:w], in0=sg[k][:, 0:w],
                                                                     in1=gc[k][:, 0:w], op=ALU.mult),
                         [("sg", k), ("gc", k)], [("t1", k)])
                    p.op("pool", lambda e, k=k, j=j, n0=n0, n1=n1, w=w: e.tensor_tensor(
                        out=aT[:, j, n0:n1], in0=t1[k][:, 0:w], in1=uc[k][:, 0:w], op=ALU.mult),
                        [("t1", k), ("uc", k)], ["aT"])
            while nxt_steps:
                f_dma, f_cast = nxt_steps.pop(0)
                f_dma()
                if pend_cast is not None:
                    pend_cast()
                pend_cast = f_cast
            if pend_cast is not None:
                pend_cast()
            if ex + 1 < NEXP:
                xg_load(ex + 1)
            for st in range(NT):
                ys = st8["ycnt"] % 2
                st8["ycnt"] += 1
                for half in range(2):
                    yk = (st8["ycnt"] + half) % 2
                    pairs = [(aT[:, j, st * 128:(st + 1) * 128], wdn[ws][:, j, half * 512:(half + 1) * 512])
                             for j in range(8)]
                    pairs.append((ones1[0:1, 0:128], bdb[ws][0:1, half * 512:(half + 1) * 512]))
                    mm_group(p, psy[yk][:, :], pairs, ["aT", ("wdn", ws), ("bdb", ws), "ones1"], ["psy%d" % yk])
                    p.op("act", lambda e, ys=ys, yk=yk, half=half: e.activation(
                        out=yout[ys][:, half * 512:(half + 1) * 512], in_=psy[yk][:, :], func=AF.Copy),
                        ["psy%d" % yk], [("yout", ys)])
                r0 = ex * CAP + st * 128
                dma(p, "sp", c.s["yg"][r0:r0 + 128, :], yout[ys][:], [("yout", ys)], [("d_yg", ex, st)])
        p.barrier()
        p.emit()


def phase_ln2(c, L, dst, sparse=True):
    nc, p = c.nc, c.p
    with ExitStack() as ph:
        c.ph = ph
        lng = _sb(c, "lng2", [128, D], F32)
        lnb = _sb(c, "lnb2", [128, D], F32)
        xa = [_sb(c, "xa%d" % i, [128, D], F32) for i in range(2)]
        ya = [_sb(c, "ya%d" % i, [128, D], F32) for i in range(2)]
        yk_ = [[_sb(c, "yk%d_%d" % (i, k), [128, D], F32) for k in range(4)] for i in range(2)]
        sl = [_sb(c, "sl%d" % i, [128, 4], I32) for i in range(2)]
        gk = [_sb(c, "gk%d" % i, [128, 4], F32) for i in range(2)]
        tsb = [_sb(c, "tsb%d" % i, [128, D], F32) for i in range(2)]
        out = [_sb(c, "out%d" % i, [128, D], F32) for i in range(2)]
        junk = _sb(c, "junk2", [128, D], F32)
        sm = [_sb(c, "sm%d" % i, [128, 8], F32) for i in range(2)]
        dma(p, "sp", lng[:], c.w["ln2g"][L], [], ["lngb"])
        dma(p, "sp", lnb[:], c.w["ln2b"][L], [], ["lngb"])
        for t in range(32):
            s = t % 2
            dma(p, "sp", xa[s][:], c.s["x1"][t * 128:(t + 1) * 128, :], [], [("xa", s)])
            if sparse:
                dma(p, "sp", sl[s][:], c.s["slots"][t * 128:(t + 1) * 128, :], [], [("sl", s)])
                dma(p, "sp", gk[s][:], c.s["gks"][t * 128:(t + 1) * 128, :], [], [("gk", s)])
                for k in range(4):
                    p.op("pool", lambda e, s=s, k=k: e.indirect_dma_start(
                        out=yk_[s][k][:], out_offset=None, in_=c.s["yg"][:, :],
                        in_offset=bass.IndirectOffsetOnAxis(ap=sl[s][:, k:k + 1], axis=0),
                        bounds_check=c.breg, oob_is_err=False), [("sl", s)], [("yk", s, k)], dma=True)
                p.op("dve", lambda e, s=s: e.tensor_scalar(out=ya[s][:], in0=yk_[s][0][:], scalar1=gk[s][:, 0:1],
                                                           scalar2=None, op0=ALU.mult),
                     [("yk", s, 0), ("gk", s)], [("ya", s)])
                for k in range(1, 4):
                    p.op("dve", lambda e, s=s, k=k: e.scalar_tensor_tensor(
                        out=ya[s][:], in0=yk_[s][k][:], scalar=gk[s][:, k:k + 1], in1=ya[s][:],
                        op0=ALU.mult, op1=ALU.add), [("yk", s, k), ("gk", s), ("ya", s)], [("ya", s)])
            else:
                dma(p, "sp", ya[s][:], c.s["ymoe"][t * 128:(t + 1) * 128, :], [], [("ya", s)])
            p.op("dve", lambda e, s=s: e.scalar_tensor_tensor(
                out=tsb[s][:], in0=xa[s][:], scalar=DN_ALPHA, in1=ya[s][:], op0=ALU.mult, op1=ALU.add),
                [("xa", s), ("ya", s)], [("tsb", s)])
            layer_norm_tile(c, tsb[s], ("tsb", s), junk, "junk", sm[s], ("sm", s), lng, lnb, "lngb",
                            out[s], ("out", s))
            dma(p, "sp", dst[t * 128:(t + 1) * 128, :], out[s][:], [("out", s)], [("d_out", t)])
        p.barrier()
        p.emit()


ALL_PHASES = ("p1", "na", "gqa", "mla", "merge", "moes", "ln2")
_NC_CACHE = {}


def kernel(**inputs):
    inp = {k: np.asarray(v) for k, v in inputs.items()}
    x = np.ascontiguousarray(inp["x"].astype(np.float32))
    nb = x.shape[0]
    if "nc" not in _NC_CACHE:
        _NC_CACHE["nc"] = build()
    consts = [prep_consts(hf) for hf in range(2)]
    wts = [prep_weights(inp, [0, 1], hf) for hf in range(2)]
    in_maps = []
    for cid in range(2 * nb):
        b, hf = cid // 2, cid % 2
        m = {}
        m.update(wts[hf])
        m.update(consts[hf])
        m.update(prep_acts(x[b], hf))
        in_maps.append(m)
    res = run_bass_kernel_spmd(_NC_CACHE["nc"], in_maps, core_ids=list(range(2 * nb)))
    return np.stack([np.concatenate([np.asarray(res.results[2 * b]["y"]),
                                     np.asarray(res.results[2 * b + 1]["y"])], 0)
                     for b in range(nb)], 0).astype(np.float32)
```

```python
from contextlib import ExitStack
import numpy as np
import ml_dtypes
import concourse.bass as bass
import concourse.mybir as mybir
from concourse.bass_utils import run_bass_kernel_spmd

F32 = mybir.dt.float32
BF16 = mybir.dt.bfloat16
I32 = mybir.dt.int32
U32 = mybir.dt.uint32
AF = mybir.ActivationFunctionType
ALU = mybir.AluOpType
AX = mybir.AxisListType

D = 1024
S = 8192
HALF = 4096
DIN = 5536
NEXP = 32
DEXP = 1024
LN_EPS = 1e-5
RMS_EPS = 1e-6
DN_ALPHA = 4.0 ** 0.25
NEG = -30000.0
NA_ROWS = 72

COMPUTE = ("pe", "act", "dve", "pool")


class Prog:
    def __init__(self, nc, stack, ring=8):
        self.nc = nc
        self.e = {"pe": nc.tensor, "act": nc.scalar, "dve": nc.vector,
                  "pool": nc.gpsimd, "sp": nc.sync}
        self.ops = []
        self.done = 0
        self.sem = {k: stack.enter_context(nc.semaphore("s_" + k)) for k in COMPUTE}
        self.ticket = {k: 0 for k in COMPUTE}
        self.ring = {q: [stack.enter_context(nc.semaphore("d_%s%d" % (q, i))) for i in range(ring)]
                     for q in ("sp", "pool")}
        self.ring_cnt = {q: [0] * ring for q in ("sp", "pool")}
        self.ring_last = {q: [None] * ring for q in ("sp", "pool")}
        self.ring_pos = {q: 0 for q in ("sp", "pool")}
        self.waited = {k: {} for k in self.e}
        self.last_w = {}
        self.readers = {}
        self.sig = {}
        self.eidx = {}
        self.ecount = {k: 0 for k in self.e}
        self.last_op = {k: None for k in self.e}
        self.dma_open = []

    def op(self, eng, fn, r=(), w=(), dma=False):
        self.ops.append((eng, fn, tuple(r), tuple(w), dma))

    def barrier(self):
        self.ops.append(("BAR", None, (), (), False))

    def _wait(self, eng, sem, val):
        cur = self.waited[eng].get(sem, 0)
        if cur < val:
            self.e[eng].wait_ge(sem, val)
            self.waited[eng][sem] = val

    def emit(self):
        ops = self.ops
        n = len(ops)
        start = self.done
        deps = {}
        needed = set()
        last_w, readers = self.last_w, self.readers
        eidx, ecount = self.eidx, self.ecount
        openg = {}
        for i in range(start, n):
            eng, fn, r, w, dma = ops[i]
            if eng == "BAR":
                continue
            eidx[i] = ecount[eng]
            ecount[eng] += 1
            openg[i] = (eng, dma)
            d = set()
            raw = set()
            for k in r:
                if k in last_w:
                    d.add(last_w[k])
                    raw.add(last_w[k])
                if isinstance(k, str) and k.startswith("ps"):
                    rd = readers.get(k)
                    if rd:
                        d.update(rd[0].values())
                        d.update(rd[1])
            for k in w:
                if k in last_w:
                    d.add(last_w[k])
                rd = readers.get(k)
                if rd:
                    d.update(rd[0].values())
                    d.update(rd[1])
            for k in r:
                rd = readers.setdefault(k, ({}, []))
                if dma:
                    rd[1].append(i)
                else:
                    rd[0][eng] = i
            for k in w:
                last_w[k] = i
                readers[k] = ({}, [])
            d.discard(i)
            keep = set()
            for j in d:
                if j < start and j not in self.sig and j not in openg:
                    continue
                jeng, jdma = self._opinfo(j, openg)
                if (not dma) and (not jdma) and jeng == eng:
                    if eng == "pe":
                        continue
                    if j in raw and eidx[i] - eidx[j] <= 2:
                        keep.add(j)
                    continue
                keep.add(j)
            deps[i] = keep
            needed.update(keep)
        self._openg_all = getattr(self, "_openg_all", {})
        self._openg_all.update(openg)
        for i in range(start, n):
            eng, fn, r, w, dma = ops[i]
            if eng == "BAR":
                self._emit_barrier()
                continue
            if dma:
                q = eng
                pos = self.ring_pos[q]
                self.ring_pos[q] = (pos + 1) % len(self.ring[q])
                sem = self.ring[q][pos]
                prev = self.ring_last[q][pos]
                if prev is not None:
                    self._wait(eng, sem, prev)
            for j in sorted(deps[i]):
                if j in self.sig:
                    s, v = self.sig[j]
                    self._wait(eng, s, v)
            ins = fn(self.e[eng])
            if dma:
                self.ring_cnt[q][pos] += 16
                val = self.ring_cnt[q][pos]
                ins.then_inc(sem, 16)
                self.ring_last[q][pos] = val
                self.sig[i] = (sem, val)
                self.dma_open.append(i)
            else:
                self.last_op[eng] = i
                if i in needed:
                    self.ticket[eng] += 1
                    ins.then_inc(self.sem[eng], 1)
                    self.sig[i] = (self.sem[eng], self.ticket[eng])
        self.done = n

    def _opinfo(self, j, openg):
        if j in openg:
            return openg[j]
        return self._openg_all[j]

    def _emit_barrier(self):
        marks = []
        for eng in COMPUTE:
            self.ticket[eng] += 1
            self.e[eng].drain().then_inc(self.sem[eng], 1)
            marks.append((self.sem[eng], self.ticket[eng]))
        dmas = [self.sig[i] for i in self.dma_open]
        self.dma_open = []
        for eng in self.e:
            for s, v in marks:
                self._wait(eng, s, v)
            for s, v in dmas:
                self._wait(eng, s, v)
        self.last_w.clear()
        self.readers.clear()


class Ctx:
    pass


_UID = [0]


def _sb(c, name, shape, dt):
    _UID[0] += 1
    return c.ph.enter_context(c.nc.sbuf_tensor("sb%d_%s" % (_UID[0], name), list(shape), dt))


def _ps(c, name, shape, dt):
    _UID[0] += 1
    return c.ph.enter_context(c.nc.psum_tensor("pp%d_%s" % (_UID[0], name), list(shape), dt))


def mm_group(p, out_ap, pairs, r, w):
    n = len(pairs)

    def fn(e):
        ins = None
        for i, (l, rr) in enumerate(pairs):
            ins = e.matmul(out_ap, l, rr, start=(i == 0), stop=(i == n - 1))
        return ins
    p.op("pe", fn, r, w)


def tr_group(p, outs_ins, ident, r, w):
    def fn(e):
        ins = None
        for o, i_ in outs_ins:
            ins = e.transpose(o, i_, ident)
        return ins
    p.op("pe", fn, r, w)


def dma(p, q, out_ap, in_ap, r, w):
    p.op(q, lambda e: e.dma_start(out=out_ap, in_=in_ap), r, w, dma=True)


def load_cast_cols(p, dst, src, ncols, r, w, step=2048):
    for c0 in range(0, ncols, step):
        c1 = min(ncols, c0 + step)
        dma(p, "pool", dst[:, c0:c1], src[:, c0:c1], r, w)


def phase1(c, L):
    nc, p = c.nc, c.p
    with ExitStack() as ph:
        c.ph = ph
        winb = _sb(c, "winb", [128, 8, DIN], BF16)
        wuqb = _sb(c, "wuqb", [128, 3, 384], BF16)
        wukvb = _sb(c, "wukvb", [128, 2, 512], BF16)
        identb = _sb(c, "identb", [128, 128], BF16)
        gq = _sb(c, "gq", [128, 384], F32)
        gk = _sb(c, "gk", [128, 128], F32)
        mq = _sb(c, "mq", [128, 384], F32)
        mkv = _sb(c, "mkv", [128, 256], F32)
        xf = [_sb(c, "xf%d" % i, [128, D], F32) for i in range(2)]
        xb = [_sb(c, "xb%d" % i, [128, D], BF16) for i in range(2)]
        xT = [_sb(c, "xT%d" % i, [128, 8, 512], BF16) for i in range(2)]
        rp = [_sb(c, "rp%d" % i, [128, 192], F32) for i in range(2)]
        wk = [_sb(c, "wk%d" % i, [128, 384], F32) for i in range(6)]
        sm = [_sb(c, "sm%d" % i, [128, 8], F32) for i in range(4)]
        ob = [_sb(c, "ob%d" % i, [128, 384], BF16) for i in range(4)]
        tT = [_sb(c, "tT%d" % i, [128, 512], BF16) for i in range(2)]
        blk = {k: [_sb(c, "blk_%s%d" % (k, i), [128, 6 * 512], BF16) for i in range(2)]
               for k in ("a", "b", "c", "d")}
        vna = [_sb(c, "vna%d" % i, [128, 6, 65], BF16) for i in range(2)]
        vg = [_sb(c, "vg%d" % i, [128, 2, 65], BF16) for i in range(2)]
        vm = [_sb(c, "vm%d" % i, [128, 4, 65], BF16) for i in range(2)]
        gout = [_sb(c, "gout%d" % i, [128, 512], BF16) for i in range(3)]
        psT = _ps(c, "psT", [128, 8, 128], BF16)
        psR = _ps(c, "psR", [128, 8, 128], BF16)
        psM = [_ps(c, "psM%d" % i, [128, 512], F32) for i in range(4)]
        psG = [_ps(c, "psG%d" % i, [128, 512], F32) for i in range(2)]

        w_in = c.w["w_in"][L]
        w_in_v = w_in.rearrange("(c p) n -> p c n", p=128)
        for ch in range(8):
            load_cast_cols(p, winb[:, ch, :], w_in_v[:, ch, :], DIN, [], ["winb"])
        wuq_v = c.w["w_uq"][L].rearrange("(c p) n -> p c n", p=128)
        for ch in range(3):
            load_cast_cols(p, wuqb[:, ch, :], wuq_v[:, ch, :], 384, [], ["wuqb"])
        wukv_v = c.w["w_ukv"][L].rearrange("(c p) n -> p c n", p=128)
        for ch in range(2):
            load_cast_cols(p, wukvb[:, ch, :], wukv_v[:, ch, :], 512, [], ["wukvb"])
        dma(p, "sp", identb[:], c.w["identb"][:, :], [], ["identb"])
        dma(p, "sp", gq[:], c.w["gq_rep"][L], [], ["gq"])
        dma(p, "sp", gk[:], c.w["gk_rep"][L], [], ["gk"])
        dma(p, "sp", mq[:], c.w["mq_rep"][L], [], ["mq"])
        dma(p, "sp", mkv[:], c.w["mkv_rep"][L], [], ["mkv"])
        for i in range(2):
            p.op("pool", lambda e, t=vna[i]: e.memset(t[:], 1.0), [], [("vna", i)])
            p.op("pool", lambda e, t=vg[i]: e.memset(t[:], 1.0), [], [("vg", i)])
            p.op("pool", lambda e, t=vm[i]: e.memset(t[:], 1.0), [], [("vm", i)])

        cnt = {"tile": 0, "psM": 0, "psG": 0, "wk": 0, "sm": 0, "ob": 0, "tT": 0, "gout": 0}
        pending = []

        def st(out_ap, in_ap, r, w):
            pending.append((out_ap, in_ap, r, w))

        def flush():
            for o_, i_, r_, w_ in pending:
                dma(p, "sp", o_, i_, r_, w_)
            del pending[:]

        def nxt(k, n):
            v = cnt[k] % n
            cnt[k] += 1
            return v

        def load_tile(src_ap, rope_ap, to_xT=None, mask_col=None, defer_tr=False):
            s = nxt("tile", 2)
            dma(p, "sp", xf[s][:], src_ap, [], [("xf", s)])
            if rope_ap is not None:
                dma(p, "sp", rp[s][:], rope_ap, [], [("rp", s)])
            if mask_col is not None:
                p.op("dve", lambda e: e.tensor_scalar(out=xf[s][:], in0=xf[s][:],
                                                      scalar1=c.hm[:, mask_col:mask_col + 1], scalar2=None,
                                                      op0=ALU.mult), [("xf", s), "hm"], [("xf", s)])
            p.op("dve", lambda e: e.tensor_copy(xb[s][:], xf[s][:]), [("xf", s)], [("xb", s)])
            if to_xT is None:
                dst, key = tT_full[s][:], ("tTf", s)
            else:
                dst, key = to_xT

            def fin():
                tr_group(p, [(psT[:, ch, :], xb[s][:, ch * 128:(ch + 1) * 128]) for ch in range(8)],
                         identb[:], [("xb", s), "identb"], ["psT"])
                p.op("act", lambda e: e.activation(out=dst, in_=psT[:], func=AF.Copy), ["psT"], [key])
            if defer_tr:
                return dst, key, rp[s], ("rp", s), fin
            fin()
            return dst, key, rp[s], ("rp", s)

        tT_full = [_sb(c, "tTf%d" % i, [128, 8, 128], BF16) for i in range(2)]

        def tok_mm(xTa, xTkey, c0, c1):
            b = nxt("psM", 4)
            mm_group(p, psM[b][:, 0:c1 - c0],
                     [(xTa[:, ch, :], winb[:, ch, c0:c1]) for ch in range(8)],
                     [xTkey, "winb"], ["psM%d" % b])
            return psM[b], "psM%d" % b

        def transp_out(src_bf, src_key, nh, dh, dst_ap, dst_key):
            tr_group(p, [(psR[0:dh, h, :], src_bf[:, h * dh:(h + 1) * dh]) for h in range(nh)],
                     identb[:], [src_key, "identb"], ["psR"])
            p.op("dve", lambda e: e.tensor_copy(dst_ap, psR[0:dh, 0:nh, :]), ["psR"], [dst_key])

        def rms_rope(ps_ap, pskey, nh, dh, g_ap, gkey, rope_t, rpkey, coff, out_bf, outkey):
            n = nh * dh
            hd = dh // 2
            a = nxt("wk", 6); b2 = nxt("wk", 6); c2 = nxt("wk", 6); d2 = nxt("wk", 6); e2 = nxt("wk", 6)
            s1 = nxt("sm", 4)
            A, B, C, Dd, E = wk[a], wk[b2], wk[c2], wk[d2], wk[e2]
            p.op("act", lambda e: e.activation(out=A[:, 0:n], in_=ps_ap, func=AF.Square),
                 [pskey], [("wk", a)])
            p.op("dve", lambda e: e.tensor_reduce(
                out=sm[s1][:, 0:nh], in_=A[:, 0:n].rearrange("p (h d) -> p h d", h=nh),
                axis=AX.X, op=ALU.add), [("wk", a)], [("sm", s1)])
            p.op("act", lambda e: e.activation(
                out=sm[s1][:, 0:nh], in_=sm[s1][:, 0:nh], func=AF.Sqrt, scale=1.0 / dh, bias=c.eps_rms[:, 0:1]),
                [("sm", s1)], [("sm", s1)])
            p.op("dve", lambda e: e.reciprocal(out=sm[s1][:, 0:nh], in_=sm[s1][:, 0:nh]),
                 [("sm", s1)], [("sm", s1)])
            p.op("dve", lambda e: e.tensor_tensor(
                out=B[:, 0:n].rearrange("p (h d) -> p h d", h=nh),
                in0=ps_ap.rearrange("p (h d) -> p h d", h=nh),
                in1=sm[s1][:, 0:nh].unsqueeze(2).to_broadcast([128, nh, dh]),
                op=ALU.mult), [pskey, ("sm", s1)], [("wk", b2)])
            p.op("dve", lambda e: e.tensor_tensor(out=C[:, 0:n], in0=B[:, 0:n], in1=g_ap, op=ALU.mult),
                 [("wk", b2), gkey], [("wk", c2)])
            rope(C, ("wk", c2), nh, dh, rope_t, rpkey, coff, Dd, ("wk", d2), E, ("wk", e2),
                 out_bf[:, 0:n].rearrange("p (h d) -> p h d", h=nh), outkey)

        def rope(X, xkey, nh, dh, rope_t, rpkey, coff, Dd, dkey, E, ekey, out3, outkey, xview=None):
            n = nh * dh
            hd = dh // 2
            x3 = xview if xview is not None else X[:, 0:n].rearrange("p (h d) -> p h d", h=nh)
            cs = rope_t[:, coff:coff + dh].unsqueeze(1).to_broadcast([128, nh, dh])
            sslo = rope_t[:, coff + dh:coff + dh + hd].unsqueeze(1).to_broadcast([128, nh, hd])
            sshi = rope_t[:, coff + dh + hd:coff + 2 * dh].unsqueeze(1).to_broadcast([128, nh, hd])
            d3 = Dd[:, 0:n].rearrange("p (h d) -> p h d", h=nh)
            e3 = E[:, 0:n].rearrange("p (h d) -> p h d", h=nh)
            p.op("dve", lambda e: e.tensor_tensor(out=d3, in0=x3, in1=cs, op=ALU.mult),
                 [xkey, rpkey], [dkey])
            p.op("dve", lambda e: e.tensor_tensor(out=e3[:, :, 0:hd], in0=x3[:, :, hd:dh], in1=sslo,
                                                  op=ALU.mult), [xkey, rpkey], [ekey])
            p.op("dve", lambda e: e.tensor_tensor(out=e3[:, :, hd:dh], in0=x3[:, :, 0:hd], in1=sshi,
                                                  op=ALU.mult), [xkey, rpkey, ekey], [ekey])
            p.op("pool", lambda e: e.tensor_tensor(out=out3, in0=d3, in1=e3, op=ALU.add),
                 [dkey, ekey], [outkey])

        def rms_full(ps_ap, pskey, n, g_ap, gkey, out_bf, outkey):
            a = nxt("wk", 6)
            s1 = nxt("sm", 4)
            p.op("act", lambda e: e.activation(out=wk[a][:, 0:n], in_=ps_ap, func=AF.Square,
                                               accum_out=sm[s1][:, 0:1]),
                 [pskey], [("wk", a), ("sm", s1)])
            p.op("act", lambda e: e.activation(
                out=sm[s1][:, 1:2], in_=sm[s1][:, 0:1], func=AF.Sqrt, scale=1.0 / n, bias=c.eps_rms[:, 0:1]),
                [("sm", s1)], [("sm", s1)])
            p.op("dve", lambda e: e.reciprocal(out=sm[s1][:, 2:3], in_=sm[s1][:, 1:2]),
                 [("sm", s1)], [("sm", s1)])
            p.op("dve", lambda e: e.scalar_tensor_tensor(
                out=out_bf, in0=ps_ap, scalar=sm[s1][:, 2:3], in1=g_ap,
                op0=ALU.mult, op1=ALU.mult), [pskey, ("sm", s1), gkey], [outkey])

        def na_kv(xTa, xTkey, tok_off, sub, bs):
            ps, pk = tok_mm(xTa, xTkey, 384, 768)
            o = nxt("ob", 4)
            p.op("act", lambda e, ps=ps, o=o: e.activation(out=ob[o][:, 0:384], in_=ps[:, 0:384],
                                                           func=AF.Copy), [pk], [("ob", o)])
            kb = blk["b"][bs][0:64, :].rearrange("p (h t) -> p h t", h=6)
            transp_out(ob[o], ("ob", o), 6, 64, kb[:, :, sub * 128:(sub + 1) * 128], ("blk_b", bs))
            ps, pk = tok_mm(xTa, xTkey, 768, 1152)
            s = nxt("tile", 2) if False else (cnt["tile"] - 1) % 2
            p.op("act", lambda e, ps=ps, s=s: e.activation(
                out=vna[s][:, :, 0:64], in_=ps[:, 0:384].rearrange("p (h d) -> p h d", h=6),
                func=AF.Copy), [pk], [("vna", s)])
            st(c.s["v_na"][tok_off:tok_off + 128, :], vna[s][:].rearrange("p h d -> p (h d)"),
                [("vna", s)], [("d_v_na", tok_off)])

        def flush_blk(name, bs, dh, nh, dst, t0, nt):
            src = blk[name][bs][0:dh, 0:nh * 512].rearrange("p (h t) -> p h t", h=nh)[:, :, 0:nt]
            st(dst[:, :, t0:t0 + nt].rearrange("h d t -> d h t"), src,
               [("blk_" + name, bs)], [("d_" + name, t0)])

        def own_load(t):
            bi_, sub_ = divmod(t, 4)
            return load_tile(c.q_src[t * 128:(t + 1) * 128, :], c.rope_q[t * 128:(t + 1) * 128, :],
                             to_xT=(xT[bi_ % 2][:, :, sub_ * 128:(sub_ + 1) * 128], ("xT", bi_ % 2)),
                             defer_tr=True)
        nxt_h = own_load(0)
        nxt_h[4]()
        for bi in range(8):
            bs = bi % 2
            for sub in range(4):
                t = bi * 4 + sub
                xTa, xTkey, rpt, rpk = nxt_h[0:4]
                if t + 1 < 32:
                    nxt_h = own_load(t + 1)
                flush()
                ps, pk = tok_mm(xTa, xTkey, 0, 384)
                o = nxt("ob", 4)
                p.op("act", lambda e, o=o, ps=ps: e.activation(out=ob[o][:, 0:384], in_=ps[:, 0:384],
                                                               func=AF.Copy), [pk], [("ob", o)])
                qa = blk["a"][bs][0:64, :].rearrange("p (h t) -> p h t", h=6)
                transp_out(ob[o], ("ob", o), 6, 64, qa[:, :, sub * 128:(sub + 1) * 128], ("blk_a", bs))
                na_kv(xTa, xTkey, 256 + t * 128, sub, bs)
                ps, pk = tok_mm(xTa, xTkey, 1152, 1536)
                o = nxt("ob", 4)
                rms_rope(ps[:, 0:384], pk, 6, 64, gq[:], "gq", rpt, rpk, 0, ob[o], ("ob", o))
                qg = blk["c"][bs][0:64, :].rearrange("p (h t) -> p h t", h=6)
                transp_out(ob[o], ("ob", o), 6, 64, qg[:, :, sub * 128:(sub + 1) * 128], ("blk_c", bs))
                ps, pk = tok_mm(xTa, xTkey, 1792, 2176)
                o = nxt("ob", 4)
                rms_full(ps[:, 0:384], pk, 384, mq[:], "mq", ob[o][:, 0:384], ("ob", o))
                tt = nxt("tT", 2)
                tr_group(p, [(psR[:, ch, :], ob[o][:, ch * 128:(ch + 1) * 128]) for ch in range(3)],
                         identb[:], [("ob", o), "identb"], ["psR"])
                p.op("act", lambda e, tt=tt: e.activation(
                    out=tT[tt][:, 0:384].rearrange("p (c t) -> p c t", c=3), in_=psR[:, 0:3, :],
                    func=AF.Copy), ["psR"], [("tT", tt)])
                g = nxt("psG", 2)
                tT3 = tT[tt][:, 0:384].rearrange("p (c t) -> p c t", c=3)
                mm_group(p, psG[g][:, 0:384], [(tT3[:, ch, :], wuqb[:, ch, :]) for ch in range(3)],
                         [("tT", tt), "wuqb"], ["psG%d" % g])
                o2 = nxt("ob", 4)
                qc3 = psG[g][:, 0:384].rearrange("p (h d) -> p h d", h=4)
                out3 = ob[o2][:, 0:384].rearrange("p (h d) -> p h d", h=4)
                p.op("act", lambda e, qc3=qc3, out3=out3: e.activation(
                    out=out3[:, :, 0:64], in_=qc3[:, :, 0:64], func=AF.Copy),
                    ["psG%d" % g], [("ob", o2)])
                a = nxt("wk", 6); d2 = nxt("wk", 6); e2 = nxt("wk", 6)
                xr3 = wk[a][:, 0:128].rearrange("p (h d) -> p h d", h=4)
                p.op("act", lambda e, qc3=qc3, xr3=xr3: e.activation(out=xr3, in_=qc3[:, :, 64:96],
                                                                     func=AF.Copy),
                     ["psG%d" % g], [("wk", a)])
                rope(wk[a], ("wk", a), 4, 32, rpt, rpk, 128, wk[d2], ("wk", d2), wk[e2], ("wk", e2),
                     out3[:, :, 64:96], ("ob", o2))
                qm = blk["d"][bs][0:96, 0:2048].rearrange("p (h t) -> p h t", h=4)
                transp_out(ob[o2], ("ob", o2), 4, 96, qm[:, :, sub * 128:(sub + 1) * 128], ("blk_d", bs))
                if t + 1 < 32:
                    nxt_h[4]()
            flush_blk("a", bs, 64, 6, c.s["qT_na"], bi * 512, 512)
            flush_blk("b", bs, 64, 6, c.s["kT_na"], 256 + bi * 512, 512)
            flush_blk("c", bs, 64, 6, c.s["qT_gqa"], bi * 512, 512)
            flush_blk("d", bs, 96, 4, c.s["qT_mla"], bi * 512, 512)
            for cc in range(24):
                g = nxt("psG", 2)
                c0 = 2464 + cc * 128
                mm_group(p, psG[g][:, :], [(winb[:, ch, c0:c0 + 128], xT[bs][:, ch, :]) for ch in range(8)],
                         [("xT", bs), "winb"], ["psG%d" % g])
                go = nxt("gout", 3)
                p.op("act", lambda e, g=g, go=go: e.activation(out=gout[go][:], in_=psG[g][:, :],
                                                               func=AF.Sigmoid),
                     ["psG%d" % g], [("gout", go)])
                dma(p, "sp", c.s["gT"][cc * 128:(cc + 1) * 128, bi * 512:(bi + 1) * 512], gout[go][:],
                    [("gout", go)], [("d_gT", cc, bi)])

        for hi in range(4):
            hsrc = (c.c_src[HALF - 256 + hi * 128:HALF - 256 + (hi + 1) * 128, :] if hi < 2 else
                    c.c_src[(hi - 2) * 128:(hi - 1) * 128, :])
            xTa, xTkey, rpt, rpk = load_tile(hsrc, None, mask_col=c.hm_cols[0 if hi < 2 else 1])
            flush()
            tok_off = hi * 128 if hi < 2 else 4352 + (hi - 2) * 128
            na_kv(xTa, xTkey, tok_off, hi % 2, 0)
            if hi % 2 == 1:
                flush_blk("b", 0, 64, 6, c.s["kT_na"], 0 if hi == 1 else 4352, 256)

        def full_load(t):
            return load_tile(c.full_src[t * 128:(t + 1) * 128, :], c.rope_full[t * 128:(t + 1) * 128, :],
                             defer_tr=True)
        flush()
        if c.do_full:
            nxt_h = full_load(0)
            nxt_h[4]()
        for bi in range(16 if c.do_full else 0):
            bs = bi % 2
            for sub in range(4):
                t = bi * 4 + sub
                xTa, xTkey, rpt, rpk = nxt_h[0:4]
                if t + 1 < 64:
                    nxt_h = full_load(t + 1)
                flush()
                s = (cnt["tile"] - 1) % 2
                ps, pk = tok_mm(xTa, xTkey, 1536, 1792)
                o = nxt("ob", 4)
                rms_rope(ps[:, 0:128], pk, 2, 64, gk[:], "gk", rpt, rpk, 0, ob[o], ("ob", o))
                kg = blk["a"][bs][0:64, 0:1024].rearrange("p (h t) -> p h t", h=2)
                transp_out(ob[o], ("ob", o), 2, 64, kg[:, :, sub * 128:(sub + 1) * 128], ("blk_a", bs))
                p.op("act", lambda e, ps=ps, s=s: e.activation(
                    out=vg[s][:, :, 0:64], in_=ps[:, 128:256].rearrange("p (h d) -> p h d", h=2),
                    func=AF.Copy), [pk], [("vg", s)])
                st(c.s["v_gqa"][t * 128:(t + 1) * 128, :], vg[s][:].rearrange("p h d -> p (h d)"),
                    [("vg", s)], [("d_v_gqa", t)])
                ps, pk = tok_mm(xTa, xTkey, 2176, 2464)
                o = nxt("ob", 4)
                rms_full(ps[:, 0:256], pk, 256, mkv[:], "mkv", ob[o][:, 0:256], ("ob", o))
                a = nxt("wk", 6)
                p.op("act", lambda e, ps=ps, a=a: e.activation(out=wk[a][:, 0:32], in_=ps[:, 256:288],
                                                               func=AF.Copy), [pk], [("wk", a)])
                tt = nxt("tT", 2)
                tr_group(p, [(psR[:, ch, :], ob[o][:, ch * 128:(ch + 1) * 128]) for ch in range(2)],
                         identb[:], [("ob", o), "identb"], ["psR"])
                tT2 = tT[tt][:, 0:256].rearrange("p (c t) -> p c t", c=2)
                p.op("act", lambda e, tT2=tT2: e.activation(out=tT2, in_=psR[:, 0:2, :], func=AF.Copy),
                     ["psR"], [("tT", tt)])
                g = nxt("psG", 2)
                mm_group(p, psG[g][:, :], [(tT2[:, ch, :], wukvb[:, ch, :]) for ch in range(2)],
                         [("tT", tt), "wukvb"], ["psG%d" % g])
                kv3 = psG[g][:, :].rearrange("p (h d) -> p h d", h=4)
                o2 = nxt("ob", 4)
                out3 = ob[o2][:, 0:384].rearrange("p (h d) -> p h d", h=4)
                p.op("act", lambda e, kv3=kv3, out3=out3: e.activation(
                    out=out3[:, :, 0:64], in_=kv3[:, :, 0:64], func=AF.Copy),
                    ["psG%d" % g], [("ob", o2)])
                p.op("dve", lambda e, kv3=kv3, s=s: e.tensor_copy(vm[s][:, :, 0:64], kv3[:, :, 64:128]),
                     ["psG%d" % g], [("vm", s)])
                st(c.s["v_mla"][t * 128:(t + 1) * 128, :], vm[s][:].rearrange("p h d -> p (h d)"),
                    [("vm", s)], [("d_v_mla", t)])
                d2 = nxt("wk", 6); e2 = nxt("wk", 6); f2 = nxt("wk", 6)
                kr3 = wk[f2][:, 0:32].rearrange("p (h d) -> p h d", h=1)
                rope(wk[a], ("wk", a), 1, 32, rpt, rpk, 128, wk[d2], ("wk", d2), wk[e2], ("wk", e2),
                     kr3, ("wk", f2))
                p.op("pool", lambda e, out3=out3, f2=f2: e.tensor_copy(
                    out3[:, :, 64:96], wk[f2][:, 0:32].unsqueeze(1).to_broadcast([128, 4, 32])),
                    [("wk", f2), ("ob", o2)], [("ob", o2)])
                km = blk["d"][bs][0:96, 0:2048].rearrange("p (h t) -> p h t", h=4)
                transp_out(ob[o2], ("ob", o2), 4, 96, km[:, :, sub * 128:(sub + 1) * 128], ("blk_d", bs))
                if t + 1 < 64:
                    nxt_h[4]()
            flush_blk("a", bs, 64, 2, c.s["kT_gqa"], bi * 512, 512)
            flush_blk("d", bs, 96, 4, c.s["kT_mla"], bi * 512, 512)
        flush()
        p.barrier()
        p.emit()


W_SHAPES = {
    "w_in": ([D, DIN], F32), "w_uq": ([384, 384], F32), "w_ukv": ([256, 512], F32),
    "w_ba": ([384, D], F32), "w_bb": ([384, D], F32), "w_bc": ([256, D], F32),
    "w_out": ([D, D], F32), "w_router": ([D, NEXP], F32),
    "w_gu": ([NEXP, D, 2 * DEXP], F32), "w_dn": ([NEXP, DEXP, D], F32),
    "gq_rep": ([128, 384], F32), "gk_rep": ([128, 128], F32),
    "mq_rep": ([128, 384], F32), "mkv_rep": ([128, 256], F32),
    "ln1g": ([128, D], F32), "ln1b": ([128, D], F32), "ln2g": ([128, D], F32), "ln2b": ([128, D], F32),
    "brouter": ([128, NEXP], F32), "bgu_t": ([128, NEXP, 16], F32), "bdn": ([NEXP, D], F32),
    "na_bint": ([64, 6, 8, 64], F32), "na_bbnd": ([7, 64, 6, 12, 64], F32),
    "na_bbnd_o": ([7, 64, 6, 12, 64], F32),
}
C_SHAPES = {
    "rope_loc": ([S, 192], F32), "hmask": ([128, 4], F32),
    "identb": ([128, 128], BF16), "identf": ([128, 128], F32),
    "triu": ([128, 128], F32), "ones128": ([128, 128], F32),
    "iota_e": ([128, NEXP], F32), "iota_cap": ([128, NEXP], F32),
}
CAP = 768
NSLOT = NEXP * CAP
S_SHAPES = {
    "qT_na": ([6, 64, HALF], BF16), "kT_na": ([6, 64, NA_ROWS * 64], BF16),
    "v_na": ([NA_ROWS * 64, 390], BF16),
    "qT_gqa": ([6, 64, HALF], BF16), "kT_gqa": ([2, 64, S], BF16), "v_gqa": ([S, 130], BF16),
    "qT_mla": ([4, 96, HALF], BF16), "kT_mla": ([4, 96, S], BF16), "v_mla": ([S, 260], BF16),
    "gT": ([3 * D, HALF], BF16),
    "oT": ([16, 64, HALF], BF16),
    "x1": ([HALF, D], F32),
    "x1T": ([D, HALF], BF16), "gateT": ([NEXP, HALF], F32), "ymoe": ([HALF, D], F32),
    "xg": ([NSLOT, D], BF16), "yg": ([NSLOT, D], F32),
    "slots": ([HALF, 4], I32), "gks": ([HALF, 4], F32),
}


def build(taps=(), passes=("A", "B", "C"), phases=None, dst_override=None):
    nc = bass.Bass("TRN2", target_bir_lowering=False)
    phases = phases or ALL_PHASES
    c = Ctx()
    c.nc = nc
    c.w = {}
    for k, (shp, dt) in W_SHAPES.items():
        c.w[k] = nc.dram_tensor(k, [2] + shp, dt, kind="ExternalInput").ap()
    for k, (shp, dt) in C_SHAPES.items():
        c.w[k] = nc.dram_tensor(k, shp, dt, kind="ExternalInput").ap()
    x_loc = nc.dram_tensor("x_loc", [S, D], F32, kind="ExternalInput").ap()
    c.s = {}
    for k, (shp, dt) in S_SHAPES.items():
        kind = "ExternalOutput" if k in taps else "Internal"
        c.s[k] = nc.dram_tensor("s_" + k, shp, dt, kind=kind).ap()
    y01 = nc.dram_tensor("y01", [S, D], F32, kind="Internal").ap()
    c.y = nc.dram_tensor("y", [HALF, D], F32, kind="ExternalOutput").ap()
    with ExitStack() as stack:
        c.p = Prog(nc, stack)
        c.breg = nc.gpsimd.to_reg(NSLOT - 1)
        c.eps_rms = stack.enter_context(nc.sbuf_tensor("eps_rms", [128, 1], F32))
        c.eps_ln = stack.enter_context(nc.sbuf_tensor("eps_ln", [128, 1], F32))
        c.hm = stack.enter_context(nc.sbuf_tensor("hmask_sb", [128, 4], F32))
        c.p.op("pool", lambda e: e.memset(c.eps_rms[:], RMS_EPS), [], ["eps"])
        c.p.op("pool", lambda e: e.memset(c.eps_ln[:], LN_EPS), [], ["eps"])
        dma(c.p, "sp", c.hm[:], c.w["hmask"][:, :], [], ["hm"])
        c.p.barrier()
        c.p.emit()
        rope = c.w["rope_loc"]
        c.rope_full = rope
        with ExitStack() as ph:
            zt = ph.enter_context(nc.sbuf_tensor("zt", [128, 8, D], BF16))
            c.p.op("pool", lambda e: e.memset(zt[:], 0.0), [], ["zt"])
            xg_v = c.s["xg"].rearrange("(n p) d -> p n d", p=128)
            for i in range(NSLOT // 1024):
                dma(c.p, "sp", xg_v[:, i * 8:(i + 1) * 8, :], zt[:], ["zt"], [("d_xg0", i)])
            c.p.barrier()
            c.p.emit()
        for ps_ in passes:
            if ps_ == "A":
                L, c.q_src, c.c_src, c.full_src = 0, x_loc[0:HALF, :], x_loc[HALF:S, :], x_loc
                c.rope_q, c.bbnd, c.hm_cols, c.do_full, dst = rope[0:HALF, :], c.w["na_bbnd"][0], (0, 1), True, y01[0:HALF, :]
            elif ps_ == "B":
                L, c.q_src, c.c_src, c.full_src = 0, x_loc[HALF:S, :], x_loc[0:HALF, :], x_loc
                c.rope_q, c.bbnd, c.hm_cols, c.do_full, dst = rope[HALF:S, :], c.w["na_bbnd_o"][0], (2, 3), False, y01[HALF:S, :]
            else:
                L, c.q_src, c.c_src, c.full_src = 1, y01[0:HALF, :], y01[HALF:S, :], y01
                c.rope_q, c.bbnd, c.hm_cols, c.do_full, dst = rope[0:HALF, :], c.w["na_bbnd"][1], (0, 1), True, c.y
            if "p1" in phases:
                phase1(c, L)
            if "na" in phases:
                phase_na(c, L)
            if "gqa" in phases:
                phase_dense_attn(c, L, "gqa")
            if "mla" in phases:
                phase_dense_attn(c, L, "mla")
            if "merge" in phases:
                phase_merge(c, L)
            if "moe" in phases:
                phase_moe(c, L)
            if "moes" in phases:
                phase_moe_sparse(c, L)
            if "ln2" in phases:
                phase_ln2(c, L, dst if dst_override is None else dst_override(c), sparse=("moes" in phases))
        c.p.barrier()
        c.p.emit()
    return nc


def rope_tables():
    t = np.arange(S)
    row = (t // 64).astype(np.float32)
    col = (t % 64).astype(np.float32)
    out = []
    for dim in (64, 32):
        quarter = dim // 4
        inv = (10000.0 ** (-np.arange(quarter, dtype=np.float32) / quarter)).astype(np.float32)
        ang = np.concatenate([row[:, None] * inv, col[:, None] * inv], -1).astype(np.float32)
        cs, sn = np.cos(ang).astype(np.float32), np.sin(ang).astype(np.float32)
        out.append(np.concatenate([cs, cs], -1))
        out.append(np.concatenate([-sn, sn], -1))
    return np.ascontiguousarray(np.concatenate(out, -1).astype(np.float32))


def na_bias_tables(rpb, hf):
    cols = np.arange(64)
    c0 = np.clip(cols - 8, 0, 48)
    in_win = (cols[None, :] >= c0[:, None]) & (cols[None, :] < c0[:, None] + 16)
    idx_c = np.clip(cols[None, :] - cols[:, None] + 15, 0, 30)

    def tab(j, rows):
        r = hf * 64 + j
        r0 = int(np.clip(r - 4, 0, 120))
        out = np.full((64, 6, len(rows), 64), NEG, np.float32)
        for ii, lr in enumerate(rows):
            gr = hf * 64 + lr - 4
            i = gr - r0
            if i < 0 or i >= 8 or gr < 0 or gr >= 128:
                continue
            ir = gr - r + 7
            b = rpb[:, ir][:, idx_c]
            b = np.where(in_win[None], b, NEG)
            out[:, :, ii, :] = b.transpose(2, 0, 1)
        return out
    interior = tab(10, list(range(10, 18)))
    bnd = []
    for j in (0, 1, 2, 3):
        bnd.append(tab(j, list(range(0, 12))))
    for j in (61, 62, 63):
        bnd.append(tab(j, list(range(60, 72))))
    return interior, np.stack(bnd, 0)


def prep_weights(inp, layers, hf):
    w = {}
    ls = list(layers)
    st = lambda f: np.ascontiguousarray(np.stack([f(l) for l in ls], 0))
    w["w_in"] = st(lambda l: inp["w_in"][l])
    w["w_uq"] = st(lambda l: inp["w_uq"][l])
    w["w_ukv"] = st(lambda l: inp["w_ukv"][l])
    w["w_ba"] = st(lambda l: inp["w_branch_a"][l])
    w["w_bb"] = st(lambda l: inp["w_branch_b"][l])
    w["w_bc"] = st(lambda l: inp["w_branch_c"][l])
    w["w_out"] = st(lambda l: inp["w_out"][l])
    w["w_router"] = st(lambda l: inp["w_router"][l])
    w["w_gu"] = st(lambda l: inp["w_gate_up"][l])
    w["w_dn"] = st(lambda l: inp["w_down"][l])
    w["gq_rep"] = st(lambda l: np.tile(inp["gqa_q_norm"][l][None, :], (128, 6)))
    w["gk_rep"] = st(lambda l: np.tile(inp["gqa_k_norm"][l][None, :], (128, 2)))
    w["mq_rep"] = st(lambda l: np.tile(inp["mla_q_norm"][l][None, :], (128, 1)))
    w["mkv_rep"] = st(lambda l: np.tile(inp["mla_kv_norm"][l][None, :], (128, 1)))
    for k, src in (("ln1g", "ln1_g"), ("ln1b", "ln1_b"), ("ln2g", "ln2_g"), ("ln2b", "ln2_b")):
        w[k] = st(lambda l: np.tile(inp[src][l][None, :], (128, 1)))
    w["brouter"] = st(lambda l: np.tile(inp["b_router"][l][None, :], (128, 1)))
    w["bgu_t"] = st(lambda l: inp["b_gate_up"][l].reshape(NEXP, 16, 128).transpose(2, 0, 1))
    w["bdn"] = st(lambda l: inp["b_down"][l])
    bi, bb = zip(*[na_bias_tables(inp["na_rpb"][l], hf) for l in ls])
    w["na_bint"] = np.ascontiguousarray(np.stack(bi, 0))
    w["na_bbnd"] = np.ascontiguousarray(np.stack(bb, 0))
    w["na_bbnd_o"] = np.ascontiguousarray(np.stack([na_bias_tables(inp["na_rpb"][l], 1 - hf)[1] for l in ls], 0))
    return {k: np.ascontiguousarray(v.astype(np.float32)) for k, v in w.items()}


def prep_consts(hf):
    rt = rope_tables()
    own, oth = rt[hf * HALF:(hf + 1) * HALF], rt[(1 - hf) * HALF:(2 - hf) * HALF]
    hm = np.zeros((128, 4), np.float32)
    hm[:, 0], hm[:, 1], hm[:, 2], hm[:, 3] = hf, 1 - hf, 1 - hf, hf
    return {
        "rope_loc": np.ascontiguousarray(np.concatenate([own, oth], 0)),
        "hmask": hm,
        "identb": np.eye(128, dtype=np.float32).astype(ml_dtypes.bfloat16),
        "identf": np.eye(128, dtype=np.float32),
        "triu": np.triu(np.ones((128, 128), np.float32), 1),
        "ones128": np.ones((128, 128), np.float32),
        "iota_e": np.tile(np.arange(NEXP, dtype=np.float32)[None, :], (128, 1)),
        "iota_cap": np.tile((np.arange(NEXP, dtype=np.float32) * CAP)[None, :], (128, 1)),
    }


def prep_acts(xb, hf):
    own, oth = xb[hf * HALF:(hf + 1) * HALF], xb[(1 - hf) * HALF:(2 - hf) * HALF]
    return {"x_loc": np.ascontiguousarray(np.concatenate([own, oth], 0))}


def _normalize(c, psO, okey, ncol, rsb, rkey, psB, ou, oukey, onesf, dst_ap, dst_key, nh=1):
    p = c.p
    p.op("dve", lambda e: e.reciprocal(out=rsb[64:65, 0:ncol], in_=psO[64:65, 0:ncol]), [okey], [rkey])
    p.op("pe", lambda e: e.matmul(psB[0:64, 0:ncol], onesf[64:65, 0:64], rsb[64:65, 0:ncol],
                                  start=True, stop=True), [rkey, "onesf"], ["psB"])
    p.op("act", lambda e: e.activation(out=ou[0:64, 0:ncol], in_=psO[0:64, 0:ncol], func=AF.Copy),
         [okey], [oukey])
    a0 = ou[0:64, 0:ncol]
    a1 = psB[0:64, 0:ncol]
    if nh > 1:
        a0 = a0.rearrange("p (h q) -> p h q", h=nh)
        a1 = a1.rearrange("p (h q) -> p h q", h=nh)
    p.op("dve", lambda e: e.tensor_tensor(out=dst_ap, in0=a0, in1=a1, op=ALU.mult),
         [oukey, "psB"], [dst_key])


def phase_dense_attn(c, L, kind):
    nc, p = c.nc, c.p
    if kind == "gqa":
        nH, dh, nK, nV, scale, obase = 6, 64, 2, 2, 64 ** -0.5, 6
        qT, kT, vS = c.s["qT_gqa"], c.s["kT_gqa"], c.s["v_gqa"]
        kmap = [0, 0, 0, 1, 1, 1]
    else:
        nH, dh, nK, nV, scale, obase = 4, 96, 4, 4, 96 ** -0.5, 12
        qT, kT, vS = c.s["qT_mla"], c.s["kT_mla"], c.s["v_mla"]
        kmap = [0, 1, 2, 3]
    with ExitStack() as ph:
        c.ph = ph
        KT = _sb(c, "KT", [128, nK, S], BF16)
        V = _sb(c, "V", [128, 64, nV * 65], BF16)
        Q = [_sb(c, "Q%d" % i, [128, nH, 512], BF16) for i in range(2)]
        pT = [_sb(c, "pT%d" % i, [128, 1024], BF16) for i in range(3)]
        rsb = _sb(c, "rsb", [128, 512], F32)
        ou = _sb(c, "ou", [128, 512], F32)
        ot = [_sb(c, "ot%d" % i, [128, 512], BF16) for i in range(2)]
        onesf = _sb(c, "onesf", [128, 64], F32)
        psS = [_ps(c, "psS%d" % i, [128, 1024], F32) for i in range(2)]
        psO = [_ps(c, "psO%d" % i, [128, 512], F32) for i in range(2)]
        psB = _ps(c, "psB", [128, 512], F32)
        p.op("pool", lambda e: e.memset(onesf[:], 1.0), [], ["onesf"])
        pack = (kind == "gqa")
        if pack:
            kT2 = kT.rearrange("g d t -> (g d) t")
            for half in range(2):
                dma(p, "sp", KT[:, 0, half * HALF:(half + 1) * HALF], kT2[:, half * HALF:(half + 1) * HALF],
                    [], ["KT"])
            for i in range(2):
                p.op("pool", lambda e, i=i: e.memset(Q[i][:], 0.0), [], [("Q", i)])
        else:
            for k in range(nK):
                for half in range(2):
                    dma(p, "sp", KT[0:dh, k, half * HALF:(half + 1) * HALF],
                        kT[k, :, half * HALF:(half + 1) * HALF], [], ["KT"])
        vv = vS.rearrange("(t p) c -> p t c", p=128)
        for q4 in range(16):
            dma(p, "sp", V[:, q4 * 4:(q4 + 1) * 4, :], vv[:, q4 * 4:(q4 + 1) * 4, :], [], ["V"])
        it = 0
        for qb in range(8):
            qs = qb % 2
            if pack:
                dma(p, "sp", Q[qs][0:64, 0:3, :], qT[0:3, :, qb * 512:(qb + 1) * 512].rearrange("h d t -> d h t"),
                    [], [("Q", qs)])
                dma(p, "sp", Q[qs][64:128, 3:6, :], qT[3:6, :, qb * 512:(qb + 1) * 512].rearrange("h d t -> d h t"),
                    [], [("Q", qs)])
            else:
                dma(p, "sp", Q[qs][0:dh, :, :], qT[:, :, qb * 512:(qb + 1) * 512].rearrange("h d t -> d h t"),
                    [], [("Q", qs)])
            for h in range(nH):
                ob_ = it % 2
                it += 1
                okey = "psO%d" % ob_
                ki = kmap[h]

                def qk2(k2, h=h, ki=ki, qs=qs):
                    b = k2 % 2

                    def fn(e):
                        ins = None
                        for u in range(2):
                            kt = 2 * k2 + u
                            if pack:
                                ins = e.matmul(psS[b][:, u * 512:(u + 1) * 512], KT[:, 0, kt * 128:(kt + 1) * 128],
                                               Q[qs][:, h, :], start=True, stop=True)
                            else:
                                ins = e.matmul(psS[b][:, u * 512:(u + 1) * 512],
                                               KT[0:dh, ki, kt * 128:(kt + 1) * 128], Q[qs][0:dh, h, :],
                                               start=True, stop=True)
                        return ins
                    p.op("pe", fn, ["KT", ("Q", qs)], ["psS%d" % b])
                qk2(0)
                for k2 in range(32):
                    b = k2 % 2
                    pb = k2 % 3
                    if k2 + 1 < 32:
                        qk2(k2 + 1)
                    p.op("act", lambda e, b=b, pb=pb: e.activation(out=pT[pb][:], in_=psS[b][:, :],
                                                                   func=AF.Exp, scale=scale),
                         ["psS%d" % b], [("pT", pb)])

                    def fpv(e, k2=k2, pb=pb, ob_=ob_, ki=ki):
                        ins = None
                        for u in range(2):
                            kt = 2 * k2 + u
                            ins = e.matmul(psO[ob_][0:65, :], V[:, kt, ki * 65:(ki + 1) * 65],
                                           pT[pb][:, u * 512:(u + 1) * 512],
                                           start=(kt == 0), stop=(kt == 63))
                        return ins
                    p.op("pe", fpv, ["V", ("pT", pb)], [okey])
                os_ = it % 2
                _normalize(c, psO[ob_], okey, 512, rsb, "rsb", psB, ou, "ou", onesf,
                           ot[os_][0:64, :], ("ot", os_))
                dma(p, "sp", c.s["oT"][obase + h, :, qb * 512:(qb + 1) * 512], ot[os_][0:64, :],
                    [("ot", os_)], [("d_oT", obase + h, qb)])
        p.barrier()
        p.emit()


def phase_na(c, L):
    nc, p = c.nc, c.p
    with ExitStack() as ph:
        c.ph = ph
        Qb = [_sb(c, "Qb%d" % i, [128, 6, 512], BF16) for i in range(2)]
        Kb = [_sb(c, "Kb%d" % i, [128, 6, 1024], BF16) for i in range(2)]
        Vb = [_sb(c, "Vb%d" % i, [128, 16, 390], BF16) for i in range(2)]
        bint = _sb(c, "bint", [128, 6, 8, 64], F32)
        bbnd = _sb(c, "bbnd", [128, 6, 12, 64], F32)
        sc = [_sb(c, "sc%d" % i, [128, 768], F32) for i in range(2)]
        pp = [_sb(c, "pp%d" % i, [128, 768], BF16) for i in range(2)]
        rsb = _sb(c, "rsb", [128, 512], F32)
        ou = _sb(c, "ou", [128, 512], F32)
        ot = [_sb(c, "ot%d" % i, [128, 6, 512], BF16) for i in range(2)]
        onesf = _sb(c, "onesf", [128, 64], F32)
        psS = [_ps(c, "psS%d" % i, [128, 1024], F32) for i in range(2)]
        psO = [_ps(c, "psO%d" % i, [128, 512], F32) for i in range(2)]
        psB = _ps(c, "psB", [128, 512], F32)
        p.op("pool", lambda e: e.memset(onesf[:], 1.0), [], ["onesf"])
        dma(p, "sp", bint[0:64], c.w["na_bint"][L], [], ["bint"])
        vrow = c.s["v_na"].rearrange("(r k) c -> k r c", k=64)
        it = 0
        for b8 in range(8):
            s = b8 % 2
            dma(p, "sp", Qb[s][0:64, :, :], c.s["qT_na"][:, :, b8 * 512:(b8 + 1) * 512].rearrange("h d t -> d h t"),
                [], [("Qb", s)])
            dma(p, "sp", Kb[s][0:64, :, :], c.s["kT_na"][:, :, b8 * 512:b8 * 512 + 1024].rearrange("h d t -> d h t"),
                [], [("Kb", s)])
            for hv in range(2):
                dma(p, "sp", Vb[s][0:64, hv * 8:(hv + 1) * 8, :], vrow[:, b8 * 8 + hv * 8:b8 * 8 + (hv + 1) * 8, :],
                    [], [("Vb", s)])
            for jj in range(8):
                j = b8 * 8 + jj
                if j < 4:
                    rows = list(range(0, 12)); bidx = j
                elif j > 60:
                    rows = list(range(60, 72)); bidx = 4 + (j - 61)
                else:
                    rows = list(range(j, j + 8)); bidx = None
                nr = len(rows)
                if bidx is not None:
                    dma(p, "sp", bbnd[0:64], c.bbnd[bidx], [], ["bbnd"])
                    btile, bkey = bbnd, "bbnd"
                else:
                    btile, bkey = bint, "bint"
                ob_ = j % 2
                okey = "psO%d" % ob_
                for h in range(6):
                    sb_ = it % 2
                    it += 1
                    skey = "psS%d" % sb_

                    def fqk(e, h=h, sb_=sb_, rows=rows, jj=jj, s=s, b8=b8):
                        ins = None
                        for i, lr in enumerate(rows):
                            ins = e.matmul(psS[sb_][0:64, i * 64:(i + 1) * 64],
                                           Kb[s][0:64, h, (lr - b8 * 8) * 64:(lr - b8 * 8 + 1) * 64],
                                           Qb[s][0:64, h, jj * 64:(jj + 1) * 64], start=True, stop=True)
                        return ins
                    p.op("pe", fqk, [("Qb", s), ("Kb", s)], [skey])
                    p.op("dve", lambda e, h=h, sb_=sb_, nr=nr, btile=btile: e.scalar_tensor_tensor(
                        out=sc[sb_][0:64, 0:nr * 64], in0=psS[sb_][0:64, 0:nr * 64], scalar=0.125,
                        in1=btile[0:64, h, 0:nr, :].rearrange("p r q -> p (r q)"),
                        op0=ALU.mult, op1=ALU.add), [skey, bkey], [("sc", sb_)])
                    p.op("act", lambda e, sb_=sb_, nr=nr: e.activation(
                        out=pp[sb_][0:64, 0:nr * 64], in_=sc[sb_][0:64, 0:nr * 64], func=AF.Exp),
                        [("sc", sb_)], [("pp", sb_)])

                    def fpv(e, h=h, sb_=sb_, rows=rows, ob_=ob_, s=s, b8=b8):
                        ins = None
                        n = len(rows)
                        for i, lr in enumerate(rows):
                            ins = e.matmul(psO[ob_][0:65, h * 64:(h + 1) * 64],
                                           Vb[s][0:64, lr - b8 * 8, h * 65:(h + 1) * 65],
                                           pp[sb_][0:64, i * 64:(i + 1) * 64],
                                           start=(i == 0), stop=(i == n - 1))
                        return ins
                    p.op("pe", fpv, [("Vb", s), ("pp", sb_)], [okey])
                _normalize(c, psO[ob_], okey, 384, rsb, "rsb", psB, ou, "ou", onesf,
                           ot[s][0:64, :, jj * 64:(jj + 1) * 64],
                           ("ot", s), nh=6)
            dma(p, "sp", c.s["oT"][0:6, :, b8 * 512:(b8 + 1) * 512].rearrange("h d t -> d h t"),
                ot[s][0:64, :, :], [("ot", s)], [("d_oT_na", b8)])
        p.barrier()
        p.emit()


def layer_norm_tile(c, tsb, tkey, junk, jkey, sm, smkey, g_t, b_t, gbkey, out_t, outkey):
    p = c.p
    p.op("act", lambda e: e.activation(out=junk[:], in_=tsb[:], func=AF.Copy, accum_out=sm[:, 0:1]),
         [tkey], [jkey, smkey])
    p.op("act", lambda e: e.activation(out=junk[:], in_=tsb[:], func=AF.Square, accum_out=sm[:, 1:2]),
         [tkey], [jkey, smkey])
    p.op("dve", lambda e: e.tensor_scalar(out=sm[:, 2:3], in0=sm[:, 0:1], scalar1=1.0 / D, scalar2=None,
                                          op0=ALU.mult), [smkey], [smkey])
    p.op("dve", lambda e: e.tensor_tensor(out=sm[:, 3:4], in0=sm[:, 2:3], in1=sm[:, 2:3], op=ALU.mult),
         [smkey], [smkey])
    p.op("dve", lambda e: e.scalar_tensor_tensor(out=sm[:, 4:5], in0=sm[:, 1:2], scalar=1.0 / D,
                                                 in1=sm[:, 3:4], op0=ALU.mult, op1=ALU.subtract),
         [smkey], [smkey])
    p.op("act", lambda e: e.activation(out=sm[:, 5:6], in_=sm[:, 4:5], func=AF.Sqrt,
                                       bias=c.eps_ln[:, 0:1]), [smkey], [smkey])
    p.op("dve", lambda e: e.reciprocal(out=sm[:, 6:7], in_=sm[:, 5:6]), [smkey], [smkey])
    p.op("dve", lambda e: e.tensor_scalar(out=junk[:], in0=tsb[:], scalar1=sm[:, 2:3], scalar2=sm[:, 6:7],
                                          op0=ALU.subtract, op1=ALU.mult), [tkey, smkey], [jkey])
    p.op("pool", lambda e: e.tensor_tensor(out=junk[:], in0=junk[:], in1=g_t[:], op=ALU.mult),
         [jkey, gbkey], [jkey])
    p.op("pool", lambda e: e.tensor_tensor(out=out_t[:], in0=junk[:], in1=b_t[:], op=ALU.add),
         [jkey, gbkey], [outkey])


def phase_merge(c, L):
    nc, p = c.nc, c.p
    with ExitStack() as ph:
        c.ph = ph
        wb = _sb(c, "wb", [128, 16, D], BF16)
        woutb = _sb(c, "woutb", [128, 8, D], BF16)
        lng = _sb(c, "lng", [128, D], F32)
        lnb = _sb(c, "lnb", [128, D], F32)
        wr = _sb(c, "wr", [128, 8, NEXP], F32)
        br = _sb(c, "br", [128, NEXP], F32)
        identb = _sb(c, "identb", [128, 128], BF16)
        identf = _sb(c, "identf", [128, 128], F32)
        oTb = [_sb(c, "oTb%d" % i, [128, 16, 512], BF16) for i in range(2)]
        gTc = [_sb(c, "gTc%d" % i, [128, 3, 512], BF16) for i in range(2)]
        mixT = [_sb(c, "mixT%d" % i, [128, 8, 512], BF16) for i in range(2)]
        mt = [_sb(c, "mt%d" % i, [128, 512], F32) for i in range(6)]
        xo = [_sb(c, "xo%d" % i, [128, D], F32) for i in range(2)]
        tsb = [_sb(c, "tsb%d" % i, [128, D], F32) for i in range(2)]
        junk = _sb(c, "junk", [128, D], F32)
        x1t = [_sb(c, "x1t%d" % i, [128, D], F32) for i in range(2)]
        x1b = [_sb(c, "x1b%d" % i, [128, D], BF16) for i in range(2)]
        x1Tb = [_sb(c, "x1Tb%d" % i, [128, 8, 128], BF16) for i in range(2)]
        x1Tf = [_sb(c, "x1Tf%d" % i, [128, 8, 128], F32) for i in range(2)]
        sm = [_sb(c, "sm%d" % i, [128, 8], F32) for i in range(2)]
        rt_ = [_sb(c, "rt%d" % i, [128, 160], F32) for i in range(2)]
        gTt = [_sb(c, "gTt%d" % i, [128, 128], F32) for i in range(2)]
        psY = [_ps(c, "psY%d" % i, [128, 512], F32) for i in range(3)]
        psOut = [_ps(c, "psOut%d" % i, [128, 512], F32) for i in range(2)]
        psT = _ps(c, "psT", [128, 8, 128], BF16)
        psTf = _ps(c, "psTf", [128, 8, 128], F32)
        triu = _sb(c, "triu", [128, 128], F32)
        ones128 = _sb(c, "ones128", [128, 128], F32)
        iota_e = _sb(c, "iota_e", [128, NEXP], F32)
        iota_cap = _sb(c, "iota_cap", [128, NEXP], F32)
        msum = _sb(c, "msum", [128, NEXP], F32)
        rx = [_sb(c, "rx%d" % i, [128, 160], F32) for i in range(2)]
        idxu = [_sb(c, "idxu%d" % i, [128, 8], U32) for i in range(2)]
        slotu = [_sb(c, "slotu%d" % i, [128, 4], I32) for i in range(2)]
        dma(p, "sp", triu[:], c.w["triu"][:, :], [], ["triu"])
        dma(p, "sp", ones128[:], c.w["ones128"][:, :], [], ["ones128"])
        dma(p, "sp", iota_e[:], c.w["iota_e"][:, :], [], ["iota_e"])
        dma(p, "sp", iota_cap[:], c.w["iota_cap"][:, :], [], ["iota_cap"])
        p.op("pool", lambda e: e.memset(msum[:], 0.0), [], ["msum"])

        for h in range(16):
            src = (c.w["w_ba"][L][h * 64:(h + 1) * 64, :] if h < 6 else
                   c.w["w_bb"][L][(h - 6) * 64:(h - 5) * 64, :] if h < 12 else
                   c.w["w_bc"][L][(h - 12) * 64:(h - 11) * 64, :])
            dma(p, "pool", wb[0:64, h, :], src, [], ["wb"])
        wo_v = c.w["w_out"][L].rearrange("(c p) n -> p c n", p=128)
        for ch in range(8):
            dma(p, "pool", woutb[:, ch, :], wo_v[:, ch, :], [], ["woutb"])
        dma(p, "sp", lng[:], c.w["ln1g"][L], [], ["lngb"])
        dma(p, "sp", lnb[:], c.w["ln1b"][L], [], ["lngb"])
        dma(p, "sp", wr[:], c.w["w_router"][L].rearrange("(c p) e -> p c e", p=128), [], ["wr"])
        dma(p, "sp", br[:], c.w["brouter"][L], [], ["br"])
        dma(p, "sp", identb[:], c.w["identb"][:, :], [], ["identb"])
        dma(p, "sp", identf[:], c.w["identf"][:, :], [], ["identf"])
        gT_v = c.s["gT"].rearrange("(i c p) t -> p i c t", i=3, c=8, p=128)
        x1T_v = c.s["x1T"].rearrange("(c p) t -> p c t", p=128)
        gcnt = 0
        for blk in range(8):
            s = blk % 2
            for hv in range(2):
                dma(p, "sp", oTb[s][0:64, hv * 8:(hv + 1) * 8, :],
                    c.s["oT"][hv * 8:(hv + 1) * 8, :, blk * 512:(blk + 1) * 512].rearrange("h d t -> d h t"),
                    [], [("oTb", s)])
            for dc in range(8):
                gs = gcnt % 2
                gcnt += 1
                dma(p, "sp", gTc[gs][:], gT_v[:, :, dc, blk * 512:(blk + 1) * 512], [], [("gTc", gs)])
                for i, (h0, nh) in enumerate(((0, 6), (6, 6), (12, 4))):
                    mm_group(p, psY[i][:, :],
                             [(wb[0:64, h0 + k, dc * 128:(dc + 1) * 128], oTb[s][0:64, h0 + k, :])
                              for k in range(nh)], ["wb", ("oTb", s)], ["psY%d" % i])
                m3 = [(gcnt * 3 + i) % 6 for i in range(3)]
                for i in range(3):
                    p.op("dve", lambda e, i=i, gs=gs, m=m3[i]: e.tensor_tensor(
                        out=mt[m][:], in0=psY[i][:, :], in1=gTc[gs][:, i, :], op=ALU.mult),
                        ["psY%d" % i, ("gTc", gs)], [("mt", m3[i])])
                p.op("pool", lambda e, m3=m3: e.tensor_tensor(out=mt[m3[0]][:], in0=mt[m3[0]][:],
                                                              in1=mt[m3[1]][:], op=ALU.add),
                     [("mt", m3[0]), ("mt", m3[1])], [("mt", m3[0])])
                p.op("pool", lambda e, m3=m3, s=s, dc=dc: e.tensor_tensor(
                    out=mixT[s][:, dc, :], in0=mt[m3[0]][:], in1=mt[m3[2]][:], op=ALU.add),
                    [("mt", m3[0]), ("mt", m3[2])], [("mixT", s)])
            for tt in range(4):
                t = blk * 4 + tt
                ts_ = t % 2
                dma(p, "sp", xo[ts_][:], c.q_src[t * 128:(t + 1) * 128, :], [], [("xo", ts_)])
                for half in range(2):
                    mm_group(p, psOut[half][:, :],
                             [(mixT[s][:, dc, tt * 128:(tt + 1) * 128], woutb[:, dc, half * 512:(half + 1) * 512])
                              for dc in range(8)], [("mixT", s), "woutb"], ["psOut%d" % half])
                    p.op("dve", lambda e, half=half, ts_=ts_: e.scalar_tensor_tensor(
                        out=tsb[ts_][:, half * 512:(half + 1) * 512], in0=xo[ts_][:, half * 512:(half + 1) * 512],
                        scalar=DN_ALPHA, in1=psOut[half][:, :], op0=ALU.mult, op1=ALU.add),
                        [("xo", ts_), "psOut%d" % half], [("tsb", ts_)])
                layer_norm_tile(c, tsb[ts_], ("tsb", ts_), junk, "junk", sm[ts_], ("sm", ts_),
                                lng, lnb, "lngb", x1t[ts_], ("x1t", ts_))
                dma(p, "sp", c.s["x1"][t * 128:(t + 1) * 128, :], x1t[ts_][:], [("x1t", ts_)], [("d_x1", t)])
                p.op("act", lambda e, ts_=ts_: e.activation(out=x1b[ts_][:], in_=x1t[ts_][:], func=AF.Copy),
                     [("x1t", ts_)], [("x1b", ts_)])
                tr_group(p, [(psT[:, ch, :], x1b[ts_][:, ch * 128:(ch + 1) * 128]) for ch in range(8)],
                         identb[:], [("x1b", ts_), "identb"], ["psT"])
                p.op("dve", lambda e, ts_=ts_: e.tensor_copy(x1Tb[ts_][:], psT[:]), ["psT"], [("x1Tb", ts_)])
                for hv in range(2):
                    dma(p, "sp", x1T_v[:, hv * 4:(hv + 1) * 4, t * 128:(t + 1) * 128], x1Tb[ts_][:, hv * 4:(hv + 1) * 4, :],
                        [("x1Tb", ts_)], [("d_x1T", t, hv)])
                tr_group(p, [(psTf[:, ch, :], x1t[ts_][:, ch * 128:(ch + 1) * 128]) for ch in range(8)],
                         identf[:], [("x1t", ts_), "identf"], ["psTf"])
                p.op("act", lambda e, ts_=ts_: e.activation(out=x1Tf[ts_][:], in_=psTf[:], func=AF.Copy),
                     ["psTf"], [("x1Tf", ts_)])
                mm_group(p, psY[0][:, 0:NEXP], [(x1Tf[ts_][:, ch, :], wr[:, ch, :]) for ch in range(8)],
                         [("x1Tf", ts_), "wr"], ["psY0"])
                R = rt_[ts_]
                rk = ("rt", ts_)
                lg, mx, msk, ex, exm = R[:, 0:32], R[:, 32:40], R[:, 40:72], R[:, 72:104], R[:, 104:136]
                nm, ssum, rs = R[:, 136:137], R[:, 137:138], R[:, 138:139]
                p.op("dve", lambda e, lg=lg: e.tensor_tensor(out=lg, in0=psY[0][:, 0:NEXP], in1=br[:], op=ALU.add),
                     ["psY0", "br"], [rk])
                p.op("dve", lambda e, lg=lg, mx=mx: e.max(out=mx, in_=lg), [rk], [rk])
                p.op("dve", lambda e, lg=lg, mx=mx, msk=msk: e.tensor_scalar(
                    out=msk, in0=lg, scalar1=mx[:, 3:4], scalar2=None, op0=ALU.is_ge), [rk], [rk])
                p.op("dve", lambda e, mx=mx, nm=nm: e.tensor_scalar(
                    out=nm, in0=mx[:, 0:1], scalar1=-1.0, scalar2=None, op0=ALU.mult), [rk], [rk])
                p.op("act", lambda e, lg=lg, ex=ex, nm=nm: e.activation(out=ex, in_=lg, func=AF.Exp, bias=nm),
                     [rk], [rk])
                p.op("dve", lambda e, ex=ex, msk=msk, exm=exm: e.tensor_tensor(out=exm, in0=ex, in1=msk,
                                                                               op=ALU.mult), [rk], [rk])
                p.op("dve", lambda e, exm=exm, ssum=ssum: e.reduce_sum(out=ssum, in_=exm, axis=AX.X), [rk], [rk])
                p.op("dve", lambda e, ssum=ssum, rs=rs: e.reciprocal(out=rs, in_=ssum), [rk], [rk])
                p.op("dve", lambda e, exm=exm, rs=rs: e.tensor_scalar(
                    out=exm, in0=exm, scalar1=rs, scalar2=None, op0=ALU.mult), [rk], [rk])
                p.op("pe", lambda e, exm=exm: e.transpose(psY[1][0:32, 0:128], exm, identf[:]),
                     [rk, "identf"], ["psY1"])
                p.op("act", lambda e, ts_=ts_: e.activation(out=gTt[ts_][0:32, :], in_=psY[1][0:32, 0:128],
                                                            func=AF.Copy), ["psY1"], [("gTt", ts_)])
                dma(p, "sp", c.s["gateT"][:, t * 128:(t + 1) * 128], gTt[ts_][0:32, :],
                    [("gTt", ts_)], [("d_gateT", t)])
                X = rx[ts_]
                xk = ("rx", ts_)
                idxf, sv, ov, eq, tmp = X[:, 0:8], X[:, 8:40], X[:, 40:72], X[:, 72:104], X[:, 104:136]
                slotf, gkf = X[:, 136:140], X[:, 140:144]
                p.op("dve", lambda e, ts_=ts_, lg=lg, mx=mx: e.max_index(out=idxu[ts_][:], in_max=mx, in_values=lg),
                     [rk], [("idxu", ts_)])
                p.op("dve", lambda e, ts_=ts_, idxf=idxf: e.tensor_copy(idxf, idxu[ts_][:]),
                     [("idxu", ts_)], [xk])
                p.op("pe", lambda e, msk=msk: e.matmul(psY[2][:, 0:NEXP], triu[:], msk, start=True, stop=False),
                     [rk, "triu"], ["psY2"])
                p.op("pe", lambda e: e.matmul(psY[2][:, 0:NEXP], ones128[:], msum[:], start=False, stop=True),
                     ["msum", "ones128"], ["psY2"])
                p.op("dve", lambda e, ov=ov: e.tensor_scalar(out=ov, in0=psY[2][:, 0:NEXP], scalar1=float(CAP),
                                                             scalar2=1.0e6, op0=ALU.is_ge, op1=ALU.mult),
                     ["psY2"], [xk])
                p.op("dve", lambda e, sv=sv: e.tensor_tensor(out=sv, in0=psY[2][:, 0:NEXP], in1=iota_cap[:],
                                                             op=ALU.add), ["psY2", "iota_cap"], [xk])
                p.op("dve", lambda e, sv=sv, ov=ov: e.tensor_tensor(out=sv, in0=sv, in1=ov, op=ALU.add), [xk], [xk])
                p.op("pool", lambda e, msk=msk: e.tensor_tensor(out=msum[:], in0=msum[:], in1=msk, op=ALU.add),
                     [rk, "msum"], ["msum"])
                for k4 in range(4):
                    p.op("dve", lambda e, eq=eq, idxf=idxf, k4=k4: e.tensor_scalar(
                        out=eq, in0=iota_e[:], scalar1=idxf[:, k4:k4 + 1], scalar2=None, op0=ALU.is_equal),
                        [xk, "iota_e"], [xk])
                    p.op("dve", lambda e, eq=eq, sv=sv, tmp=tmp: e.tensor_tensor(out=tmp, in0=eq, in1=sv, op=ALU.mult),
                         [xk], [xk])
                    p.op("dve", lambda e, tmp=tmp, slotf=slotf, k4=k4: e.reduce_sum(
                        out=slotf[:, k4:k4 + 1], in_=tmp, axis=AX.X), [xk], [xk])
                    p.op("dve", lambda e, eq=eq, exm=exm, tmp=tmp: e.tensor_tensor(out=tmp, in0=eq, in1=exm,
                                                                                   op=ALU.mult), [xk, rk], [xk])
                    p.op("dve", lambda e, tmp=tmp, gkf=gkf, k4=k4: e.reduce_sum(
                        out=gkf[:, k4:k4 + 1], in_=tmp, axis=AX.X), [xk], [xk])
                p.op("dve", lambda e, ts_=ts_, slotf=slotf: e.tensor_copy(slotu[ts_][:], slotf),
                     [xk], [("slotu", ts_)])
                for k4 in range(4):
                    p.op("pool", lambda e, ts_=ts_, k4=k4: e.indirect_dma_start(
                        out=c.s["xg"][:, :], out_offset=bass.IndirectOffsetOnAxis(ap=slotu[ts_][:, k4:k4 + 1], axis=0),
                        in_=x1b[ts_][:], in_offset=None, bounds_check=c.breg, oob_is_err=False),
                        [("slotu", ts_), ("x1b", ts_)], [("d_xg", t, k4)], dma=True)
                dma(p, "sp", c.s["slots"][t * 128:(t + 1) * 128, :], slotu[ts_][:], [("slotu", ts_)], [("d_slots", t)])
                dma(p, "sp", c.s["gks"][t * 128:(t + 1) * 128, :], gkf, [xk], [("d_gks", t)])
        p.barrier()
        p.emit()


SIG_MAX = float(1.0 / (1.0 + np.exp(-1.702 * 7.0)))


def phase_moe(c, L):
    nc, p = c.nc, c.p
    with ExitStack() as ph:
        c.ph = ph
        wgu = [_sb(c, "wgu%d" % i, [128, 8, 2 * DEXP], BF16) for i in range(2)]
        wdn = [_sb(c, "wdn%d" % i, [128, 8, D], BF16) for i in range(2)]
        xT = _sb(c, "xTsb", [128, 8, 1024], BF16)
        gT = _sb(c, "gTsb", [128, 1024], F32)
        yacc = _sb(c, "yacc", [128, 8, D], F32)
        bgu = _sb(c, "bgu", [128, NEXP, 16], F32)
        bgs = _sb(c, "bgs", [128, NEXP, 8], F32)
        bdn = _sb(c, "bdn", [128, D], F32)
        identf = _sb(c, "identf", [128, 128], F32)
        sg = [_sb(c, "sg%d" % i, [128, 512], F32) for i in range(2)]
        gc = [_sb(c, "gc%d" % i, [128, 512], F32) for i in range(2)]
        uc = [_sb(c, "uc%d" % i, [128, 512], F32) for i in range(2)]
        t1 = [_sb(c, "t1%d" % i, [128, 512], F32) for i in range(2)]
        t2 = [_sb(c, "t2%d" % i, [128, 512], F32) for i in range(2)]
        aT = [_sb(c, "aT%d" % i, [128, 8, 512], BF16) for i in range(2)]
        yev = [_sb(c, "yev%d" % i, [128, 512], F32) for i in range(2)]
        psg = [_ps(c, "psg%d" % i, [128, 512], F32) for i in range(2)]
        psu = [_ps(c, "psu%d" % i, [128, 512], F32) for i in range(2)]
        psGb = _ps(c, "psGb", [128, 512], F32)
        psy = [_ps(c, "psy%d" % i, [128, 512], F32) for i in range(2)]

        dma(p, "sp", bgu[:], c.w["bgu_t"][L], [], ["bgu"])
        dma(p, "sp", bdn[0:32, :], c.w["bdn"][L], [], ["bdn"])
        dma(p, "sp", identf[:], c.w["identf"][:, :], [], ["identf"])
        p.op("dve", lambda e: e.tensor_scalar(out=bgs[:], in0=bgu[:, :, 0:8], scalar1=1.702, scalar2=None,
                                              op0=ALU.mult), ["bgu"], ["bgs"])
        x1T_v = c.s["x1T"].rearrange("(c p) t -> p c t", p=128)
        ym_v = c.s["ymoe"].rearrange("(t p) d -> p t d", p=128)
        cnt = 0
        ycnt = 0
        for sb in range(4):
            dma(p, "sp", xT[:], x1T_v[:, :, sb * 1024:(sb + 1) * 1024], [], ["xTsb"])
            dma(p, "sp", gT[0:32, :], c.s["gateT"][:, sb * 1024:(sb + 1) * 1024], [], ["gTsb"])
            for ex in range(NEXP):
                ws = ex % 2
                gu_v = c.w["w_gu"][L, ex].rearrange("(c p) n -> p c n", p=128)
                dn_v = c.w["w_dn"][L, ex].rearrange("(c p) n -> p c n", p=128)
                for ch in range(8):
                    dma(p, "pool", wgu[ws][:, ch, :], gu_v[:, ch, :], [], [("wgu", ws)])
                for ch in range(8):
                    dma(p, "pool", wdn[ws][:, ch, :], dn_v[:, ch, :], [], [("wdn", ws)])
                for tb in range(2):
                    as_ = (ex * 2 + tb) % 2
                    p.op("pe", lambda e, ex=ex, tb=tb: e.matmul(
                        psGb[:, :], identf[0:32, ex:ex + 1].to_broadcast([32, 128]),
                        gT[0:32, tb * 512:(tb + 1) * 512], start=True, stop=True),
                        ["identf", "gTsb"], ["psGb"])
                    for j in range(8):
                        k = cnt % 2
                        cnt += 1
                        mm_group(p, psg[k][:, :],
                                 [(wgu[ws][:, ch, j * 128:(j + 1) * 128], xT[:, ch, tb * 512:(tb + 1) * 512])
                                  for ch in range(8)], [("wgu", ws), "xTsb"], ["psg%d" % k])
                        mm_group(p, psu[k][:, :],
                                 [(wgu[ws][:, ch, DEXP + j * 128:DEXP + (j + 1) * 128],
                                   xT[:, ch, tb * 512:(tb + 1) * 512]) for ch in range(8)],
                                 [("wgu", ws), "xTsb"], ["psu%d" % k])
                        p.op("dve", lambda e, k=k, ex=ex, j=j: e.tensor_scalar(
                            out=gc[k][:], in0=psg[k][:, :], scalar1=bgu[:, ex, j:j + 1], scalar2=7.0,
                            op0=ALU.add, op1=ALU.min), ["psg%d" % k, "bgu"], [("gc", k)])
                        p.op("act", lambda e, k=k: e.activation(
                            out=sg[k][:], in_=gc[k][:], func=AF.Sigmoid, scale=1.702),
                            [("gc", k)], [("sg", k)])
                        p.op("dve", lambda e, k=k, ex=ex, j=j: e.tensor_scalar(
                            out=uc[k][:], in0=psu[k][:, :], scalar1=bgu[:, ex, 8 + j:9 + j], scalar2=7.0,
                            op0=ALU.add, op1=ALU.min), ["psu%d" % k, "bgu"], [("uc", k)])
                        p.op("dve", lambda e, k=k: e.tensor_scalar(
                            out=uc[k][:], in0=uc[k][:], scalar1=-7.0, scalar2=1.0,
                            op0=ALU.max, op1=ALU.add), [("uc", k)], [("uc", k)])
                        p.op("pool", lambda e, k=k: e.tensor_tensor(
                            out=t1[k][:], in0=sg[k][:], in1=gc[k][:], op=ALU.mult),
                            [("sg", k), ("gc", k)], [("t1", k)])
                        p.op("dve", lambda e, k=k: e.tensor_tensor(out=t2[k][:], in0=uc[k][:], in1=psGb[:, :],
                                                                   op=ALU.mult), [("uc", k), "psGb"], [("t2", k)])
                        p.op("pool", lambda e, k=k, j=j, as_=as_: e.tensor_tensor(
                            out=aT[as_][:, j, :], in0=t1[k][:], in1=t2[k][:], op=ALU.mult),
                            [("t1", k), ("t2", k)], [("aT", as_)])
                    for tt in range(4):
                        tile = tb * 4 + tt
                        for half in range(2):
                            yk = ycnt % 2
                            ycnt += 1
                            pairs = [(aT[as_][:, j, tt * 128:(tt + 1) * 128],
                                      wdn[ws][:, j, half * 512:(half + 1) * 512]) for j in range(8)]
                            rds = [("aT", as_), ("wdn", ws)]
                            if ex == 0:
                                pairs.append((gT[0:32, tile * 128:(tile + 1) * 128],
                                              bdn[0:32, half * 512:(half + 1) * 512]))
                                rds += ["gTsb", "bdn"]
                            mm_group(p, psy[yk][:, :], pairs, rds, ["psy%d" % yk])
                            ya = yacc[:, tile, half * 512:(half + 1) * 512]
                            if ex == 0:
                                p.op("act", lambda e, ya=ya, yk=yk: e.activation(out=ya, in_=psy[yk][:, :],
                                                                                 func=AF.Copy),
                                     ["psy%d" % yk], [("yacc", tile, half)])
                            else:
                                p.op("act", lambda e, yk=yk: e.activation(out=yev[yk][:], in_=psy[yk][:, :],
                                                                          func=AF.Copy),
                                     ["psy%d" % yk], [("yev", yk)])
                                p.op("pool", lambda e, ya=ya, yk=yk: e.tensor_tensor(out=ya, in0=ya, in1=yev[yk][:],
                                                                                     op=ALU.add),
                                     [("yev", yk), ("yacc", tile, half)], [("yacc", tile, half)])
            dma(p, "sp", ym_v[:, sb * 8:(sb + 1) * 8, :], yacc[:],
                [("yacc", t_, h_) for t_ in range(8) for h_ in range(2)], [("d_ymoe", sb)])
        p.barrier()
        p.emit()


def load_w_cast(c, dst_ap, src_ap, stg, skey, dkey, eng="pool"):
    p = c.p
    dma(p, "sp", stg, src_ap, [], [skey])
    if eng == "act":
        p.op("act", lambda e: e.activation(out=dst_ap, in_=stg, func=AF.Copy), [skey], [dkey])
    else:
        p.op(eng, lambda e: e.tensor_copy(dst_ap, stg), [skey], [dkey])


def phase_moe_sparse(c, L):
    nc, p = c.nc, c.p
    NT = CAP // 128
    groups = [(0, 512), (512, CAP)] if CAP > 512 else [(0, CAP)]
    with ExitStack() as ph:
        c.ph = ph
        wgu = [_sb(c, "wgu%d" % i, [128, 8, 2 * DEXP], BF16) for i in range(2)]
        wdn = [_sb(c, "wdn%d" % i, [128, 8, D], BF16) for i in range(2)]
        stg = [_sb(c, "stg%d" % i, [128, 2 * DEXP], F32) for i in range(3)]
        xgt = [_sb(c, "xgt%d" % i, [128, D], BF16) for i in range(NT)]
        xgT = [_sb(c, "xgT%d" % i, [128, 8, CAP], BF16) for i in range(2)]
        bgu = _sb(c, "bgu", [128, NEXP, 16], F32)
        bdr1 = _sb(c, "bdr", [128, D], F32)
        bdr = [bdr1, bdr1]
        bdb = [_sb(c, "bdb%d" % i, [128, D], BF16) for i in range(2)]
        ones1 = _sb(c, "ones1", [128, 128], BF16)
        identb = _sb(c, "identb", [128, 128], BF16)
        sg = [_sb(c, "sg%d" % i, [128, 512], F32) for i in range(2)]
        gc = [_sb(c, "gc%d" % i, [128, 512], F32) for i in range(2)]
        uc = [_sb(c, "uc%d" % i, [128, 512], F32) for i in range(2)]
        t1 = [_sb(c, "t1%d" % i, [128, 512], F32) for i in range(2)]
        aT = _sb(c, "aT", [128, 8, CAP], BF16)
        yout = [_sb(c, "yout%d" % i, [128, D], F32) for i in range(2)]
        psT2 = [_ps(c, "psT%d" % i, [128, 8, 128], BF16) for i in range(2)]
        psg = [_ps(c, "psg%d" % i, [128, 512], F32) for i in range(2)]
        psu = [_ps(c, "psu%d" % i, [128, 512], F32) for i in range(2)]
        psy = [_ps(c, "psy%d" % i, [128, 512], F32) for i in range(2)]
        dma(p, "sp", bgu[:], c.w["bgu_t"][L], [], ["bgu"])
        dma(p, "sp", identb[:], c.w["identb"][:, :], [], ["identb"])
        p.op("pool", lambda e: e.memset(ones1[:], 1.0), [], ["ones1"])
        st8 = {"scnt": 0, "cnt": 0, "ycnt": 0, "xcnt": 0}
        cast_rr = ("dve", "act", "pool")

        def wload_steps(ex):
            ws = ex % 2
            gu_v = c.w["w_gu"][L, ex].rearrange("(c p) n -> p c n", p=128)
            dn_v = c.w["w_dn"][L, ex].rearrange("(c p) n -> p c n", p=128)
            steps = []
            for ch in range(12):
                si = st8["scnt"] % 3
                eng = cast_rr[st8["scnt"] % 3]
                st8["scnt"] += 1
                if ch < 8:
                    src, stv, dstv, dkey = gu_v[:, ch, :], stg[si][:], wgu[ws][:, ch, :], ("wgu", ws)
                else:
                    c2 = ch - 8
                    src = dn_v[:, 2 * c2:2 * c2 + 2, :]
                    stv = stg[si][:].rearrange("p (c n) -> p c n", c=2)
                    dstv, dkey = wdn[ws][:, 2 * c2:2 * c2 + 2, :], ("wdn", ws)

                def f_dma(src=src, stv=stv, si=si):
                    dma(p, "sp", stv, src, [], [("stg", si)])

                def f_cast(stv=stv, dstv=dstv, si=si, dkey=dkey, eng=eng):
                    if eng == "act":
                        p.op("act", lambda e: e.activation(out=dstv, in_=stv, func=AF.Copy), [("stg", si)], [dkey])
                    else:
                        p.op(eng, lambda e: e.tensor_copy(dstv, stv), [("stg", si)], [dkey])
                steps.append((f_dma, f_cast))
            steps.append((lambda ws=ws, ex=ex: dma(p, "sp", bdr[ws][0:1, :], c.w["bdn"][L, ex:ex + 1, :], [],
                                                   ["bdr"]),
                          lambda ws=ws: p.op("pool", lambda e: e.tensor_copy(bdb[ws][0:1, :], bdr[ws][0:1, :]),
                                             ["bdr"], [("bdb", ws)])))
            return steps

        def xg_dma(ex):
            for st in range(NT):
                r0 = ex * CAP + st * 128
                dma(p, "sp", xgt[st][:], c.s["xg"][r0:r0 + 128, :], [], [("xgt", st)])

        def xg_load(ex):
            xb_ = ex % 2
            for st in range(NT):
                pt = st8["xcnt"] % 2
                st8["xcnt"] += 1
                tr_group(p, [(psT2[pt][:, ch, :], xgt[st][:, ch * 128:(ch + 1) * 128]) for ch in range(8)],
                         identb[:], [("xgt", st), "identb"], ["psT%d" % pt])
                eng = "act" if st % 2 == 0 else "dve"
                if eng == "act":
                    p.op("act", lambda e, st=st, xb_=xb_, pt=pt: e.activation(
                        out=xgT[xb_][:, :, st * 128:(st + 1) * 128], in_=psT2[pt][:], func=AF.Copy),
                        ["psT%d" % pt], [("xgT", xb_)])
                else:
                    p.op("dve", lambda e, st=st, xb_=xb_, pt=pt: e.tensor_copy(
                        xgT[xb_][:, :, st * 128:(st + 1) * 128], psT2[pt][:]), ["psT%d" % pt], [("xgT", xb_)])

        pend_cast = None
        for f_dma, f_cast in wload_steps(0):
            f_dma()
            if pend_cast is not None:
                pend_cast()
            pend_cast = f_cast
        pend_cast()
        xg_dma(0)
        xg_load(0)
        for ex in range(NEXP):
            ws = ex % 2
            xb_ = ex % 2
            nxt_steps = wload_steps(ex + 1) if ex + 1 < NEXP else []
            pend_cast = None
            if ex + 1 < NEXP:
                xg_dma(ex + 1)
            for (n0, n1) in groups:
                w = n1 - n0
                for j in range(8):
                    k = st8["cnt"] % 2
                    st8["cnt"] += 1
                    if nxt_steps:
                        f_dma, f_cast = nxt_steps.pop(0)
                        f_dma()
                        if pend_cast is not None:
                            pend_cast()
                        pend_cast = f_cast
                    mm_group(p, psg[k][:, 0:w], [(wgu[ws][:, ch, j * 128:(j + 1) * 128], xgT[xb_][:, ch, n0:n1])
                                                 for ch in range(8)], [("wgu", ws), ("xgT", xb_)], ["psg%d" % k])
                    mm_group(p, psu[k][:, 0:w], [(wgu[ws][:, ch, DEXP + j * 128:DEXP + (j + 1) * 128],
                                                  xgT[xb_][:, ch, n0:n1]) for ch in range(8)],
                             [("wgu", ws), ("xgT", xb_)], ["psu%d" % k])
                    p.op("dve", lambda e, k=k, ex=ex, j=j, w=w: e.tensor_scalar(
                        out=gc[k][:, 0:w], in0=psg[k][:, 0:w], scalar1=bgu[:, ex, j:j + 1], scalar2=7.0,
                        op0=ALU.add, op1=ALU.min), ["psg%d" % k, "bgu"], [("gc", k)])
                    p.op("act", lambda e, k=k, w=w: e.activation(out=sg[k][:, 0:w], in_=gc[k][:, 0:w],
                                                                 func=AF.Sigmoid, scale=1.702),
                         [("gc", k)], [("sg", k)])
                    p.op("dve", lambda e, k=k, ex=ex, j=j, w=w: e.tensor_scalar(
                        out=uc[k][:, 0:w], in0=psu[k][:, 0:w], scalar1=bgu[:, ex, 8 + j:9 + j], scalar2=7.0,
                        op0=ALU.add, op1=ALU.min), ["psu%d" % k, "bgu"], [("uc", k)])
                    p.op("dve", lambda e, k=k, w=w: e.tensor_scalar(
                        out=uc[k][:, 0:w], in0=uc[k][:, 0:w], scalar1=-7.0, scalar2=1.0,
                        op0=ALU.max, op1=ALU.add), [("uc", k)], [("uc", k)])
                    p.op("pool", lambda e, k=k, w=w: e.tensor_tensor(out=t1[k][:, 0:w], in0=sg[k][:, 0:w],
                                                                     in1=gc[k][:, 0:w], op=ALU.mult),
                         [("sg", k), ("gc", k)], [("t1", k)])
                    p.op("pool", lambda e, k=k, j=j, n0=n0, n1=n1, w=w: e.tensor_tensor(
                        out=aT[:, j, n0:n1], in0=t1[k][:, 0:w], in1=uc[k][:, 0:w], op=ALU.mult),
                        [("t1", k), ("uc", k)], ["aT"])
            while nxt_steps:
                f_dma, f_cast = nxt_steps.pop(0)
                f_dma()
                if pend_cast is not None:
                    pend_cast()
                pend_cast = f_cast
            if pend_cast is not None:
                pend_cast()
            if ex + 1 < NEXP:
                xg_load(ex + 1)
            for st in range(NT):
                ys = st8["ycnt"] % 2
                st8["ycnt"] += 1
                for half in range(2):
                    yk = (st8["ycnt"] + half) % 2
                    pairs = [(aT[:, j, st * 128:(st + 1) * 128], wdn[ws][:, j, half * 512:(half + 1) * 512])
                             for j in range(8)]
                    pairs.append((ones1[0:1, 0:128], bdb[ws][0:1, half * 512:(half + 1) * 512]))
                    mm_group(p, psy[yk][:, :], pairs, ["aT", ("wdn", ws), ("bdb", ws), "ones1"], ["psy%d" % yk])
                    p.op("act", lambda e, ys=ys, yk=yk, half=half: e.activation(
                        out=yout[ys][:, half * 512:(half + 1) * 512], in_=psy[yk][:, :], func=AF.Copy),
                        ["psy%d" % yk], [("yout", ys)])
                r0 = ex * CAP + st * 128
                dma(p, "sp", c.s["yg"][r0:r0 + 128, :], yout[ys][:], [("yout", ys)], [("d_yg", ex, st)])
        p.barrier()
        p.emit()


def phase_ln2(c, L, dst, sparse=True):
    nc, p = c.nc, c.p
    with ExitStack() as ph:
        c.ph = ph
        lng = _sb(c, "lng2", [128, D], F32)
        lnb = _sb(c, "lnb2", [128, D], F32)
        xa = [_sb(c, "xa%d" % i, [128, D], F32) for i in range(2)]
        ya = [_sb(c, "ya%d" % i, [128, D], F32) for i in range(2)]
        yk_ = [[_sb(c, "yk%d_%d" % (i, k), [128, D], F32) for k in range(4)] for i in range(2)]
        sl = [_sb(c, "sl%d" % i, [128, 4], I32) for i in range(2)]
        gk = [_sb(c, "gk%d" % i, [128, 4], F32) for i in range(2)]
        tsb = [_sb(c, "tsb%d" % i, [128, D], F32) for i in range(2)]
        out = [_sb(c, "out%d" % i, [128, D], F32) for i in range(2)]
        junk = _sb(c, "junk2", [128, D], F32)
        sm = [_sb(c, "sm%d" % i, [128, 8], F32) for i in range(2)]
        dma(p, "sp", lng[:], c.w["ln2g"][L], [], ["lngb"])
        dma(p, "sp", lnb[:], c.w["ln2b"][L], [], ["lngb"])
        for t in range(32):
            s = t % 2
            dma(p, "sp", xa[s][:], c.s["x1"][t * 128:(t + 1) * 128, :], [], [("xa", s)])
            if sparse:
                dma(p, "sp", sl[s][:], c.s["slots"][t * 128:(t + 1) * 128, :], [], [("sl", s)])
                dma(p, "sp", gk[s][:], c.s["gks"][t * 128:(t + 1) * 128, :], [], [("gk", s)])
                for k in range(4):
                    p.op("pool", lambda e, s=s, k=k: e.indirect_dma_start(
                        out=yk_[s][k][:], out_offset=None, in_=c.s["yg"][:, :],
                        in_offset=bass.IndirectOffsetOnAxis(ap=sl[s][:, k:k + 1], axis=0),
                        bounds_check=c.breg, oob_is_err=False), [("sl", s)], [("yk", s, k)], dma=True)
                p.op("dve", lambda e, s=s: e.tensor_scalar(out=ya[s][:], in0=yk_[s][0][:], scalar1=gk[s][:, 0:1],
                                                           scalar2=None, op0=ALU.mult),
                     [("yk", s, 0), ("gk", s)], [("ya", s)])
                for k in range(1, 4):
                    p.op("dve", lambda e, s=s, k=k: e.scalar_tensor_tensor(
                        out=ya[s][:], in0=yk_[s][k][:], scalar=gk[s][:, k:k + 1], in1=ya[s][:],
                        op0=ALU.mult, op1=ALU.add), [("yk", s, k), ("gk", s), ("ya", s)], [("ya", s)])
            else:
                dma(p, "sp", ya[s][:], c.s["ymoe"][t * 128:(t + 1) * 128, :], [], [("ya", s)])
            p.op("dve", lambda e, s=s: e.scalar_tensor_tensor(
                out=tsb[s][:], in0=xa[s][:], scalar=DN_ALPHA, in1=ya[s][:], op0=ALU.mult, op1=ALU.add),
                [("xa", s), ("ya", s)], [("tsb", s)])
            layer_norm_tile(c, tsb[s], ("tsb", s), junk, "junk", sm[s], ("sm", s), lng, lnb, "lngb",
                            out[s], ("out", s))
            dma(p, "sp", dst[t * 128:(t + 1) * 128, :], out[s][:], [("out", s)], [("d_out", t)])
        p.barrier()
        p.emit()


ALL_PHASES = ("p1", "na", "gqa", "mla", "merge", "moes", "ln2")
_NC_CACHE = {}


def kernel(**inputs):
    inp = {k: np.asarray(v) for k, v in inputs.items()}
    x = np.ascontiguousarray(inp["x"].astype(np.float32))
    nb = x.shape[0]
    if "nc" not in _NC_CACHE:
        _NC_CACHE["nc"] = build()
    consts = [prep_consts(hf) for hf in range(2)]
    wts = [prep_weights(inp, [0, 1], hf) for hf in range(2)]
    in_maps = []
    for cid in range(2 * nb):
        b, hf = cid // 2, cid % 2
        m = {}
        m.update(wts[hf])
        m.update(consts[hf])
        m.update(prep_acts(x[b], hf))
        in_maps.append(m)
    res = run_bass_kernel_spmd(_NC_CACHE["nc"], in_maps, core_ids=list(range(2 * nb)))
    return np.stack([np.concatenate([np.asarray(res.results[2 * b]["y"]),
                                     np.asarray(res.results[2 * b + 1]["y"])], 0)
                     for b in range(nb)], 0).astype(np.float32)
```

```python
from contextlib import ExitStack
import numpy as np
import ml_dtypes
import concourse.bass as bass
import concourse.mybir as mybir
from concourse.bass_utils import run_bass_kernel_spmd

F32 = mybir.dt.float32
BF16 = mybir.dt.bfloat16
I32 = mybir.dt.int32
U32 = mybir.dt.uint32
AF = mybir.ActivationFunctionType
ALU = mybir.AluOpType
AX = mybir.AxisListType

D = 1024
S = 8192
HALF = 4096
DIN = 5536
NEXP = 32
DEXP = 1024
LN_EPS = 1e-5
RMS_EPS = 1e-6
DN_ALPHA = 4.0 ** 0.25
NEG = -30000.0
NA_ROWS = 72

COMPUTE = ("pe", "act", "dve", "pool")


class Prog:
    def __init__(self, nc, stack, ring=8):
        self.nc = nc
        self.e = {"pe": nc.tensor, "act": nc.scalar, "dve": nc.vector,
                  "pool": nc.gpsimd, "sp": nc.sync}
        self.ops = []
        self.done = 0
        self.sem = {k: stack.enter_context(nc.semaphore("s_" + k)) for k in COMPUTE}
        self.ticket = {k: 0 for k in COMPUTE}
        self.ring = {q: [stack.enter_context(nc.semaphore("d_%s%d" % (q, i))) for i in range(ring)]
                     for q in ("sp", "pool")}
        self.ring_cnt = {q: [0] * ring for q in ("sp", "pool")}
        self.ring_last = {q: [None] * ring for q in ("sp", "pool")}
        self.ring_pos = {q: 0 for q in ("sp", "pool")}
        self.waited = {k: {} for k in self.e}
        self.last_w = {}
        self.readers = {}
        self.sig = {}
        self.eidx = {}
        self.ecount = {k: 0 for k in self.e}
        self.last_op = {k: None for k in self.e}
        self.dma_open = []

    def op(self, eng, fn, r=(), w=(), dma=False):
        self.ops.append((eng, fn, tuple(r), tuple(w), dma))

    def barrier(self):
        self.ops.append(("BAR", None, (), (), False))

    def _wait(self, eng, sem, val):
        cur = self.waited[eng].get(sem, 0)
        if cur < val:
            self.e[eng].wait_ge(sem, val)
            self.waited[eng][sem] = val

    def emit(self):
        ops = self.ops
        n = len(ops)
        start = self.done
        deps = {}
        needed = set()
        last_w, readers = self.last_w, self.readers
        eidx, ecount = self.eidx, self.ecount
        openg = {}
        for i in range(start, n):
            eng, fn, r, w, dma = ops[i]
            if eng == "BAR":
                continue
            eidx[i] = ecount[eng]
            ecount[eng] += 1
            openg[i] = (eng, dma)
            d = set()
            raw = set()
            for k in r:
                if k in last_w:
                    d.add(last_w[k])
                    raw.add(last_w[k])
                if isinstance(k, str) and k.startswith("ps"):
                    rd = readers.get(k)
                    if rd:
                        d.update(rd[0].values())
                        d.update(rd[1])
            for k in w:
                if k in last_w:
                    d.add(last_w[k])
                rd = readers.get(k)
                if rd:
                    d.update(rd[0].values())
                    d.update(rd[1])
            for k in r:
                rd = readers.setdefault(k, ({}, []))
                if dma:
                    rd[1].append(i)
                else:
                    rd[0][eng] = i
            for k in w:
                last_w[k] = i
                readers[k] = ({}, [])
            d.discard(i)
            keep = set()
            for j in d:
                if j < start and j not in self.sig and j not in openg:
                    continue
                jeng, jdma = self._opinfo(j, openg)
                if (not dma) and (not jdma) and jeng == eng:
                    if eng == "pe":
                        continue
                    if j in raw and eidx[i] - eidx[j] <= 2:
                        keep.add(j)
                    continue
                keep.add(j)
            deps[i] = keep
            needed.update(keep)
        self._openg_all = getattr(self, "_openg_all", {})
        self._openg_all.update(openg)
        for i in range(start, n):
            eng, fn, r, w, dma = ops[i]
            if eng == "BAR":
                self._emit_barrier()
                continue
            if dma:
                q = eng
                pos = self.ring_pos[q]
                self.ring_pos[q] = (pos + 1) % len(self.ring[q])
                sem = self.ring[q][pos]
                prev = self.ring_last[q][pos]
                if prev is not None:
                    self._wait(eng, sem, prev)
            for j in sorted(deps[i]):
                if j in self.sig:
                    s, v = self.sig[j]
                    self._wait(eng, s, v)
            ins = fn(self.e[eng])
            if dma:
                self.ring_cnt[q][pos] += 16
                val = self.ring_cnt[q][pos]
                ins.then_inc(sem, 16)
                self.ring_last[q][pos] = val
                self.sig[i] = (sem, val)
                self.dma_open.append(i)
            else:
                self.last_op[eng] = i
                if i in needed:
                    self.ticket[eng] += 1
                    ins.then_inc(self.sem[eng], 1)
                    self.sig[i] = (self.sem[eng], self.ticket[eng])
        self.done = n

    def _opinfo(self, j, openg):
        if j in openg:
            return openg[j]
        return self._openg_all[j]

    def _emit_barrier(self):
        marks = []
        for eng in COMPUTE:
            self.ticket[eng] += 1
            self.e[eng].drain().then_inc(self.sem[eng], 1)
            marks.append((self.sem[eng], self.ticket[eng]))
        dmas = [self.sig[i] for i in self.dma_open]
        self.dma_open = []
        for eng in self.e:
            for s, v in marks:
                self._wait(eng, s, v)
            for s, v in dmas:
                self._wait(eng, s, v)
        self.last_w.clear()
        self.readers.clear()


class Ctx:
    pass


_UID = [0]


def _sb(c, name, shape, dt):
    _UID[0] += 1
    return c.ph.enter_context(c.nc.sbuf_tensor("sb%d_%s" % (_UID[0], name), list(shape), dt))


def _ps(c, name, shape, dt):
    _UID[0] += 1
    return c.ph.enter_context(c.nc.psum_tensor("pp%d_%s" % (_UID[0], name), list(shape), dt))


def mm_group(p, out_ap, pairs, r, w):
    n = len(pairs)

    def fn(e):
        ins = None
        for i, (l, rr) in enumerate(pairs):
            ins = e.matmul(out_ap, l, rr, start=(i == 0), stop=(i == n - 1))
        return ins
    p.op("pe", fn, r, w)


def tr_group(p, outs_ins, ident, r, w):
    def fn(e):
        ins = None
        for o, i_ in outs_ins:
            ins = e.transpose(o, i_, ident)
        return ins
    p.op("pe", fn, r, w)


def dma(p, q, out_ap, in_ap, r, w):
    p.op(q, lambda e: e.dma_start(out=out_ap, in_=in_ap), r, w, dma=True)


def load_cast_cols(p, dst, src, ncols, r, w, step=2048):
    for c0 in range(0, ncols, step):
        c1 = min(ncols, c0 + step)
        dma(p, "pool", dst[:, c0:c1], src[:, c0:c1], r, w)


def phase1(c, L):
    nc, p = c.nc, c.p
    with ExitStack() as ph:
        c.ph = ph
        winb = _sb(c, "winb", [128, 8, DIN], BF16)
        wuqb = _sb(c, "wuqb", [128, 3, 384], BF16)
        wukvb = _sb(c, "wukvb", [128, 2, 512], BF16)
        identb = _sb(c, "identb", [128, 128], BF16)
        gq = _sb(c, "gq", [128, 384], F32)
        gk = _sb(c, "gk", [128, 128], F32)
        mq = _sb(c, "mq", [128, 384], F32)
        mkv = _sb(c, "mkv", [128, 256], F32)
        xf = [_sb(c, "xf%d" % i, [128, D], F32) for i in range(2)]
        xb = [_sb(c, "xb%d" % i, [128, D], BF16) for i in range(2)]
        xT = [_sb(c, "xT%d" % i, [128, 8, 512], BF16) for i in range(2)]
        rp = [_sb(c, "rp%d" % i, [128, 192], F32) for i in range(2)]
        wk = [_sb(c, "wk%d" % i, [128, 384], F32) for i in range(6)]
        sm = [_sb(c, "sm%d" % i, [128, 8], F32) for i in range(4)]
        ob = [_sb(c, "ob%d" % i, [128, 384], BF16) for i in range(8)]
        tT = [_sb(c, "tT%d" % i, [128, 512], BF16) for i in range(2)]
        blk = {k: [_sb(c, "blk_%s%d" % (k, i), [128, 6 * 512], BF16) for i in range(2)]
               for k in ("a", "b", "c", "d")}
        vna = [_sb(c, "vna%d" % i, [128, 6, 65], BF16) for i in range(2)]
        vg = [_sb(c, "vg%d" % i, [128, 2, 65], BF16) for i in range(2)]
        vm = [_sb(c, "vm%d" % i, [128, 4, 65], BF16) for i in range(2)]
        gout = [_sb(c, "gout%d" % i, [128, 512], BF16) for i in range(3)]
        psT = _ps(c, "psT", [128, 8, 128], BF16)
        psR = _ps(c, "psR", [128, 8, 128], BF16)
        psM = [_ps(c, "psM%d" % i, [128, 512], F32) for i in range(4)]
        psG = [_ps(c, "psG%d" % i, [128, 512], F32) for i in range(2)]

        w_in = c.w["w_in"][L]
        w_in_v = w_in.rearrange("(c p) n -> p c n", p=128)
        for ch in range(8):
            load_cast_cols(p, winb[:, ch, :], w_in_v[:, ch, :], DIN, [], ["winb"])
        wuq_v = c.w["w_uq"][L].rearrange("(c p) n -> p c n", p=128)
        for ch in range(3):
            load_cast_cols(p, wuqb[:, ch, :], wuq_v[:, ch, :], 384, [], ["wuqb"])
        wukv_v = c.w["w_ukv"][L].rearrange("(c p) n -> p c n", p=128)
        for ch in range(2):
            load_cast_cols(p, wukvb[:, ch, :], wukv_v[:, ch, :], 512, [], ["wukvb"])
        dma(p, "sp", identb[:], c.w["identb"][:, :], [], ["identb"])
        dma(p, "sp", gq[:], c.w["gq_rep"][L], [], ["gq"])
        dma(p, "sp", gk[:], c.w["gk_rep"][L], [], ["gk"])
        dma(p, "sp", mq[:], c.w["mq_rep"][L], [], ["mq"])
        dma(p, "sp", mkv[:], c.w["mkv_rep"][L], [], ["mkv"])
        for i in range(2):
            p.op("pool", lambda e, t=vna[i]: e.memset(t[:], 1.0), [], [("vna", i)])
            p.op("pool", lambda e, t=vg[i]: e.memset(t[:], 1.0), [], [("vg", i)])
            p.op("pool", lambda e, t=vm[i]: e.memset(t[:], 1.0), [], [("vm", i)])

        cnt = {"tile": 0, "psM": 0, "psG": 0, "wk": 0, "sm": 0, "ob": 0, "tT": 0, "gout": 0}
        pending = []

        def st(out_ap, in_ap, r, w):
            dma(p, "sp", out_ap, in_ap, r, w)

        def flush():
            for o_, i_, r_, w_ in pending:
                dma(p, "sp", o_, i_, r_, w_)
            del pending[:]

        def nxt(k, n):
            v = cnt[k] % n
            cnt[k] += 1
            return v

        def load_tile(src_ap, rope_ap, to_xT=None, mask_col=None, defer_tr=False):
            s = nxt("tile", 2)
            dma(p, "sp", xf[s][:], src_ap, [], [("xf", s)])
            if rope_ap is not None:
                dma(p, "sp", rp[s][:], rope_ap, [], [("rp", s)])
            if mask_col is not None:
                p.op("dve", lambda e: e.tensor_scalar(out=xf[s][:], in0=xf[s][:],
                                                      scalar1=c.hm[:, mask_col:mask_col + 1], scalar2=None,
                                                      op0=ALU.mult), [("xf", s), "hm"], [("xf", s)])
            p.op("dve", lambda e: e.tensor_copy(xb[s][:], xf[s][:]), [("xf", s)], [("xb", s)])
            if to_xT is None:
                dst, key = tT_full[s][:], ("tTf", s)
            else:
                dst, key = to_xT

            def fin():
                tr_group(p, [(psT[:, ch, :], xb[s][:, ch * 128:(ch + 1) * 128]) for ch in range(8)],
                         identb[:], [("xb", s), "identb"], ["psT"])
                p.op("act", lambda e: e.activation(out=dst, in_=psT[:], func=AF.Copy), ["psT"], [key])
            if defer_tr:
                return dst, key, rp[s], ("rp", s), fin
            fin()
            return dst, key, rp[s], ("rp", s)

        tT_full = [_sb(c, "tTf%d" % i, [128, 8, 128], BF16) for i in range(2)]

        def tok_mm(xTa, xTkey, c0, c1):
            b = nxt("psM", 4)
            mm_group(p, psM[b][:, 0:c1 - c0],
                     [(xTa[:, ch, :], winb[:, ch, c0:c1]) for ch in range(8)],
                     [xTkey, "winb"], ["psM%d" % b])
            return psM[b], "psM%d" % b

        def transp_out(src_bf, src_key, nh, dh, dst_ap, dst_key):
            tr_group(p, [(psR[0:dh, h, :], src_bf[:, h * dh:(h + 1) * dh]) for h in range(nh)],
                     identb[:], [src_key, "identb"], ["psR"])
            p.op("dve", lambda e: e.tensor_copy(dst_ap, psR[0:dh, 0:nh, :]), ["psR"], [dst_key])

        def rms_rope(ps_ap, pskey, nh, dh, g_ap, gkey, rope_t, rpkey, coff, out_bf, outkey):
            n = nh * dh
            hd = dh // 2
            a = nxt("wk", 6); b2 = nxt("wk", 6); c2 = nxt("wk", 6); d2 = nxt("wk", 6); e2 = nxt("wk", 6)
            s1 = nxt("sm", 4)
            A, B, C, Dd, E = wk[a], wk[b2], wk[c2], wk[d2], wk[e2]
            p.op("act", lambda e: e.activation(out=A[:, 0:n], in_=ps_ap, func=AF.Square),
                 [pskey], [("wk", a)])
            p.op("dve", lambda e: e.tensor_reduce(
                out=sm[s1][:, 0:nh], in_=A[:, 0:n].rearrange("p (h d) -> p h d", h=nh),
                axis=AX.X, op=ALU.add), [("wk", a)], [("sm", s1)])
            p.op("act", lambda e: e.activation(
                out=sm[s1][:, 0:nh], in_=sm[s1][:, 0:nh], func=AF.Sqrt, scale=1.0 / dh, bias=c.eps_rms[:, 0:1]),
                [("sm", s1)], [("sm", s1)])
            p.op("dve", lambda e: e.reciprocal(out=sm[s1][:, 0:nh], in_=sm[s1][:, 0:nh]),
                 [("sm", s1)], [("sm", s1)])
            p.op("dve", lambda e: e.tensor_tensor(
                out=B[:, 0:n].rearrange("p (h d) -> p h d", h=nh),
                in0=ps_ap.rearrange("p (h d) -> p h d", h=nh),
                in1=sm[s1][:, 0:nh].unsqueeze(2).to_broadcast([128, nh, dh]),
                op=ALU.mult), [pskey, ("sm", s1)], [("wk", b2)])
            p.op("dve", lambda e: e.tensor_tensor(out=C[:, 0:n], in0=B[:, 0:n], in1=g_ap, op=ALU.mult),
                 [("wk", b2), gkey], [("wk", c2)])
            rope(C, ("wk", c2), nh, dh, rope_t, rpkey, coff, Dd, ("wk", d2), E, ("wk", e2),
                 out_bf[:, 0:n].rearrange("p (h d) -> p h d", h=nh), outkey)

        def rope(X, xkey, nh, dh, rope_t, rpkey, coff, Dd, dkey, E, ekey, out3, outkey, xview=None):
            n = nh * dh
            hd = dh // 2
            x3 = xview if xview is not None else X[:, 0:n].rearrange("p (h d) -> p h d", h=nh)
            cs = rope_t[:, coff:coff + dh].unsqueeze(1).to_broadcast([128, nh, dh])
            sslo = rope_t[:, coff + dh:coff + dh + hd].unsqueeze(1).to_broadcast([128, nh, hd])
            sshi = rope_t[:, coff + dh + hd:coff + 2 * dh].unsqueeze(1).to_broadcast([128, nh, hd])
            d3 = Dd[:, 0:n].rearrange("p (h d) -> p h d", h=nh)
            e3 = E[:, 0:n].rearrange("p (h d) -> p h d", h=nh)
            p.op("dve", lambda e: e.tensor_tensor(out=d3, in0=x3, in1=cs, op=ALU.mult),
                 [xkey, rpkey], [dkey])
            p.op("dve", lambda e: e.tensor_tensor(out=e3[:, :, 0:hd], in0=x3[:, :, hd:dh], in1=sslo,
                                                  op=ALU.mult), [xkey, rpkey], [ekey])
            p.op("dve", lambda e: e.tensor_tensor(out=e3[:, :, hd:dh], in0=x3[:, :, 0:hd], in1=sshi,
                                                  op=ALU.mult), [xkey, rpkey, ekey], [ekey])
            p.op("pool", lambda e: e.tensor_tensor(out=out3, in0=d3, in1=e3, op=ALU.add),
                 [dkey, ekey], [outkey])

        def rms_full(ps_ap, pskey, n, g_ap, gkey, out_bf, outkey):
            a = nxt("wk", 6)
            s1 = nxt("sm", 4)
            p.op("act", lambda e: e.activation(out=wk[a][:, 0:n], in_=ps_ap, func=AF.Square,
                                               accum_out=sm[s1][:, 0:1]),
                 [pskey], [("wk", a), ("sm", s1)])
            p.op("act", lambda e: e.activation(
                out=sm[s1][:, 1:2], in_=sm[s1][:, 0:1], func=AF.Sqrt, scale=1.0 / n, bias=c.eps_rms[:, 0:1]),
                [("sm", s1)], [("sm", s1)])
            p.op("dve", lambda e: e.reciprocal(out=sm[s1][:, 2:3], in_=sm[s1][:, 1:2]),
                 [("sm", s1)], [("sm", s1)])
            p.op("dve", lambda e: e.scalar_tensor_tensor(
                out=out_bf, in0=ps_ap, scalar=sm[s1][:, 2:3], in1=g_ap,
                op0=ALU.mult, op1=ALU.mult), [pskey, ("sm", s1), gkey], [outkey])

        def na_kv(xTa, xTkey, tok_off, sub, bs, vs=None):
            ps, pk = tok_mm(xTa, xTkey, 384, 768)
            o = nxt("ob", 8)
            p.op("act", lambda e, ps=ps, o=o: e.activation(out=ob[o][:, 0:384], in_=ps[:, 0:384],
                                                           func=AF.Copy), [pk], [("ob", o)])
            kb = blk["b"][bs][0:64, :].rearrange("p (h t) -> p h t", h=6)
            transp_out(ob[o], ("ob", o), 6, 64, kb[:, :, sub * 128:(sub + 1) * 128], ("blk_b", bs))
            ps, pk = tok_mm(xTa, xTkey, 768, 1152)
            s = (cnt["tile"] - 1) % 2 if vs is None else vs
            p.op("act", lambda e, ps=ps, s=s: e.activation(
                out=vna[s][:, :, 0:64], in_=ps[:, 0:384].rearrange("p (h d) -> p h d", h=6),
                func=AF.Copy), [pk], [("vna", s)])
            st(c.s["v_na"][tok_off:tok_off + 128, :], vna[s][:].rearrange("p h d -> p (h d)"),
                [("vna", s)], [("d_v_na", tok_off)])

        def flush_blk(name, bs, dh, nh, dst, t0, nt):
            src = blk[name][bs][0:dh, 0:nh * 512].rearrange("p (h t) -> p h t", h=nh)[:, :, 0:nt]
            st(dst[:, :, t0:t0 + nt].rearrange("h d t -> d h t"), src,
               [("blk_" + name, bs)], [("d_" + name, t0)])

        def run_interleaved(gens):
            active = list(gens)
            while active:
                for g_ in list(active):
                    try:
                        next(g_)
                    except StopIteration:
                        active.remove(g_)

        def own_tile(bi, sub):
            bs = bi % 2
            t = bi * 4 + sub
            xTa, xTkey, rpt, rpk = load_tile(
                c.q_src[t * 128:(t + 1) * 128, :], c.rope_q[t * 128:(t + 1) * 128, :],
                to_xT=(xT[bs][:, :, sub * 128:(sub + 1) * 128], ("xT", bs)))
            vs = (cnt["tile"] - 1) % 2
            ps, pk = tok_mm(xTa, xTkey, 1152, 1536)
            o = nxt("ob", 8)
            rms_rope(ps[:, 0:384], pk, 6, 64, gq[:], "gq", rpt, rpk, 0, ob[o], ("ob", o))
            yield
            ps, pk = tok_mm(xTa, xTkey, 0, 384)
            oq = nxt("ob", 8)
            p.op("act", lambda e, oq=oq, ps=ps: e.activation(out=ob[oq][:, 0:384], in_=ps[:, 0:384],
                                                             func=AF.Copy), [pk], [("ob", oq)])
            qa = blk["a"][bs][0:64, :].rearrange("p (h t) -> p h t", h=6)
            transp_out(ob[oq], ("ob", oq), 6, 64, qa[:, :, sub * 128:(sub + 1) * 128], ("blk_a", bs))
            qg = blk["c"][bs][0:64, :].rearrange("p (h t) -> p h t", h=6)
            transp_out(ob[o], ("ob", o), 6, 64, qg[:, :, sub * 128:(sub + 1) * 128], ("blk_c", bs))
            yield
            ps, pk = tok_mm(xTa, xTkey, 1792, 2176)
            o = nxt("ob", 8)
            rms_full(ps[:, 0:384], pk, 384, mq[:], "mq", ob[o][:, 0:384], ("ob", o))
            yield
            na_kv(xTa, xTkey, 256 + t * 128, sub, bs, vs)
            tt = nxt("tT", 2)
            tr_group(p, [(psR[:, ch, :], ob[o][:, ch * 128:(ch + 1) * 128]) for ch in range(3)],
                     identb[:], [("ob", o), "identb"], ["psR"])
            p.op("act", lambda e, tt=tt: e.activation(
                out=tT[tt][:, 0:384].rearrange("p (c t) -> p c t", c=3), in_=psR[:, 0:3, :],
                func=AF.Copy), ["psR"], [("tT", tt)])
            g = nxt("psG", 2)
            tT3 = tT[tt][:, 0:384].rearrange("p (c t) -> p c t", c=3)
            mm_group(p, psG[g][:, 0:384], [(tT3[:, ch, :], wuqb[:, ch, :]) for ch in range(3)],
                     [("tT", tt), "wuqb"], ["psG%d" % g])
            o2 = nxt("ob", 8)
            qc3 = psG[g][:, 0:384].rearrange("p (h d) -> p h d", h=4)
            out3 = ob[o2][:, 0:384].rearrange("p (h d) -> p h d", h=4)
            p.op("act", lambda e, qc3=qc3, out3=out3: e.activation(
                out=out3[:, :, 0:64], in_=qc3[:, :, 0:64], func=AF.Copy),
                ["psG%d" % g], [("ob", o2)])
            a = nxt("wk", 6); d2 = nxt("wk", 6); e2 = nxt("wk", 6)
            xr3 = wk[a][:, 0:128].rearrange("p (h d) -> p h d", h=4)
            p.op("act", lambda e, qc3=qc3, xr3=xr3: e.activation(out=xr3, in_=qc3[:, :, 64:96],
                                                                 func=AF.Copy),
                 ["psG%d" % g], [("wk", a)])
            rope(wk[a], ("wk", a), 4, 32, rpt, rpk, 128, wk[d2], ("wk", d2), wk[e2], ("wk", e2),
                 out3[:, :, 64:96], ("ob", o2))
            yield
            qm = blk["d"][bs][0:96, 0:2048].rearrange("p (h t) -> p h t", h=4)
            transp_out(ob[o2], ("ob", o2), 4, 96, qm[:, :, sub * 128:(sub + 1) * 128], ("blk_d", bs))

        for bi in range(8):
            bs = bi % 2
            run_interleaved([own_tile(bi, 0), own_tile(bi, 1)])
            run_interleaved([own_tile(bi, 2), own_tile(bi, 3)])
            flush_blk("a", bs, 64, 6, c.s["qT_na"], bi * 512, 512)
            flush_blk("b", bs, 64, 6, c.s["kT_na"], 256 + bi * 512, 512)
            flush_blk("c", bs, 64, 6, c.s["qT_gqa"], bi * 512, 512)
            flush_blk("d", bs, 96, 4, c.s["qT_mla"], bi * 512, 512)
            flush()
            for cc in range(24):
                g = nxt("psG", 2)
                c0 = 2464 + cc * 128
                mm_group(p, psG[g][:, :], [(winb[:, ch, c0:c0 + 128], xT[bs][:, ch, :]) for ch in range(8)],
                         [("xT", bs), "winb"], ["psG%d" % g])
                go = nxt("gout", 3)
                p.op("act", lambda e, g=g, go=go: e.activation(out=gout[go][:], in_=psG[g][:, :],
                                                               func=AF.Sigmoid),
                     ["psG%d" % g], [("gout", go)])
                dma(p, "sp", c.s["gT"][cc * 128:(cc + 1) * 128, bi * 512:(bi + 1) * 512], gout[go][:],
                    [("gout", go)], [("d_gT", cc, bi)])

        for hi in range(4):
            hsrc = (c.c_src[HALF - 256 + hi * 128:HALF - 256 + (hi + 1) * 128, :] if hi < 2 else
                    c.c_src[(hi - 2) * 128:(hi - 1) * 128, :])
            xTa, xTkey, rpt, rpk = load_tile(hsrc, None, mask_col=c.hm_cols[0 if hi < 2 else 1])
            flush()
            tok_off = hi * 128 if hi < 2 else 4352 + (hi - 2) * 128
            na_kv(xTa, xTkey, tok_off, hi % 2, 0)
            if hi % 2 == 1:
                flush_blk("b", 0, 64, 6, c.s["kT_na"], 0 if hi == 1 else 4352, 256)

        def full_tile(bi, sub):
            bs = bi % 2
            t = bi * 4 + sub
            xTa, xTkey, rpt, rpk = load_tile(
                c.full_src[t * 128:(t + 1) * 128, :], c.rope_full[t * 128:(t + 1) * 128, :])
            s = (cnt["tile"] - 1) % 2
            ps, pk = tok_mm(xTa, xTkey, 1536, 1792)
            o = nxt("ob", 8)
            rms_rope(ps[:, 0:128], pk, 2, 64, gk[:], "gk", rpt, rpk, 0, ob[o], ("ob", o))
            p.op("act", lambda e, ps=ps, s=s: e.activation(
                out=vg[s][:, :, 0:64], in_=ps[:, 128:256].rearrange("p (h d) -> p h d", h=2),
                func=AF.Copy), [pk], [("vg", s)])
            st(c.s["v_gqa"][t * 128:(t + 1) * 128, :], vg[s][:].rearrange("p h d -> p (h d)"),
               [("vg", s)], [("d_v_gqa", t)])
            yield
            ps, pk = tok_mm(xTa, xTkey, 2176, 2464)
            o3 = nxt("ob", 8)
            rms_full(ps[:, 0:256], pk, 256, mkv[:], "mkv", ob[o3][:, 0:256], ("ob", o3))
            a = nxt("wk", 6)
            p.op("act", lambda e, ps=ps, a=a: e.activation(out=wk[a][:, 0:32], in_=ps[:, 256:288],
                                                           func=AF.Copy), [pk], [("wk", a)])
            d2 = nxt("wk", 6); e2 = nxt("wk", 6); f2 = nxt("wk", 6)
            kr3 = wk[f2][:, 0:32].rearrange("p (h d) -> p h d", h=1)
            rope(wk[a], ("wk", a), 1, 32, rpt, rpk, 128, wk[d2], ("wk", d2), wk[e2], ("wk", e2),
                 kr3, ("wk", f2))
            o2 = nxt("ob", 8)
            out3 = ob[o2][:, 0:384].rearrange("p (h d) -> p h d", h=4)
            p.op("pool", lambda e, out3=out3, f2=f2: e.tensor_copy(
                out3[:, :, 64:96], wk[f2][:, 0:32].unsqueeze(1).to_broadcast([128, 4, 32])),
                [("wk", f2)], [("ob", o2)])
            yield
            kg = blk["a"][bs][0:64, 0:1024].rearrange("p (h t) -> p h t", h=2)
            transp_out(ob[o], ("ob", o), 2, 64, kg[:, :, sub * 128:(sub + 1) * 128], ("blk_a", bs))
            tt = nxt("tT", 2)
            tr_group(p, [(psR[:, ch, :], ob[o3][:, ch * 128:(ch + 1) * 128]) for ch in range(2)],
                     identb[:], [("ob", o3), "identb"], ["psR"])
            tT2 = tT[tt][:, 0:256].rearrange("p (c t) -> p c t", c=2)
            p.op("act", lambda e, tT2=tT2: e.activation(out=tT2, in_=psR[:, 0:2, :], func=AF.Copy),
                 ["psR"], [("tT", tt)])
            g = nxt("psG", 2)
            mm_group(p, psG[g][:, :], [(tT2[:, ch, :], wukvb[:, ch, :]) for ch in range(2)],
                     [("tT", tt), "wukvb"], ["psG%d" % g])
            kv3 = psG[g][:, :].rearrange("p (h d) -> p h d", h=4)
            p.op("act", lambda e, kv3=kv3, out3=out3: e.activation(
                out=out3[:, :, 0:64], in_=kv3[:, :, 0:64], func=AF.Copy),
                ["psG%d" % g, ("ob", o2)], [("ob", o2)])
            p.op("dve", lambda e, kv3=kv3, s=s: e.tensor_copy(vm[s][:, :, 0:64], kv3[:, :, 64:128]),
                 ["psG%d" % g], [("vm", s)])
            st(c.s["v_mla"][t * 128:(t + 1) * 128, :], vm[s][:].rearrange("p h d -> p (h d)"),
               [("vm", s)], [("d_v_mla", t)])
            yield
            km = blk["d"][bs][0:96, 0:2048].rearrange("p (h t) -> p h t", h=4)
            transp_out(ob[o2], ("ob", o2), 4, 96, km[:, :, sub * 128:(sub + 1) * 128], ("blk_d", bs))

        flush()
        for bi in range(16 if c.do_full else 0):
            bs = bi % 2
            run_interleaved([full_tile(bi, 0), full_tile(bi, 1)])
            run_interleaved([full_tile(bi, 2), full_tile(bi, 3)])
            flush_blk("a", bs, 64, 2, c.s["kT_gqa"], bi * 512, 512)
            flush_blk("d", bs, 96, 4, c.s["kT_mla"], bi * 512, 512)
        flush()
        p.barrier()
        p.emit()


W_SHAPES = {
    "w_in": ([D, DIN], F32), "w_uq": ([384, 384], F32), "w_ukv": ([256, 512], F32),
    "w_ba": ([384, D], F32), "w_bb": ([384, D], F32), "w_bc": ([256, D], F32),
    "w_out": ([D, D], F32), "w_router": ([D, NEXP], F32),
    "w_gu": ([NEXP, D, 2 * DEXP], F32), "w_dn": ([NEXP, DEXP, D], F32),
    "gq_rep": ([128, 384], F32), "gk_rep": ([128, 128], F32),
    "mq_rep": ([128, 384], F32), "mkv_rep": ([128, 256], F32),
    "ln1g": ([128, D], F32), "ln1b": ([128, D], F32), "ln2g": ([128, D], F32), "ln2b": ([128, D], F32),
    "brouter": ([128, NEXP], F32), "bgu_t": ([128, NEXP, 16], F32), "bdn": ([NEXP, D], F32),
    "na_bint": ([64, 6, 8, 64], F32), "na_bbnd": ([7, 64, 6, 12, 64], F32),
    "na_bbnd_o": ([7, 64, 6, 12, 64], F32),
}
C_SHAPES = {
    "rope_loc": ([S, 192], F32), "hmask": ([128, 4], F32),
    "identb": ([128, 128], BF16), "identf": ([128, 128], F32),
    "triu": ([128, 128], F32), "ones128": ([128, 128], F32),
    "iota_e": ([128, NEXP], F32), "iota_cap": ([128, NEXP], F32),
}
CAP = 768
NSLOT = NEXP * CAP
S_SHAPES = {
    "qT_na": ([6, 64, HALF], BF16), "kT_na": ([6, 64, NA_ROWS * 64], BF16),
    "v_na": ([NA_ROWS * 64, 390], BF16),
    "qT_gqa": ([6, 64, HALF], BF16), "kT_gqa": ([2, 64, S], BF16), "v_gqa": ([S, 130], BF16),
    "qT_mla": ([4, 96, HALF], BF16), "kT_mla": ([4, 96, S], BF16), "v_mla": ([S, 260], BF16),
    "gT": ([3 * D, HALF], BF16),
    "oT": ([16, 64, HALF], BF16),
    "x1": ([HALF, D], F32),
    "x1T": ([D, HALF], BF16), "gateT": ([NEXP, HALF], F32), "ymoe": ([HALF, D], F32),
    "xg": ([NSLOT, D], BF16), "yg": ([NSLOT, D], F32),
    "slots": ([HALF, 4], I32), "gks": ([HALF, 4], F32),
}


def build(taps=(), passes=("A", "B", "C"), phases=None, dst_override=None):
    nc = bass.Bass("TRN2", target_bir_lowering=False)
    phases = phases or ALL_PHASES
    c = Ctx()
    c.nc = nc
    c.w = {}
    for k, (shp, dt) in W_SHAPES.items():
        c.w[k] = nc.dram_tensor(k, [2] + shp, dt, kind="ExternalInput").ap()
    for k, (shp, dt) in C_SHAPES.items():
        c.w[k] = nc.dram_tensor(k, shp, dt, kind="ExternalInput").ap()
    x_loc = nc.dram_tensor("x_loc", [S, D], F32, kind="ExternalInput").ap()
    c.s = {}
    for k, (shp, dt) in S_SHAPES.items():
        kind = "ExternalOutput" if k in taps else "Internal"
        c.s[k] = nc.dram_tensor("s_" + k, shp, dt, kind=kind).ap()
    y01 = nc.dram_tensor("y01", [S, D], F32, kind="Internal").ap()
    c.y = nc.dram_tensor("y", [HALF, D], F32, kind="ExternalOutput").ap()
    with ExitStack() as stack:
        c.p = Prog(nc, stack)
        c.breg = nc.gpsimd.to_reg(NSLOT - 1)
        c.eps_rms = stack.enter_context(nc.sbuf_tensor("eps_rms", [128, 1], F32))
        c.eps_ln = stack.enter_context(nc.sbuf_tensor("eps_ln", [128, 1], F32))
        c.hm = stack.enter_context(nc.sbuf_tensor("hmask_sb", [128, 4], F32))
        c.p.op("pool", lambda e: e.memset(c.eps_rms[:], RMS_EPS), [], ["eps"])
        c.p.op("pool", lambda e: e.memset(c.eps_ln[:], LN_EPS), [], ["eps"])
        dma(c.p, "sp", c.hm[:], c.w["hmask"][:, :], [], ["hm"])
        c.p.barrier()
        c.p.emit()
        rope = c.w["rope_loc"]
        c.rope_full = rope
        with ExitStack() as ph:
            zt = ph.enter_context(nc.sbuf_tensor("zt", [128, 8, D], BF16))
            c.p.op("pool", lambda e: e.memset(zt[:], 0.0), [], ["zt"])
            xg_v = c.s["xg"].rearrange("(n p) d -> p n d", p=128)
            for i in range(NSLOT // 1024):
                dma(c.p, "sp", xg_v[:, i * 8:(i + 1) * 8, :], zt[:], ["zt"], [("d_xg0", i)])
            c.p.barrier()
            c.p.emit()
        for ps_ in passes:
            if ps_ == "A":
                L, c.q_src, c.c_src, c.full_src = 0, x_loc[0:HALF, :], x_loc[HALF:S, :], x_loc
                c.rope_q, c.bbnd, c.hm_cols, c.do_full, dst = rope[0:HALF, :], c.w["na_bbnd"][0], (0, 1), True, y01[0:HALF, :]
            elif ps_ == "B":
                L, c.q_src, c.c_src, c.full_src = 0, x_loc[HALF:S, :], x_loc[0:HALF, :], x_loc
                c.rope_q, c.bbnd, c.hm_cols, c.do_full, dst = rope[HALF:S, :], c.w["na_bbnd_o"][0], (2, 3), False, y01[HALF:S, :]
            else:
                L, c.q_src, c.c_src, c.full_src = 1, y01[0:HALF, :], y01[HALF:S, :], y01
                c.rope_q, c.bbnd, c.hm_cols, c.do_full, dst = rope[0:HALF, :], c.w["na_bbnd"][1], (0, 1), True, c.y
            if "p1" in phases:
                phase1(c, L)
            if "na" in phases:
                phase_na(c, L)
            if "gqa" in phases:
                phase_dense_attn(c, L, "gqa")
            if "mla" in phases:
                phase_dense_attn(c, L, "mla")
            if "merge" in phases:
                phase_merge(c, L)
            if "moe" in phases:
                phase_moe(c, L)
            if "moes" in phases:
                phase_moe_sparse(c, L)
            if "ln2" in phases:
                phase_ln2(c, L, dst if dst_override is None else dst_override(c), sparse=("moes" in phases))
        c.p.barrier()
        c.p.emit()
    return nc


def rope_tables():
    t = np.arange(S)
    row = (t // 64).astype(np.float32)
    col = (t % 64).astype(np.float32)
    out = []
    for dim in (64, 32):
        quarter = dim // 4
        inv = (10000.0 ** (-np.arange(quarter, dtype=np.float32) / quarter)).astype(np.float32)
        ang = np.concatenate([row[:, None] * inv, col[:, None] * inv], -1).astype(np.float32)
        cs, sn = np.cos(ang).astype(np.float32), np.sin(ang).astype(np.float32)
        out.append(np.concatenate([cs, cs], -1))
        out.append(np.concatenate([-sn, sn], -1))
    return np.ascontiguousarray(np.concatenate(out, -1).astype(np.float32))


def na_bias_tables(rpb, hf):
    cols = np.arange(64)
    c0 = np.clip(cols - 8, 0, 48)
    in_win = (cols[None, :] >= c0[:, None]) & (cols[None, :] < c0[:, None] + 16)
    idx_c = np.clip(cols[None, :] - cols[:, None] + 15, 0, 30)

    def tab(j, rows):
        r = hf * 64 + j
        r0 = int(np.clip(r - 4, 0, 120))
        out = np.full((64, 6, len(rows), 64), NEG, np.float32)
        for ii, lr in enumerate(rows):
            gr = hf * 64 + lr - 4
            i = gr - r0
            if i < 0 or i >= 8 or gr < 0 or gr >= 128:
                continue
            ir = gr - r + 7
            b = rpb[:, ir][:, idx_c]
            b = np.where(in_win[None], b, NEG)
            out[:, :, ii, :] = b.transpose(2, 0, 1)
        return out
    interior = tab(10, list(range(10, 18)))
    bnd = []
    for j in (0, 1, 2, 3):
        bnd.append(tab(j, list(range(0, 12))))
    for j in (61, 62, 63):
        bnd.append(tab(j, list(range(60, 72))))
    return interior, np.stack(bnd, 0)


def prep_weights(inp, layers, hf):
    w = {}
    ls = list(layers)
    st = lambda f: np.ascontiguousarray(np.stack([f(l) for l in ls], 0))
    w["w_in"] = st(lambda l: inp["w_in"][l])
    w["w_uq"] = st(lambda l: inp["w_uq"][l])
    w["w_ukv"] = st(lambda l: inp["w_ukv"][l])
    w["w_ba"] = st(lambda l: inp["w_branch_a"][l])
    w["w_bb"] = st(lambda l: inp["w_branch_b"][l])
    w["w_bc"] = st(lambda l: inp["w_branch_c"][l])
    w["w_out"] = st(lambda l: inp["w_out"][l])
    w["w_router"] = st(lambda l: inp["w_router"][l])
    w["w_gu"] = st(lambda l: inp["w_gate_up"][l])
    w["w_dn"] = st(lambda l: inp["w_down"][l])
    w["gq_rep"] = st(lambda l: np.tile(inp["gqa_q_norm"][l][None, :], (128, 6)))
    w["gk_rep"] = st(lambda l: np.tile(inp["gqa_k_norm"][l][None, :], (128, 2)))
    w["mq_rep"] = st(lambda l: np.tile(inp["mla_q_norm"][l][None, :], (128, 1)))
    w["mkv_rep"] = st(lambda l: np.tile(inp["mla_kv_norm"][l][None, :], (128, 1)))
    for k, src in (("ln1g", "ln1_g"), ("ln1b", "ln1_b"), ("ln2g", "ln2_g"), ("ln2b", "ln2_b")):
        w[k] = st(lambda l: np.tile(inp[src][l][None, :], (128, 1)))
    w["brouter"] = st(lambda l: np.tile(inp["b_router"][l][None, :], (128, 1)))
    w["bgu_t"] = st(lambda l: inp["b_gate_up"][l].reshape(NEXP, 16, 128).transpose(2, 0, 1))
    w["bdn"] = st(lambda l: inp["b_down"][l])
    bi, bb = zip(*[na_bias_tables(inp["na_rpb"][l], hf) for l in ls])
    w["na_bint"] = np.ascontiguousarray(np.stack(bi, 0))
    w["na_bbnd"] = np.ascontiguousarray(np.stack(bb, 0))
    w["na_bbnd_o"] = np.ascontiguousarray(np.stack([na_bias_tables(inp["na_rpb"][l], 1 - hf)[1] for l in ls], 0))
    return {k: np.ascontiguousarray(v.astype(np.float32)) for k, v in w.items()}


def prep_consts(hf):
    rt = rope_tables()
    own, oth = rt[hf * HALF:(hf + 1) * HALF], rt[(1 - hf) * HALF:(2 - hf) * HALF]
    hm = np.zeros((128, 4), np.float32)
    hm[:, 0], hm[:, 1], hm[:, 2], hm[:, 3] = hf, 1 - hf, 1 - hf, hf
    return {
        "rope_loc": np.ascontiguousarray(np.concatenate([own, oth], 0)),
        "hmask": hm,
        "identb": np.eye(128, dtype=np.float32).astype(ml_dtypes.bfloat16),
        "identf": np.eye(128, dtype=np.float32),
        "triu": np.triu(np.ones((128, 128), np.float32), 1),
        "ones128": np.ones((128, 128), np.float32),
        "iota_e": np.tile(np.arange(NEXP, dtype=np.float32)[None, :], (128, 1)),
        "iota_cap": np.tile((np.arange(NEXP, dtype=np.float32) * CAP)[None, :], (128, 1)),
    }


def prep_acts(xb, hf):
    own, oth = xb[hf * HALF:(hf + 1) * HALF], xb[(1 - hf) * HALF:(2 - hf) * HALF]
    return {"x_loc": np.ascontiguousarray(np.concatenate([own, oth], 0))}


def _normalize(c, psO, okey, ncol, rsb, rkey, psB, ou, oukey, onesf, dst_ap, dst_key, nh=1):
    p = c.p
    p.op("dve", lambda e: e.reciprocal(out=rsb[64:65, 0:ncol], in_=psO[64:65, 0:ncol]), [okey], [rkey])
    p.op("pe", lambda e: e.matmul(psB[0:64, 0:ncol], onesf[64:65, 0:64], rsb[64:65, 0:ncol],
                                  start=True, stop=True), [rkey, "onesf"], ["psB"])
    p.op("act", lambda e: e.activation(out=ou[0:64, 0:ncol], in_=psO[0:64, 0:ncol], func=AF.Copy),
         [okey], [oukey])
    a0 = ou[0:64, 0:ncol]
    a1 = psB[0:64, 0:ncol]
    if nh > 1:
        a0 = a0.rearrange("p (h q) -> p h q", h=nh)
        a1 = a1.rearrange("p (h q) -> p h q", h=nh)
    p.op("dve", lambda e: e.tensor_tensor(out=dst_ap, in0=a0, in1=a1, op=ALU.mult),
         [oukey, "psB"], [dst_key])


def phase_dense_attn(c, L, kind):
    nc, p = c.nc, c.p
    if kind == "gqa":
        nH, dh, nK, nV, scale, obase = 6, 64, 2, 2, 64 ** -0.5, 6
        qT, kT, vS = c.s["qT_gqa"], c.s["kT_gqa"], c.s["v_gqa"]
        kmap = [0, 0, 0, 1, 1, 1]
    else:
        nH, dh, nK, nV, scale, obase = 4, 96, 4, 4, 96 ** -0.5, 12
        qT, kT, vS = c.s["qT_mla"], c.s["kT_mla"], c.s["v_mla"]
        kmap = [0, 1, 2, 3]
    with ExitStack() as ph:
        c.ph = ph
        KT = _sb(c, "KT", [128, nK, S], BF16)
        V = _sb(c, "V", [128, 64, nV * 65], BF16)
        Q = [_sb(c, "Q%d" % i, [128, nH, 512], BF16) for i in range(2)]
        pT = [_sb(c, "pT%d" % i, [128, 1024], BF16) for i in range(3)]
        rsb = _sb(c, "rsb", [128, 512], F32)
        ou = _sb(c, "ou", [128, 512], F32)
        ot = [_sb(c, "ot%d" % i, [128, 512], BF16) for i in range(2)]
        onesf = _sb(c, "onesf", [128, 64], F32)
        psS = [_ps(c, "psS%d" % i, [128, 1024], F32) for i in range(2)]
        psO = [_ps(c, "psO%d" % i, [128, 512], F32) for i in range(2)]
        psB = _ps(c, "psB", [128, 512], F32)
        p.op("pool", lambda e: e.memset(onesf[:], 1.0), [], ["onesf"])
        pack = (kind == "gqa")
        if pack:
            kT2 = kT.rearrange("g d t -> (g d) t")
            for half in range(2):
                dma(p, "sp", KT[:, 0, half * HALF:(half + 1) * HALF], kT2[:, half * HALF:(half + 1) * HALF],
                    [], ["KT"])
            for i in range(2):
                p.op("pool", lambda e, i=i: e.memset(Q[i][:], 0.0), [], [("Q", i)])
        else:
            for k in range(nK):
                for half in range(2):
                    dma(p, "sp", KT[0:dh, k, half * HALF:(half + 1) * HALF],
                        kT[k, :, half * HALF:(half + 1) * HALF], [], ["KT"])
        vv = vS.rearrange("(t p) c -> p t c", p=128)
        for q4 in range(16):
            dma(p, "sp", V[:, q4 * 4:(q4 + 1) * 4, :], vv[:, q4 * 4:(q4 + 1) * 4, :], [], ["V"])
        it = 0
        for qb in range(8):
            qs = qb % 2
            if pack:
                dma(p, "sp", Q[qs][0:64, 0:3, :], qT[0:3, :, qb * 512:(qb + 1) * 512].rearrange("h d t -> d h t"),
                    [], [("Q", qs)])
                dma(p, "sp", Q[qs][64:128, 3:6, :], qT[3:6, :, qb * 512:(qb + 1) * 512].rearrange("h d t -> d h t"),
                    [], [("Q", qs)])
            else:
                dma(p, "sp", Q[qs][0:dh, :, :], qT[:, :, qb * 512:(qb + 1) * 512].rearrange("h d t -> d h t"),
                    [], [("Q", qs)])
            for h in range(nH):
                ob_ = it % 2
                it += 1
                okey = "psO%d" % ob_
                ki = kmap[h]

                def qk2(k2, h=h, ki=ki, qs=qs):
                    b = k2 % 2

                    def fn(e):
                        ins = None
                        for u in range(2):
                            kt = 2 * k2 + u
                            if pack:
                                ins = e.matmul(psS[b][:, u * 512:(u + 1) * 512], KT[:, 0, kt * 128:(kt + 1) * 128],
                                               Q[qs][:, h, :], start=True, stop=True)
                            else:
                                ins = e.matmul(psS[b][:, u * 512:(u + 1) * 512],
                                               KT[0:dh, ki, kt * 128:(kt + 1) * 128], Q[qs][0:dh, h, :],
                                               start=True, stop=True)
                        return ins
                    p.op("pe", fn, ["KT", ("Q", qs)], ["psS%d" % b])
                qk2(0)
                for k2 in range(32):
                    b = k2 % 2
                    pb = k2 % 3
                    if k2 + 1 < 32:
                        qk2(k2 + 1)
                    p.op("act", lambda e, b=b, pb=pb: e.activation(out=pT[pb][:], in_=psS[b][:, :],
                                                                   func=AF.Exp, scale=scale),
                         ["psS%d" % b], [("pT", pb)])

                    def fpv(e, k2=k2, pb=pb, ob_=ob_, ki=ki):
                        ins = None
                        for u in range(2):
                            kt = 2 * k2 + u
                            ins = e.matmul(psO[ob_][0:65, :], V[:, kt, ki * 65:(ki + 1) * 65],
                                           pT[pb][:, u * 512:(u + 1) * 512],
                                           start=(kt == 0), stop=(kt == 63))
                        return ins
                    p.op("pe", fpv, ["V", ("pT", pb)], [okey])
                os_ = it % 2
                _normalize(c, psO[ob_], okey, 512, rsb, "rsb", psB, ou, "ou", onesf,
                           ot[os_][0:64, :], ("ot", os_))
                dma(p, "sp", c.s["oT"][obase + h, :, qb * 512:(qb + 1) * 512], ot[os_][0:64, :],
                    [("ot", os_)], [("d_oT", obase + h, qb)])
        p.barrier()
        p.emit()


def phase_na(c, L):
    nc, p = c.nc, c.p
    with ExitStack() as ph:
        c.ph = ph
        Qb = [_sb(c, "Qb%d" % i, [128, 6, 512], BF16) for i in range(2)]
        Kb = [_sb(c, "Kb%d" % i, [128, 6, 1024], BF16) for i in range(2)]
        Vb = [_sb(c, "Vb%d" % i, [128, 16, 390], BF16) for i in range(2)]
        bint = _sb(c, "bint", [128, 6, 8, 64], F32)
        bbnd = _sb(c, "bbnd", [128, 6, 12, 64], F32)
        sc = [_sb(c, "sc%d" % i, [128, 768], F32) for i in range(2)]
        pp = [_sb(c, "pp%d" % i, [128, 768], BF16) for i in range(2)]
        rsb = _sb(c, "rsb", [128, 512], F32)
        ou = _sb(c, "ou", [128, 512], F32)
        ot = [_sb(c, "ot%d" % i, [128, 6, 512], BF16) for i in range(2)]
        onesf = _sb(c, "onesf", [128, 64], F32)
        psS = [_ps(c, "psS%d" % i, [128, 1024], F32) for i in range(2)]
        psO = [_ps(c, "psO%d" % i, [128, 512], F32) for i in range(2)]
        psB = _ps(c, "psB", [128, 512], F32)
        p.op("pool", lambda e: e.memset(onesf[:], 1.0), [], ["onesf"])
        dma(p, "sp", bint[0:64], c.w["na_bint"][L], [], ["bint"])
        vrow = c.s["v_na"].rearrange("(r k) c -> k r c", k=64)
        it = 0
        for b8 in range(8):
            s = b8 % 2
            dma(p, "sp", Qb[s][0:64, :, :], c.s["qT_na"][:, :, b8 * 512:(b8 + 1) * 512].rearrange("h d t -> d h t"),
                [], [("Qb", s)])
            dma(p, "sp", Kb[s][0:64, :, :], c.s["kT_na"][:, :, b8 * 512:b8 * 512 + 1024].rearrange("h d t -> d h t"),
                [], [("Kb", s)])
            for hv in range(2):
                dma(p, "sp", Vb[s][0:64, hv * 8:(hv + 1) * 8, :], vrow[:, b8 * 8 + hv * 8:b8 * 8 + (hv + 1) * 8, :],
                    [], [("Vb", s)])
            for jj in range(8):
                j = b8 * 8 + jj
                if j < 4:
                    rows = list(range(0, 12)); bidx = j
                elif j > 60:
                    rows = list(range(60, 72)); bidx = 4 + (j - 61)
                else:
                    rows = list(range(j, j + 8)); bidx = None
                nr = len(rows)
                if bidx is not None:
                    dma(p, "sp", bbnd[0:64], c.bbnd[bidx], [], ["bbnd"])
                    btile, bkey = bbnd, "bbnd"
                else:
                    btile, bkey = bint, "bint"
                ob_ = j % 2
                okey = "psO%d" % ob_
                for h in range(6):
                    sb_ = it % 2
                    it += 1
                    skey = "psS%d" % sb_

                    def fqk(e, h=h, sb_=sb_, rows=rows, jj=jj, s=s, b8=b8):
                        ins = None
                        for i, lr in enumerate(rows):
                            ins = e.matmul(psS[sb_][0:64, i * 64:(i + 1) * 64],
                                           Kb[s][0:64, h, (lr - b8 * 8) * 64:(lr - b8 * 8 + 1) * 64],
                                           Qb[s][0:64, h, jj * 64:(jj + 1) * 64], start=True, stop=True)
                        return ins
                    p.op("pe", fqk, [("Qb", s), ("Kb", s)], [skey])
                    p.op("dve", lambda e, h=h, sb_=sb_, nr=nr, btile=btile: e.scalar_tensor_tensor(
                        out=sc[sb_][0:64, 0:nr * 64], in0=psS[sb_][0:64, 0:nr * 64], scalar=0.125,
                        in1=btile[0:64, h, 0:nr, :].rearrange("p r q -> p (r q)"),
                        op0=ALU.mult, op1=ALU.add), [skey, bkey], [("sc", sb_)])
                    p.op("act", lambda e, sb_=sb_, nr=nr: e.activation(
                        out=pp[sb_][0:64, 0:nr * 64], in_=sc[sb_][0:64, 0:nr * 64], func=AF.Exp),
                        [("sc", sb_)], [("pp", sb_)])

                    def fpv(e, h=h, sb_=sb_, rows=rows, ob_=ob_, s=s, b8=b8):
                        ins = None
                        n = len(rows)
                        for i, lr in enumerate(rows):
                            ins = e.matmul(psO[ob_][0:65, h * 64:(h + 1) * 64],
                                           Vb[s][0:64, lr - b8 * 8, h * 65:(h + 1) * 65],
                                           pp[sb_][0:64, i * 64:(i + 1) * 64],
                                           start=(i == 0), stop=(i == n - 1))
                        return ins
                    p.op("pe", fpv, [("Vb", s), ("pp", sb_)], [okey])
                _normalize(c, psO[ob_], okey, 384, rsb, "rsb", psB, ou, "ou", onesf,
                           ot[s][0:64, :, jj * 64:(jj + 1) * 64],
                           ("ot", s), nh=6)
            dma(p, "sp", c.s["oT"][0:6, :, b8 * 512:(b8 + 1) * 512].rearrange("h d t -> d h t"),
                ot[s][0:64, :, :], [("ot", s)], [("d_oT_na", b8)])
        p.barrier()
        p.emit()


def layer_norm_tile(c, tsb, tkey, junk, jkey, sm, smkey, g_t, b_t, gbkey, out_t, outkey):
    p = c.p
    p.op("act", lambda e: e.activation(out=junk[:], in_=tsb[:], func=AF.Copy, accum_out=sm[:, 0:1]),
         [tkey], [jkey, smkey])
    p.op("act", lambda e: e.activation(out=junk[:], in_=tsb[:], func=AF.Square, accum_out=sm[:, 1:2]),
         [tkey], [jkey, smkey])
    p.op("dve", lambda e: e.tensor_scalar(out=sm[:, 2:3], in0=sm[:, 0:1], scalar1=1.0 / D, scalar2=None,
                                          op0=ALU.mult), [smkey], [smkey])
    p.op("dve", lambda e: e.tensor_tensor(out=sm[:, 3:4], in0=sm[:, 2:3], in1=sm[:, 2:3], op=ALU.mult),
         [smkey], [smkey])
    p.op("dve", lambda e: e.scalar_tensor_tensor(out=sm[:, 4:5], in0=sm[:, 1:2], scalar=1.0 / D,
                                                 in1=sm[:, 3:4], op0=ALU.mult, op1=ALU.subtract),
         [smkey], [smkey])
    p.op("act", lambda e: e.activation(out=sm[:, 5:6], in_=sm[:, 4:5], func=AF.Sqrt,
                                       bias=c.eps_ln[:, 0:1]), [smkey], [smkey])
    p.op("dve", lambda e: e.reciprocal(out=sm[:, 6:7], in_=sm[:, 5:6]), [smkey], [smkey])
    p.op("dve", lambda e: e.tensor_scalar(out=junk[:], in0=tsb[:], scalar1=sm[:, 2:3], scalar2=sm[:, 6:7],
                                          op0=ALU.subtract, op1=ALU.mult), [tkey, smkey], [jkey])
    p.op("pool", lambda e: e.tensor_tensor(out=junk[:], in0=junk[:], in1=g_t[:], op=ALU.mult),
         [jkey, gbkey], [jkey])
    p.op("pool", lambda e: e.tensor_tensor(out=out_t[:], in0=junk[:], in1=b_t[:], op=ALU.add),
         [jkey, gbkey], [outkey])


def phase_merge(c, L):
    nc, p = c.nc, c.p
    with ExitStack() as ph:
        c.ph = ph
        wb = _sb(c, "wb", [128, 16, D], BF16)
        woutb = _sb(c, "woutb", [128, 8, D], BF16)
        lng = _sb(c, "lng", [128, D], F32)
        lnb = _sb(c, "lnb", [128, D], F32)
        wr = _sb(c, "wr", [128, 8, NEXP], F32)
        br = _sb(c, "br", [128, NEXP], F32)
        identb = _sb(c, "identb", [128, 128], BF16)
        identf = _sb(c, "identf", [128, 128], F32)
        oTb = [_sb(c, "oTb%d" % i, [128, 16, 512], BF16) for i in range(2)]
        gTc = [_sb(c, "gTc%d" % i, [128, 3, 512], BF16) for i in range(2)]
        mixT = [_sb(c, "mixT%d" % i, [128, 8, 512], BF16) for i in range(2)]
        mt = [_sb(c, "mt%d" % i, [128, 512], F32) for i in range(6)]
        xo = [_sb(c, "xo%d" % i, [128, D], F32) for i in range(2)]
        tsb = [_sb(c, "tsb%d" % i, [128, D], F32) for i in range(2)]
        junk = _sb(c, "junk", [128, D], F32)
        x1t = [_sb(c, "x1t%d" % i, [128, D], F32) for i in range(2)]
        x1b = [_sb(c, "x1b%d" % i, [128, D], BF16) for i in range(2)]
        x1Tb = [_sb(c, "x1Tb%d" % i, [128, 8, 128], BF16) for i in range(2)]
        x1Tf = [_sb(c, "x1Tf%d" % i, [128, 8, 128], F32) for i in range(2)]
        sm = [_sb(c, "sm%d" % i, [128, 8], F32) for i in range(2)]
        rt_ = [_sb(c, "rt%d" % i, [128, 160], F32) for i in range(2)]
        gTt = [_sb(c, "gTt%d" % i, [128, 128], F32) for i in range(2)]
        psY = [_ps(c, "psY%d" % i, [128, 512], F32) for i in range(3)]
        psOut = [_ps(c, "psOut%d" % i, [128, 512], F32) for i in range(2)]
        psT = _ps(c, "psT", [128, 8, 128], BF16)
        psTf = _ps(c, "psTf", [128, 8, 128], F32)
        triu = _sb(c, "triu", [128, 128], F32)
        ones128 = _sb(c, "ones128", [128, 128], F32)
        iota_e = _sb(c, "iota_e", [128, NEXP], F32)
        iota_cap = _sb(c, "iota_cap", [128, NEXP], F32)
        msum = _sb(c, "msum", [128, NEXP], F32)
        rx = [_sb(c, "rx%d" % i, [128, 160], F32) for i in range(2)]
        idxu = [_sb(c, "idxu%d" % i, [128, 8], U32) for i in range(2)]
        slotu = [_sb(c, "slotu%d" % i, [128, 4], I32) for i in range(2)]
        dma(p, "sp", triu[:], c.w["triu"][:, :], [], ["triu"])
        dma(p, "sp", ones128[:], c.w["ones128"][:, :], [], ["ones128"])
        dma(p, "sp", iota_e[:], c.w["iota_e"][:, :], [], ["iota_e"])
        dma(p, "sp", iota_cap[:], c.w["iota_cap"][:, :], [], ["iota_cap"])
        p.op("pool", lambda e: e.memset(msum[:], 0.0), [], ["msum"])

        for h in range(16):
            src = (c.w["w_ba"][L][h * 64:(h + 1) * 64, :] if h < 6 else
                   c.w["w_bb"][L][(h - 6) * 64:(h - 5) * 64, :] if h < 12 else
                   c.w["w_bc"][L][(h - 12) * 64:(h - 11) * 64, :])
            dma(p, "pool", wb[0:64, h, :], src, [], ["wb"])
        wo_v = c.w["w_out"][L].rearrange("(c p) n -> p c n", p=128)
        for ch in range(8):
            dma(p, "pool", woutb[:, ch, :], wo_v[:, ch, :], [], ["woutb"])
        dma(p, "sp", lng[:], c.w["ln1g"][L], [], ["lngb"])
        dma(p, "sp", lnb[:], c.w["ln1b"][L], [], ["lngb"])
        dma(p, "sp", wr[:], c.w["w_router"][L].rearrange("(c p) e -> p c e", p=128), [], ["wr"])
        dma(p, "sp", br[:], c.w["brouter"][L], [], ["br"])
        dma(p, "sp", identb[:], c.w["identb"][:, :], [], ["identb"])
        dma(p, "sp", identf[:], c.w["identf"][:, :], [], ["identf"])
        gT_v = c.s["gT"].rearrange("(i c p) t -> p i c t", i=3, c=8, p=128)
        x1T_v = c.s["x1T"].rearrange("(c p) t -> p c t", p=128)
        gcnt = 0
        for blk in range(8):
            s = blk % 2
            for hv in range(2):
                dma(p, "sp", oTb[s][0:64, hv * 8:(hv + 1) * 8, :],
                    c.s["oT"][hv * 8:(hv + 1) * 8, :, blk * 512:(blk + 1) * 512].rearrange("h d t -> d h t"),
                    [], [("oTb", s)])
            for dc in range(8):
                gs = gcnt % 2
                gcnt += 1
                dma(p, "sp", gTc[gs][:], gT_v[:, :, dc, blk * 512:(blk + 1) * 512], [], [("gTc", gs)])
                for i, (h0, nh) in enumerate(((0, 6), (6, 6), (12, 4))):
                    mm_group(p, psY[i][:, :],
                             [(wb[0:64, h0 + k, dc * 128:(dc + 1) * 128], oTb[s][0:64, h0 + k, :])
                              for k in range(nh)], ["wb", ("oTb", s)], ["psY%d" % i])
                m3 = [(gcnt * 3 + i) % 6 for i in range(3)]
                for i in range(3):
                    p.op("dve", lambda e, i=i, gs=gs, m=m3[i]: e.tensor_tensor(
                        out=mt[m][:], in0=psY[i][:, :], in1=gTc[gs][:, i, :], op=ALU.mult),
                        ["psY%d" % i, ("gTc", gs)], [("mt", m3[i])])
                p.op("pool", lambda e, m3=m3: e.tensor_tensor(out=mt[m3[0]][:], in0=mt[m3[0]][:],
                                                              in1=mt[m3[1]][:], op=ALU.add),
                     [("mt", m3[0]), ("mt", m3[1])], [("mt", m3[0])])
                p.op("pool", lambda e, m3=m3, s=s, dc=dc: e.tensor_tensor(
                    out=mixT[s][:, dc, :], in0=mt[m3[0]][:], in1=mt[m3[2]][:], op=ALU.add),
                    [("mt", m3[0]), ("mt", m3[2])], [("mixT", s)])
            for tt in range(4):
                t = blk * 4 + tt
                ts_ = t % 2
                dma(p, "sp", xo[ts_][:], c.q_src[t * 128:(t + 1) * 128, :], [], [("xo", ts_)])
                for half in range(2):
                    mm_group(p, psOut[half][:, :],
                             [(mixT[s][:, dc, tt * 128:(tt + 1) * 128], woutb[:, dc, half * 512:(half + 1) * 512])
                              for dc in range(8)], [("mixT", s), "woutb"], ["psOut%d" % half])
                    p.op("dve", lambda e, half=half, ts_=ts_: e.scalar_tensor_tensor(
                        out=tsb[ts_][:, half * 512:(half + 1) * 512], in0=xo[ts_][:, half * 512:(half + 1) * 512],
                        scalar=DN_ALPHA, in1=psOut[half][:, :], op0=ALU.mult, op1=ALU.add),
                        [("xo", ts_), "psOut%d" % half], [("tsb", ts_)])
                layer_norm_tile(c, tsb[ts_], ("tsb", ts_), junk, "junk", sm[ts_], ("sm", ts_),
                                lng, lnb, "lngb", x1t[ts_], ("x1t", ts_))
                dma(p, "sp", c.s["x1"][t * 128:(t + 1) * 128, :], x1t[ts_][:], [("x1t", ts_)], [("d_x1", t)])
                p.op("act", lambda e, ts_=ts_: e.activation(out=x1b[ts_][:], in_=x1t[ts_][:], func=AF.Copy),
                     [("x1t", ts_)], [("x1b", ts_)])
                tr_group(p, [(psT[:, ch, :], x1b[ts_][:, ch * 128:(ch + 1) * 128]) for ch in range(8)],
                         identb[:], [("x1b", ts_), "identb"], ["psT"])
                p.op("dve", lambda e, ts_=ts_: e.tensor_copy(x1Tb[ts_][:], psT[:]), ["psT"], [("x1Tb", ts_)])
                for hv in range(2):
                    dma(p, "sp", x1T_v[:, hv * 4:(hv + 1) * 4, t * 128:(t + 1) * 128], x1Tb[ts_][:, hv * 4:(hv + 1) * 4, :],
                        [("x1Tb", ts_)], [("d_x1T", t, hv)])
                tr_group(p, [(psTf[:, ch, :], x1t[ts_][:, ch * 128:(ch + 1) * 128]) for ch in range(8)],
                         identf[:], [("x1t", ts_), "identf"], ["psTf"])
                p.op("act", lambda e, ts_=ts_: e.activation(out=x1Tf[ts_][:], in_=psTf[:], func=AF.Copy),
                     ["psTf"], [("x1Tf", ts_)])
                mm_group(p, psY[0][:, 0:NEXP], [(x1Tf[ts_][:, ch, :], wr[:, ch, :]) for ch in range(8)],
                         [("x1Tf", ts_), "wr"], ["psY0"])
                R = rt_[ts_]
                rk = ("rt", ts_)
                lg, mx, msk, ex, exm = R[:, 0:32], R[:, 32:40], R[:, 40:72], R[:, 72:104], R[:, 104:136]
                nm, ssum, rs = R[:, 136:137], R[:, 137:138], R[:, 138:139]
                p.op("dve", lambda e, lg=lg: e.tensor_tensor(out=lg, in0=psY[0][:, 0:NEXP], in1=br[:], op=ALU.add),
                     ["psY0", "br"], [rk])
                p.op("dve", lambda e, lg=lg, mx=mx: e.max(out=mx, in_=lg), [rk], [rk])
                p.op("dve", lambda e, lg=lg, mx=mx, msk=msk: e.tensor_scalar(
                    out=msk, in0=lg, scalar1=mx[:, 3:4], scalar2=None, op0=ALU.is_ge), [rk], [rk])
                p.op("dve", lambda e, mx=mx, nm=nm: e.tensor_scalar(
                    out=nm, in0=mx[:, 0:1], scalar1=-1.0, scalar2=None, op0=ALU.mult), [rk], [rk])
                p.op("act", lambda e, lg=lg, ex=ex, nm=nm: e.activation(out=ex, in_=lg, func=AF.Exp, bias=nm),
                     [rk], [rk])
                p.op("dve", lambda e, ex=ex, msk=msk, exm=exm: e.tensor_tensor(out=exm, in0=ex, in1=msk,
                                                                               op=ALU.mult), [rk], [rk])
                p.op("dve", lambda e, exm=exm, ssum=ssum: e.reduce_sum(out=ssum, in_=exm, axis=AX.X), [rk], [rk])
                p.op("dve", lambda e, ssum=ssum, rs=rs: e.reciprocal(out=rs, in_=ssum), [rk], [rk])
                p.op("dve", lambda e, exm=exm, rs=rs: e.tensor_scalar(
                    out=exm, in0=exm, scalar1=rs, scalar2=None, op0=ALU.mult), [rk], [rk])
                p.op("pe", lambda e, exm=exm: e.transpose(psY[1][0:32, 0:128], exm, identf[:]),
                     [rk, "identf"], ["psY1"])
                p.op("act", lambda e, ts_=ts_: e.activation(out=gTt[ts_][0:32, :], in_=psY[1][0:32, 0:128],
                                                            func=AF.Copy), ["psY1"], [("gTt", ts_)])
                dma(p, "sp", c.s["gateT"][:, t * 128:(t + 1) * 128], gTt[ts_][0:32, :],
                    [("gTt", ts_)], [("d_gateT", t)])
                X = rx[ts_]
                xk = ("rx", ts_)
                idxf, sv, ov, eq, tmp = X[:, 0:8], X[:, 8:40], X[:, 40:72], X[:, 72:104], X[:, 104:136]
                slotf, gkf = X[:, 136:140], X[:, 140:144]
                p.op("dve", lambda e, ts_=ts_, lg=lg, mx=mx: e.max_index(out=idxu[ts_][:], in_max=mx, in_values=lg),
                     [rk], [("idxu", ts_)])
                p.op("dve", lambda e, ts_=ts_, idxf=idxf: e.tensor_copy(idxf, idxu[ts_][:]),
                     [("idxu", ts_)], [xk])
                p.op("pe", lambda e, msk=msk: e.matmul(psY[2][:, 0:NEXP], triu[:], msk, start=True, stop=False),
                     [rk, "triu"], ["psY2"])
                p.op("pe", lambda e: e.matmul(psY[2][:, 0:NEXP], ones128[:], msum[:], start=False, stop=True),
                     ["msum", "ones128"], ["psY2"])
                p.op("dve", lambda e, ov=ov: e.tensor_scalar(out=ov, in0=psY[2][:, 0:NEXP], scalar1=float(CAP),
                                                             scalar2=1.0e6, op0=ALU.is_ge, op1=ALU.mult),
                     ["psY2"], [xk])
                p.op("dve", lambda e, sv=sv: e.tensor_tensor(out=sv, in0=psY[2][:, 0:NEXP], in1=iota_cap[:],
                                                             op=ALU.add), ["psY2", "iota_cap"], [xk])
                p.op("dve", lambda e, sv=sv, ov=ov: e.tensor_tensor(out=sv, in0=sv, in1=ov, op=ALU.add), [xk], [xk])
                p.op("pool", lambda e, msk=msk: e.tensor_tensor(out=msum[:], in0=msum[:], in1=msk, op=ALU.add),
                     [rk, "msum"], ["msum"])
                for k4 in range(4):
                    p.op("dve", lambda e, eq=eq, idxf=idxf, k4=k4: e.tensor_scalar(
                        out=eq, in0=iota_e[:], scalar1=idxf[:, k4:k4 + 1], scalar2=None, op0=ALU.is_equal),
                        [xk, "iota_e"], [xk])
                    p.op("dve", lambda e, eq=eq, sv=sv, tmp=tmp: e.tensor_tensor(out=tmp, in0=eq, in1=sv, op=ALU.mult),
                         [xk], [xk])
                    p.op("dve", lambda e, tmp=tmp, slotf=slotf, k4=k4: e.reduce_sum(
                        out=slotf[:, k4:k4 + 1], in_=tmp, axis=AX.X), [xk], [xk])
                    p.op("dve", lambda e, eq=eq, exm=exm, tmp=tmp: e.tensor_tensor(out=tmp, in0=eq, in1=exm,
                                                                                   op=ALU.mult), [xk, rk], [xk])
                    p.op("dve", lambda e, tmp=tmp, gkf=gkf, k4=k4: e.reduce_sum(
                        out=gkf[:, k4:k4 + 1], in_=tmp, axis=AX.X), [xk], [xk])
                p.op("dve", lambda e, ts_=ts_, slotf=slotf: e.tensor_copy(slotu[ts_][:], slotf),
                     [xk], [("slotu", ts_)])
                for k4 in range(4):
                    p.op("pool", lambda e, ts_=ts_, k4=k4: e.indirect_dma_start(
                        out=c.s["xg"][:, :], out_offset=bass.IndirectOffsetOnAxis(ap=slotu[ts_][:, k4:k4 + 1], axis=0),
                        in_=x1b[ts_][:], in_offset=None, bounds_check=c.breg, oob_is_err=False),
                        [("slotu", ts_), ("x1b", ts_)], [("d_xg", t, k4)], dma=True)
                dma(p, "sp", c.s["slots"][t * 128:(t + 1) * 128, :], slotu[ts_][:], [("slotu", ts_)], [("d_slots", t)])
                dma(p, "sp", c.s["gks"][t * 128:(t + 1) * 128, :], gkf, [xk], [("d_gks", t)])
        p.barrier()
        p.emit()


SIG_MAX = float(1.0 / (1.0 + np.exp(-1.702 * 7.0)))


def phase_moe(c, L):
    nc, p = c.nc, c.p
    with ExitStack() as ph:
        c.ph = ph
        wgu = [_sb(c, "wgu%d" % i, [128, 8, 2 * DEXP], BF16) for i in range(2)]
        wdn = [_sb(c, "wdn%d" % i, [128, 8, D], BF16) for i in range(2)]
        xT = _sb(c, "xTsb", [128, 8, 1024], BF16)
        gT = _sb(c, "gTsb", [128, 1024], F32)
        yacc = _sb(c, "yacc", [128, 8, D], F32)
        bgu = _sb(c, "bgu", [128, NEXP, 16], F32)
        bgs = _sb(c, "bgs", [128, NEXP, 8], F32)
        bdn = _sb(c, "bdn", [128, D], F32)
        identf = _sb(c, "identf", [128, 128], F32)
        sg = [_sb(c, "sg%d" % i, [128, 512], F32) for i in range(2)]
        gc = [_sb(c, "gc%d" % i, [128, 512], F32) for i in range(2)]
        uc = [_sb(c, "uc%d" % i, [128, 512], F32) for i in range(2)]
        t1 = [_sb(c, "t1%d" % i, [128, 512], F32) for i in range(2)]
        t2 = [_sb(c, "t2%d" % i, [128, 512], F32) for i in range(2)]
        aT = [_sb(c, "aT%d" % i, [128, 8, 512], BF16) for i in range(2)]
        yev = [_sb(c, "yev%d" % i, [128, 512], F32) for i in range(2)]
        psg = [_ps(c, "psg%d" % i, [128, 512], F32) for i in range(2)]
        psu = [_ps(c, "psu%d" % i, [128, 512], F32) for i in range(2)]
        psGb = _ps(c, "psGb", [128, 512], F32)
        psy = [_ps(c, "psy%d" % i, [128, 512], F32) for i in range(2)]

        dma(p, "sp", bgu[:], c.w["bgu_t"][L], [], ["bgu"])
        dma(p, "sp", bdn[0:32, :], c.w["bdn"][L], [], ["bdn"])
        dma(p, "sp", identf[:], c.w["identf"][:, :], [], ["identf"])
        p.op("dve", lambda e: e.tensor_scalar(out=bgs[:], in0=bgu[:, :, 0:8], scalar1=1.702, scalar2=None,
                                              op0=ALU.mult), ["bgu"], ["bgs"])
        x1T_v = c.s["x1T"].rearrange("(c p) t -> p c t", p=128)
        ym_v = c.s["ymoe"].rearrange("(t p) d -> p t d", p=128)
        cnt = 0
        ycnt = 0
        for sb in range(4):
            dma(p, "sp", xT[:], x1T_v[:, :, sb * 1024:(sb + 1) * 1024], [], ["xTsb"])
            dma(p, "sp", gT[0:32, :], c.s["gateT"][:, sb * 1024:(sb + 1) * 1024], [], ["gTsb"])
            for ex in range(NEXP):
                ws = ex % 2
                gu_v = c.w["w_gu"][L, ex].rearrange("(c p) n -> p c n", p=128)
                dn_v = c.w["w_dn"][L, ex].rearrange("(c p) n -> p c n", p=128)
                for ch in range(8):
                    dma(p, "pool", wgu[ws][:, ch, :], gu_v[:, ch, :], [], [("wgu", ws)])
                for ch in range(8):
                    dma(p, "pool", wdn[ws][:, ch, :], dn_v[:, ch, :], [], [("wdn", ws)])
                for tb in range(2):
                    as_ = (ex * 2 + tb) % 2
                    p.op("pe", lambda e, ex=ex, tb=tb: e.matmul(
                        psGb[:, :], identf[0:32, ex:ex + 1].to_broadcast([32, 128]),
                        gT[0:32, tb * 512:(tb + 1) * 512], start=True, stop=True),
                        ["identf", "gTsb"], ["psGb"])
                    for j in range(8):
                        k = cnt % 2
                        cnt += 1
                        mm_group(p, psg[k][:, :],
                                 [(wgu[ws][:, ch, j * 128:(j + 1) * 128], xT[:, ch, tb * 512:(tb + 1) * 512])
                                  for ch in range(8)], [("wgu", ws), "xTsb"], ["psg%d" % k])
                        mm_group(p, psu[k][:, :],
                                 [(wgu[ws][:, ch, DEXP + j * 128:DEXP + (j + 1) * 128],
                                   xT[:, ch, tb * 512:(tb + 1) * 512]) for ch in range(8)],
                                 [("wgu", ws), "xTsb"], ["psu%d" % k])
                        p.op("dve", lambda e, k=k, ex=ex, j=j: e.tensor_scalar(
                            out=gc[k][:], in0=psg[k][:, :], scalar1=bgu[:, ex, j:j + 1], scalar2=7.0,
                            op0=ALU.add, op1=ALU.min), ["psg%d" % k, "bgu"], [("gc", k)])
                        p.op("act", lambda e, k=k: e.activation(
                            out=sg[k][:], in_=gc[k][:], func=AF.Sigmoid, scale=1.702),
                            [("gc", k)], [("sg", k)])
                        p.op("dve", lambda e, k=k, ex=ex, j=j: e.tensor_scalar(
                            out=uc[k][:], in0=psu[k][:, :], scalar1=bgu[:, ex, 8 + j:9 + j], scalar2=7.0,
                            op0=ALU.add, op1=ALU.min), ["psu%d" % k, "bgu"], [("uc", k)])
                        p.op("dve", lambda e, k=k: e.tensor_scalar(
                            out=uc[k][:], in0=uc[k][:], scalar1=-7.0, scalar2=1.0,
                            op0=ALU.max, op1=ALU.add), [("uc", k)], [("uc", k)])
                        p.op("pool", lambda e, k=k: e.tensor_tensor(
                            out=t1[k][:], in0=sg[k][:], in1=gc[k][:], op=ALU.mult),
                            [("sg", k), ("gc", k)], [("t1", k)])
                        p.op("dve", lambda e, k=k: e.tensor_tensor(out=t2[k][:], in0=uc[k][:], in1=psGb[:, :],
                                                                   op=ALU.mult), [("uc", k), "psGb"], [("t2", k)])
                        p.op("pool", lambda e, k=k, j=j, as_=as_: e.tensor_tensor(
                            out=aT[as_][:, j, :], in0=t1[k][:], in1=t2[k][:], op=ALU.mult),
                            [("t1", k), ("t2", k)], [("aT", as_)])
                    for tt in range(4):
                        tile = tb * 4 + tt
                        for half in range(2):
                            yk = ycnt % 2
                            ycnt += 1
                            pairs = [(aT[as_][:, j, tt * 128:(tt + 1) * 128],
                                      wdn[ws][:, j, half * 512:(half + 1) * 512]) for j in range(8)]
                            rds = [("aT", as_), ("wdn", ws)]
                            if ex == 0:
                                pairs.append((gT[0:32, tile * 128:(tile + 1) * 128],
                                              bdn[0:32, half * 512:(half + 1) * 512]))
                                rds += ["gTsb", "bdn"]
                            mm_group(p, psy[yk][:, :], pairs, rds, ["psy%d" % yk])
                            ya = yacc[:, tile, half * 512:(half + 1) * 512]
                            if ex == 0:
                                p.op("act", lambda e, ya=ya, yk=yk: e.activation(out=ya, in_=psy[yk][:, :],
                                                                                 func=AF.Copy),
                                     ["psy%d" % yk], [("yacc", tile, half)])
                            else:
                                p.op("act", lambda e, yk=yk: e.activation(out=yev[yk][:], in_=psy[yk][:, :],
                                                                          func=AF.Copy),
                                     ["psy%d" % yk], [("yev", yk)])
                                p.op("pool", lambda e, ya=ya, yk=yk: e.tensor_tensor(out=ya, in0=ya, in1=yev[yk][:],
                                                                                     op=ALU.add),
                                     [("yev", yk), ("yacc", tile, half)], [("yacc", tile, half)])
            dma(p, "sp", ym_v[:, sb * 8:(sb + 1) * 8, :], yacc[:],
                [("yacc", t_, h_) for t_ in range(8) for h_ in range(2)], [("d_ymoe", sb)])
        p.barrier()
        p.emit()


def load_w_cast(c, dst_ap, src_ap, stg, skey, dkey, eng="pool"):
    p = c.p
    dma(p, "sp", stg, src_ap, [], [skey])
    if eng == "act":
        p.op("act", lambda e: e.activation(out=dst_ap, in_=stg, func=AF.Copy), [skey], [dkey])
    else:
        p.op(eng, lambda e: e.tensor_copy(dst_ap, stg), [skey], [dkey])


def phase_moe_sparse(c, L):
    nc, p = c.nc, c.p
    NT = CAP // 128
    groups = [(0, 512), (512, CAP)] if CAP > 512 else [(0, CAP)]
    with ExitStack() as ph:
        c.ph = ph
        wgu = [_sb(c, "wgu%d" % i, [128, 8, 2 * DEXP], BF16) for i in range(2)]
        wdn = [_sb(c, "wdn%d" % i, [128, 8, D], BF16) for i in range(2)]
        stg = [_sb(c, "stg%d" % i, [128, 2 * DEXP], F32) for i in range(3)]
        xgt = [_sb(c, "xgt%d" % i, [128, D], BF16) for i in range(NT)]
        xgT = [_sb(c, "xgT%d" % i, [128, 8, CAP], BF16) for i in range(2)]
        bgu = _sb(c, "bgu", [128, NEXP, 16], F32)
        bdr1 = _sb(c, "bdr", [128, D], F32)
        bdr = [bdr1, bdr1]
        bdb = [_sb(c, "bdb%d" % i, [128, D], BF16) for i in range(2)]
        ones1 = _sb(c, "ones1", [128, 128], BF16)
        identb = _sb(c, "identb", [128, 128], BF16)
        sg = [_sb(c, "sg%d" % i, [128, 512], F32) for i in range(2)]
        gc = [_sb(c, "gc%d" % i, [128, 512], F32) for i in range(2)]
        uc = [_sb(c, "uc%d" % i, [128, 512], F32) for i in range(2)]
        t1 = [_sb(c, "t1%d" % i, [128, 512], F32) for i in range(2)]
        aT = _sb(c, "aT", [128, 8, CAP], BF16)
        yout = [_sb(c, "yout%d" % i, [128, D], F32) for i in range(2)]
        psT2 = [_ps(c, "psT%d" % i, [128, 8, 128], BF16) for i in range(2)]
        psg = [_ps(c, "psg%d" % i, [128, 512], F32) for i in range(2)]
        psu = [_ps(c, "psu%d" % i, [128, 512], F32) for i in range(2)]
        psy = [_ps(c, "psy%d" % i, [128, 512], F32) for i in range(2)]
        dma(p, "sp", bgu[:], c.w["bgu_t"][L], [], ["bgu"])
        dma(p, "sp", identb[:], c.w["identb"][:, :], [], ["identb"])
        p.op("pool", lambda e: e.memset(ones1[:], 1.0), [], ["ones1"])
        st8 = {"scnt": 0, "cnt": 0, "ycnt": 0, "xcnt": 0}
        cast_rr = ("dve", "act", "pool")

        def wload_steps(ex):
            ws = ex % 2
            gu_v = c.w["w_gu"][L, ex].rearrange("(c p) n -> p c n", p=128)
            dn_v = c.w["w_dn"][L, ex].rearrange("(c p) n -> p c n", p=128)
            steps = []
            for ch in range(12):
                si = st8["scnt"] % 3
                eng = cast_rr[st8["scnt"] % 3]
                st8["scnt"] += 1
                if ch < 8:
                    src, stv, dstv, dkey = gu_v[:, ch, :], stg[si][:], wgu[ws][:, ch, :], ("wgu", ws)
                else:
                    c2 = ch - 8
                    src = dn_v[:, 2 * c2:2 * c2 + 2, :]
                    stv = stg[si][:].rearrange("p (c n) -> p c n", c=2)
                    dstv, dkey = wdn[ws][:, 2 * c2:2 * c2 + 2, :], ("wdn", ws)

                def f_dma(src=src, stv=stv, si=si):
                    dma(p, "sp", stv, src, [], [("stg", si)])

                def f_cast(stv=stv, dstv=dstv, si=si, dkey=dkey, eng=eng):
                    if eng == "act":
                        p.op("act", lambda e: e.activation(out=dstv, in_=stv, func=AF.Copy), [("stg", si)], [dkey])
                    else:
                        p.op(eng, lambda e: e.tensor_copy(dstv, stv), [("stg", si)], [dkey])
                steps.append((f_dma, f_cast))
            steps.append((lambda ws=ws, ex=ex: dma(p, "sp", bdr[ws][0:1, :], c.w["bdn"][L, ex:ex + 1, :], [],
                                                   ["bdr"]),
                          lambda ws=ws: p.op("pool", lambda e: e.tensor_copy(bdb[ws][0:1, :], bdr[ws][0:1, :]),
                                             ["bdr"], [("bdb", ws)])))
            return steps

        def xg_dma(ex):
            for st in range(NT):
                r0 = ex * CAP + st * 128
                dma(p, "sp", xgt[st][:], c.s["xg"][r0:r0 + 128, :], [], [("xgt", st)])

        def xg_load(ex):
            xb_ = ex % 2
            for st in range(NT):
                pt = st8["xcnt"] % 2
                st8["xcnt"] += 1
                tr_group(p, [(psT2[pt][:, ch, :], xgt[st][:, ch * 128:(ch + 1) * 128]) for ch in range(8)],
                         identb[:], [("xgt", st), "identb"], ["psT%d" % pt])
                eng = "act" if st % 2 == 0 else "dve"
                if eng == "act":
                    p.op("act", lambda e, st=st, xb_=xb_, pt=pt: e.activation(
                        out=xgT[xb_][:, :, st * 128:(st + 1) * 128], in_=psT2[pt][:], func=AF.Copy),
                        ["psT%d" % pt], [("xgT", xb_)])
                else:
                    p.op("dve", lambda e, st=st, xb_=xb_, pt=pt: e.tensor_copy(
                        xgT[xb_][:, :, st * 128:(st + 1) * 128], psT2[pt][:]), ["psT%d" % pt], [("xgT", xb_)])

        pend_cast = None
        for f_dma, f_cast in wload_steps(0):
            f_dma()
            if pend_cast is not None:
                pend_cast()
            pend_cast = f_cast
        pend_cast()
        xg_dma(0)
        xg_load(0)
        for ex in range(NEXP):
            ws = ex % 2
            xb_ = ex % 2
            nxt_steps = wload_steps(ex + 1) if ex + 1 < NEXP else []
            pend_cast = None
            if ex + 1 < NEXP:
                xg_dma(ex + 1)
            for (n0, n1) in groups:
                w = n1 - n0
                for j in range(8):
                    k = st8["cnt"] % 2
                    st8["cnt"] += 1
                    if nxt_steps:
                        f_dma, f_cast = nxt_steps.pop(0)
                        f_dma()
                        if pend_cast is not None:
                            pend_cast()
                        pend_cast = f_cast
                    mm_group(p, psg[k][:, 0:w], [(wgu[ws][:, ch, j * 128:(j + 1) * 128], xgT[xb_][:, ch, n0:n1])
                                                 for ch in range(8)], [("wgu", ws), ("xgT", xb_)], ["psg%d" % k])
                    mm_group(p, psu[k][:, 0:w], [(wgu[ws][:, ch, DEXP + j * 128:DEXP + (j + 1) * 128],
                                                  xgT[xb_][:, ch, n0:n1]) for ch in range(8)],
                             [("wgu", ws), ("xgT", xb_)], ["psu%d" % k])
                    p.op("dve", lambda e, k=k, ex=ex, j=j, w=w: e.tensor_scalar(
                        out=gc[k][:, 0:w], in0=psg[k][:, 0:w], scalar1=bgu[:, ex, j:j + 1], scalar2=7.0,
                        op0=ALU.add, op1=ALU.min), ["psg%d" % k, "bgu"], [("gc", k)])
                    p.op("act", lambda e, k=k, w=w: e.activation(out=sg[k][:, 0:w], in_=gc[k][:, 0:w],
                                                                 func=AF.Sigmoid, scale=1.702),
                         [("gc", k)], [("sg", k)])
                    p.op("dve", lambda e, k=k, ex=ex, j=j, w=w: e.tensor_scalar(
                        out=uc[k][:, 0:w], in0=psu[k][:, 0:w], scalar1=bgu[:, ex, 8 + j:9 + j], scalar2=7.0,
                        op0=ALU.add, op1=ALU.min), ["psu%d" % k, "bgu"], [("uc", k)])
                    p.op("dve", lambda e, k=k, w=w: e.tensor_scalar(
                        out=uc[k][:, 0:w], in0=uc[k][:, 0:w], scalar1=-7.0, scalar2=1.0,
                        op0=ALU.max, op1=ALU.add), [("uc", k)], [("uc", k)])
                    p.op("pool", lambda e, k=k, w=w: e.tensor_tensor(out=t1[k][:, 0:w], in0=sg[k][:, 0:w],
                                                                     in1=gc[k][:, 0:w], op=ALU.mult),
                         [("sg", k), ("gc", k)], [("t1", k)])
                    p.op("pool", lambda e, k=k, j=j, n0=n0, n1=n1, w=w: e.tensor_tensor(
                        out=aT[:, j, n0:n1], in0=t1[k][:, 0:w], in1=uc[k][:, 0:w], op=ALU.mult),
                        [("t1", k), ("uc", k)], ["aT"])
            while nxt_steps:
                f_dma, f_cast = nxt_steps.pop(0)
                f_dma()
                if pend_cast is not None:
                    pend_cast()
                pend_cast = f_cast
            if pend_cast is not None:
                pend_cast()
            if ex + 1 < NEXP:
                xg_load(ex + 1)
            for st in range(NT):
                ys = st8["ycnt"] % 2
                st8["ycnt"] += 1
                for half in range(2):
                    yk = (st8["ycnt"] + half) % 2
                    pairs = [(aT[:, j, st * 128:(st + 1) * 128], wdn[ws][:, j, half * 512:(half + 1) * 512])
                             for j in range(8)]
                    pairs.append((ones1[0:1, 0:128], bdb[ws][0:1, half * 512:(half + 1) * 512]))
                    mm_group(p, psy[yk][:, :], pairs, ["aT", ("wdn", ws), ("bdb", ws), "ones1"], ["psy%d" % yk])
                    p.op("act", lambda e, ys=ys, yk=yk, half=half: e.activation(
                        out=yout[ys][:, half * 512:(half + 1) * 512], in_=psy[yk][:, :], func=AF.Copy),
                        ["psy%d" % yk], [("yout", ys)])
                r0 = ex * CAP + st * 128
                dma(p, "sp", c.s["yg"][r0:r0 + 128, :], yout[ys][:], [("yout", ys)], [("d_yg", ex, st)])
        p.barrier()
        p.emit()


def phase_ln2(c, L, dst, sparse=True):
    nc, p = c.nc, c.p
    with ExitStack() as ph:
        c.ph = ph
        lng = _sb(c, "lng2", [128, D], F32)
        lnb = _sb(c, "lnb2", [128, D], F32)
        xa = [_sb(c, "xa%d" % i, [128, D], F32) for i in range(2)]
        ya = [_sb(c, "ya%d" % i, [128, D], F32) for i in range(2)]
        yk_ = [[_sb(c, "yk%d_%d" % (i, k), [128, D], F32) for k in range(4)] for i in range(2)]
        sl = [_sb(c, "sl%d" % i, [128, 4], I32) for i in range(2)]
        gk = [_sb(c, "gk%d" % i, [128, 4], F32) for i in range(2)]
        tsb = [_sb(c, "tsb%d" % i, [128, D], F32) for i in range(2)]
        out = [_sb(c, "out%d" % i, [128, D], F32) for i in range(2)]
        junk = _sb(c, "junk2", [128, D], F32)
        sm = [_sb(c, "sm%d" % i, [128, 8], F32) for i in range(2)]
        dma(p, "sp", lng[:], c.w["ln2g"][L], [], ["lngb"])
        dma(p, "sp", lnb[:], c.w["ln2b"][L], [], ["lngb"])
        for t in range(32):
            s = t % 2
            dma(p, "sp", xa[s][:], c.s["x1"][t * 128:(t + 1) * 128, :], [], [("xa", s)])
            if sparse:
                dma(p, "sp", sl[s][:], c.s["slots"][t * 128:(t + 1) * 128, :], [], [("sl", s)])
                dma(p, "sp", gk[s][:], c.s["gks"][t * 128:(t + 1) * 128, :], [], [("gk", s)])
                for k in range(4):
                    p.op("pool", lambda e, s=s, k=k: e.indirect_dma_start(
                        out=yk_[s][k][:], out_offset=None, in_=c.s["yg"][:, :],
                        in_offset=bass.IndirectOffsetOnAxis(ap=sl[s][:, k:k + 1], axis=0),
                        bounds_check=c.breg, oob_is_err=False), [("sl", s)], [("yk", s, k)], dma=True)
                p.op("dve", lambda e, s=s: e.tensor_scalar(out=ya[s][:], in0=yk_[s][0][:], scalar1=gk[s][:, 0:1],
                                                           scalar2=None, op0=ALU.mult),
                     [("yk", s, 0), ("gk", s)], [("ya", s)])
                for k in range(1, 4):
                    p.op("dve", lambda e, s=s, k=k: e.scalar_tensor_tensor(
                        out=ya[s][:], in0=yk_[s][k][:], scalar=gk[s][:, k:k + 1], in1=ya[s][:],
                        op0=ALU.mult, op1=ALU.add), [("yk", s, k), ("gk", s), ("ya", s)], [("ya", s)])
            else:
                dma(p, "sp", ya[s][:], c.s["ymoe"][t * 128:(t + 1) * 128, :], [], [("ya", s)])
            p.op("dve", lambda e, s=s: e.scalar_tensor_tensor(
                out=tsb[s][:], in0=xa[s][:], scalar=DN_ALPHA, in1=ya[s][:], op0=ALU.mult, op1=ALU.add),
                [("xa", s), ("ya", s)], [("tsb", s)])
            layer_norm_tile(c, tsb[s], ("tsb", s), junk, "junk", sm[s], ("sm", s), lng, lnb, "lngb",
                            out[s], ("out", s))
            dma(p, "sp", dst[t * 128:(t + 1) * 128, :], out[s][:], [("out", s)], [("d_out", t)])
        p.barrier()
        p.emit()


ALL_PHASES = ("p1", "na", "gqa", "mla", "merge", "moes", "ln2")
_NC_CACHE = {}


def kernel(**inputs):
    inp = {k: np.asarray(v) for k, v in inputs.items()}
    x = np.ascontiguousarray(inp["x"].astype(np.float32))
    nb = x.shape[0]
    if "nc" not in _NC_CACHE:
        _NC_CACHE["nc"] = build()
    consts = [prep_consts(hf) for hf in range(2)]
    wts = [prep_weights(inp, [0, 1], hf) for hf in range(2)]
    in_maps = []
    for cid in range(2 * nb):
        b, hf = cid // 2, cid % 2
        m = {}
        m.update(wts[hf])
        m.update(consts[hf])
        m.update(prep_acts(x[b], hf))
        in_maps.append(m)
    res = run_bass_kernel_spmd(_NC_CACHE["nc"], in_maps, core_ids=list(range(2 * nb)))
    return np.stack([np.concatenate([np.asarray(res.results[2 * b]["y"]),
                                     np.asarray(res.results[2 * b + 1]["y"])], 0)
                     for b in range(nb)], 0).astype(np.float32)
```

```python
from contextlib import ExitStack
import numpy as np
import ml_dtypes
import concourse.bass as bass
import concourse.mybir as mybir
from concourse.bass_utils import run_bass_kernel_spmd

F32 = mybir.dt.float32
BF16 = mybir.dt.bfloat16
I32 = mybir.dt.int32
U32 = mybir.dt.uint32
AF = mybir.ActivationFunctionType
ALU = mybir.AluOpType
AX = mybir.AxisListType

D = 1024
S = 8192
HALF = 4096
DIN = 5536
NEXP = 32
DEXP = 1024
LN_EPS = 1e-5
RMS_EPS = 1e-6
DN_ALPHA = 4.0 ** 0.25
NEG = -30000.0
NA_ROWS = 72

COMPUTE = ("pe", "act", "dve", "pool")


class Prog:
    def __init__(self, nc, stack, ring=8):
        self.nc = nc
        self.e = {"pe": nc.tensor, "act": nc.scalar, "dve": nc.vector,
                  "pool": nc.gpsimd, "sp": nc.sync}
        self.ops = []
        self.done = 0
        self.sem = {k: stack.enter_context(nc.semaphore("s_" + k)) for k in COMPUTE}
        self.ticket = {k: 0 for k in COMPUTE}
        self.ring = {q: [stack.enter_context(nc.semaphore("d_%s%d" % (q, i))) for i in range(ring)]
                     for q in ("sp", "pool")}
        self.ring_cnt = {q: [0] * ring for q in ("sp", "pool")}
        self.ring_last = {q: [None] * ring for q in ("sp", "pool")}
        self.ring_pos = {q: 0 for q in ("sp", "pool")}
        self.waited = {k: {} for k in self.e}
        self.last_w = {}
        self.readers = {}
        self.sig = {}
        self.eidx = {}
        self.ecount = {k: 0 for k in self.e}
        self.last_op = {k: None for k in self.e}
        self.dma_open = []

    def op(self, eng, fn, r=(), w=(), dma=False):
        self.ops.append((eng, fn, tuple(r), tuple(w), dma))

    def barrier(self):
        self.ops.append(("BAR", None, (), (), False))

    def _wait(self, eng, sem, val):
        cur = self.waited[eng].get(sem, 0)
        if cur < val:
            self.e[eng].wait_ge(sem, val)
            self.waited[eng][sem] = val

    def emit(self):
        ops = self.ops
        n = len(ops)
        start = self.done
        deps = {}
        needed = set()
        last_w, readers = self.last_w, self.readers
        eidx, ecount = self.eidx, self.ecount
        openg = {}
        for i in range(start, n):
            eng, fn, r, w, dma = ops[i]
            if eng == "BAR":
                continue
            eidx[i] = ecount[eng]
            ecount[eng] += 1
            openg[i] = (eng, dma)
            d = set()
            raw = set()
            for k in r:
                if k in last_w:
                    d.add(last_w[k])
                    raw.add(last_w[k])
                if isinstance(k, str) and k.startswith("ps"):
                    rd = readers.get(k)
                    if rd:
                        d.update(rd[0].values())
                        d.update(rd[1])
            for k in w:
                if k in last_w:
                    d.add(last_w[k])
                rd = readers.get(k)
                if rd:
                    d.update(rd[0].values())
                    d.update(rd[1])
            for k in r:
                rd = readers.setdefault(k, ({}, []))
                if dma:
                    rd[1].append(i)
                else:
                    rd[0][eng] = i
            for k in w:
                last_w[k] = i
                readers[k] = ({}, [])
            d.discard(i)
            keep = set()
            for j in d:
                if j < start and j not in self.sig and j not in openg:
                    continue
                jeng, jdma = self._opinfo(j, openg)
                if (not dma) and (not jdma) and jeng == eng:
                    if eng == "pe":
                        continue
                    if j in raw and eidx[i] - eidx[j] <= 2:
                        keep.add(j)
                    continue
                keep.add(j)
            deps[i] = keep
            needed.update(keep)
        self._openg_all = getattr(self, "_openg_all", {})
        self._openg_all.update(openg)
        for i in range(start, n):
            eng, fn, r, w, dma = ops[i]
            if eng == "BAR":
                self._emit_barrier()
                continue
            if dma:
                q = eng
                pos = self.ring_pos[q]
                self.ring_pos[q] = (pos + 1) % len(self.ring[q])
                sem = self.ring[q][pos]
                prev = self.ring_last[q][pos]
                if prev is not None:
                    self._wait(eng, sem, prev)
            for j in sorted(deps[i]):
                if j in self.sig:
                    s, v = self.sig[j]
                    self._wait(eng, s, v)
            ins = fn(self.e[eng])
            if dma:
                self.ring_cnt[q][pos] += 16
                val = self.ring_cnt[q][pos]
                ins.then_inc(sem, 16)
                self.ring_last[q][pos] = val
                self.sig[i] = (sem, val)
                self.dma_open.append(i)
            else:
                self.last_op[eng] = i
                if i in needed:
                    self.ticket[eng] += 1
                    ins.then_inc(self.sem[eng], 1)
                    self.sig[i] = (self.sem[eng], self.ticket[eng])
        self.done = n

    def _opinfo(self, j, openg):
        if j in openg:
            return openg[j]
        return self._openg_all[j]

    def _emit_barrier(self):
        marks = []
        for eng in COMPUTE:
            self.ticket[eng] += 1
            self.e[eng].drain().then_inc(self.sem[eng], 1)
            marks.append((self.sem[eng], self.ticket[eng]))
        dmas = [self.sig[i] for i in self.dma_open]
        self.dma_open = []
        for eng in self.e:
            for s, v in marks:
                self._wait(eng, s, v)
            for s, v in dmas:
                self._wait(eng, s, v)
        self.last_w.clear()
        self.readers.clear()


class Ctx:
    pass


_UID = [0]


def _sb(c, name, shape, dt):
    _UID[0] += 1
    return c.ph.enter_context(c.nc.sbuf_tensor("sb%d_%s" % (_UID[0], name), list(shape), dt))


def _ps(c, name, shape, dt):
    _UID[0] += 1
    return c.ph.enter_context(c.nc.psum_tensor("pp%d_%s" % (_UID[0], name), list(shape), dt))


def mm_group(p, out_ap, pairs, r, w):
    n = len(pairs)

    def fn(e):
        ins = None
        for i, (l, rr) in enumerate(pairs):
            ins = e.matmul(out_ap, l, rr, start=(i == 0), stop=(i == n - 1))
        return ins
    p.op("pe", fn, r, w)


def tr_group(p, outs_ins, ident, r, w):
    def fn(e):
        ins = None
        for o, i_ in outs_ins:
            ins = e.transpose(o, i_, ident)
        return ins
    p.op("pe", fn, r, w)


def dma(p, q, out_ap, in_ap, r, w):
    p.op(q, lambda e: e.dma_start(out=out_ap, in_=in_ap), r, w, dma=True)


def load_cast_cols(p, dst, src, ncols, r, w, step=2048):
    for c0 in range(0, ncols, step):
        c1 = min(ncols, c0 + step)
        dma(p, "pool", dst[:, c0:c1], src[:, c0:c1], r, w)


def phase1(c, L):
    nc, p = c.nc, c.p
    with ExitStack() as ph:
        c.ph = ph
        winb = _sb(c, "winb", [128, 8, DIN], BF16)
        wuqb = _sb(c, "wuqb", [128, 3, 384], BF16)
        wukvb = _sb(c, "wukvb", [128, 2, 512], BF16)
        identb = _sb(c, "identb", [128, 128], BF16)
        gq = _sb(c, "gq", [128, 384], F32)
        gk = _sb(c, "gk", [128, 128], F32)
        mq = _sb(c, "mq", [128, 384], F32)
        mkv = _sb(c, "mkv", [128, 256], F32)
        xf = [_sb(c, "xf%d" % i, [128, D], F32) for i in range(2)]
        xb = [_sb(c, "xb%d" % i, [128, D], BF16) for i in range(2)]
        xT = [_sb(c, "xT%d" % i, [128, 8, 512], BF16) for i in range(2)]
        rp = [_sb(c, "rp%d" % i, [128, 192], F32) for i in range(2)]
        wk = [_sb(c, "wk%d" % i, [128, 384], F32) for i in range(6)]
        sm = [_sb(c, "sm%d" % i, [128, 8], F32) for i in range(4)]
        ob = [_sb(c, "ob%d" % i, [128, 384], BF16) for i in range(8)]
        tT = [_sb(c, "tT%d" % i, [128, 512], BF16) for i in range(2)]
        blk = {k: [_sb(c, "blk_%s%d" % (k, i), [128, 6 * 512], BF16) for i in range(2)]
               for k in ("a", "b", "c", "d")}
        vna = [_sb(c, "vna%d" % i, [128, 6, 65], BF16) for i in range(2)]
        vg = [_sb(c, "vg%d" % i, [128, 2, 65], BF16) for i in range(2)]
        vm = [_sb(c, "vm%d" % i, [128, 4, 65], BF16) for i in range(2)]
        gout = [_sb(c, "gout%d" % i, [128, 512], BF16) for i in range(3)]
        psT = _ps(c, "psT", [128, 8, 128], BF16)
        psR = _ps(c, "psR", [128, 8, 128], BF16)
        psM = [_ps(c, "psM%d" % i, [128, 512], F32) for i in range(4)]
        psG = [_ps(c, "psG%d" % i, [128, 512], F32) for i in range(2)]

        w_in = c.w["w_in"][L]
        w_in_v = w_in.rearrange("(c p) n -> p c n", p=128)
        NSTG = 2
        stgw = [_sb(c, "stgw%d" % i, [128, 692], F32) for i in range(NSTG)]
        wi = 0
        for ch in range(8):
            for pc in range(8):
                c0 = pc * 692
                si = wi % NSTG
                dma(p, "sp", stgw[si][:], w_in_v[:, ch, c0:c0 + 692], [], [("stgw", si)])
                if wi % 2 == 0:
                    p.op("dve", lambda e, si=si, ch=ch, c0=c0: e.tensor_copy(winb[:, ch, c0:c0 + 692], stgw[si][:]),
                         [("stgw", si)], ["winb"])
                else:
                    p.op("act", lambda e, si=si, ch=ch, c0=c0: e.activation(out=winb[:, ch, c0:c0 + 692],
                                                                           in_=stgw[si][:], func=AF.Copy),
                         [("stgw", si)], ["winb"])
                wi += 1
        wuq_v = c.w["w_uq"][L].rearrange("(c p) n -> p c n", p=128)
        for ch in range(3):
            load_cast_cols(p, wuqb[:, ch, :], wuq_v[:, ch, :], 384, [], ["wuqb"])
        wukv_v = c.w["w_ukv"][L].rearrange("(c p) n -> p c n", p=128)
        for ch in range(2):
            load_cast_cols(p, wukvb[:, ch, :], wukv_v[:, ch, :], 512, [], ["wukvb"])
        dma(p, "sp", identb[:], c.w["identb"][:, :], [], ["identb"])
        dma(p, "sp", gq[:], c.w["gq_rep"][L], [], ["gq"])
        dma(p, "sp", gk[:], c.w["gk_rep"][L], [], ["gk"])
        dma(p, "sp", mq[:], c.w["mq_rep"][L], [], ["mq"])
        dma(p, "sp", mkv[:], c.w["mkv_rep"][L], [], ["mkv"])
        for i in range(2):
            p.op("pool", lambda e, t=vna[i]: e.memset(t[:], 1.0), [], [("vna", i)])
            p.op("pool", lambda e, t=vg[i]: e.memset(t[:], 1.0), [], [("vg", i)])
            p.op("pool", lambda e, t=vm[i]: e.memset(t[:], 1.0), [], [("vm", i)])

        cnt = {"tile": 0, "psM": 0, "psG": 0, "wk": 0, "sm": 0, "ob": 0, "tT": 0, "gout": 0}
        pending = []

        def st(out_ap, in_ap, r, w):
            dma(p, "sp", out_ap, in_ap, r, w)

        def flush():
            for o_, i_, r_, w_ in pending:
                dma(p, "sp", o_, i_, r_, w_)
            del pending[:]

        def nxt(k, n):
            v = cnt[k] % n
            cnt[k] += 1
            return v

        def load_tile(src_ap, rope_ap, to_xT=None, mask_col=None, defer_tr=False):
            s = nxt("tile", 2)
            dma(p, "sp", xf[s][:], src_ap, [], [("xf", s)])
            if rope_ap is not None:
                dma(p, "sp", rp[s][:], rope_ap, [], [("rp", s)])
            if mask_col is not None:
                p.op("dve", lambda e: e.tensor_scalar(out=xf[s][:], in0=xf[s][:],
                                                      scalar1=c.hm[:, mask_col:mask_col + 1], scalar2=None,
                                                      op0=ALU.mult), [("xf", s), "hm"], [("xf", s)])
            p.op("dve", lambda e: e.tensor_copy(xb[s][:], xf[s][:]), [("xf", s)], [("xb", s)])
            if to_xT is None:
                dst, key = tT_full[s][:], ("tTf", s)
            else:
                dst, key = to_xT

            def fin():
                tr_group(p, [(psT[:, ch, :], xb[s][:, ch * 128:(ch + 1) * 128]) for ch in range(8)],
                         identb[:], [("xb", s), "identb"], ["psT"])
                p.op("act", lambda e: e.activation(out=dst, in_=psT[:], func=AF.Copy), ["psT"], [key])
            if defer_tr:
                return dst, key, rp[s], ("rp", s), fin
            fin()
            return dst, key, rp[s], ("rp", s)

        tT_full = [_sb(c, "tTf%d" % i, [128, 8, 128], BF16) for i in range(2)]

        def tok_mm(xTa, xTkey, c0, c1):
            b = nxt("psM", 4)
            mm_group(p, psM[b][:, 0:c1 - c0],
                     [(xTa[:, ch, :], winb[:, ch, c0:c1]) for ch in range(8)],
                     [xTkey, "winb"], ["psM%d" % b])
            return psM[b], "psM%d" % b

        def transp_out(src_bf, src_key, nh, dh, dst_ap, dst_key):
            tr_group(p, [(psR[0:dh, h, :], src_bf[:, h * dh:(h + 1) * dh]) for h in range(nh)],
                     identb[:], [src_key, "identb"], ["psR"])
            p.op("dve", lambda e: e.tensor_copy(dst_ap, psR[0:dh, 0:nh, :]), ["psR"], [dst_key])

        def rms_rope(ps_ap, pskey, nh, dh, g_ap, gkey, rope_t, rpkey, coff, out_bf, outkey):
            n = nh * dh
            hd = dh // 2
            a = nxt("wk", 6); b2 = nxt("wk", 6); c2 = nxt("wk", 6); d2 = nxt("wk", 6); e2 = nxt("wk", 6)
            s1 = nxt("sm", 4)
            A, B, C, Dd, E = wk[a], wk[b2], wk[c2], wk[d2], wk[e2]
            p.op("act", lambda e: e.activation(out=A[:, 0:n], in_=ps_ap, func=AF.Square),
                 [pskey], [("wk", a)])
            p.op("dve", lambda e: e.tensor_reduce(
                out=sm[s1][:, 0:nh], in_=A[:, 0:n].rearrange("p (h d) -> p h d", h=nh),
                axis=AX.X, op=ALU.add), [("wk", a)], [("sm", s1)])
            p.op("act", lambda e: e.activation(
                out=sm[s1][:, 0:nh], in_=sm[s1][:, 0:nh], func=AF.Sqrt, scale=1.0 / dh, bias=c.eps_rms[:, 0:1]),
                [("sm", s1)], [("sm", s1)])
            p.op("dve", lambda e: e.reciprocal(out=sm[s1][:, 0:nh], in_=sm[s1][:, 0:nh]),
                 [("sm", s1)], [("sm", s1)])
            p.op("dve", lambda e: e.tensor_tensor(
                out=B[:, 0:n].rearrange("p (h d) -> p h d", h=nh),
                in0=ps_ap.rearrange("p (h d) -> p h d", h=nh),
                in1=sm[s1][:, 0:nh].unsqueeze(2).to_broadcast([128, nh, dh]),
                op=ALU.mult), [pskey, ("sm", s1)], [("wk", b2)])
            p.op("dve", lambda e: e.tensor_tensor(out=C[:, 0:n], in0=B[:, 0:n], in1=g_ap, op=ALU.mult),
                 [("wk", b2), gkey], [("wk", c2)])
            rope(C, ("wk", c2), nh, dh, rope_t, rpkey, coff, Dd, ("wk", d2), E, ("wk", e2),
                 out_bf[:, 0:n].rearrange("p (h d) -> p h d", h=nh), outkey)

        def rope(X, xkey, nh, dh, rope_t, rpkey, coff, Dd, dkey, E, ekey, out3, outkey, xview=None):
            n = nh * dh
            hd = dh // 2
            x3 = xview if xview is not None else X[:, 0:n].rearrange("p (h d) -> p h d", h=nh)
            cs = rope_t[:, coff:coff + dh].unsqueeze(1).to_broadcast([128, nh, dh])
            sslo = rope_t[:, coff + dh:coff + dh + hd].unsqueeze(1).to_broadcast([128, nh, hd])
            sshi = rope_t[:, coff + dh + hd:coff + 2 * dh].unsqueeze(1).to_broadcast([128, nh, hd])
            d3 = Dd[:, 0:n].rearrange("p (h d) -> p h d", h=nh)
            e3 = E[:, 0:n].rearrange("p (h d) -> p h d", h=nh)
            p.op("dve", lambda e: e.tensor_tensor(out=d3, in0=x3, in1=cs, op=ALU.mult),
                 [xkey, rpkey], [dkey])
            p.op("dve", lambda e: e.tensor_tensor(out=e3[:, :, 0:hd], in0=x3[:, :, hd:dh], in1=sslo,
                                                  op=ALU.mult), [xkey, rpkey], [ekey])
            p.op("dve", lambda e: e.tensor_tensor(out=e3[:, :, hd:dh], in0=x3[:, :, 0:hd], in1=sshi,
                                                  op=ALU.mult), [xkey, rpkey, ekey], [ekey])
            p.op("pool", lambda e: e.tensor_tensor(out=out3, in0=d3, in1=e3, op=ALU.add),
                 [dkey, ekey], [outkey])

        def rms_full(ps_ap, pskey, n, g_ap, gkey, out_bf, outkey):
            a = nxt("wk", 6)
            s1 = nxt("sm", 4)
            p.op("act", lambda e: e.activation(out=wk[a][:, 0:n], in_=ps_ap, func=AF.Square,
                                               accum_out=sm[s1][:, 0:1]),
                 [pskey], [("wk", a), ("sm", s1)])
            p.op("act", lambda e: e.activation(
                out=sm[s1][:, 1:2], in_=sm[s1][:, 0:1], func=AF.Sqrt, scale=1.0 / n, bias=c.eps_rms[:, 0:1]),
                [("sm", s1)], [("sm", s1)])
            p.op("dve", lambda e: e.reciprocal(out=sm[s1][:, 2:3], in_=sm[s1][:, 1:2]),
                 [("sm", s1)], [("sm", s1)])
            p.op("dve", lambda e: e.scalar_tensor_tensor(
                out=out_bf, in0=ps_ap, scalar=sm[s1][:, 2:3], in1=g_ap,
                op0=ALU.mult, op1=ALU.mult), [pskey, ("sm", s1), gkey], [outkey])

        def na_kv(xTa, xTkey, tok_off, sub, bs, vs=None):
            ps, pk = tok_mm(xTa, xTkey, 384, 768)
            o = nxt("ob", 8)
            p.op("act", lambda e, ps=ps, o=o: e.activation(out=ob[o][:, 0:384], in_=ps[:, 0:384],
                                                           func=AF.Copy), [pk], [("ob", o)])
            kb = blk["b"][bs][0:64, :].rearrange("p (h t) -> p h t", h=6)
            transp_out(ob[o], ("ob", o), 6, 64, kb[:, :, sub * 128:(sub + 1) * 128], ("blk_b", bs))
            ps, pk = tok_mm(xTa, xTkey, 768, 1152)
            s = (cnt["tile"] - 1) % 2 if vs is None else vs
            p.op("act", lambda e, ps=ps, s=s: e.activation(
                out=vna[s][:, :, 0:64], in_=ps[:, 0:384].rearrange("p (h d) -> p h d", h=6),
                func=AF.Copy), [pk], [("vna", s)])
            st(c.s["v_na"][tok_off:tok_off + 128, :], vna[s][:].rearrange("p h d -> p (h d)"),
                [("vna", s)], [("d_v_na", tok_off)])

        def flush_blk(name, bs, dh, nh, dst, t0, nt):
            src = blk[name][bs][0:dh, 0:nh * 512].rearrange("p (h t) -> p h t", h=nh)[:, :, 0:nt]
            st(dst[:, :, t0:t0 + nt].rearrange("h d t -> d h t"), src,
               [("blk_" + name, bs)], [("d_" + name, t0)])

        def run_interleaved(gens):
            active = list(gens)
            while active:
                for g_ in list(active):
                    try:
                        next(g_)
                    except StopIteration:
                        active.remove(g_)

        def own_tile(bi, sub):
            bs = bi % 2
            t = bi * 4 + sub
            xTa, xTkey, rpt, rpk = load_tile(
                c.q_src[t * 128:(t + 1) * 128, :], c.rope_q[t * 128:(t + 1) * 128, :],
                to_xT=(xT[bs][:, :, sub * 128:(sub + 1) * 128], ("xT", bs)))
            vs = (cnt["tile"] - 1) % 2
            ps, pk = tok_mm(xTa, xTkey, 1152, 1536)
            o = nxt("ob", 8)
            rms_rope(ps[:, 0:384], pk, 6, 64, gq[:], "gq", rpt, rpk, 0, ob[o], ("ob", o))
            yield
            ps, pk = tok_mm(xTa, xTkey, 0, 384)
            oq = nxt("ob", 8)
            p.op("act", lambda e, oq=oq, ps=ps: e.activation(out=ob[oq][:, 0:384], in_=ps[:, 0:384],
                                                             func=AF.Copy), [pk], [("ob", oq)])
            qa = blk["a"][bs][0:64, :].rearrange("p (h t) -> p h t", h=6)
            transp_out(ob[oq], ("ob", oq), 6, 64, qa[:, :, sub * 128:(sub + 1) * 128], ("blk_a", bs))
            qg = blk["c"][bs][0:64, :].rearrange("p (h t) -> p h t", h=6)
            transp_out(ob[o], ("ob", o), 6, 64, qg[:, :, sub * 128:(sub + 1) * 128], ("blk_c", bs))
            yield
            ps, pk = tok_mm(xTa, xTkey, 1792, 2176)
            o = nxt("ob", 8)
            rms_full(ps[:, 0:384], pk, 384, mq[:], "mq", ob[o][:, 0:384], ("ob", o))
            yield
            na_kv(xTa, xTkey, 256 + t * 128, sub, bs, vs)
            tt = nxt("tT", 2)
            tr_group(p, [(psR[:, ch, :], ob[o][:, ch * 128:(ch + 1) * 128]) for ch in range(3)],
                     identb[:], [("ob", o), "identb"], ["psR"])
            p.op("act", lambda e, tt=tt: e.activation(
                out=tT[tt][:, 0:384].rearrange("p (c t) -> p c t", c=3), in_=psR[:, 0:3, :],
                func=AF.Copy), ["psR"], [("tT", tt)])
            g = nxt("psG", 2)
            tT3 = tT[tt][:, 0:384].rearrange("p (c t) -> p c t", c=3)
            mm_group(p, psG[g][:, 0:384], [(tT3[:, ch, :], wuqb[:, ch, :]) for ch in range(3)],
                     [("tT", tt), "wuqb"], ["psG%d" % g])
            o2 = nxt("ob", 8)
            qc3 = psG[g][:, 0:384].rearrange("p (h d) -> p h d", h=4)
            out3 = ob[o2][:, 0:384].rearrange("p (h d) -> p h d", h=4)
            p.op("act", lambda e, qc3=qc3, out3=out3: e.activation(
                out=out3[:, :, 0:64], in_=qc3[:, :, 0:64], func=AF.Copy),
                ["psG%d" % g], [("ob", o2)])
            a = nxt("wk", 6); d2 = nxt("wk", 6); e2 = nxt("wk", 6)
            xr3 = wk[a][:, 0:128].rearrange("p (h d) -> p h d", h=4)
            p.op("act", lambda e, qc3=qc3, xr3=xr3: e.activation(out=xr3, in_=qc3[:, :, 64:96],
                                                                 func=AF.Copy),
                 ["psG%d" % g], [("wk", a)])
            rope(wk[a], ("wk", a), 4, 32, rpt, rpk, 128, wk[d2], ("wk", d2), wk[e2], ("wk", e2),
                 out3[:, :, 64:96], ("ob", o2))
            yield
            qm = blk["d"][bs][0:96, 0:2048].rearrange("p (h t) -> p h t", h=4)
            transp_out(ob[o2], ("ob", o2), 4, 96, qm[:, :, sub * 128:(sub + 1) * 128], ("blk_d", bs))

        for bi in range(8):
            bs = bi % 2
            run_interleaved([own_tile(bi, 0), own_tile(bi, 1)])
            run_interleaved([own_tile(bi, 2), own_tile(bi, 3)])
            flush_blk("a", bs, 64, 6, c.s["qT_na"], bi * 512, 512)
            flush_blk("b", bs, 64, 6, c.s["kT_na"], 256 + bi * 512, 512)
            flush_blk("c", bs, 64, 6, c.s["qT_gqa"], bi * 512, 512)
            flush_blk("d", bs, 96, 4, c.s["qT_mla"], bi * 512, 512)
            flush()
            for cc in range(24):
                g = nxt("psG", 2)
                c0 = 2464 + cc * 128
                mm_group(p, psG[g][:, :], [(winb[:, ch, c0:c0 + 128], xT[bs][:, ch, :]) for ch in range(8)],
                         [("xT", bs), "winb"], ["psG%d" % g])
                go = nxt("gout", 3)
                p.op("act", lambda e, g=g, go=go: e.activation(out=gout[go][:], in_=psG[g][:, :],
                                                               func=AF.Sigmoid),
                     ["psG%d" % g], [("gout", go)])
                dma(p, "sp", c.s["gT"][cc * 128:(cc + 1) * 128, bi * 512:(bi + 1) * 512], gout[go][:],
                    [("gout", go)], [("d_gT", cc, bi)])

        for hi in range(4):
            hsrc = (c.c_src[HALF - 256 + hi * 128:HALF - 256 + (hi + 1) * 128, :] if hi < 2 else
                    c.c_src[(hi - 2) * 128:(hi - 1) * 128, :])
            xTa, xTkey, rpt, rpk = load_tile(hsrc, None, mask_col=c.hm_cols[0 if hi < 2 else 1])
            flush()
            tok_off = hi * 128 if hi < 2 else 4352 + (hi - 2) * 128
            na_kv(xTa, xTkey, tok_off, hi % 2, 0)
            if hi % 2 == 1:
                flush_blk("b", 0, 64, 6, c.s["kT_na"], 0 if hi == 1 else 4352, 256)

        def full_tile(bi, sub):
            bs = bi % 2
            t = bi * 4 + sub
            xTa, xTkey, rpt, rpk = load_tile(
                c.full_src[t * 128:(t + 1) * 128, :], c.rope_full[t * 128:(t + 1) * 128, :])
            s = (cnt["tile"] - 1) % 2
            ps, pk = tok_mm(xTa, xTkey, 1536, 1792)
            o = nxt("ob", 8)
            rms_rope(ps[:, 0:128], pk, 2, 64, gk[:], "gk", rpt, rpk, 0, ob[o], ("ob", o))
            p.op("act", lambda e, ps=ps, s=s: e.activation(
                out=vg[s][:, :, 0:64], in_=ps[:, 128:256].rearrange("p (h d) -> p h d", h=2),
                func=AF.Copy), [pk], [("vg", s)])
            st(c.s["v_gqa"][t * 128:(t + 1) * 128, :], vg[s][:].rearrange("p h d -> p (h d)"),
               [("vg", s)], [("d_v_gqa", t)])
            yield
            ps, pk = tok_mm(xTa, xTkey, 2176, 2464)
            o3 = nxt("ob", 8)
            rms_full(ps[:, 0:256], pk, 256, mkv[:], "mkv", ob[o3][:, 0:256], ("ob", o3))
            a = nxt("wk", 6)
            p.op("act", lambda e, ps=ps, a=a: e.activation(out=wk[a][:, 0:32], in_=ps[:, 256:288],
                                                           func=AF.Copy), [pk], [("wk", a)])
            d2 = nxt("wk", 6); e2 = nxt("wk", 6); f2 = nxt("wk", 6)
            kr3 = wk[f2][:, 0:32].rearrange("p (h d) -> p h d", h=1)
            rope(wk[a], ("wk", a), 1, 32, rpt, rpk, 128, wk[d2], ("wk", d2), wk[e2], ("wk", e2),
                 kr3, ("wk", f2))
            o2 = nxt("ob", 8)
            out3 = ob[o2][:, 0:384].rearrange("p (h d) -> p h d", h=4)
            p.op("pool", lambda e, out3=out3, f2=f2: e.tensor_copy(
                out3[:, :, 64:96], wk[f2][:, 0:32].unsqueeze(1).to_broadcast([128, 4, 32])),
                [("wk", f2)], [("ob", o2)])
            yield
            kg = blk["a"][bs][0:64, 0:1024].rearrange("p (h t) -> p h t", h=2)
            transp_out(ob[o], ("ob", o), 2, 64, kg[:, :, sub * 128:(sub + 1) * 128], ("blk_a", bs))
            tt = nxt("tT", 2)
            tr_group(p, [(psR[:, ch, :], ob[o3][:, ch * 128:(ch + 1) * 128]) for ch in range(2)],
                     identb[:], [("ob", o3), "identb"], ["psR"])
            tT2 = tT[tt][:, 0:256].rearrange("p (c t) -> p c t", c=2)
            p.op("act", lambda e, tT2=tT2: e.activation(out=tT2, in_=psR[:, 0:2, :], func=AF.Copy),
                 ["psR"], [("tT", tt)])
            g = nxt("psG", 2)
            mm_group(p, psG[g][:, :], [(tT2[:, ch, :], wukvb[:, ch, :]) for ch in range(2)],
                     [("tT", tt), "wukvb"], ["psG%d" % g])
            kv3 = psG[g][:, :].rearrange("p (h d) -> p h d", h=4)
            p.op("act", lambda e, kv3=kv3, out3=out3: e.activation(
                out=out3[:, :, 0:64], in_=kv3[:, :, 0:64], func=AF.Copy),
                ["psG%d" % g, ("ob", o2)], [("ob", o2)])
            p.op("dve", lambda e, kv3=kv3, s=s: e.tensor_copy(vm[s][:, :, 0:64], kv3[:, :, 64:128]),
                 ["psG%d" % g], [("vm", s)])
            st(c.s["v_mla"][t * 128:(t + 1) * 128, :], vm[s][:].rearrange("p h d -> p (h d)"),
               [("vm", s)], [("d_v_mla", t)])
            yield
            km = blk["d"][bs][0:96, 0:2048].rearrange("p (h t) -> p h t", h=4)
            transp_out(ob[o2], ("ob", o2), 4, 96, km[:, :, sub * 128:(sub + 1) * 128], ("blk_d", bs))

        flush()
        for bi in range(16 if c.do_full else 0):
            bs = bi % 2
            run_interleaved([full_tile(bi, 0), full_tile(bi, 1)])
            run_interleaved([full_tile(bi, 2), full_tile(bi, 3)])
            flush_blk("a", bs, 64, 2, c.s["kT_gqa"], bi * 512, 512)
            flush_blk("d", bs, 96, 4, c.s["kT_mla"], bi * 512, 512)
        flush()
        p.barrier()
        p.emit()


W_SHAPES = {
    "w_in": ([D, DIN], F32), "w_uq": ([384, 384], F32), "w_ukv": ([256, 512], F32),
    "w_ba": ([384, D], F32), "w_bb": ([384, D], F32), "w_bc": ([256, D], F32),
    "w_out": ([D, D], F32), "w_router": ([D, NEXP], F32),
    "w_gu": ([NEXP, D, 2 * DEXP], F32), "w_dn": ([NEXP, DEXP, D], F32),
    "gq_rep": ([128, 384], F32), "gk_rep": ([128, 128], F32),
    "mq_rep": ([128, 384], F32), "mkv_rep": ([128, 256], F32),
    "ln1g": ([128, D], F32), "ln1b": ([128, D], F32), "ln2g": ([128, D], F32), "ln2b": ([128, D], F32),
    "brouter": ([128, NEXP], F32), "bgu_t": ([128, NEXP, 16], F32), "bdn": ([NEXP, D], F32),
    "na_bint": ([64, 6, 8, 64], F32), "na_bbnd": ([7, 64, 6, 12, 64], F32),
    "na_bbnd_o": ([7, 64, 6, 12, 64], F32),
}
C_SHAPES = {
    "rope_loc": ([S, 192], F32), "hmask": ([128, 4], F32),
    "identb": ([128, 128], BF16), "identf": ([128, 128], F32),
    "triu": ([128, 128], F32), "ones128": ([128, 128], F32),
    "iota_e": ([128, NEXP], F32), "iota_cap": ([128, NEXP], F32),
}
CAP = 768
NSLOT = NEXP * CAP
S_SHAPES = {
    "qT_na": ([6, 64, HALF], BF16), "kT_na": ([6, 64, NA_ROWS * 64], BF16),
    "v_na": ([NA_ROWS * 64, 390], BF16),
    "qT_gqa": ([6, 64, HALF], BF16), "kT_gqa": ([2, 64, S], BF16), "v_gqa": ([S, 130], BF16),
    "qT_mla": ([4, 96, HALF], BF16), "kT_mla": ([4, 96, S], BF16), "v_mla": ([S, 260], BF16),
    "gT": ([3 * D, HALF], BF16),
    "oT": ([16, 64, HALF], BF16),
    "x1": ([HALF, D], F32),
    "x1T": ([D, HALF], BF16), "gateT": ([NEXP, HALF], F32), "ymoe": ([HALF, D], F32),
    "xg": ([NSLOT, D], BF16), "yg": ([NSLOT, D], F32),
    "slots": ([HALF, 4], I32), "gks": ([HALF, 4], F32),
}


def build(taps=(), passes=("A", "B", "C"), phases=None, dst_override=None):
    nc = bass.Bass("TRN2", target_bir_lowering=False)
    phases = phases or ALL_PHASES
    c = Ctx()
    c.nc = nc
    c.w = {}
    for k, (shp, dt) in W_SHAPES.items():
        c.w[k] = nc.dram_tensor(k, [2] + shp, dt, kind="ExternalInput").ap()
    for k, (shp, dt) in C_SHAPES.items():
        c.w[k] = nc.dram_tensor(k, shp, dt, kind="ExternalInput").ap()
    x_loc = nc.dram_tensor("x_loc", [S, D], F32, kind="ExternalInput").ap()
    c.s = {}
    for k, (shp, dt) in S_SHAPES.items():
        kind = "ExternalOutput" if k in taps else "Internal"
        c.s[k] = nc.dram_tensor("s_" + k, shp, dt, kind=kind).ap()
    y01 = nc.dram_tensor("y01", [S, D], F32, kind="Internal").ap()
    c.y = nc.dram_tensor("y", [HALF, D], F32, kind="ExternalOutput").ap()
    with ExitStack() as stack:
        c.p = Prog(nc, stack)
        c.breg = nc.gpsimd.to_reg(NSLOT - 1)
        c.eps_rms = stack.enter_context(nc.sbuf_tensor("eps_rms", [128, 1], F32))
        c.eps_ln = stack.enter_context(nc.sbuf_tensor("eps_ln", [128, 1], F32))
        c.hm = stack.enter_context(nc.sbuf_tensor("hmask_sb", [128, 4], F32))
        c.p.op("pool", lambda e: e.memset(c.eps_rms[:], RMS_EPS), [], ["eps"])
        c.p.op("pool", lambda e: e.memset(c.eps_ln[:], LN_EPS), [], ["eps"])
        dma(c.p, "sp", c.hm[:], c.w["hmask"][:, :], [], ["hm"])
        c.p.barrier()
        c.p.emit()
        rope = c.w["rope_loc"]
        c.rope_full = rope
        with ExitStack() as ph:
            zt = ph.enter_context(nc.sbuf_tensor("zt", [128, 8, D], BF16))
            c.p.op("pool", lambda e: e.memset(zt[:], 0.0), [], ["zt"])
            xg_v = c.s["xg"].rearrange("(n p) d -> p n d", p=128)
            for i in range(NSLOT // 1024):
                dma(c.p, "sp", xg_v[:, i * 8:(i + 1) * 8, :], zt[:], ["zt"], [("d_xg0", i)])
            c.p.barrier()
            c.p.emit()
        for ps_ in passes:
            if ps_ == "A":
                L, c.q_src, c.c_src, c.full_src = 0, x_loc[0:HALF, :], x_loc[HALF:S, :], x_loc
                c.rope_q, c.bbnd, c.hm_cols, c.do_full, dst = rope[0:HALF, :], c.w["na_bbnd"][0], (0, 1), True, y01[0:HALF, :]
            elif ps_ == "B":
                L, c.q_src, c.c_src, c.full_src = 0, x_loc[HALF:S, :], x_loc[0:HALF, :], x_loc
                c.rope_q, c.bbnd, c.hm_cols, c.do_full, dst = rope[HALF:S, :], c.w["na_bbnd_o"][0], (2, 3), False, y01[HALF:S, :]
            else:
                L, c.q_src, c.c_src, c.full_src = 1, y01[0:HALF, :], y01[HALF:S, :], y01
                c.rope_q, c.bbnd, c.hm_cols, c.do_full, dst = rope[0:HALF, :], c.w["na_bbnd"][1], (0, 1), True, c.y
            if "p1" in phases:
                phase1(c, L)
            if "na" in phases:
                phase_na(c, L)
            if "gqa" in phases:
                phase_dense_attn(c, L, "gqa")
            if "mla" in phases:
                phase_dense_attn(c, L, "mla")
            if "merge" in phases:
                phase_merge(c, L)
            if "moe" in phases:
                phase_moe(c, L)
            if "moes" in phases:
                phase_moe_sparse(c, L)
            if "ln2" in phases:
                phase_ln2(c, L, dst if dst_override is None else dst_override(c), sparse=("moes" in phases))
        c.p.barrier()
        c.p.emit()
    return nc


def rope_tables():
    t = np.arange(S)
    row = (t // 64).astype(np.float32)
    col = (t % 64).astype(np.float32)
    out = []
    for dim in (64, 32):
        quarter = dim // 4
        inv = (10000.0 ** (-np.arange(quarter, dtype=np.float32) / quarter)).astype(np.float32)
        ang = np.concatenate([row[:, None] * inv, col[:, None] * inv], -1).astype(np.float32)
        cs, sn = np.cos(ang).astype(np.float32), np.sin(ang).astype(np.float32)
        out.append(np.concatenate([cs, cs], -1))
        out.append(np.concatenate([-sn, sn], -1))
    return np.ascontiguousarray(np.concatenate(out, -1).astype(np.float32))


def na_bias_tables(rpb, hf):
    cols = np.arange(64)
    c0 = np.clip(cols - 8, 0, 48)
    in_win = (cols[None, :] >= c0[:, None]) & (cols[None, :] < c0[:, None] + 16)
    idx_c = np.clip(cols[None, :] - cols[:, None] + 15, 0, 30)

    def tab(j, rows):
        r = hf * 64 + j
        r0 = int(np.clip(r - 4, 0, 120))
        out = np.full((64, 6, len(rows), 64), NEG, np.float32)
        for ii, lr in enumerate(rows):
            gr = hf * 64 + lr - 4
            i = gr - r0
            if i < 0 or i >= 8 or gr < 0 or gr >= 128:
                continue
            ir = gr - r + 7
            b = rpb[:, ir][:, idx_c]
            b = np.where(in_win[None], b, NEG)
            out[:, :, ii, :] = b.transpose(2, 0, 1)
        return out
    interior = tab(10, list(range(10, 18)))
    bnd = []
    for j in (0, 1, 2, 3):
        bnd.append(tab(j, list(range(0, 12))))
    for j in (61, 62, 63):
        bnd.append(tab(j, list(range(60, 72))))
    return interior, np.stack(bnd, 0)


def prep_weights(inp, layers, hf):
    w = {}
    ls = list(layers)
    st = lambda f: np.ascontiguousarray(np.stack([f(l) for l in ls], 0))
    w["w_in"] = st(lambda l: inp["w_in"][l])
    w["w_uq"] = st(lambda l: inp["w_uq"][l])
    w["w_ukv"] = st(lambda l: inp["w_ukv"][l])
    w["w_ba"] = st(lambda l: inp["w_branch_a"][l])
    w["w_bb"] = st(lambda l: inp["w_branch_b"][l])
    w["w_bc"] = st(lambda l: inp["w_branch_c"][l])
    w["w_out"] = st(lambda l: inp["w_out"][l])
    w["w_router"] = st(lambda l: inp["w_router"][l])
    w["w_gu"] = st(lambda l: inp["w_gate_up"][l])
    w["w_dn"] = st(lambda l: inp["w_down"][l])
    w["gq_rep"] = st(lambda l: np.tile(inp["gqa_q_norm"][l][None, :], (128, 6)))
    w["gk_rep"] = st(lambda l: np.tile(inp["gqa_k_norm"][l][None, :], (128, 2)))
    w["mq_rep"] = st(lambda l: np.tile(inp["mla_q_norm"][l][None, :], (128, 1)))
    w["mkv_rep"] = st(lambda l: np.tile(inp["mla_kv_norm"][l][None, :], (128, 1)))
    for k, src in (("ln1g", "ln1_g"), ("ln1b", "ln1_b"), ("ln2g", "ln2_g"), ("ln2b", "ln2_b")):
        w[k] = st(lambda l: np.tile(inp[src][l][None, :], (128, 1)))
    w["brouter"] = st(lambda l: np.tile(inp["b_router"][l][None, :], (128, 1)))
    w["bgu_t"] = st(lambda l: inp["b_gate_up"][l].reshape(NEXP, 16, 128).transpose(2, 0, 1))
    w["bdn"] = st(lambda l: inp["b_down"][l])
    bi, bb = zip(*[na_bias_tables(inp["na_rpb"][l], hf) for l in ls])
    w["na_bint"] = np.ascontiguousarray(np.stack(bi, 0))
    w["na_bbnd"] = np.ascontiguousarray(np.stack(bb, 0))
    w["na_bbnd_o"] = np.ascontiguousarray(np.stack([na_bias_tables(inp["na_rpb"][l], 1 - hf)[1] for l in ls], 0))
    return {k: np.ascontiguousarray(v.astype(np.float32)) for k, v in w.items()}


def prep_consts(hf):
    rt = rope_tables()
    own, oth = rt[hf * HALF:(hf + 1) * HALF], rt[(1 - hf) * HALF:(2 - hf) * HALF]
    hm = np.zeros((128, 4), np.float32)
    hm[:, 0], hm[:, 1], hm[:, 2], hm[:, 3] = hf, 1 - hf, 1 - hf, hf
    return {
        "rope_loc": np.ascontiguousarray(np.concatenate([own, oth], 0)),
        "hmask": hm,
        "identb": np.eye(128, dtype=np.float32).astype(ml_dtypes.bfloat16),
        "identf": np.eye(128, dtype=np.float32),
        "triu": np.triu(np.ones((128, 128), np.float32), 1),
        "ones128": np.ones((128, 128), np.float32),
        "iota_e": np.tile(np.arange(NEXP, dtype=np.float32)[None, :], (128, 1)),
        "iota_cap": np.tile((np.arange(NEXP, dtype=np.float32) * CAP)[None, :], (128, 1)),
    }


def prep_acts(xb, hf):
    own, oth = xb[hf * HALF:(hf + 1) * HALF], xb[(1 - hf) * HALF:(2 - hf) * HALF]
    return {"x_loc": np.ascontiguousarray(np.concatenate([own, oth], 0))}


def _normalize(c, psO, okey, ncol, rsb, rkey, psB, ou, oukey, onesf, dst_ap, dst_key, nh=1):
    p = c.p
    p.op("dve", lambda e: e.reciprocal(out=rsb[64:65, 0:ncol], in_=psO[64:65, 0:ncol]), [okey], [rkey])
    p.op("pe", lambda e: e.matmul(psB[0:64, 0:ncol], onesf[64:65, 0:64], rsb[64:65, 0:ncol],
                                  start=True, stop=True), [rkey, "onesf"], ["psB"])
    p.op("act", lambda e: e.activation(out=ou[0:64, 0:ncol], in_=psO[0:64, 0:ncol], func=AF.Copy),
         [okey], [oukey])
    a0 = ou[0:64, 0:ncol]
    a1 = psB[0:64, 0:ncol]
    if nh > 1:
        a0 = a0.rearrange("p (h q) -> p h q", h=nh)
        a1 = a1.rearrange("p (h q) -> p h q", h=nh)
    p.op("dve", lambda e: e.tensor_tensor(out=dst_ap, in0=a0, in1=a1, op=ALU.mult),
         [oukey, "psB"], [dst_key])


def phase_dense_attn(c, L, kind):
    nc, p = c.nc, c.p
    if kind == "gqa":
        nH, dh, nK, nV, scale, obase = 6, 64, 2, 2, 64 ** -0.5, 6
        qT, kT, vS = c.s["qT_gqa"], c.s["kT_gqa"], c.s["v_gqa"]
        kmap = [0, 0, 0, 1, 1, 1]
    else:
        nH, dh, nK, nV, scale, obase = 4, 96, 4, 4, 96 ** -0.5, 12
        qT, kT, vS = c.s["qT_mla"], c.s["kT_mla"], c.s["v_mla"]
        kmap = [0, 1, 2, 3]
    with ExitStack() as ph:
        c.ph = ph
        KT = _sb(c, "KT", [128, nK, S], BF16)
        V = _sb(c, "V", [128, 64, nV * 65], BF16)
        Q = [_sb(c, "Q%d" % i, [128, nH, 512], BF16) for i in range(2)]
        pT = [_sb(c, "pT%d" % i, [128, 1024], BF16) for i in range(3)]
        rsb = _sb(c, "rsb", [128, 512], F32)
        ou = _sb(c, "ou", [128, 512], F32)
        ot = [_sb(c, "ot%d" % i, [128, 512], BF16) for i in range(2)]
        onesf = _sb(c, "onesf", [128, 64], F32)
        psS = [_ps(c, "psS%d" % i, [128, 1024], F32) for i in range(2)]
        psO = [_ps(c, "psO%d" % i, [128, 512], F32) for i in range(2)]
        psB = _ps(c, "psB", [128, 512], F32)
        p.op("pool", lambda e: e.memset(onesf[:], 1.0), [], ["onesf"])
        pack = (kind == "gqa")
        if pack:
            kT2 = kT.rearrange("g d t -> (g d) t")
            for half in range(2):
                dma(p, "sp", KT[:, 0, half * HALF:(half + 1) * HALF], kT2[:, half * HALF:(half + 1) * HALF],
                    [], ["KT"])
            for i in range(2):
                p.op("pool", lambda e, i=i: e.memset(Q[i][:], 0.0), [], [("Q", i)])
        else:
            for k in range(nK):
                for half in range(2):
                    dma(p, "sp", KT[0:dh, k, half * HALF:(half + 1) * HALF],
                        kT[k, :, half * HALF:(half + 1) * HALF], [], ["KT"])
        vv = vS.rearrange("(t p) c -> p t c", p=128)
        for q4 in range(16):
            dma(p, "sp", V[:, q4 * 4:(q4 + 1) * 4, :], vv[:, q4 * 4:(q4 + 1) * 4, :], [], ["V"])
        it = 0
        for qb in range(8):
            qs = qb % 2
            if pack:
                dma(p, "sp", Q[qs][0:64, 0:3, :], qT[0:3, :, qb * 512:(qb + 1) * 512].rearrange("h d t -> d h t"),
                    [], [("Q", qs)])
                dma(p, "sp", Q[qs][64:128, 3:6, :], qT[3:6, :, qb * 512:(qb + 1) * 512].rearrange("h d t -> d h t"),
                    [], [("Q", qs)])
            else:
                dma(p, "sp", Q[qs][0:dh, :, :], qT[:, :, qb * 512:(qb + 1) * 512].rearrange("h d t -> d h t"),
                    [], [("Q", qs)])
            for h in range(nH):
                ob_ = it % 2
                it += 1
                okey = "psO%d" % ob_
                ki = kmap[h]

                def qk2(k2, h=h, ki=ki, qs=qs):
                    b = k2 % 2

                    def fn(e):
                        ins = None
                        for u in range(2):
                            kt = 2 * k2 + u
                            if pack:
                                ins = e.matmul(psS[b][:, u * 512:(u + 1) * 512], KT[:, 0, kt * 128:(kt + 1) * 128],
                                               Q[qs][:, h, :], start=True, stop=True)
                            else:
                                ins = e.matmul(psS[b][:, u * 512:(u + 1) * 512],
                                               KT[0:dh, ki, kt * 128:(kt + 1) * 128], Q[qs][0:dh, h, :],
                                               start=True, stop=True)
                        return ins
                    p.op("pe", fn, ["KT", ("Q", qs)], ["psS%d" % b])
                qk2(0)
                for k2 in range(32):
                    b = k2 % 2
                    pb = k2 % 3
                    if k2 + 1 < 32:
                        qk2(k2 + 1)
                    p.op("act", lambda e, b=b, pb=pb: e.activation(out=pT[pb][:], in_=psS[b][:, :],
                                                                   func=AF.Exp, scale=scale),
                         ["psS%d" % b], [("pT", pb)])

                    def fpv(e, k2=k2, pb=pb, ob_=ob_, ki=ki):
                        ins = None
                        for u in range(2):
                            kt = 2 * k2 + u
                            ins = e.matmul(psO[ob_][0:65, :], V[:, kt, ki * 65:(ki + 1) * 65],
                                           pT[pb][:, u * 512:(u + 1) * 512],
                                           start=(kt == 0), stop=(kt == 63))
                        return ins
                    p.op("pe", fpv, ["V", ("pT", pb)], [okey])
                os_ = it % 2
                _normalize(c, psO[ob_], okey, 512, rsb, "rsb", psB, ou, "ou", onesf,
                           ot[os_][0:64, :], ("ot", os_))
                dma(p, "sp", c.s["oT"][obase + h, :, qb * 512:(qb + 1) * 512], ot[os_][0:64, :],
                    [("ot", os_)], [("d_oT", obase + h, qb)])
        p.barrier()
        p.emit()


def phase_na(c, L):
    nc, p = c.nc, c.p
    with ExitStack() as ph:
        c.ph = ph
        Qb = [_sb(c, "Qb%d" % i, [128, 6, 512], BF16) for i in range(2)]
        Kb = [_sb(c, "Kb%d" % i, [128, 6, 1024], BF16) for i in range(2)]
        Vb = [_sb(c, "Vb%d" % i, [128, 16, 390], BF16) for i in range(2)]
        bint = _sb(c, "bint", [128, 6, 8, 64], F32)
        bbnd = _sb(c, "bbnd", [128, 6, 12, 64], F32)
        sc = [_sb(c, "sc%d" % i, [128, 768], F32) for i in range(2)]
        pp = [_sb(c, "pp%d" % i, [128, 768], BF16) for i in range(2)]
        rsb = _sb(c, "rsb", [128, 512], F32)
        ou = _sb(c, "ou", [128, 512], F32)
        ot = [_sb(c, "ot%d" % i, [128, 6, 512], BF16) for i in range(2)]
        onesf = _sb(c, "onesf", [128, 64], F32)
        psS = [_ps(c, "psS%d" % i, [128, 1024], F32) for i in range(2)]
        psO = [_ps(c, "psO%d" % i, [128, 512], F32) for i in range(2)]
        psB = _ps(c, "psB", [128, 512], F32)
        p.op("pool", lambda e: e.memset(onesf[:], 1.0), [], ["onesf"])
        dma(p, "sp", bint[0:64], c.w["na_bint"][L], [], ["bint"])
        vrow = c.s["v_na"].rearrange("(r k) c -> k r c", k=64)
        it = 0
        for b8 in range(8):
            s = b8 % 2
            dma(p, "sp", Qb[s][0:64, :, :], c.s["qT_na"][:, :, b8 * 512:(b8 + 1) * 512].rearrange("h d t -> d h t"),
                [], [("Qb", s)])
            dma(p, "sp", Kb[s][0:64, :, :], c.s["kT_na"][:, :, b8 * 512:b8 * 512 + 1024].rearrange("h d t -> d h t"),
                [], [("Kb", s)])
            for hv in range(2):
                dma(p, "sp", Vb[s][0:64, hv * 8:(hv + 1) * 8, :], vrow[:, b8 * 8 + hv * 8:b8 * 8 + (hv + 1) * 8, :],
                    [], [("Vb", s)])
            for jj in range(8):
                j = b8 * 8 + jj
                if j < 4:
                    rows = list(range(0, 12)); bidx = j
                elif j > 60:
                    rows = list(range(60, 72)); bidx = 4 + (j - 61)
                else:
                    rows = list(range(j, j + 8)); bidx = None
                nr = len(rows)
                if bidx is not None:
                    dma(p, "sp", bbnd[0:64], c.bbnd[bidx], [], ["bbnd"])
                    btile, bkey = bbnd, "bbnd"
                else:
                    btile, bkey = bint, "bint"
                ob_ = j % 2
                okey = "psO%d" % ob_
                for h in range(6):
                    sb_ = it % 2
                    it += 1
                    skey = "psS%d" % sb_

                    def fqk(e, h=h, sb_=sb_, rows=rows, jj=jj, s=s, b8=b8):
                        ins = None
                        for i, lr in enumerate(rows):
                            ins = e.matmul(psS[sb_][0:64, i * 64:(i + 1) * 64],
                                           Kb[s][0:64, h, (lr - b8 * 8) * 64:(lr - b8 * 8 + 1) * 64],
                                           Qb[s][0:64, h, jj * 64:(jj + 1) * 64], start=True, stop=True)
                        return ins
                    p.op("pe", fqk, [("Qb", s), ("Kb", s)], [skey])
                    p.op("dve", lambda e, h=h, sb_=sb_, nr=nr, btile=btile: e.scalar_tensor_tensor(
                        out=sc[sb_][0:64, 0:nr * 64], in0=psS[sb_][0:64, 0:nr * 64], scalar=0.125,
                        in1=btile[0:64, h, 0:nr, :].rearrange("p r q -> p (r q)"),
                        op0=ALU.mult, op1=ALU.add), [skey, bkey], [("sc", sb_)])
                    p.op("act", lambda e, sb_=sb_, nr=nr: e.activation(
                        out=pp[sb_][0:64, 0:nr * 64], in_=sc[sb_][0:64, 0:nr * 64], func=AF.Exp),
                        [("sc", sb_)], [("pp", sb_)])

                    def fpv(e, h=h, sb_=sb_, rows=rows, ob_=ob_, s=s, b8=b8):
                        ins = None
                        n = len(rows)
                        for i, lr in enumerate(rows):
                            ins = e.matmul(psO[ob_][0:65, h * 64:(h + 1) * 64],
                                           Vb[s][0:64, lr - b8 * 8, h * 65:(h + 1) * 65],
                                           pp[sb_][0:64, i * 64:(i + 1) * 64],
                                           start=(i == 0), stop=(i == n - 1))
                        return ins
                    p.op("pe", fpv, [("Vb", s), ("pp", sb_)], [okey])
                _normalize(c, psO[ob_], okey, 384, rsb, "rsb", psB, ou, "ou", onesf,
                           ot[s][0:64, :, jj * 64:(jj + 1) * 64],
                           ("ot", s), nh=6)
            dma(p, "sp", c.s["oT"][0:6, :, b8 * 512:(b8 + 1) * 512].rearrange("h d t -> d h t"),
                ot[s][0:64, :, :], [("ot", s)], [("d_oT_na", b8)])
        p.barrier()
        p.emit()


def layer_norm_tile(c, tsb, tkey, junk, jkey, sm, smkey, g_t, b_t, gbkey, out_t, outkey):
    p = c.p
    p.op("act", lambda e: e.activation(out=junk[:], in_=tsb[:], func=AF.Copy, accum_out=sm[:, 0:1]),
         [tkey], [jkey, smkey])
    p.op("act", lambda e: e.activation(out=junk[:], in_=tsb[:], func=AF.Square, accum_out=sm[:, 1:2]),
         [tkey], [jkey, smkey])
    p.op("dve", lambda e: e.tensor_scalar(out=sm[:, 2:3], in0=sm[:, 0:1], scalar1=1.0 / D, scalar2=None,
                                          op0=ALU.mult), [smkey], [smkey])
    p.op("dve", lambda e: e.tensor_tensor(out=sm[:, 3:4], in0=sm[:, 2:3], in1=sm[:, 2:3], op=ALU.mult),
         [smkey], [smkey])
    p.op("dve", lambda e: e.scalar_tensor_tensor(out=sm[:, 4:5], in0=sm[:, 1:2], scalar=1.0 / D,
                                                 in1=sm[:, 3:4], op0=ALU.mult, op1=ALU.subtract),
         [smkey], [smkey])
    p.op("act", lambda e: e.activation(out=sm[:, 5:6], in_=sm[:, 4:5], func=AF.Sqrt,
                                       bias=c.eps_ln[:, 0:1]), [smkey], [smkey])
    p.op("dve", lambda e: e.reciprocal(out=sm[:, 6:7], in_=sm[:, 5:6]), [smkey], [smkey])
    p.op("dve", lambda e: e.tensor_scalar(out=junk[:], in0=tsb[:], scalar1=sm[:, 2:3], scalar2=sm[:, 6:7],
                                          op0=ALU.subtract, op1=ALU.mult), [tkey, smkey], [jkey])
    p.op("pool", lambda e: e.tensor_tensor(out=junk[:], in0=junk[:], in1=g_t[:], op=ALU.mult),
         [jkey, gbkey], [jkey])
    p.op("pool", lambda e: e.tensor_tensor(out=out_t[:], in0=junk[:], in1=b_t[:], op=ALU.add),
         [jkey, gbkey], [outkey])


def phase_merge(c, L):
    nc, p = c.nc, c.p
    with ExitStack() as ph:
        c.ph = ph
        wb = _sb(c, "wb", [128, 16, D], BF16)
        woutb = _sb(c, "woutb", [128, 8, D], BF16)
        lng = _sb(c, "lng", [128, D], F32)
        lnb = _sb(c, "lnb", [128, D], F32)
        wr = _sb(c, "wr", [128, 8, NEXP], F32)
        br = _sb(c, "br", [128, NEXP], F32)
        identb = _sb(c, "identb", [128, 128], BF16)
        identf = _sb(c, "identf", [128, 128], F32)
        oTb = [_sb(c, "oTb%d" % i, [128, 16, 512], BF16) for i in range(2)]
        gTc = [_sb(c, "gTc%d" % i, [128, 3, 512], BF16) for i in range(2)]
        mixT = [_sb(c, "mixT%d" % i, [128, 8, 512], BF16) for i in range(2)]
        mt = [_sb(c, "mt%d" % i, [128, 512], F32) for i in range(6)]
        xo = [_sb(c, "xo%d" % i, [128, D], F32) for i in range(2)]
        tsb = [_sb(c, "tsb%d" % i, [128, D], F32) for i in range(2)]
        junk = _sb(c, "junk", [128, D], F32)
        x1t = [_sb(c, "x1t%d" % i, [128, D], F32) for i in range(2)]
        x1b = [_sb(c, "x1b%d" % i, [128, D], BF16) for i in range(2)]
        x1Tb = [_sb(c, "x1Tb%d" % i, [128, 8, 128], BF16) for i in range(2)]
        x1Tf = [_sb(c, "x1Tf%d" % i, [128, 8, 128], F32) for i in range(2)]
        sm = [_sb(c, "sm%d" % i, [128, 8], F32) for i in range(2)]
        rt_ = [_sb(c, "rt%d" % i, [128, 160], F32) for i in range(2)]
        gTt = [_sb(c, "gTt%d" % i, [128, 128], F32) for i in range(2)]
        psY = [_ps(c, "psY%d" % i, [128, 512], F32) for i in range(3)]
        psOut = [_ps(c, "psOut%d" % i, [128, 512], F32) for i in range(2)]
        psT = _ps(c, "psT", [128, 8, 128], BF16)
        psTf = _ps(c, "psTf", [128, 8, 128], F32)
        triu = _sb(c, "triu", [128, 128], F32)
        ones128 = _sb(c, "ones128", [128, 128], F32)
        iota_e = _sb(c, "iota_e", [128, NEXP], F32)
        iota_cap = _sb(c, "iota_cap", [128, NEXP], F32)
        msum = _sb(c, "msum", [128, NEXP], F32)
        rx = [_sb(c, "rx%d" % i, [128, 160], F32) for i in range(2)]
        idxu = [_sb(c, "idxu%d" % i, [128, 8], U32) for i in range(2)]
        slotu = [_sb(c, "slotu%d" % i, [128, 4], I32) for i in range(2)]
        dma(p, "sp", triu[:], c.w["triu"][:, :], [], ["triu"])
        dma(p, "sp", ones128[:], c.w["ones128"][:, :], [], ["ones128"])
        dma(p, "sp", iota_e[:], c.w["iota_e"][:, :], [], ["iota_e"])
        dma(p, "sp", iota_cap[:], c.w["iota_cap"][:, :], [], ["iota_cap"])
        p.op("pool", lambda e: e.memset(msum[:], 0.0), [], ["msum"])

        for h in range(16):
            src = (c.w["w_ba"][L][h * 64:(h + 1) * 64, :] if h < 6 else
                   c.w["w_bb"][L][(h - 6) * 64:(h - 5) * 64, :] if h < 12 else
                   c.w["w_bc"][L][(h - 12) * 64:(h - 11) * 64, :])
            dma(p, "pool", wb[0:64, h, :], src, [], ["wb"])
        wo_v = c.w["w_out"][L].rearrange("(c p) n -> p c n", p=128)
        for ch in range(8):
            dma(p, "pool", woutb[:, ch, :], wo_v[:, ch, :], [], ["woutb"])
        dma(p, "sp", lng[:], c.w["ln1g"][L], [], ["lngb"])
        dma(p, "sp", lnb[:], c.w["ln1b"][L], [], ["lngb"])
        dma(p, "sp", wr[:], c.w["w_router"][L].rearrange("(c p) e -> p c e", p=128), [], ["wr"])
        dma(p, "sp", br[:], c.w["brouter"][L], [], ["br"])
        dma(p, "sp", identb[:], c.w["identb"][:, :], [], ["identb"])
        dma(p, "sp", identf[:], c.w["identf"][:, :], [], ["identf"])
        gT_v = c.s["gT"].rearrange("(i c p) t -> p i c t", i=3, c=8, p=128)
        x1T_v = c.s["x1T"].rearrange("(c p) t -> p c t", p=128)
        gcnt = 0
        for blk in range(8):
            s = blk % 2
            for hv in range(2):
                dma(p, "sp", oTb[s][0:64, hv * 8:(hv + 1) * 8, :],
                    c.s["oT"][hv * 8:(hv + 1) * 8, :, blk * 512:(blk + 1) * 512].rearrange("h d t -> d h t"),
                    [], [("oTb", s)])
            for dc in range(8):
                gs = gcnt % 2
                gcnt += 1
                dma(p, "sp", gTc[gs][:], gT_v[:, :, dc, blk * 512:(blk + 1) * 512], [], [("gTc", gs)])
                for i, (h0, nh) in enumerate(((0, 6), (6, 6), (12, 4))):
                    mm_group(p, psY[i][:, :],
                             [(wb[0:64, h0 + k, dc * 128:(dc + 1) * 128], oTb[s][0:64, h0 + k, :])
                              for k in range(nh)], ["wb", ("oTb", s)], ["psY%d" % i])
                m3 = [(gcnt * 3 + i) % 6 for i in range(3)]
                for i in range(3):
                    p.op("dve", lambda e, i=i, gs=gs, m=m3[i]: e.tensor_tensor(
                        out=mt[m][:], in0=psY[i][:, :], in1=gTc[gs][:, i, :], op=ALU.mult),
                        ["psY%d" % i, ("gTc", gs)], [("mt", m3[i])])
                p.op("pool", lambda e, m3=m3: e.tensor_tensor(out=mt[m3[0]][:], in0=mt[m3[0]][:],
                                                              in1=mt[m3[1]][:], op=ALU.add),
                     [("mt", m3[0]), ("mt", m3[1])], [("mt", m3[0])])
                p.op("pool", lambda e, m3=m3, s=s, dc=dc: e.tensor_tensor(
                    out=mixT[s][:, dc, :], in0=mt[m3[0]][:], in1=mt[m3[2]][:], op=ALU.add),
                    [("mt", m3[0]), ("mt", m3[2])], [("mixT", s)])
            for tt in range(4):
                t = blk * 4 + tt
                ts_ = t % 2
                dma(p, "sp", xo[ts_][:], c.q_src[t * 128:(t + 1) * 128, :], [], [("xo", ts_)])
                for half in range(2):
                    mm_group(p, psOut[half][:, :],
                             [(mixT[s][:, dc, tt * 128:(tt + 1) * 128], woutb[:, dc, half * 512:(half + 1) * 512])
                              for dc in range(8)], [("mixT", s), "woutb"], ["psOut%d" % half])
                    p.op("dve", lambda e, half=half, ts_=ts_: e.scalar_tensor_tensor(
                        out=tsb[ts_][:, half * 512:(half + 1) * 512], in0=xo[ts_][:, half * 512:(half + 1) * 512],
                        scalar=DN_ALPHA, in1=psOut[half][:, :], op0=ALU.mult, op1=ALU.add),
                        [("xo", ts_), "psOut%d" % half], [("tsb", ts_)])
                layer_norm_tile(c, tsb[ts_], ("tsb", ts_), junk, "junk", sm[ts_], ("sm", ts_),
                                lng, lnb, "lngb", x1t[ts_], ("x1t", ts_))
                dma(p, "sp", c.s["x1"][t * 128:(t + 1) * 128, :], x1t[ts_][:], [("x1t", ts_)], [("d_x1", t)])
                p.op("act", lambda e, ts_=ts_: e.activation(out=x1b[ts_][:], in_=x1t[ts_][:], func=AF.Copy),
                     [("x1t", ts_)], [("x1b", ts_)])
                tr_group(p, [(psT[:, ch, :], x1b[ts_][:, ch * 128:(ch + 1) * 128]) for ch in range(8)],
                         identb[:], [("x1b", ts_), "identb"], ["psT"])
                p.op("dve", lambda e, ts_=ts_: e.tensor_copy(x1Tb[ts_][:], psT[:]), ["psT"], [("x1Tb", ts_)])
                for hv in range(2):
                    dma(p, "sp", x1T_v[:, hv * 4:(hv + 1) * 4, t * 128:(t + 1) * 128], x1Tb[ts_][:, hv * 4:(hv + 1) * 4, :],
                        [("x1Tb", ts_)], [("d_x1T", t, hv)])
                tr_group(p, [(psTf[:, ch, :], x1t[ts_][:, ch * 128:(ch + 1) * 128]) for ch in range(8)],
                         identf[:], [("x1t", ts_), "identf"], ["psTf"])
                p.op("act", lambda e, ts_=ts_: e.activation(out=x1Tf[ts_][:], in_=psTf[:], func=AF.Copy),
                     ["psTf"], [("x1Tf", ts_)])
                mm_group(p, psY[0][:, 0:NEXP], [(x1Tf[ts_][:, ch, :], wr[:, ch, :]) for ch in range(8)],
                         [("x1Tf", ts_), "wr"], ["psY0"])
                R = rt_[ts_]
                rk = ("rt", ts_)
                lg, mx, msk, ex, exm = R[:, 0:32], R[:, 32:40], R[:, 40:72], R[:, 72:104], R[:, 104:136]
                nm, ssum, rs = R[:, 136:137], R[:, 137:138], R[:, 138:139]
                p.op("dve", lambda e, lg=lg: e.tensor_tensor(out=lg, in0=psY[0][:, 0:NEXP], in1=br[:], op=ALU.add),
                     ["psY0", "br"], [rk])
                p.op("dve", lambda e, lg=lg, mx=mx: e.max(out=mx, in_=lg), [rk], [rk])
                p.op("dve", lambda e, lg=lg, mx=mx, msk=msk: e.tensor_scalar(
                    out=msk, in0=lg, scalar1=mx[:, 3:4], scalar2=None, op0=ALU.is_ge), [rk], [rk])
                p.op("dve", lambda e, mx=mx, nm=nm: e.tensor_scalar(
                    out=nm, in0=mx[:, 0:1], scalar1=-1.0, scalar2=None, op0=ALU.mult), [rk], [rk])
                p.op("act", lambda e, lg=lg, ex=ex, nm=nm: e.activation(out=ex, in_=lg, func=AF.Exp, bias=nm),
                     [rk], [rk])
                p.op("dve", lambda e, ex=ex, msk=msk, exm=exm: e.tensor_tensor(out=exm, in0=ex, in1=msk,
                                                                               op=ALU.mult), [rk], [rk])
                p.op("dve", lambda e, exm=exm, ssum=ssum: e.reduce_sum(out=ssum, in_=exm, axis=AX.X), [rk], [rk])
                p.op("dve", lambda e, ssum=ssum, rs=rs: e.reciprocal(out=rs, in_=ssum), [rk], [rk])
                p.op("dve", lambda e, exm=exm, rs=rs: e.tensor_scalar(
                    out=exm, in0=exm, scalar1=rs, scalar2=None, op0=ALU.mult), [rk], [rk])
                p.op("pe", lambda e, exm=exm: e.transpose(psY[1][0:32, 0:128], exm, identf[:]),
                     [rk, "identf"], ["psY1"])
                p.op("act", lambda e, ts_=ts_: e.activation(out=gTt[ts_][0:32, :], in_=psY[1][0:32, 0:128],
                                                            func=AF.Copy), ["psY1"], [("gTt", ts_)])
                dma(p, "sp", c.s["gateT"][:, t * 128:(t + 1) * 128], gTt[ts_][0:32, :],
                    [("gTt", ts_)], [("d_gateT", t)])
                X = rx[ts_]
                xk = ("rx", ts_)
                idxf, sv, ov, eq, tmp = X[:, 0:8], X[:, 8:40], X[:, 40:72], X[:, 72:104], X[:, 104:136]
                slotf, gkf = X[:, 136:140], X[:, 140:144]
                p.op("dve", lambda e, ts_=ts_, lg=lg, mx=mx: e.max_index(out=idxu[ts_][:], in_max=mx, in_values=lg),
                     [rk], [("idxu", ts_)])
                p.op("dve", lambda e, ts_=ts_, idxf=idxf: e.tensor_copy(idxf, idxu[ts_][:]),
                     [("idxu", ts_)], [xk])
                p.op("pe", lambda e, msk=msk: e.matmul(psY[2][:, 0:NEXP], triu[:], msk, start=True, stop=False),
                     [rk, "triu"], ["psY2"])
                p.op("pe", lambda e: e.matmul(psY[2][:, 0:NEXP], ones128[:], msum[:], start=False, stop=True),
                     ["msum", "ones128"], ["psY2"])
                p.op("dve", lambda e, ov=ov: e.tensor_scalar(out=ov, in0=psY[2][:, 0:NEXP], scalar1=float(CAP),
                                                             scalar2=1.0e6, op0=ALU.is_ge, op1=ALU.mult),
                     ["psY2"], [xk])
                p.op("dve", lambda e, sv=sv: e.tensor_tensor(out=sv, in0=psY[2][:, 0:NEXP], in1=iota_cap[:],
                                                             op=ALU.add), ["psY2", "iota_cap"], [xk])
                p.op("dve", lambda e, sv=sv, ov=ov: e.tensor_tensor(out=sv, in0=sv, in1=ov, op=ALU.add), [xk], [xk])
                p.op("pool", lambda e, msk=msk: e.tensor_tensor(out=msum[:], in0=msum[:], in1=msk, op=ALU.add),
                     [rk, "msum"], ["msum"])
                for k4 in range(4):
                    p.op("dve", lambda e, eq=eq, idxf=idxf, k4=k4: e.tensor_scalar(
                        out=eq, in0=iota_e[:], scalar1=idxf[:, k4:k4 + 1], scalar2=None, op0=ALU.is_equal),
                        [xk, "iota_e"], [xk])
                    p.op("dve", lambda e, eq=eq, sv=sv, tmp=tmp: e.tensor_tensor(out=tmp, in0=eq, in1=sv, op=ALU.mult),
                         [xk], [xk])
                    p.op("dve", lambda e, tmp=tmp, slotf=slotf, k4=k4: e.reduce_sum(
                        out=slotf[:, k4:k4 + 1], in_=tmp, axis=AX.X), [xk], [xk])
                    p.op("dve", lambda e, eq=eq, exm=exm, tmp=tmp: e.tensor_tensor(out=tmp, in0=eq, in1=exm,
                                                                                   op=ALU.mult), [xk, rk], [xk])
                    p.op("dve", lambda e, tmp=tmp, gkf=gkf, k4=k4: e.reduce_sum(
                        out=gkf[:, k4:k4 + 1], in_=tmp, axis=AX.X), [xk], [xk])
                p.op("dve", lambda e, ts_=ts_, slotf=slotf: e.tensor_copy(slotu[ts_][:], slotf),
                     [xk], [("slotu", ts_)])
                for k4 in range(4):
                    p.op("pool", lambda e, ts_=ts_, k4=k4: e.indirect_dma_start(
                        out=c.s["xg"][:, :], out_offset=bass.IndirectOffsetOnAxis(ap=slotu[ts_][:, k4:k4 + 1], axis=0),
                        in_=x1b[ts_][:], in_offset=None, bounds_check=c.breg, oob_is_err=False),
                        [("slotu", ts_), ("x1b", ts_)], [("d_xg", t, k4)], dma=True)
                dma(p, "sp", c.s["slots"][t * 128:(t + 1) * 128, :], slotu[ts_][:], [("slotu", ts_)], [("d_slots", t)])
                dma(p, "sp", c.s["gks"][t * 128:(t + 1) * 128, :], gkf, [xk], [("d_gks", t)])
        p.barrier()
        p.emit()


SIG_MAX = float(1.0 / (1.0 + np.exp(-1.702 * 7.0)))


def phase_moe(c, L):
    nc, p = c.nc, c.p
    with ExitStack() as ph:
        c.ph = ph
        wgu = [_sb(c, "wgu%d" % i, [128, 8, 2 * DEXP], BF16) for i in range(2)]
        wdn = [_sb(c, "wdn%d" % i, [128, 8, D], BF16) for i in range(2)]
        xT = _sb(c, "xTsb", [128, 8, 1024], BF16)
        gT = _sb(c, "gTsb", [128, 1024], F32)
        yacc = _sb(c, "yacc", [128, 8, D], F32)
        bgu = _sb(c, "bgu", [128, NEXP, 16], F32)
        bgs = _sb(c, "bgs", [128, NEXP, 8], F32)
        bdn = _sb(c, "bdn", [128, D], F32)
        identf = _sb(c, "identf", [128, 128], F32)
        sg = [_sb(c, "sg%d" % i, [128, 512], F32) for i in range(2)]
        gc = [_sb(c, "gc%d" % i, [128, 512], F32) for i in range(2)]
        uc = [_sb(c, "uc%d" % i, [128, 512], F32) for i in range(2)]
        t1 = [_sb(c, "t1%d" % i, [128, 512], F32) for i in range(2)]
        t2 = [_sb(c, "t2%d" % i, [128, 512], F32) for i in range(2)]
        aT = [_sb(c, "aT%d" % i, [128, 8, 512], BF16) for i in range(2)]
        yev = [_sb(c, "yev%d" % i, [128, 512], F32) for i in range(2)]
        psg = [_ps(c, "psg%d" % i, [128, 512], F32) for i in range(2)]
        psu = [_ps(c, "psu%d" % i, [128, 512], F32) for i in range(2)]
        psGb = _ps(c, "psGb", [128, 512], F32)
        psy = [_ps(c, "psy%d" % i, [128, 512], F32) for i in range(2)]

        dma(p, "sp", bgu[:], c.w["bgu_t"][L], [], ["bgu"])
        dma(p, "sp", bdn[0:32, :], c.w["bdn"][L], [], ["bdn"])
        dma(p, "sp", identf[:], c.w["identf"][:, :], [], ["identf"])
        p.op("dve", lambda e: e.tensor_scalar(out=bgs[:], in0=bgu[:, :, 0:8], scalar1=1.702, scalar2=None,
                                              op0=ALU.mult), ["bgu"], ["bgs"])
        x1T_v = c.s["x1T"].rearrange("(c p) t -> p c t", p=128)
        ym_v = c.s["ymoe"].rearrange("(t p) d -> p t d", p=128)
        cnt = 0
        ycnt = 0
        for sb in range(4):
            dma(p, "sp", xT[:], x1T_v[:, :, sb * 1024:(sb + 1) * 1024], [], ["xTsb"])
            dma(p, "sp", gT[0:32, :], c.s["gateT"][:, sb * 1024:(sb + 1) * 1024], [], ["gTsb"])
            for ex in range(NEXP):
                ws = ex % 2
                gu_v = c.w["w_gu"][L, ex].rearrange("(c p) n -> p c n", p=128)
                dn_v = c.w["w_dn"][L, ex].rearrange("(c p) n -> p c n", p=128)
                for ch in range(8):
                    dma(p, "pool", wgu[ws][:, ch, :], gu_v[:, ch, :], [], [("wgu", ws)])
                for ch in range(8):
                    dma(p, "pool", wdn[ws][:, ch, :], dn_v[:, ch, :], [], [("wdn", ws)])
                for tb in range(2):
                    as_ = (ex * 2 + tb) % 2
                    p.op("pe", lambda e, ex=ex, tb=tb: e.matmul(
                        psGb[:, :], identf[0:32, ex:ex + 1].to_broadcast([32, 128]),
                        gT[0:32, tb * 512:(tb + 1) * 512], start=True, stop=True),
                        ["identf", "gTsb"], ["psGb"])
                    for j in range(8):
                        k = cnt % 2
                        cnt += 1
                        mm_group(p, psg[k][:, :],
                                 [(wgu[ws][:, ch, j * 128:(j + 1) * 128], xT[:, ch, tb * 512:(tb + 1) * 512])
                                  for ch in range(8)], [("wgu", ws), "xTsb"], ["psg%d" % k])
                        mm_group(p, psu[k][:, :],
                                 [(wgu[ws][:, ch, DEXP + j * 128:DEXP + (j + 1) * 128],
                                   xT[:, ch, tb * 512:(tb + 1) * 512]) for ch in range(8)],
                                 [("wgu", ws), "xTsb"], ["psu%d" % k])
                        p.op("dve", lambda e, k=k, ex=ex, j=j: e.tensor_scalar(
                            out=gc[k][:], in0=psg[k][:, :], scalar1=bgu[:, ex, j:j + 1], scalar2=7.0,
                            op0=ALU.add, op1=ALU.min), ["psg%d" % k, "bgu"], [("gc", k)])
                        p.op("act", lambda e, k=k: e.activation(
                            out=sg[k][:], in_=gc[k][:], func=AF.Sigmoid, scale=1.702),
                            [("gc", k)], [("sg", k)])
                        p.op("dve", lambda e, k=k, ex=ex, j=j: e.tensor_scalar(
                            out=uc[k][:], in0=psu[k][:, :], scalar1=bgu[:, ex, 8 + j:9 + j], scalar2=7.0,
                            op0=ALU.add, op1=ALU.min), ["psu%d" % k, "bgu"], [("uc", k)])
                        p.op("dve", lambda e, k=k: e.tensor_scalar(
                            out=uc[k][:], in0=uc[k][:], scalar1=-7.0, scalar2=1.0,
                            op0=ALU.max, op1=ALU.add), [("uc", k)], [("uc", k)])
                        p.op("pool", lambda e, k=k: e.tensor_tensor(
                            out=t1[k][:], in0=sg[k][:], in1=gc[k][:], op=ALU.mult),
                            [("sg", k), ("gc", k)], [("t1", k)])
                        p.op("dve", lambda e, k=k: e.tensor_tensor(out=t2[k][:], in0=uc[k][:], in1=psGb[:, :],
                                                                   op=ALU.mult), [("uc", k), "psGb"], [("t2", k)])
                        p.op("pool", lambda e, k=k, j=j, as_=as_: e.tensor_tensor(
                            out=aT[as_][:, j, :], in0=t1[k][:], in1=t2[k][:], op=ALU.mult),
                            [("t1", k), ("t2", k)], [("aT", as_)])
                    for tt in range(4):
                        tile = tb * 4 + tt
                        for half in range(2):
                            yk = ycnt % 2
                            ycnt += 1
                            pairs = [(aT[as_][:, j, tt * 128:(tt + 1) * 128],
                                      wdn[ws][:, j, half * 512:(half + 1) * 512]) for j in range(8)]
                            rds = [("aT", as_), ("wdn", ws)]
                            if ex == 0:
                                pairs.append((gT[0:32, tile * 128:(tile + 1) * 128],
                                              bdn[0:32, half * 512:(half + 1) * 512]))
                                rds += ["gTsb", "bdn"]
                            mm_group(p, psy[yk][:, :], pairs, rds, ["psy%d" % yk])
                            ya = yacc[:, tile, half * 512:(half + 1) * 512]
                            if ex == 0:
                                p.op("act", lambda e, ya=ya, yk=yk: e.activation(out=ya, in_=psy[yk][:, :],
                                                                                 func=AF.Copy),
                                     ["psy%d" % yk], [("yacc", tile, half)])
                            else:
                                p.op("act", lambda e, yk=yk: e.activation(out=yev[yk][:], in_=psy[yk][:, :],
                                                                          func=AF.Copy),
                                     ["psy%d" % yk], [("yev", yk)])
                                p.op("pool", lambda e, ya=ya, yk=yk: e.tensor_tensor(out=ya, in0=ya, in1=yev[yk][:],
                                                                                     op=ALU.add),
                                     [("yev", yk), ("yacc", tile, half)], [("yacc", tile, half)])
            dma(p, "sp", ym_v[:, sb * 8:(sb + 1) * 8, :], yacc[:],
                [("yacc", t_, h_) for t_ in range(8) for h_ in range(2)], [("d_ymoe", sb)])
        p.barrier()
        p.emit()


def load_w_cast(c, dst_ap, src_ap, stg, skey, dkey, eng="pool"):
    p = c.p
    dma(p, "sp", stg, src_ap, [], [skey])
    if eng == "act":
        p.op("act", lambda e: e.activation(out=dst_ap, in_=stg, func=AF.Copy), [skey], [dkey])
    else:
        p.op(eng, lambda e: e.tensor_copy(dst_ap, stg), [skey], [dkey])


def phase_moe_sparse(c, L):
    nc, p = c.nc, c.p
    NT = CAP // 128
    groups = [(0, 512), (512, CAP)] if CAP > 512 else [(0, CAP)]
    with ExitStack() as ph:
        c.ph = ph
        wgu = [_sb(c, "wgu%d" % i, [128, 8, 2 * DEXP], BF16) for i in range(2)]
        wdn = [_sb(c, "wdn%d" % i, [128, 8, D], BF16) for i in range(2)]
        stg = [_sb(c, "stg%d" % i, [128, 2 * DEXP], F32) for i in range(3)]
        xgt = [_sb(c, "xgt%d" % i, [128, D], BF16) for i in range(NT)]
        xgT = [_sb(c, "xgT%d" % i, [128, 8, CAP], BF16) for i in range(2)]
        bgu = _sb(c, "bgu", [128, NEXP, 16], F32)
        bdr1 = _sb(c, "bdr", [128, D], F32)
        bdr = [bdr1, bdr1]
        bdb = [_sb(c, "bdb%d" % i, [128, D], BF16) for i in range(2)]
        ones1 = _sb(c, "ones1", [128, 128], BF16)
        identb = _sb(c, "identb", [128, 128], BF16)
        sg = [_sb(c, "sg%d" % i, [128, 512], F32) for i in range(2)]
        gc = [_sb(c, "gc%d" % i, [128, 512], F32) for i in range(2)]
        uc = [_sb(c, "uc%d" % i, [128, 512], F32) for i in range(2)]
        t1 = [_sb(c, "t1%d" % i, [128, 512], F32) for i in range(2)]
        aT = _sb(c, "aT", [128, 8, CAP], BF16)
        yout = [_sb(c, "yout%d" % i, [128, D], F32) for i in range(2)]
        psT2 = [_ps(c, "psT%d" % i, [128, 8, 128], BF16) for i in range(2)]
        psg = [_ps(c, "psg%d" % i, [128, 512], F32) for i in range(2)]
        psu = [_ps(c, "psu%d" % i, [128, 512], F32) for i in range(2)]
        psy = [_ps(c, "psy%d" % i, [128, 512], F32) for i in range(2)]
        dma(p, "sp", bgu[:], c.w["bgu_t"][L], [], ["bgu"])
        dma(p, "sp", identb[:], c.w["identb"][:, :], [], ["identb"])
        p.op("pool", lambda e: e.memset(ones1[:], 1.0), [], ["ones1"])
        st8 = {"scnt": 0, "cnt": 0, "ycnt": 0, "xcnt": 0}
        cast_rr = ("act", "dve", "act")

        def wload_steps(ex):
            ws = ex % 2
            gu_v = c.w["w_gu"][L, ex].rearrange("(c p) n -> p c n", p=128)
            dn_v = c.w["w_dn"][L, ex].rearrange("(c p) n -> p c n", p=128)
            steps = []
            for ch in range(12):
                si = st8["scnt"] % 3
                eng = cast_rr[st8["scnt"] % 3]
                st8["scnt"] += 1
                if ch < 8:
                    src, stv, dstv, dkey = gu_v[:, ch, :], stg[si][:], wgu[ws][:, ch, :], ("wgu", ws)
                else:
                    c2 = ch - 8
                    src = dn_v[:, 2 * c2:2 * c2 + 2, :]
                    stv = stg[si][:].rearrange("p (c n) -> p c n", c=2)
                    dstv, dkey = wdn[ws][:, 2 * c2:2 * c2 + 2, :], ("wdn", ws)

                def f_dma(src=src, stv=stv, si=si):
                    dma(p, "sp", stv, src, [], [("stg", si)])

                def f_cast(stv=stv, dstv=dstv, si=si, dkey=dkey, eng=eng):
                    if eng == "act":
                        p.op("act", lambda e: e.activation(out=dstv, in_=stv, func=AF.Copy), [("stg", si)], [dkey])
                    else:
                        p.op(eng, lambda e: e.tensor_copy(dstv, stv), [("stg", si)], [dkey])
                steps.append((f_dma, f_cast))
            steps.append((lambda ws=ws, ex=ex: dma(p, "sp", bdr[ws][0:1, :], c.w["bdn"][L, ex:ex + 1, :], [],
                                                   ["bdr"]),
                          lambda ws=ws: p.op("pool", lambda e: e.tensor_copy(bdb[ws][0:1, :], bdr[ws][0:1, :]),
                                             ["bdr"], [("bdb", ws)])))
            return steps

        def xg_dma(ex):
            for st in range(NT):
                r0 = ex * CAP + st * 128
                dma(p, "sp", xgt[st][:], c.s["xg"][r0:r0 + 128, :], [], [("xgt", st)])

        def xg_load(ex):
            xb_ = ex % 2
            for st in range(NT):
                pt = st8["xcnt"] % 2
                st8["xcnt"] += 1
                tr_group(p, [(psT2[pt][:, ch, :], xgt[st][:, ch * 128:(ch + 1) * 128]) for ch in range(8)],
                         identb[:], [("xgt", st), "identb"], ["psT%d" % pt])
                eng = "act" if st % 2 == 0 else "dve"
                if eng == "act":
                    p.op("act", lambda e, st=st, xb_=xb_, pt=pt: e.activation(
                        out=xgT[xb_][:, :, st * 128:(st + 1) * 128], in_=psT2[pt][:], func=AF.Copy),
                        ["psT%d" % pt], [("xgT", xb_)])
                else:
                    p.op("dve", lambda e, st=st, xb_=xb_, pt=pt: e.tensor_copy(
                        xgT[xb_][:, :, st * 128:(st + 1) * 128], psT2[pt][:]), ["psT%d" % pt], [("xgT", xb_)])

        pend_cast = None
        for f_dma, f_cast in wload_steps(0):
            f_dma()
            if pend_cast is not None:
                pend_cast()
            pend_cast = f_cast
        pend_cast()
        xg_dma(0)
        xg_load(0)
        for ex in range(NEXP):
            ws = ex % 2
            xb_ = ex % 2
            nxt_steps = wload_steps(ex + 1) if ex + 1 < NEXP else []
            pend_cast = None
            if ex + 1 < NEXP:
                xg_dma(ex + 1)
            for (n0, n1) in groups:
                w = n1 - n0
                for j in range(8):
                    k = st8["cnt"] % 2
                    st8["cnt"] += 1
                    if nxt_steps:
                        f_dma, f_cast = nxt_steps.pop(0)
                        f_dma()
                        if pend_cast is not None:
                            pend_cast()
                        pend_cast = f_cast
                    mm_group(p, psg[k][:, 0:w], [(wgu[ws][:, ch, j * 128:(j + 1) * 128], xgT[xb_][:, ch, n0:n1])
                                                 for ch in range(8)], [("wgu", ws), ("xgT", xb_)], ["psg%d" % k])
                    mm_group(p, psu[k][:, 0:w], [(wgu[ws][:, ch, DEXP + j * 128:DEXP + (j + 1) * 128],
                                                  xgT[xb_][:, ch, n0:n1]) for ch in range(8)],
                             [("wgu", ws), ("xgT", xb_)], ["psu%d" % k])
                    p.op("dve", lambda e, k=k, ex=ex, j=j, w=w: e.tensor_scalar(
                        out=gc[k][:, 0:w], in0=psg[k][:, 0:w], scalar1=bgu[:, ex, j:j + 1], scalar2=7.0,
                        op0=ALU.add, op1=ALU.min), ["psg%d" % k, "bgu"], [("gc", k)])
                    p.op("act", lambda e, k=k, w=w: e.activation(out=sg[k][:, 0:w], in_=gc[k][:, 0:w],
                                                                 func=AF.Sigmoid, scale=1.702),
                         [("gc", k)], [("sg", k)])
                    p.op("dve", lambda e, k=k, ex=ex, j=j, w=w: e.tensor_scalar(
                        out=uc[k][:, 0:w], in0=psu[k][:, 0:w], scalar1=bgu[:, ex, 8 + j:9 + j], scalar2=7.0,
                        op0=ALU.add, op1=ALU.min), ["psu%d" % k, "bgu"], [("uc", k)])
                    p.op("dve", lambda e, k=k, w=w: e.tensor_scalar(
                        out=uc[k][:, 0:w], in0=uc[k][:, 0:w], scalar1=-7.0, scalar2=1.0,
                        op0=ALU.max, op1=ALU.add), [("uc", k)], [("uc", k)])
                    p.op("pool", lambda e, k=k, w=w: e.tensor_tensor(out=t1[k][:, 0:w], in0=sg[k][:, 0:w],
                                                                     in1=gc[k][:, 0:w], op=ALU.mult),
                         [("sg", k), ("gc", k)], [("t1", k)])
                    p.op("pool", lambda e, k=k, j=j, n0=n0, n1=n1, w=w: e.tensor_tensor(
                        out=aT[:, j, n0:n1], in0=t1[k][:, 0:w], in1=uc[k][:, 0:w], op=ALU.mult),
                        [("t1", k), ("uc", k)], ["aT"])
            while nxt_steps:
                f_dma, f_cast = nxt_steps.pop(0)
                f_dma()
                if pend_cast is not None:
                    pend_cast()
                pend_cast = f_cast
            if pend_cast is not None:
                pend_cast()
            if ex + 1 < NEXP:
                xg_load(ex + 1)
            for st in range(NT):
                ys = st8["ycnt"] % 2
                st8["ycnt"] += 1
                for half in range(2):
                    yk = (st8["ycnt"] + half) % 2
                    pairs = [(aT[:, j, st * 128:(st + 1) * 128], wdn[ws][:, j, half * 512:(half + 1) * 512])
                             for j in range(8)]
                    pairs.append((ones1[0:1, 0:128], bdb[ws][0:1, half * 512:(half + 1) * 512]))
                    mm_group(p, psy[yk][:, :], pairs, ["aT", ("wdn", ws), ("bdb", ws), "ones1"], ["psy%d" % yk])
                    p.op("act", lambda e, ys=ys, yk=yk, half=half: e.activation(
                        out=yout[ys][:, half * 512:(half + 1) * 512], in_=psy[yk][:, :], func=AF.Copy),
                        ["psy%d" % yk], [("yout", ys)])
                r0 = ex * CAP + st * 128
                dma(p, "sp", c.s["yg"][r0:r0 + 128, :], yout[ys][:], [("yout", ys)], [("d_yg", ex, st)])
        p.barrier()
        p.emit()


def phase_ln2(c, L, dst, sparse=True):
    nc, p = c.nc, c.p
    with ExitStack() as ph:
        c.ph = ph
        lng = _sb(c, "lng2", [128, D], F32)
        lnb = _sb(c, "lnb2", [128, D], F32)
        xa = [_sb(c, "xa%d" % i, [128, D], F32) for i in range(2)]
        ya = [_sb(c, "ya%d" % i, [128, D], F32) for i in range(2)]
        yk_ = [[_sb(c, "yk%d_%d" % (i, k), [128, D], F32) for k in range(4)] for i in range(2)]
        sl = [_sb(c, "sl%d" % i, [128, 4], I32) for i in range(2)]
        gk = [_sb(c, "gk%d" % i, [128, 4], F32) for i in range(2)]
        tsb = [_sb(c, "tsb%d" % i, [128, D], F32) for i in range(2)]
        out = [_sb(c, "out%d" % i, [128, D], F32) for i in range(2)]
        junk = _sb(c, "junk2", [128, D], F32)
        sm = [_sb(c, "sm%d" % i, [128, 8], F32) for i in range(2)]
        dma(p, "sp", lng[:], c.w["ln2g"][L], [], ["lngb"])
        dma(p, "sp", lnb[:], c.w["ln2b"][L], [], ["lngb"])
        for t in range(32):
            s = t % 2
            dma(p, "sp", xa[s][:], c.s["x1"][t * 128:(t + 1) * 128, :], [], [("xa", s)])
            if sparse:
                dma(p, "sp", sl[s][:], c.s["slots"][t * 128:(t + 1) * 128, :], [], [("sl", s)])
                dma(p, "sp", gk[s][:], c.s["gks"][t * 128:(t + 1) * 128, :], [], [("gk", s)])
                for k in range(4):
                    p.op("pool", lambda e, s=s, k=k: e.indirect_dma_start(
                        out=yk_[s][k][:], out_offset=None, in_=c.s["yg"][:, :],
                        in_offset=bass.IndirectOffsetOnAxis(ap=sl[s][:, k:k + 1], axis=0),
                        bounds_check=c.breg, oob_is_err=False), [("sl", s)], [("yk", s, k)], dma=True)
                p.op("dve", lambda e, s=s: e.tensor_scalar(out=ya[s][:], in0=yk_[s][0][:], scalar1=gk[s][:, 0:1],
                                                           scalar2=None, op0=ALU.mult),
                     [("yk", s, 0), ("gk", s)], [("ya", s)])
                for k in range(1, 4):
                    p.op("dve", lambda e, s=s, k=k: e.scalar_tensor_tensor(
                        out=ya[s][:], in0=yk_[s][k][:], scalar=gk[s][:, k:k + 1], in1=ya[s][:],
                        op0=ALU.mult, op1=ALU.add), [("yk", s, k), ("gk", s), ("ya", s)], [("ya", s)])
            else:
                dma(p, "sp", ya[s][:], c.s["ymoe"][t * 128:(t + 1) * 128, :], [], [("ya", s)])
            p.op("dve", lambda e, s=s: e.scalar_tensor_tensor(
                out=tsb[s][:], in0=xa[s][:], scalar=DN_ALPHA, in1=ya[s][:], op0=ALU.mult, op1=ALU.add),
                [("xa", s), ("ya", s)], [("tsb", s)])
            layer_norm_tile(c, tsb[s], ("tsb", s), junk, "junk", sm[s], ("sm", s), lng, lnb, "lngb",
                            out[s], ("out", s))
            dma(p, "sp", dst[t * 128:(t + 1) * 128, :], out[s][:], [("out", s)], [("d_out", t)])
        p.barrier()
        p.emit()


ALL_PHASES = ("p1", "na", "gqa", "mla", "merge", "moes", "ln2")
_NC_CACHE = {}


def kernel(**inputs):
    inp = {k: np.asarray(v) for k, v in inputs.items()}
    x = np.ascontiguousarray(inp["x"].astype(np.float32))
    nb = x.shape[0]
    if "nc" not in _NC_CACHE:
        _NC_CACHE["nc"] = build()
    consts = [prep_consts(hf) for hf in range(2)]
    wts = [prep_weights(inp, [0, 1], hf) for hf in range(2)]
    in_maps = []
    for cid in range(2 * nb):
        b, hf = cid // 2, cid % 2
        m = {}
        m.update(wts[hf])
        m.update(consts[hf])
        m.update(prep_acts(x[b], hf))
        in_maps.append(m)
    res = run_bass_kernel_spmd(_NC_CACHE["nc"], in_maps, core_ids=list(range(2 * nb)))
    return np.stack([np.concatenate([np.asarray(res.results[2 * b]["y"]),
                                     np.asarray(res.results[2 * b + 1]["y"])], 0)
                     for b in range(nb)], 0).astype(np.float32)
```

```python
from contextlib import ExitStack
import numpy as np
import ml_dtypes
import concourse.bass as bass
import concourse.mybir as mybir
from concourse.bass_utils import run_bass_kernel_spmd

F32 = mybir.dt.float32
BF16 = mybir.dt.bfloat16
I32 = mybir.dt.int32
U32 = mybir.dt.uint32
AF = mybir.ActivationFunctionType
ALU = mybir.AluOpType
AX = mybir.AxisListType

D = 1024
S = 8192
HALF = 4096
DIN = 5536
NEXP = 32
DEXP = 1024
LN_EPS = 1e-5
RMS_EPS = 1e-6
DN_ALPHA = 4.0 ** 0.25
NEG = -30000.0
NA_ROWS = 72

COMPUTE = ("pe", "act", "dve", "pool")


class Prog:
    def __init__(self, nc, stack, ring=8):
        self.nc = nc
        self.e = {"pe": nc.tensor, "act": nc.scalar, "dve": nc.vector,
                  "pool": nc.gpsimd, "sp": nc.sync}
        self.ops = []
        self.done = 0
        self.sem = {k: stack.enter_context(nc.semaphore("s_" + k)) for k in COMPUTE}
        self.ticket = {k: 0 for k in COMPUTE}
        self.ring = {q: [stack.enter_context(nc.semaphore("d_%s%d" % (q, i))) for i in range(ring)]
                     for q in ("sp", "pool")}
        self.ring_cnt = {q: [0] * ring for q in ("sp", "pool")}
        self.ring_last = {q: [None] * ring for q in ("sp", "pool")}
        self.ring_pos = {q: 0 for q in ("sp", "pool")}
        self.waited = {k: {} for k in self.e}
        self.last_w = {}
        self.readers = {}
        self.sig = {}
        self.eidx = {}
        self.ecount = {k: 0 for k in self.e}
        self.last_op = {k: None for k in self.e}
        self.dma_open = []

    def op(self, eng, fn, r=(), w=(), dma=False):
        self.ops.append((eng, fn, tuple(r), tuple(w), dma))

    def barrier(self):
        self.ops.append(("BAR", None, (), (), False))

    def _wait(self, eng, sem, val):
        cur = self.waited[eng].get(sem, 0)
        if cur < val:
            self.e[eng].wait_ge(sem, val)
            self.waited[eng][sem] = val

    def emit(self):
        ops = self.ops
        n = len(ops)
        start = self.done
        deps = {}
        needed = set()
        last_w, readers = self.last_w, self.readers
        eidx, ecount = self.eidx, self.ecount
        openg = {}
        for i in range(start, n):
            eng, fn, r, w, dma = ops[i]
            if eng == "BAR":
                continue
            eidx[i] = ecount[eng]
            ecount[eng] += 1
            openg[i] = (eng, dma)
            d = set()
            raw = set()
            for k in r:
                if k in last_w:
                    d.add(last_w[k])
                    raw.add(last_w[k])
                if isinstance(k, str) and k.startswith("ps"):
                    rd = readers.get(k)
                    if rd:
                        d.update(rd[0].values())
                        d.update(rd[1])
            for k in w:
                if k in last_w:
                    d.add(last_w[k])
                rd = readers.get(k)
                if rd:
                    d.update(rd[0].values())
                    d.update(rd[1])
            for k in r:
                rd = readers.setdefault(k, ({}, []))
                if dma:
                    rd[1].append(i)
                else:
                    rd[0][eng] = i
            for k in w:
                last_w[k] = i
                readers[k] = ({}, [])
            d.discard(i)
            keep = set()
            for j in d:
                if j < start and j not in self.sig and j not in openg:
                    continue
                jeng, jdma = self._opinfo(j, openg)
                if (not dma) and (not jdma) and jeng == eng:
                    if eng == "pe":
                        continue
                    if j in raw and eidx[i] - eidx[j] <= 2:
                        keep.add(j)
                    continue
                keep.add(j)
            deps[i] = keep
            needed.update(keep)
        self._openg_all = getattr(self, "_openg_all", {})
        self._openg_all.update(openg)
        for i in range(start, n):
            eng, fn, r, w, dma = ops[i]
            if eng == "BAR":
                self._emit_barrier()
                continue
            if dma:
                q = eng
                pos = self.ring_pos[q]
                self.ring_pos[q] = (pos + 1) % len(self.ring[q])
                sem = self.ring[q][pos]
                prev = self.ring_last[q][pos]
                if prev is not None:
                    self._wait(eng, sem, prev)
            for j in sorted(deps[i]):
                if j in self.sig:
                    s, v = self.sig[j]
                    self._wait(eng, s, v)
            ins = fn(self.e[eng])
            if dma:
                self.ring_cnt[q][pos] += 16
                val = self.ring_cnt[q][pos]
                ins.then_inc(sem, 16)
                self.ring_last[q][pos] = val
                self.sig[i] = (sem, val)
                self.dma_open.append(i)
            else:
                self.last_op[eng] = i
                if i in needed:
                    self.ticket[eng] += 1
                    ins.then_inc(self.sem[eng], 1)
                    self.sig[i] = (self.sem[eng], self.ticket[eng])
        self.done = n

    def _opinfo(self, j, openg):
        if j in openg:
            return openg[j]
        return self._openg_all[j]

    def _emit_barrier(self):
        marks = []
        for eng in COMPUTE:
            self.ticket[eng] += 1
            self.e[eng].drain().then_inc(self.sem[eng], 1)
            marks.append((self.sem[eng], self.ticket[eng]))
        dmas = [self.sig[i] for i in self.dma_open]
        self.dma_open = []
        for eng in self.e:
            for s, v in marks:
                self._wait(eng, s, v)
            for s, v in dmas:
                self._wait(eng, s, v)
        self.last_w.clear()
        self.readers.clear()


class Ctx:
    pass


_UID = [0]


def _sb(c, name, shape, dt):
    _UID[0] += 1
    return c.ph.enter_context(c.nc.sbuf_tensor("sb%d_%s" % (_UID[0], name), list(shape), dt))


def _ps(c, name, shape, dt):
    _UID[0] += 1
    return c.ph.enter_context(c.nc.psum_tensor("pp%d_%s" % (_UID[0], name), list(shape), dt))


def mm_group(p, out_ap, pairs, r, w):
    n = len(pairs)

    def fn(e):
        ins = None
        for i, (l, rr) in enumerate(pairs):
            ins = e.matmul(out_ap, l, rr, start=(i == 0), stop=(i == n - 1))
        return ins
    p.op("pe", fn, r, w)


def tr_group(p, outs_ins, ident, r, w):
    def fn(e):
        ins = None
        for o, i_ in outs_ins:
            ins = e.transpose(o, i_, ident)
        return ins
    p.op("pe", fn, r, w)


def dma(p, q, out_ap, in_ap, r, w):
    p.op(q, lambda e: e.dma_start(out=out_ap, in_=in_ap), r, w, dma=True)


def load_cast_cols(p, dst, src, ncols, r, w, step=2048):
    for c0 in range(0, ncols, step):
        c1 = min(ncols, c0 + step)
        dma(p, "pool", dst[:, c0:c1], src[:, c0:c1], r, w)


def phase1(c, L):
    nc, p = c.nc, c.p
    with ExitStack() as ph:
        c.ph = ph
        winb = _sb(c, "winb", [128, 8, DIN], BF16)
        wuqb = _sb(c, "wuqb", [128, 3, 384], BF16)
        wukvb = _sb(c, "wukvb", [128, 2, 512], BF16)
        identb = _sb(c, "identb", [128, 128], BF16)
        gq = _sb(c, "gq", [128, 384], F32)
        gk = _sb(c, "gk", [128, 128], F32)
        mq = _sb(c, "mq", [128, 384], F32)
        mkv = _sb(c, "mkv", [128, 256], F32)
        xf = [_sb(c, "xf%d" % i, [128, D], F32) for i in range(2)]
        xb = [_sb(c, "xb%d" % i, [128, D], BF16) for i in range(2)]
        xT = [_sb(c, "xT%d" % i, [128, 8, 512], BF16) for i in range(2)]
        rp = [_sb(c, "rp%d" % i, [128, 192], F32) for i in range(2)]
        wk = [_sb(c, "wk%d" % i, [128, 384], F32) for i in range(6)]
        sm = [_sb(c, "sm%d" % i, [128, 8], F32) for i in range(4)]
        ob = [_sb(c, "ob%d" % i, [128, 384], BF16) for i in range(8)]
        tT = [_sb(c, "tT%d" % i, [128, 512], BF16) for i in range(2)]
        blk = {k: [_sb(c, "blk_%s%d" % (k, i), [128, 6 * 512], BF16) for i in range(2)]
               for k in ("a", "b", "c", "d")}
        vna = [_sb(c, "vna%d" % i, [128, 6, 65], BF16) for i in range(2)]
        vg = [_sb(c, "vg%d" % i, [128, 2, 65], BF16) for i in range(2)]
        vm = [_sb(c, "vm%d" % i, [128, 4, 65], BF16) for i in range(2)]
        gout = [_sb(c, "gout%d" % i, [128, 512], BF16) for i in range(3)]
        psT = _ps(c, "psT", [128, 8, 128], BF16)
        psR = _ps(c, "psR", [128, 8, 128], BF16)
        psM = [_ps(c, "psM%d" % i, [128, 512], F32) for i in range(4)]
        psG = [_ps(c, "psG%d" % i, [128, 512], F32) for i in range(2)]

        w_in = c.w["w_in"][L]
        w_in_v = w_in.rearrange("(c p) n -> p c n", p=128)
        NSTG = 2
        stgw = [_sb(c, "stgw%d" % i, [128, 692], F32) for i in range(NSTG)]
        wi = 0
        for ch in range(8):
            for pc in range(8):
                c0 = pc * 692
                si = wi % NSTG
                dma(p, "sp", stgw[si][:], w_in_v[:, ch, c0:c0 + 692], [], [("stgw", si)])
                if wi % 2 == 0:
                    p.op("dve", lambda e, si=si, ch=ch, c0=c0: e.tensor_copy(winb[:, ch, c0:c0 + 692], stgw[si][:]),
                         [("stgw", si)], ["winb"])
                else:
                    p.op("act", lambda e, si=si, ch=ch, c0=c0: e.activation(out=winb[:, ch, c0:c0 + 692],
                                                                           in_=stgw[si][:], func=AF.Copy),
                         [("stgw", si)], ["winb"])
                wi += 1
        wuq_v = c.w["w_uq"][L].rearrange("(c p) n -> p c n", p=128)
        for ch in range(3):
            load_cast_cols(p, wuqb[:, ch, :], wuq_v[:, ch, :], 384, [], ["wuqb"])
        wukv_v = c.w["w_ukv"][L].rearrange("(c p) n -> p c n", p=128)
        for ch in range(2):
            load_cast_cols(p, wukvb[:, ch, :], wukv_v[:, ch, :], 512, [], ["wukvb"])
        dma(p, "sp", identb[:], c.w["identb"][:, :], [], ["identb"])
        dma(p, "sp", gq[:], c.w["gq_rep"][L], [], ["gq"])
        dma(p, "sp", gk[:], c.w["gk_rep"][L], [], ["gk"])
        dma(p, "sp", mq[:], c.w["mq_rep"][L], [], ["mq"])
        dma(p, "sp", mkv[:], c.w["mkv_rep"][L], [], ["mkv"])
        for i in range(2):
            p.op("pool", lambda e, t=vna[i]: e.memset(t[:], 1.0), [], [("vna", i)])
            p.op("pool", lambda e, t=vg[i]: e.memset(t[:], 1.0), [], [("vg", i)])
            p.op("pool", lambda e, t=vm[i]: e.memset(t[:], 1.0), [], [("vm", i)])

        cnt = {"tile": 0, "psM": 0, "psG": 0, "wk": 0, "sm": 0, "ob": 0, "tT": 0, "gout": 0}
        pending = []

        def st(out_ap, in_ap, r, w):
            dma(p, "sp", out_ap, in_ap, r, w)

        def flush():
            for o_, i_, r_, w_ in pending:
                dma(p, "sp", o_, i_, r_, w_)
            del pending[:]

        def nxt(k, n):
            v = cnt[k] % n
            cnt[k] += 1
            return v

        def load_tile(src_ap, rope_ap, to_xT=None, mask_col=None, defer_tr=False):
            s = nxt("tile", 2)
            dma(p, "sp", xf[s][:], src_ap, [], [("xf", s)])
            if rope_ap is not None:
                dma(p, "sp", rp[s][:], rope_ap, [], [("rp", s)])
            if mask_col is not None:
                p.op("dve", lambda e: e.tensor_scalar(out=xf[s][:], in0=xf[s][:],
                                                      scalar1=c.hm[:, mask_col:mask_col + 1], scalar2=None,
                                                      op0=ALU.mult), [("xf", s), "hm"], [("xf", s)])
            p.op("dve", lambda e: e.tensor_copy(xb[s][:], xf[s][:]), [("xf", s)], [("xb", s)])
            if to_xT is None:
                dst, key = tT_full[s][:], ("tTf", s)
            else:
                dst, key = to_xT

            def fin():
                tr_group(p, [(psT[:, ch, :], xb[s][:, ch * 128:(ch + 1) * 128]) for ch in range(8)],
                         identb[:], [("xb", s), "identb"], ["psT"])
                p.op("act", lambda e: e.activation(out=dst, in_=psT[:], func=AF.Copy), ["psT"], [key])
            if defer_tr:
                return dst, key, rp[s], ("rp", s), fin
            fin()
            return dst, key, rp[s], ("rp", s)

        tT_full = [_sb(c, "tTf%d" % i, [128, 8, 128], BF16) for i in range(2)]

        def tok_mm(xTa, xTkey, c0, c1):
            b = nxt("psM", 4)
            mm_group(p, psM[b][:, 0:c1 - c0],
                     [(xTa[:, ch, :], winb[:, ch, c0:c1]) for ch in range(8)],
                     [xTkey, "winb"], ["psM%d" % b])
            return psM[b], "psM%d" % b

        def transp_out(src_bf, src_key, nh, dh, dst_ap, dst_key):
            tr_group(p, [(psR[0:dh, h, :], src_bf[:, h * dh:(h + 1) * dh]) for h in range(nh)],
                     identb[:], [src_key, "identb"], ["psR"])
            p.op("dve", lambda e: e.tensor_copy(dst_ap, psR[0:dh, 0:nh, :]), ["psR"], [dst_key])

        def rms_rope(ps_ap, pskey, nh, dh, g_ap, gkey, rope_t, rpkey, coff, out_bf, outkey):
            n = nh * dh
            hd = dh // 2
            a = nxt("wk", 6); b2 = nxt("wk", 6); c2 = nxt("wk", 6); d2 = nxt("wk", 6); e2 = nxt("wk", 6)
            s1 = nxt("sm", 4)
            A, B, C, Dd, E = wk[a], wk[b2], wk[c2], wk[d2], wk[e2]
            p.op("act", lambda e: e.activation(out=A[:, 0:n], in_=ps_ap, func=AF.Square),
                 [pskey], [("wk", a)])
            p.op("dve", lambda e: e.tensor_reduce(
                out=sm[s1][:, 0:nh], in_=A[:, 0:n].rearrange("p (h d) -> p h d", h=nh),
                axis=AX.X, op=ALU.add), [("wk", a)], [("sm", s1)])
            p.op("act", lambda e: e.activation(
                out=sm[s1][:, 0:nh], in_=sm[s1][:, 0:nh], func=AF.Sqrt, scale=1.0 / dh, bias=c.eps_rms[:, 0:1]),
                [("sm", s1)], [("sm", s1)])
            p.op("dve", lambda e: e.reciprocal(out=sm[s1][:, 0:nh], in_=sm[s1][:, 0:nh]),
                 [("sm", s1)], [("sm", s1)])
            p.op("dve", lambda e: e.tensor_tensor(
                out=B[:, 0:n].rearrange("p (h d) -> p h d", h=nh),
                in0=ps_ap.rearrange("p (h d) -> p h d", h=nh),
                in1=sm[s1][:, 0:nh].unsqueeze(2).to_broadcast([128, nh, dh]),
                op=ALU.mult), [pskey, ("sm", s1)], [("wk", b2)])
            p.op("dve", lambda e: e.tensor_tensor(out=C[:, 0:n], in0=B[:, 0:n], in1=g_ap, op=ALU.mult),
                 [("wk", b2), gkey], [("wk", c2)])
            rope(C, ("wk", c2), nh, dh, rope_t, rpkey, coff, Dd, ("wk", d2), E, ("wk", e2),
                 out_bf[:, 0:n].rearrange("p (h d) -> p h d", h=nh), outkey)

        def rope(X, xkey, nh, dh, rope_t, rpkey, coff, Dd, dkey, E, ekey, out3, outkey, xview=None):
            n = nh * dh
            hd = dh // 2
            x3 = xview if xview is not None else X[:, 0:n].rearrange("p (h d) -> p h d", h=nh)
            cs = rope_t[:, coff:coff + dh].unsqueeze(1).to_broadcast([128, nh, dh])
            sslo = rope_t[:, coff + dh:coff + dh + hd].unsqueeze(1).to_broadcast([128, nh, hd])
            sshi = rope_t[:, coff + dh + hd:coff + 2 * dh].unsqueeze(1).to_broadcast([128, nh, hd])
            d3 = Dd[:, 0:n].rearrange("p (h d) -> p h d", h=nh)
            e3 = E[:, 0:n].rearrange("p (h d) -> p h d", h=nh)
            p.op("dve", lambda e: e.tensor_tensor(out=d3, in0=x3, in1=cs, op=ALU.mult),
                 [xkey, rpkey], [dkey])
            p.op("dve", lambda e: e.tensor_tensor(out=e3[:, :, 0:hd], in0=x3[:, :, hd:dh], in1=sslo,
                                                  op=ALU.mult), [xkey, rpkey], [ekey])
            p.op("dve", lambda e: e.tensor_tensor(out=e3[:, :, hd:dh], in0=x3[:, :, 0:hd], in1=sshi,
                                                  op=ALU.mult), [xkey, rpkey, ekey], [ekey])
            p.op("pool", lambda e: e.tensor_tensor(out=out3, in0=d3, in1=e3, op=ALU.add),
                 [dkey, ekey], [outkey])

        def rms_full(ps_ap, pskey, n, g_ap, gkey, out_bf, outkey):
            a = nxt("wk", 6)
            s1 = nxt("sm", 4)
            p.op("act", lambda e: e.activation(out=wk[a][:, 0:n], in_=ps_ap, func=AF.Square,
                                               accum_out=sm[s1][:, 0:1]),
                 [pskey], [("wk", a), ("sm", s1)])
            p.op("act", lambda e: e.activation(
                out=sm[s1][:, 1:2], in_=sm[s1][:, 0:1], func=AF.Sqrt, scale=1.0 / n, bias=c.eps_rms[:, 0:1]),
                [("sm", s1)], [("sm", s1)])
            p.op("dve", lambda e: e.reciprocal(out=sm[s1][:, 2:3], in_=sm[s1][:, 1:2]),
                 [("sm", s1)], [("sm", s1)])
            p.op("dve", lambda e: e.scalar_tensor_tensor(
                out=out_bf, in0=ps_ap, scalar=sm[s1][:, 2:3], in1=g_ap,
                op0=ALU.mult, op1=ALU.mult), [pskey, ("sm", s1), gkey], [outkey])

        def na_kv(xTa, xTkey, tok_off, sub, bs, vs=None):
            ps, pk = tok_mm(xTa, xTkey, 384, 768)
            o = nxt("ob", 8)
            p.op("act", lambda e, ps=ps, o=o: e.activation(out=ob[o][:, 0:384], in_=ps[:, 0:384],
                                                           func=AF.Copy), [pk], [("ob", o)])
            kb = blk["b"][bs][0:64, :].rearrange("p (h t) -> p h t", h=6)
            transp_out(ob[o], ("ob", o), 6, 64, kb[:, :, sub * 128:(sub + 1) * 128], ("blk_b", bs))
            ps, pk = tok_mm(xTa, xTkey, 768, 1152)
            s = (cnt["tile"] - 1) % 2 if vs is None else vs
            p.op("act", lambda e, ps=ps, s=s: e.activation(
                out=vna[s][:, :, 0:64], in_=ps[:, 0:384].rearrange("p (h d) -> p h d", h=6),
                func=AF.Copy), [pk], [("vna", s)])
            st(c.s["v_na"][tok_off:tok_off + 128, :], vna[s][:].rearrange("p h d -> p (h d)"),
                [("vna", s)], [("d_v_na", tok_off)])

        def flush_blk(name, bs, dh, nh, dst, t0, nt):
            src = blk[name][bs][0:dh, 0:nh * 512].rearrange("p (h t) -> p h t", h=nh)[:, :, 0:nt]
            st(dst[:, :, t0:t0 + nt].rearrange("h d t -> d h t"), src,
               [("blk_" + name, bs)], [("d_" + name, t0)])

        def run_interleaved(gens):
            active = list(gens)
            while active:
                for g_ in list(active):
                    try:
                        next(g_)
                    except StopIteration:
                        active.remove(g_)

        def own_tile(bi, sub):
            bs = bi % 2
            t = bi * 4 + sub
            xTa, xTkey, rpt, rpk = load_tile(
                c.q_src[t * 128:(t + 1) * 128, :], c.rope_q[t * 128:(t + 1) * 128, :],
                to_xT=(xT[bs][:, :, sub * 128:(sub + 1) * 128], ("xT", bs)))
            vs = (cnt["tile"] - 1) % 2
            ps, pk = tok_mm(xTa, xTkey, 1152, 1536)
            o = nxt("ob", 8)
            rms_rope(ps[:, 0:384], pk, 6, 64, gq[:], "gq", rpt, rpk, 0, ob[o], ("ob", o))
            yield
            ps, pk = tok_mm(xTa, xTkey, 0, 384)
            oq = nxt("ob", 8)
            p.op("act", lambda e, oq=oq, ps=ps: e.activation(out=ob[oq][:, 0:384], in_=ps[:, 0:384],
                                                             func=AF.Copy), [pk], [("ob", oq)])
            qa = blk["a"][bs][0:64, :].rearrange("p (h t) -> p h t", h=6)
            transp_out(ob[oq], ("ob", oq), 6, 64, qa[:, :, sub * 128:(sub + 1) * 128], ("blk_a", bs))
            qg = blk["c"][bs][0:64, :].rearrange("p (h t) -> p h t", h=6)
            transp_out(ob[o], ("ob", o), 6, 64, qg[:, :, sub * 128:(sub + 1) * 128], ("blk_c", bs))
            yield
            ps, pk = tok_mm(xTa, xTkey, 1792, 2176)
            o = nxt("ob", 8)
            rms_full(ps[:, 0:384], pk, 384, mq[:], "mq", ob[o][:, 0:384], ("ob", o))
            yield
            na_kv(xTa, xTkey, 256 + t * 128, sub, bs, vs)
            tt = nxt("tT", 2)
            tr_group(p, [(psR[:, ch, :], ob[o][:, ch * 128:(ch + 1) * 128]) for ch in range(3)],
                     identb[:], [("ob", o), "identb"], ["psR"])
            p.op("act", lambda e, tt=tt: e.activation(
                out=tT[tt][:, 0:384].rearrange("p (c t) -> p c t", c=3), in_=psR[:, 0:3, :],
                func=AF.Copy), ["psR"], [("tT", tt)])
            g = nxt("psG", 2)
            tT3 = tT[tt][:, 0:384].rearrange("p (c t) -> p c t", c=3)
            mm_group(p, psG[g][:, 0:384], [(tT3[:, ch, :], wuqb[:, ch, :]) for ch in range(3)],
                     [("tT", tt), "wuqb"], ["psG%d" % g])
            o2 = nxt("ob", 8)
            qc3 = psG[g][:, 0:384].rearrange("p (h d) -> p h d", h=4)
            out3 = ob[o2][:, 0:384].rearrange("p (h d) -> p h d", h=4)
            p.op("act", lambda e, qc3=qc3, out3=out3: e.activation(
                out=out3[:, :, 0:64], in_=qc3[:, :, 0:64], func=AF.Copy),
                ["psG%d" % g], [("ob", o2)])
            a = nxt("wk", 6); d2 = nxt("wk", 6); e2 = nxt("wk", 6)
            xr3 = wk[a][:, 0:128].rearrange("p (h d) -> p h d", h=4)
            p.op("act", lambda e, qc3=qc3, xr3=xr3: e.activation(out=xr3, in_=qc3[:, :, 64:96],
                                                                 func=AF.Copy),
                 ["psG%d" % g], [("wk", a)])
            rope(wk[a], ("wk", a), 4, 32, rpt, rpk, 128, wk[d2], ("wk", d2), wk[e2], ("wk", e2),
                 out3[:, :, 64:96], ("ob", o2))
            yield
            qm = blk["d"][bs][0:96, 0:2048].rearrange("p (h t) -> p h t", h=4)
            transp_out(ob[o2], ("ob", o2), 4, 96, qm[:, :, sub * 128:(sub + 1) * 128], ("blk_d", bs))

        for bi in range(8):
            bs = bi % 2
            run_interleaved([own_tile(bi, 0), own_tile(bi, 1)])
            run_interleaved([own_tile(bi, 2), own_tile(bi, 3)])
            flush_blk("a", bs, 64, 6, c.s["qT_na"], bi * 512, 512)
            flush_blk("b", bs, 64, 6, c.s["kT_na"], 256 + bi * 512, 512)
            flush_blk("c", bs, 64, 6, c.s["qT_gqa"], bi * 512, 512)
            flush_blk("d", bs, 96, 4, c.s["qT_mla"], bi * 512, 512)
            flush()
            for cc in range(24):
                g = nxt("psG", 2)
                c0 = 2464 + cc * 128
                mm_group(p, psG[g][:, :], [(winb[:, ch, c0:c0 + 128], xT[bs][:, ch, :]) for ch in range(8)],
                         [("xT", bs), "winb"], ["psG%d" % g])
                go = nxt("gout", 3)
                p.op("act", lambda e, g=g, go=go: e.activation(out=gout[go][:], in_=psG[g][:, :],
                                                               func=AF.Sigmoid),
                     ["psG%d" % g], [("gout", go)])
                dma(p, "sp", c.s["gT"][cc * 128:(cc + 1) * 128, bi * 512:(bi + 1) * 512], gout[go][:],
                    [("gout", go)], [("d_gT", cc, bi)])

        for hi in range(4):
            hsrc = (c.c_src[HALF - 256 + hi * 128:HALF - 256 + (hi + 1) * 128, :] if hi < 2 else
                    c.c_src[(hi - 2) * 128:(hi - 1) * 128, :])
            xTa, xTkey, rpt, rpk = load_tile(hsrc, None, mask_col=c.hm_cols[0 if hi < 2 else 1])
            flush()
            tok_off = hi * 128 if hi < 2 else 4352 + (hi - 2) * 128
            na_kv(xTa, xTkey, tok_off, hi % 2, 0)
            if hi % 2 == 1:
                flush_blk("b", 0, 64, 6, c.s["kT_na"], 0 if hi == 1 else 4352, 256)

        def full_tile(bi, sub):
            bs = bi % 2
            t = bi * 4 + sub
            xTa, xTkey, rpt, rpk = load_tile(
                c.full_src[t * 128:(t + 1) * 128, :], c.rope_full[t * 128:(t + 1) * 128, :])
            s = (cnt["tile"] - 1) % 2
            ps, pk = tok_mm(xTa, xTkey, 1536, 1792)
            o = nxt("ob", 8)
            rms_rope(ps[:, 0:128], pk, 2, 64, gk[:], "gk", rpt, rpk, 0, ob[o], ("ob", o))
            p.op("act", lambda e, ps=ps, s=s: e.activation(
                out=vg[s][:, :, 0:64], in_=ps[:, 128:256].rearrange("p (h d) -> p h d", h=2),
                func=AF.Copy), [pk], [("vg", s)])
            st(c.s["v_gqa"][t * 128:(t + 1) * 128, :], vg[s][:].rearrange("p h d -> p (h d)"),
               [("vg", s)], [("d_v_gqa", t)])
            yield
            ps, pk = tok_mm(xTa, xTkey, 2176, 2464)
            o3 = nxt("ob", 8)
            rms_full(ps[:, 0:256], pk, 256, mkv[:], "mkv", ob[o3][:, 0:256], ("ob", o3))
            a = nxt("wk", 6)
            p.op("act", lambda e, ps=ps, a=a: e.activation(out=wk[a][:, 0:32], in_=ps[:, 256:288],
                                                           func=AF.Copy), [pk], [("wk", a)])
            d2 = nxt("wk", 6); e2 = nxt("wk", 6); f2 = nxt("wk", 6)
            kr3 = wk[f2][:, 0:32].rearrange("p (h d) -> p h d", h=1)
            rope(wk[a], ("wk", a), 1, 32, rpt, rpk, 128, wk[d2], ("wk", d2), wk[e2], ("wk", e2),
                 kr3, ("wk", f2))
            o2 = nxt("ob", 8)
            out3 = ob[o2][:, 0:384].rearrange("p (h d) -> p h d", h=4)
            p.op("pool", lambda e, out3=out3, f2=f2: e.tensor_copy(
                out3[:, :, 64:96], wk[f2][:, 0:32].unsqueeze(1).to_broadcast([128, 4, 32])),
                [("wk", f2)], [("ob", o2)])
            yield
            kg = blk["a"][bs][0:64, 0:1024].rearrange("p (h t) -> p h t", h=2)
            transp_out(ob[o], ("ob", o), 2, 64, kg[:, :, sub * 128:(sub + 1) * 128], ("blk_a", bs))
            tt = nxt("tT", 2)
            tr_group(p, [(psR[:, ch, :], ob[o3][:, ch * 128:(ch + 1) * 128]) for ch in range(2)],
                     identb[:], [("ob", o3), "identb"], ["psR"])
            tT2 = tT[tt][:, 0:256].rearrange("p (c t) -> p c t", c=2)
            p.op("act", lambda e, tT2=tT2: e.activation(out=tT2, in_=psR[:, 0:2, :], func=AF.Copy),
                 ["psR"], [("tT", tt)])
            g = nxt("psG", 2)
            mm_group(p, psG[g][:, :], [(tT2[:, ch, :], wukvb[:, ch, :]) for ch in range(2)],
                     [("tT", tt), "wukvb"], ["psG%d" % g])
            kv3 = psG[g][:, :].rearrange("p (h d) -> p h d", h=4)
            p.op("act", lambda e, kv3=kv3, out3=out3: e.activation(
                out=out3[:, :, 0:64], in_=kv3[:, :, 0:64], func=AF.Copy),
                ["psG%d" % g, ("ob", o2)], [("ob", o2)])
            p.op("dve", lambda e, kv3=kv3, s=s: e.tensor_copy(vm[s][:, :, 0:64], kv3[:, :, 64:128]),
                 ["psG%d" % g], [("vm", s)])
            st(c.s["v_mla"][t * 128:(t + 1) * 128, :], vm[s][:].rearrange("p h d -> p (h d)"),
               [("vm", s)], [("d_v_mla", t)])
            yield
            km = blk["d"][bs][0:96, 0:2048].rearrange("p (h t) -> p h t", h=4)
            transp_out(ob[o2], ("ob", o2), 4, 96, km[:, :, sub * 128:(sub + 1) * 128], ("blk_d", bs))

        flush()
        for bi in range(16 if c.do_full else 0):
            bs = bi % 2
            run_interleaved([full_tile(bi, 0), full_tile(bi, 1)])
            run_interleaved([full_tile(bi, 2), full_tile(bi, 3)])
            flush_blk("a", bs, 64, 2, c.s["kT_gqa"], bi * 512, 512)
            flush_blk("d", bs, 96, 4, c.s["kT_mla"], bi * 512, 512)
        flush()
        p.barrier()
        p.emit()


W_SHAPES = {
    "w_in": ([D, DIN], F32), "w_uq": ([384, 384], F32), "w_ukv": ([256, 512], F32),
    "w_ba": ([384, D], F32), "w_bb": ([384, D], F32), "w_bc": ([256, D], F32),
    "w_out": ([D, D], F32), "w_router": ([D, NEXP], F32),
    "w_gu": ([NEXP, D, 2 * DEXP], F32), "w_dn": ([NEXP, DEXP, D], F32),
    "gq_rep": ([128, 384], F32), "gk_rep": ([128, 128], F32),
    "mq_rep": ([128, 384], F32), "mkv_rep": ([128, 256], F32),
    "ln1g": ([128, D], F32), "ln1b": ([128, D], F32), "ln2g": ([128, D], F32), "ln2b": ([128, D], F32),
    "brouter": ([128, NEXP], F32), "bgu_t": ([128, NEXP, 16], F32), "bdn": ([NEXP, D], F32),
    "na_bint": ([64, 6, 8, 64], F32), "na_bbnd": ([7, 64, 6, 12, 64], F32),
    "na_bbnd_o": ([7, 64, 6, 12, 64], F32),
}
C_SHAPES = {
    "rope_loc": ([S, 192], F32), "hmask": ([128, 4], F32),
    "identb": ([128, 128], BF16), "identf": ([128, 128], F32),
    "triu": ([128, 128], F32), "ones128": ([128, 128], F32),
    "iota_e": ([128, NEXP], F32), "iota_cap": ([128, NEXP], F32),
}
CAP = 768
NSLOT = NEXP * CAP
S_SHAPES = {
    "qT_na": ([6, 64, HALF], BF16), "kT_na": ([6, 64, NA_ROWS * 64], BF16),
    "v_na": ([NA_ROWS * 64, 390], BF16),
    "qT_gqa": ([6, 64, HALF], BF16), "kT_gqa": ([2, 64, S], BF16), "v_gqa": ([S, 130], BF16),
    "qT_mla": ([4, 96, HALF], BF16), "kT_mla": ([4, 96, S], BF16), "v_mla": ([S, 260], BF16),
    "gT": ([3 * D, HALF], BF16),
    "oT": ([16, 64, HALF], BF16),
    "x1": ([HALF, D], F32),
    "x1T": ([D, HALF], BF16), "gateT": ([NEXP, HALF], F32), "ymoe": ([HALF, D], F32),
    "xg": ([NSLOT, D], BF16), "yg": ([NSLOT, D], F32),
    "slots": ([HALF, 4], I32), "gks": ([HALF, 4], F32),
}


def build(taps=(), passes=("A", "B", "C"), phases=None, dst_override=None):
    nc = bass.Bass("TRN2", target_bir_lowering=False)
    phases = phases or ALL_PHASES
    c = Ctx()
    c.nc = nc
    c.w = {}
    for k, (shp, dt) in W_SHAPES.items():
        c.w[k] = nc.dram_tensor(k, [2] + shp, dt, kind="ExternalInput").ap()
    for k, (shp, dt) in C_SHAPES.items():
        c.w[k] = nc.dram_tensor(k, shp, dt, kind="ExternalInput").ap()
    x_loc = nc.dram_tensor("x_loc", [S, D], F32, kind="ExternalInput").ap()
    c.s = {}
    for k, (shp, dt) in S_SHAPES.items():
        kind = "ExternalOutput" if k in taps else "Internal"
        c.s[k] = nc.dram_tensor("s_" + k, shp, dt, kind=kind).ap()
    y01 = nc.dram_tensor("y01", [S, D], F32, kind="Internal").ap()
    c.y = nc.dram_tensor("y", [HALF, D], F32, kind="ExternalOutput").ap()
    with ExitStack() as stack:
        c.p = Prog(nc, stack)
        c.breg = nc.gpsimd.to_reg(NSLOT - 1)
        c.eps_rms = stack.enter_context(nc.sbuf_tensor("eps_rms", [128, 1], F32))
        c.eps_ln = stack.enter_context(nc.sbuf_tensor("eps_ln", [128, 1], F32))
        c.hm = stack.enter_context(nc.sbuf_tensor("hmask_sb", [128, 4], F32))
        c.p.op("pool", lambda e: e.memset(c.eps_rms[:], RMS_EPS), [], ["eps"])
        c.p.op("pool", lambda e: e.memset(c.eps_ln[:], LN_EPS), [], ["eps"])
        dma(c.p, "sp", c.hm[:], c.w["hmask"][:, :], [], ["hm"])
        c.p.barrier()
        c.p.emit()
        rope = c.w["rope_loc"]
        c.rope_full = rope
        with ExitStack() as ph:
            zt = ph.enter_context(nc.sbuf_tensor("zt", [128, 8, D], BF16))
            c.p.op("pool", lambda e: e.memset(zt[:], 0.0), [], ["zt"])
            xg_v = c.s["xg"].rearrange("(n p) d -> p n d", p=128)
            for i in range(NSLOT // 1024):
                dma(c.p, "sp", xg_v[:, i * 8:(i + 1) * 8, :], zt[:], ["zt"], [("d_xg0", i)])
            c.p.barrier()
            c.p.emit()
        for ps_ in passes:
            if ps_ == "A":
                L, c.q_src, c.c_src, c.full_src = 0, x_loc[0:HALF, :], x_loc[HALF:S, :], x_loc
                c.rope_q, c.bbnd, c.hm_cols, c.do_full, dst = rope[0:HALF, :], c.w["na_bbnd"][0], (0, 1), True, y01[0:HALF, :]
            elif ps_ == "B":
                L, c.q_src, c.c_src, c.full_src = 0, x_loc[HALF:S, :], x_loc[0:HALF, :], x_loc
                c.rope_q, c.bbnd, c.hm_cols, c.do_full, dst = rope[HALF:S, :], c.w["na_bbnd_o"][0], (2, 3), False, y01[HALF:S, :]
            else:
                L, c.q_src, c.c_src, c.full_src = 1, y01[0:HALF, :], y01[HALF:S, :], y01
                c.rope_q, c.bbnd, c.hm_cols, c.do_full, dst = rope[0:HALF, :], c.w["na_bbnd"][1], (0, 1), True, c.y
            if "p1" in phases:
                phase1(c, L)
            if "na" in phases:
                phase_na(c, L)
            if "gqa" in phases:
                phase_dense_attn(c, L, "gqa")
            if "mla" in phases:
                phase_dense_attn(c, L, "mla")
            if "merge" in phases:
                phase_merge(c, L)
            if "moe" in phases:
                phase_moe(c, L)
            if "moes" in phases:
                phase_moe_sparse(c, L)
            if "ln2" in phases:
                phase_ln2(c, L, dst if dst_override is None else dst_override(c), sparse=("moes" in phases))
        c.p.barrier()
        c.p.emit()
    return nc


def rope_tables():
    t = np.arange(S)
    row = (t // 64).astype(np.float32)
    col = (t % 64).astype(np.float32)
    out = []
    for dim in (64, 32):
        quarter = dim // 4
        inv = (10000.0 ** (-np.arange(quarter, dtype=np.float32) / quarter)).astype(np.float32)
        ang = np.concatenate([row[:, None] * inv, col[:, None] * inv], -1).astype(np.float32)
        cs, sn = np.cos(ang).astype(np.float32), np.sin(ang).astype(np.float32)
        out.append(np.concatenate([cs, cs], -1))
        out.append(np.concatenate([-sn, sn], -1))
    return np.ascontiguousarray(np.concatenate(out, -1).astype(np.float32))


def na_bias_tables(rpb, hf):
    cols = np.arange(64)
    c0 = np.clip(cols - 8, 0, 48)
    in_win = (cols[None, :] >= c0[:, None]) & (cols[None, :] < c0[:, None] + 16)
    idx_c = np.clip(cols[None, :] - cols[:, None] + 15, 0, 30)

    def tab(j, rows):
        r = hf * 64 + j
        r0 = int(np.clip(r - 4, 0, 120))
        out = np.full((64, 6, len(rows), 64), NEG, np.float32)
        for ii, lr in enumerate(rows):
            gr = hf * 64 + lr - 4
            i = gr - r0
            if i < 0 or i >= 8 or gr < 0 or gr >= 128:
                continue
            ir = gr - r + 7
            b = rpb[:, ir][:, idx_c]
            b = np.where(in_win[None], b, NEG)
            out[:, :, ii, :] = b.transpose(2, 0, 1)
        return out
    interior = tab(10, list(range(10, 18)))
    bnd = []
    for j in (0, 1, 2, 3):
        bnd.append(tab(j, list(range(0, 12))))
    for j in (61, 62, 63):
        bnd.append(tab(j, list(range(60, 72))))
    return interior, np.stack(bnd, 0)


def prep_weights(inp, layers, hf):
    w = {}
    ls = list(layers)
    st = lambda f: np.ascontiguousarray(np.stack([f(l) for l in ls], 0))
    w["w_in"] = st(lambda l: inp["w_in"][l])
    w["w_uq"] = st(lambda l: inp["w_uq"][l])
    w["w_ukv"] = st(lambda l: inp["w_ukv"][l])
    w["w_ba"] = st(lambda l: inp["w_branch_a"][l])
    w["w_bb"] = st(lambda l: inp["w_branch_b"][l])
    w["w_bc"] = st(lambda l: inp["w_branch_c"][l])
    w["w_out"] = st(lambda l: inp["w_out"][l])
    w["w_router"] = st(lambda l: inp["w_router"][l])
    w["w_gu"] = st(lambda l: inp["w_gate_up"][l])
    w["w_dn"] = st(lambda l: inp["w_down"][l])
    w["gq_rep"] = st(lambda l: np.tile(inp["gqa_q_norm"][l][None, :], (128, 6)))
    w["gk_rep"] = st(lambda l: np.tile(inp["gqa_k_norm"][l][None, :], (128, 2)))
    w["mq_rep"] = st(lambda l: np.tile(inp["mla_q_norm"][l][None, :], (128, 1)))
    w["mkv_rep"] = st(lambda l: np.tile(inp["mla_kv_norm"][l][None, :], (128, 1)))
    for k, src in (("ln1g", "ln1_g"), ("ln1b", "ln1_b"), ("ln2g", "ln2_g"), ("ln2b", "ln2_b")):
        w[k] = st(lambda l: np.tile(inp[src][l][None, :], (128, 1)))
    w["brouter"] = st(lambda l: np.tile(inp["b_router"][l][None, :], (128, 1)))
    w["bgu_t"] = st(lambda l: inp["b_gate_up"][l].reshape(NEXP, 16, 128).transpose(2, 0, 1))
    w["bdn"] = st(lambda l: inp["b_down"][l])
    bi, bb = zip(*[na_bias_tables(inp["na_rpb"][l], hf) for l in ls])
    w["na_bint"] = np.ascontiguousarray(np.stack(bi, 0))
    w["na_bbnd"] = np.ascontiguousarray(np.stack(bb, 0))
    w["na_bbnd_o"] = np.ascontiguousarray(np.stack([na_bias_tables(inp["na_rpb"][l], 1 - hf)[1] for l in ls], 0))
    return {k: np.ascontiguousarray(v.astype(np.float32)) for k, v in w.items()}


def prep_consts(hf):
    rt = rope_tables()
    own, oth = rt[hf * HALF:(hf + 1) * HALF], rt[(1 - hf) * HALF:(2 - hf) * HALF]
    hm = np.zeros((128, 4), np.float32)
    hm[:, 0], hm[:, 1], hm[:, 2], hm[:, 3] = hf, 1 - hf, 1 - hf, hf
    return {
        "rope_loc": np.ascontiguousarray(np.concatenate([own, oth], 0)),
        "hmask": hm,
        "identb": np.eye(128, dtype=np.float32).astype(ml_dtypes.bfloat16),
        "identf": np.eye(128, dtype=np.float32),
        "triu": np.triu(np.ones((128, 128), np.float32), 1),
        "ones128": np.ones((128, 128), np.float32),
        "iota_e": np.tile(np.arange(NEXP, dtype=np.float32)[None, :], (128, 1)),
        "iota_cap": np.tile((np.arange(NEXP, dtype=np.float32) * CAP)[None, :], (128, 1)),
    }


def prep_acts(xb, hf):
    own, oth = xb[hf * HALF:(hf + 1) * HALF], xb[(1 - hf) * HALF:(2 - hf) * HALF]
    return {"x_loc": np.ascontiguousarray(np.concatenate([own, oth], 0))}


def _normalize(c, psO, okey, ncol, rsb, rkey, psB, ou, oukey, onesf, dst_ap, dst_key, nh=1):
    p = c.p
    p.op("dve", lambda e: e.reciprocal(out=rsb[64:65, 0:ncol], in_=psO[64:65, 0:ncol]), [okey], [rkey])
    p.op("pe", lambda e: e.matmul(psB[0:64, 0:ncol], onesf[64:65, 0:64], rsb[64:65, 0:ncol],
                                  start=True, stop=True), [rkey, "onesf"], ["psB"])
    p.op("act", lambda e: e.activation(out=ou[0:64, 0:ncol], in_=psO[0:64, 0:ncol], func=AF.Copy),
         [okey], [oukey])
    a0 = ou[0:64, 0:ncol]
    a1 = psB[0:64, 0:ncol]
    if nh > 1:
        a0 = a0.rearrange("p (h q) -> p h q", h=nh)
        a1 = a1.rearrange("p (h q) -> p h q", h=nh)
    p.op("dve", lambda e: e.tensor_tensor(out=dst_ap, in0=a0, in1=a1, op=ALU.mult),
         [oukey, "psB"], [dst_key])


def phase_dense_attn(c, L, kind):
    nc, p = c.nc, c.p
    if kind == "gqa":
        nH, dh, nK, nV, scale, obase = 6, 64, 2, 2, 64 ** -0.5, 6
        qT, kT, vS = c.s["qT_gqa"], c.s["kT_gqa"], c.s["v_gqa"]
        kmap = [0, 0, 0, 1, 1, 1]
    else:
        nH, dh, nK, nV, scale, obase = 4, 96, 4, 4, 96 ** -0.5, 12
        qT, kT, vS = c.s["qT_mla"], c.s["kT_mla"], c.s["v_mla"]
        kmap = [0, 1, 2, 3]
    with ExitStack() as ph:
        c.ph = ph
        KT = _sb(c, "KT", [128, nK, S], BF16)
        V = _sb(c, "V", [128, 64, nV * 65], BF16)
        Q = [_sb(c, "Q%d" % i, [128, nH, 512], BF16) for i in range(2)]
        pT = [_sb(c, "pT%d" % i, [128, 1024], BF16) for i in range(3)]
        rsb = _sb(c, "rsb", [128, 512], F32)
        ou = _sb(c, "ou", [128, 512], F32)
        ot = [_sb(c, "ot%d" % i, [128, 512], BF16) for i in range(2)]
        onesf = _sb(c, "onesf", [128, 64], F32)
        psS = [_ps(c, "psS%d" % i, [128, 1024], F32) for i in range(2)]
        psO = [_ps(c, "psO%d" % i, [128, 512], F32) for i in range(2)]
        psB = _ps(c, "psB", [128, 512], F32)
        p.op("pool", lambda e: e.memset(onesf[:], 1.0), [], ["onesf"])
        pack = (kind == "gqa")
        if pack:
            kT2 = kT.rearrange("g d t -> (g d) t")
            for half in range(2):
                dma(p, "sp", KT[:, 0, half * HALF:(half + 1) * HALF], kT2[:, half * HALF:(half + 1) * HALF],
                    [], ["KT"])
            for i in range(2):
                p.op("pool", lambda e, i=i: e.memset(Q[i][:], 0.0), [], [("Q", i)])
        else:
            for k in range(nK):
                for half in range(2):
                    dma(p, "sp", KT[0:dh, k, half * HALF:(half + 1) * HALF],
                        kT[k, :, half * HALF:(half + 1) * HALF], [], ["KT"])
        vv = vS.rearrange("(t p) c -> p t c", p=128)
        for q4 in range(16):
            dma(p, "sp", V[:, q4 * 4:(q4 + 1) * 4, :], vv[:, q4 * 4:(q4 + 1) * 4, :], [], ["V"])
        it = 0
        for qb in range(8):
            qs = qb % 2
            if pack:
                dma(p, "sp", Q[qs][0:64, 0:3, :], qT[0:3, :, qb * 512:(qb + 1) * 512].rearrange("h d t -> d h t"),
                    [], [("Q", qs)])
                dma(p, "sp", Q[qs][64:128, 3:6, :], qT[3:6, :, qb * 512:(qb + 1) * 512].rearrange("h d t -> d h t"),
                    [], [("Q", qs)])
            else:
                dma(p, "sp", Q[qs][0:dh, :, :], qT[:, :, qb * 512:(qb + 1) * 512].rearrange("h d t -> d h t"),
                    [], [("Q", qs)])
            for h in range(nH):
                ob_ = it % 2
                it += 1
                okey = "psO%d" % ob_
                ki = kmap[h]

                def qk2(k2, h=h, ki=ki, qs=qs):
                    b = k2 % 2

                    def fn(e):
                        ins = None
                        for u in range(2):
                            kt = 2 * k2 + u
                            if pack:
                                ins = e.matmul(psS[b][:, u * 512:(u + 1) * 512], KT[:, 0, kt * 128:(kt + 1) * 128],
                                               Q[qs][:, h, :], start=True, stop=True)
                            else:
                                ins = e.matmul(psS[b][:, u * 512:(u + 1) * 512],
                                               KT[0:dh, ki, kt * 128:(kt + 1) * 128], Q[qs][0:dh, h, :],
                                               start=True, stop=True)
                        return ins
                    p.op("pe", fn, ["KT", ("Q", qs)], ["psS%d" % b])
                qk2(0)
                for k2 in range(32):
                    b = k2 % 2
                    pb = k2 % 3
                    if k2 + 1 < 32:
                        qk2(k2 + 1)
                    p.op("act", lambda e, b=b, pb=pb: e.activation(out=pT[pb][:], in_=psS[b][:, :],
                                                                   func=AF.Exp, scale=scale),
                         ["psS%d" % b], [("pT", pb)])

                    def fpv(e, k2=k2, pb=pb, ob_=ob_, ki=ki):
                        ins = None
                        for u in range(2):
                            kt = 2 * k2 + u
                            ins = e.matmul(psO[ob_][0:65, :], V[:, kt, ki * 65:(ki + 1) * 65],
                                           pT[pb][:, u * 512:(u + 1) * 512],
                                           start=(kt == 0), stop=(kt == 63))
                        return ins
                    p.op("pe", fpv, ["V", ("pT", pb)], [okey])
                os_ = it % 2
                _normalize(c, psO[ob_], okey, 512, rsb, "rsb", psB, ou, "ou", onesf,
                           ot[os_][0:64, :], ("ot", os_))
                dma(p, "sp", c.s["oT"][obase + h, :, qb * 512:(qb + 1) * 512], ot[os_][0:64, :],
                    [("ot", os_)], [("d_oT", obase + h, qb)])
        p.barrier()
        p.emit()


def phase_na(c, L):
    nc, p = c.nc, c.p
    with ExitStack() as ph:
        c.ph = ph
        Qb = [_sb(c, "Qb%d" % i, [128, 6, 512], BF16) for i in range(2)]
        Kb = [_sb(c, "Kb%d" % i, [128, 6, 1024], BF16) for i in range(2)]
        Vb = [_sb(c, "Vb%d" % i, [128, 16, 390], BF16) for i in range(2)]
        bint = _sb(c, "bint", [128, 6, 8, 64], F32)
        bbnd = _sb(c, "bbnd", [128, 6, 12, 64], F32)
        sc = [_sb(c, "sc%d" % i, [128, 768], F32) for i in range(2)]
        pp = [_sb(c, "pp%d" % i, [128, 768], BF16) for i in range(2)]
        rsb = _sb(c, "rsb", [128, 512], F32)
        ou = _sb(c, "ou", [128, 512], F32)
        ot = [_sb(c, "ot%d" % i, [128, 6, 512], BF16) for i in range(2)]
        onesf = _sb(c, "onesf", [128, 64], F32)
        psS = [_ps(c, "psS%d" % i, [128, 1024], F32) for i in range(2)]
        psO = [_ps(c, "psO%d" % i, [128, 512], F32) for i in range(2)]
        psB = _ps(c, "psB", [128, 512], F32)
        p.op("pool", lambda e: e.memset(onesf[:], 1.0), [], ["onesf"])
        dma(p, "sp", bint[0:64], c.w["na_bint"][L], [], ["bint"])
        vrow = c.s["v_na"].rearrange("(r k) c -> k r c", k=64)
        it = 0
        for b8 in range(8):
            s = b8 % 2
            dma(p, "sp", Qb[s][0:64, :, :], c.s["qT_na"][:, :, b8 * 512:(b8 + 1) * 512].rearrange("h d t -> d h t"),
                [], [("Qb", s)])
            dma(p, "sp", Kb[s][0:64, :, :], c.s["kT_na"][:, :, b8 * 512:b8 * 512 + 1024].rearrange("h d t -> d h t"),
                [], [("Kb", s)])
            for hv in range(2):
                dma(p, "sp", Vb[s][0:64, hv * 8:(hv + 1) * 8, :], vrow[:, b8 * 8 + hv * 8:b8 * 8 + (hv + 1) * 8, :],
                    [], [("Vb", s)])
            for jj in range(8):
                j = b8 * 8 + jj
                if j < 4:
                    rows = list(range(0, 12)); bidx = j
                elif j > 60:
                    rows = list(range(60, 72)); bidx = 4 + (j - 61)
                else:
                    rows = list(range(j, j + 8)); bidx = None
                nr = len(rows)
                if bidx is not None:
                    dma(p, "sp", bbnd[0:64], c.bbnd[bidx], [], ["bbnd"])
                    btile, bkey = bbnd, "bbnd"
                else:
                    btile, bkey = bint, "bint"
                ob_ = j % 2
                okey = "psO%d" % ob_
                for h in range(6):
                    sb_ = it % 2
                    it += 1
                    skey = "psS%d" % sb_

                    def fqk(e, h=h, sb_=sb_, rows=rows, jj=jj, s=s, b8=b8):
                        ins = None
                        for i, lr in enumerate(rows):
                            ins = e.matmul(psS[sb_][0:64, i * 64:(i + 1) * 64],
                                           Kb[s][0:64, h, (lr - b8 * 8) * 64:(lr - b8 * 8 + 1) * 64],
                                           Qb[s][0:64, h, jj * 64:(jj + 1) * 64], start=True, stop=True)
                        return ins
                    p.op("pe", fqk, [("Qb", s), ("Kb", s)], [skey])
                    p.op("dve", lambda e, h=h, sb_=sb_, nr=nr, btile=btile: e.scalar_tensor_tensor(
                        out=sc[sb_][0:64, 0:nr * 64], in0=psS[sb_][0:64, 0:nr * 64], scalar=0.125,
                        in1=btile[0:64, h, 0:nr, :].rearrange("p r q -> p (r q)"),
                        op0=ALU.mult, op1=ALU.add), [skey, bkey], [("sc", sb_)])
                    p.op("act", lambda e, sb_=sb_, nr=nr: e.activation(
                        out=pp[sb_][0:64, 0:nr * 64], in_=sc[sb_][0:64, 0:nr * 64], func=AF.Exp),
                        [("sc", sb_)], [("pp", sb_)])

                    def fpv(e, h=h, sb_=sb_, rows=rows, ob_=ob_, s=s, b8=b8):
                        ins = None
                        n = len(rows)
                        for i, lr in enumerate(rows):
                            ins = e.matmul(psO[ob_][0:65, h * 64:(h + 1) * 64],
                                           Vb[s][0:64, lr - b8 * 8, h * 65:(h + 1) * 65],
                                           pp[sb_][0:64, i * 64:(i + 1) * 64],
                                           start=(i == 0), stop=(i == n - 1))
                        return ins
                    p.op("pe", fpv, [("Vb", s), ("pp", sb_)], [okey])
                _normalize(c, psO[ob_], okey, 384, rsb, "rsb", psB, ou, "ou", onesf,
                           ot[s][0:64, :, jj * 64:(jj + 1) * 64],
                           ("ot", s), nh=6)
            dma(p, "sp", c.s["oT"][0:6, :, b8 * 512:(b8 + 1) * 512].rearrange("h d t -> d h t"),
                ot[s][0:64, :, :], [("ot", s)], [("d_oT_na", b8)])
        p.barrier()
        p.emit()


def layer_norm_tile(c, tsb, tkey, junk, jkey, sm, smkey, g_t, b_t, gbkey, out_t, outkey):
    p = c.p
    p.op("act", lambda e: e.activation(out=junk[:], in_=tsb[:], func=AF.Copy, accum_out=sm[:, 0:1]),
         [tkey], [jkey, smkey])
    p.op("act", lambda e: e.activation(out=junk[:], in_=tsb[:], func=AF.Square, accum_out=sm[:, 1:2]),
         [tkey], [jkey, smkey])
    p.op("dve", lambda e: e.tensor_scalar(out=sm[:, 2:3], in0=sm[:, 0:1], scalar1=1.0 / D, scalar2=None,
                                          op0=ALU.mult), [smkey], [smkey])
    p.op("dve", lambda e: e.tensor_tensor(out=sm[:, 3:4], in0=sm[:, 2:3], in1=sm[:, 2:3], op=ALU.mult),
         [smkey], [smkey])
    p.op("dve", lambda e: e.scalar_tensor_tensor(out=sm[:, 4:5], in0=sm[:, 1:2], scalar=1.0 / D,
                                                 in1=sm[:, 3:4], op0=ALU.mult, op1=ALU.subtract),
         [smkey], [smkey])
    p.op("act", lambda e: e.activation(out=sm[:, 5:6], in_=sm[:, 4:5], func=AF.Sqrt,
                                       bias=c.eps_ln[:, 0:1]), [smkey], [smkey])
    p.op("dve", lambda e: e.reciprocal(out=sm[:, 6:7], in_=sm[:, 5:6]), [smkey], [smkey])
    p.op("dve", lambda e: e.tensor_scalar(out=junk[:], in0=tsb[:], scalar1=sm[:, 2:3], scalar2=sm[:, 6:7],
                                          op0=ALU.subtract, op1=ALU.mult), [tkey, smkey], [jkey])
    p.op("pool", lambda e: e.tensor_tensor(out=junk[:], in0=junk[:], in1=g_t[:], op=ALU.mult),
         [jkey, gbkey], [jkey])
    p.op("pool", lambda e: e.tensor_tensor(out=out_t[:], in0=junk[:], in1=b_t[:], op=ALU.add),
         [jkey, gbkey], [outkey])


def phase_merge(c, L):
    nc, p = c.nc, c.p
    with ExitStack() as ph:
        c.ph = ph
        wb = _sb(c, "wb", [128, 8, D], BF16)
        woutb = _sb(c, "woutb", [128, 8, D], BF16)
        lng = _sb(c, "lng", [128, D], F32)
        lnb = _sb(c, "lnb", [128, D], F32)
        wr = _sb(c, "wr", [128, 8, NEXP], F32)
        br = _sb(c, "br", [128, NEXP], F32)
        identb = _sb(c, "identb", [128, 128], BF16)
        identf = _sb(c, "identf", [128, 128], F32)
        oTb = [_sb(c, "oTb%d" % i, [128, 8, 512], BF16) for i in range(2)]
        gTc = [_sb(c, "gTc%d" % i, [128, 3, 512], BF16) for i in range(2)]
        mixT = [_sb(c, "mixT%d" % i, [128, 8, 512], BF16) for i in range(2)]
        mt = [_sb(c, "mt%d" % i, [128, 512], F32) for i in range(6)]
        xo = [_sb(c, "xo%d" % i, [128, D], F32) for i in range(2)]
        tsb = [_sb(c, "tsb%d" % i, [128, D], F32) for i in range(2)]
        junk = _sb(c, "junk", [128, D], F32)
        x1t = [_sb(c, "x1t%d" % i, [128, D], F32) for i in range(2)]
        x1b = [_sb(c, "x1b%d" % i, [128, D], BF16) for i in range(2)]
        x1Tb = [_sb(c, "x1Tb%d" % i, [128, 8, 128], BF16) for i in range(2)]
        x1Tf = [_sb(c, "x1Tf%d" % i, [128, 8, 128], F32) for i in range(2)]
        sm = [_sb(c, "sm%d" % i, [128, 8], F32) for i in range(2)]
        rt_ = [_sb(c, "rt%d" % i, [128, 160], F32) for i in range(2)]
        gTt = [_sb(c, "gTt%d" % i, [128, 128], F32) for i in range(2)]
        psY = [_ps(c, "psY%d" % i, [128, 512], F32) for i in range(3)]
        psOut = [_ps(c, "psOut%d" % i, [128, 512], F32) for i in range(2)]
        psT = _ps(c, "psT", [128, 8, 128], BF16)
        psTf = _ps(c, "psTf", [128, 8, 128], F32)
        triu = _sb(c, "triu", [128, 128], F32)
        ones128 = _sb(c, "ones128", [128, 128], F32)
        iota_e = _sb(c, "iota_e", [128, NEXP], F32)
        iota_cap = _sb(c, "iota_cap", [128, NEXP], F32)
        msum = _sb(c, "msum", [128, NEXP], F32)
        rx = [_sb(c, "rx%d" % i, [128, 160], F32) for i in range(2)]
        idxu = [_sb(c, "idxu%d" % i, [128, 8], U32) for i in range(2)]
        slotu = [_sb(c, "slotu%d" % i, [128, 4], I32) for i in range(2)]
        dma(p, "sp", triu[:], c.w["triu"][:, :], [], ["triu"])
        dma(p, "sp", ones128[:], c.w["ones128"][:, :], [], ["ones128"])
        dma(p, "sp", iota_e[:], c.w["iota_e"][:, :], [], ["iota_e"])
        dma(p, "sp", iota_cap[:], c.w["iota_cap"][:, :], [], ["iota_cap"])
        p.op("pool", lambda e: e.memset(msum[:], 0.0), [], ["msum"])

        for pp in range(8):
            src = (c.w["w_ba"][L][pp * 128:(pp + 1) * 128, :] if pp < 3 else
                   c.w["w_bb"][L][(pp - 3) * 128:(pp - 2) * 128, :] if pp < 6 else
                   c.w["w_bc"][L][(pp - 6) * 128:(pp - 5) * 128, :])
            dma(p, "pool", wb[:, pp, :], src, [], ["wb"])
        wo_v = c.w["w_out"][L].rearrange("(c p) n -> p c n", p=128)
        for ch in range(8):
            dma(p, "pool", woutb[:, ch, :], wo_v[:, ch, :], [], ["woutb"])
        dma(p, "sp", lng[:], c.w["ln1g"][L], [], ["lngb"])
        dma(p, "sp", lnb[:], c.w["ln1b"][L], [], ["lngb"])
        dma(p, "sp", wr[:], c.w["w_router"][L].rearrange("(c p) e -> p c e", p=128), [], ["wr"])
        dma(p, "sp", br[:], c.w["brouter"][L], [], ["br"])
        dma(p, "sp", identb[:], c.w["identb"][:, :], [], ["identb"])
        dma(p, "sp", identf[:], c.w["identf"][:, :], [], ["identf"])
        gT_v = c.s["gT"].rearrange("(i c p) t -> p i c t", i=3, c=8, p=128)
        x1T_v = c.s["x1T"].rearrange("(c p) t -> p c t", p=128)
        gcnt = 0
        for blk in range(8):
            s = blk % 2
            oT_pairs = c.s["oT"][:, :, blk * 512:(blk + 1) * 512].rearrange("(hp two) d t -> two d hp t", two=2)
            for hv in range(2):
                dma(p, "sp", oTb[s][hv * 64:(hv + 1) * 64, :, :], oT_pairs[hv], [], [("oTb", s)])
            for dc in range(8):
                gs = gcnt % 2
                gcnt += 1
                dma(p, "sp", gTc[gs][:], gT_v[:, :, dc, blk * 512:(blk + 1) * 512], [], [("gTc", gs)])
                for i, (p0, npair) in enumerate(((0, 3), (3, 3), (6, 2))):
                    mm_group(p, psY[i][:, :],
                             [(wb[:, p0 + k, dc * 128:(dc + 1) * 128], oTb[s][:, p0 + k, :])
                              for k in range(npair)], ["wb", ("oTb", s)], ["psY%d" % i])
                m3 = [(gcnt * 3 + i) % 6 for i in range(3)]
                for i in range(3):
                    p.op("dve", lambda e, i=i, gs=gs, m=m3[i]: e.tensor_tensor(
                        out=mt[m][:], in0=psY[i][:, :], in1=gTc[gs][:, i, :], op=ALU.mult),
                        ["psY%d" % i, ("gTc", gs)], [("mt", m3[i])])
                p.op("pool", lambda e, m3=m3: e.tensor_tensor(out=mt[m3[0]][:], in0=mt[m3[0]][:],
                                                              in1=mt[m3[1]][:], op=ALU.add),
                     [("mt", m3[0]), ("mt", m3[1])], [("mt", m3[0])])
                p.op("pool", lambda e, m3=m3, s=s, dc=dc: e.tensor_tensor(
                    out=mixT[s][:, dc, :], in0=mt[m3[0]][:], in1=mt[m3[2]][:], op=ALU.add),
                    [("mt", m3[0]), ("mt", m3[2])], [("mixT", s)])
            for tt in range(4):
                t = blk * 4 + tt
                ts_ = t % 2
                dma(p, "sp", xo[ts_][:], c.q_src[t * 128:(t + 1) * 128, :], [], [("xo", ts_)])
                for half in range(2):
                    mm_group(p, psOut[half][:, :],
                             [(mixT[s][:, dc, tt * 128:(tt + 1) * 128], woutb[:, dc, half * 512:(half + 1) * 512])
                              for dc in range(8)], [("mixT", s), "woutb"], ["psOut%d" % half])
                    p.op("dve", lambda e, half=half, ts_=ts_: e.scalar_tensor_tensor(
                        out=tsb[ts_][:, half * 512:(half + 1) * 512], in0=xo[ts_][:, half * 512:(half + 1) * 512],
                        scalar=DN_ALPHA, in1=psOut[half][:, :], op0=ALU.mult, op1=ALU.add),
                        [("xo", ts_), "psOut%d" % half], [("tsb", ts_)])
                layer_norm_tile(c, tsb[ts_], ("tsb", ts_), junk, "junk", sm[ts_], ("sm", ts_),
                                lng, lnb, "lngb", x1t[ts_], ("x1t", ts_))
                dma(p, "sp", c.s["x1"][t * 128:(t + 1) * 128, :], x1t[ts_][:], [("x1t", ts_)], [("d_x1", t)])
                p.op("act", lambda e, ts_=ts_: e.activation(out=x1b[ts_][:], in_=x1t[ts_][:], func=AF.Copy),
                     [("x1t", ts_)], [("x1b", ts_)])
                tr_group(p, [(psT[:, ch, :], x1b[ts_][:, ch * 128:(ch + 1) * 128]) for ch in range(8)],
                         identb[:], [("x1b", ts_), "identb"], ["psT"])
                p.op("dve", lambda e, ts_=ts_: e.tensor_copy(x1Tb[ts_][:], psT[:]), ["psT"], [("x1Tb", ts_)])
                for hv in range(2):
                    dma(p, "sp", x1T_v[:, hv * 4:(hv + 1) * 4, t * 128:(t + 1) * 128], x1Tb[ts_][:, hv * 4:(hv + 1) * 4, :],
                        [("x1Tb", ts_)], [("d_x1T", t, hv)])
                tr_group(p, [(psTf[:, ch, :], x1t[ts_][:, ch * 128:(ch + 1) * 128]) for ch in range(8)],
                         identf[:], [("x1t", ts_), "identf"], ["psTf"])
                p.op("act", lambda e, ts_=ts_: e.activation(out=x1Tf[ts_][:], in_=psTf[:], func=AF.Copy),
                     ["psTf"], [("x1Tf", ts_)])
                mm_group(p, psY[0][:, 0:NEXP], [(x1Tf[ts_][:, ch, :], wr[:, ch, :]) for ch in range(8)],
                         [("x1Tf", ts_), "wr"], ["psY0"])
                R = rt_[ts_]
                rk = ("rt", ts_)
                lg, mx, msk, ex, exm = R[:, 0:32], R[:, 32:40], R[:, 40:72], R[:, 72:104], R[:, 104:136]
                nm, ssum, rs = R[:, 136:137], R[:, 137:138], R[:, 138:139]
                p.op("dve", lambda e, lg=lg: e.tensor_tensor(out=lg, in0=psY[0][:, 0:NEXP], in1=br[:], op=ALU.add),
                     ["psY0", "br"], [rk])
                p.op("dve", lambda e, lg=lg, mx=mx: e.max(out=mx, in_=lg), [rk], [rk])
                p.op("dve", lambda e, lg=lg, mx=mx, msk=msk: e.tensor_scalar(
                    out=msk, in0=lg, scalar1=mx[:, 3:4], scalar2=None, op0=ALU.is_ge), [rk], [rk])
                p.op("dve", lambda e, mx=mx, nm=nm: e.tensor_scalar(
                    out=nm, in0=mx[:, 0:1], scalar1=-1.0, scalar2=None, op0=ALU.mult), [rk], [rk])
                p.op("act", lambda e, lg=lg, ex=ex, nm=nm: e.activation(out=ex, in_=lg, func=AF.Exp, bias=nm),
                     [rk], [rk])
                p.op("dve", lambda e, ex=ex, msk=msk, exm=exm: e.tensor_tensor(out=exm, in0=ex, in1=msk,
                                                                               op=ALU.mult), [rk], [rk])
                p.op("dve", lambda e, exm=exm, ssum=ssum: e.reduce_sum(out=ssum, in_=exm, axis=AX.X), [rk], [rk])
                p.op("dve", lambda e, ssum=ssum, rs=rs: e.reciprocal(out=rs, in_=ssum), [rk], [rk])
                p.op("dve", lambda e, exm=exm, rs=rs: e.tensor_scalar(
                    out=exm, in0=exm, scalar1=rs, scalar2=None, op0=ALU.mult), [rk], [rk])
                p.op("pe", lambda e, exm=exm: e.transpose(psY[1][0:32, 0:128], exm, identf[:]),
                     [rk, "identf"], ["psY1"])
                p.op("act", lambda e, ts_=ts_: e.activation(out=gTt[ts_][0:32, :], in_=psY[1][0:32, 0:128],
                                                            func=AF.Copy), ["psY1"], [("gTt", ts_)])
                dma(p, "sp", c.s["gateT"][:, t * 128:(t + 1) * 128], gTt[ts_][0:32, :],
                    [("gTt", ts_)], [("d_gateT", t)])
                X = rx[ts_]
                xk = ("rx", ts_)
                idxf, sv, ov, eq, tmp = X[:, 0:8], X[:, 8:40], X[:, 40:72], X[:, 72:104], X[:, 104:136]
                slotf, gkf = X[:, 136:140], X[:, 140:144]
                p.op("dve", lambda e, ts_=ts_, lg=lg, mx=mx: e.max_index(out=idxu[ts_][:], in_max=mx, in_values=lg),
                     [rk], [("idxu", ts_)])
                p.op("dve", lambda e, ts_=ts_, idxf=idxf: e.tensor_copy(idxf, idxu[ts_][:]),
                     [("idxu", ts_)], [xk])
                p.op("pe", lambda e, msk=msk: e.matmul(psY[2][:, 0:NEXP], triu[:], msk, start=True, stop=False),
                     [rk, "triu"], ["psY2"])
                p.op("pe", lambda e: e.matmul(psY[2][:, 0:NEXP], ones128[:], msum[:], start=False, stop=True),
                     ["msum", "ones128"], ["psY2"])
                p.op("dve", lambda e, ov=ov: e.tensor_scalar(out=ov, in0=psY[2][:, 0:NEXP], scalar1=float(CAP),
                                                             scalar2=1.0e6, op0=ALU.is_ge, op1=ALU.mult),
                     ["psY2"], [xk])
                p.op("dve", lambda e, sv=sv: e.tensor_tensor(out=sv, in0=psY[2][:, 0:NEXP], in1=iota_cap[:],
                                                             op=ALU.add), ["psY2", "iota_cap"], [xk])
                p.op("dve", lambda e, sv=sv, ov=ov: e.tensor_tensor(out=sv, in0=sv, in1=ov, op=ALU.add), [xk], [xk])
                p.op("pool", lambda e, msk=msk: e.tensor_tensor(out=msum[:], in0=msum[:], in1=msk, op=ALU.add),
                     [rk, "msum"], ["msum"])
                for k4 in range(4):
                    p.op("dve", lambda e, eq=eq, idxf=idxf, k4=k4: e.tensor_scalar(
                        out=eq, in0=iota_e[:], scalar1=idxf[:, k4:k4 + 1], scalar2=None, op0=ALU.is_equal),
                        [xk, "iota_e"], [xk])
                    p.op("dve", lambda e, eq=eq, sv=sv, tmp=tmp: e.tensor_tensor(out=tmp, in0=eq, in1=sv, op=ALU.mult),
                         [xk], [xk])
                    p.op("dve", lambda e, tmp=tmp, slotf=slotf, k4=k4: e.reduce_sum(
                        out=slotf[:, k4:k4 + 1], in_=tmp, axis=AX.X), [xk], [xk])
                    p.op("dve", lambda e, eq=eq, exm=exm, tmp=tmp: e.tensor_tensor(out=tmp, in0=eq, in1=exm,
                                                                                   op=ALU.mult), [xk, rk], [xk])
                    p.op("dve", lambda e, tmp=tmp, gkf=gkf, k4=k4: e.reduce_sum(
                        out=gkf[:, k4:k4 + 1], in_=tmp, axis=AX.X), [xk], [xk])
                p.op("dve", lambda e, ts_=ts_, slotf=slotf: e.tensor_copy(slotu[ts_][:], slotf),
                     [xk], [("slotu", ts_)])
                for k4 in range(4):
                    p.op("pool", lambda e, ts_=ts_, k4=k4: e.indirect_dma_start(
                        out=c.s["xg"][:, :], out_offset=bass.IndirectOffsetOnAxis(ap=slotu[ts_][:, k4:k4 + 1], axis=0),
                        in_=x1b[ts_][:], in_offset=None, bounds_check=c.breg, oob_is_err=False),
                        [("slotu", ts_), ("x1b", ts_)], [("d_xg", t, k4)], dma=True)
                dma(p, "sp", c.s["slots"][t * 128:(t + 1) * 128, :], slotu[ts_][:], [("slotu", ts_)], [("d_slots", t)])
                dma(p, "sp", c.s["gks"][t * 128:(t + 1) * 128, :], gkf, [xk], [("d_gks", t)])
        p.barrier()
        p.emit()


SIG_MAX = float(1.0 / (1.0 + np.exp(-1.702 * 7.0)))


def phase_moe(c, L):
    nc, p = c.nc, c.p
    with ExitStack() as ph:
        c.ph = ph
        wgu = [_sb(c, "wgu%d" % i, [128, 8, 2 * DEXP], BF16) for i in range(2)]
        wdn = [_sb(c, "wdn%d" % i, [128, 8, D], BF16) for i in range(2)]
        xT = _sb(c, "xTsb", [128, 8, 1024], BF16)
        gT = _sb(c, "gTsb", [128, 1024], F32)
        yacc = _sb(c, "yacc", [128, 8, D], F32)
        bgu = _sb(c, "bgu", [128, NEXP, 16], F32)
        bgs = _sb(c, "bgs", [128, NEXP, 8], F32)
        bdn = _sb(c, "bdn", [128, D], F32)
        identf = _sb(c, "identf", [128, 128], F32)
        sg = [_sb(c, "sg%d" % i, [128, 512], F32) for i in range(2)]
        gc = [_sb(c, "gc%d" % i, [128, 512], F32) for i in range(2)]
        uc = [_sb(c, "uc%d" % i, [128, 512], F32) for i in range(2)]
        t1 = [_sb(c, "t1%d" % i, [128, 512], F32) for i in range(2)]
        t2 = [_sb(c, "t2%d" % i, [128, 512], F32) for i in range(2)]
        aT = [_sb(c, "aT%d" % i, [128, 8, 512], BF16) for i in range(2)]
        yev = [_sb(c, "yev%d" % i, [128, 512], F32) for i in range(2)]
        psg = [_ps(c, "psg%d" % i, [128, 512], F32) for i in range(2)]
        psu = [_ps(c, "psu%d" % i, [128, 512], F32) for i in range(2)]
        psGb = _ps(c, "psGb", [128, 512], F32)
        psy = [_ps(c, "psy%d" % i, [128, 512], F32) for i in range(2)]

        dma(p, "sp", bgu[:], c.w["bgu_t"][L], [], ["bgu"])
        dma(p, "sp", bdn[0:32, :], c.w["bdn"][L], [], ["bdn"])
        dma(p, "sp", identf[:], c.w["identf"][:, :], [], ["identf"])
        p.op("dve", lambda e: e.tensor_scalar(out=bgs[:], in0=bgu[:, :, 0:8], scalar1=1.702, scalar2=None,
                                              op0=ALU.mult), ["bgu"], ["bgs"])
        x1T_v = c.s["x1T"].rearrange("(c p) t -> p c t", p=128)
        ym_v = c.s["ymoe"].rearrange("(t p) d -> p t d", p=128)
        cnt = 0
        ycnt = 0
        for sb in range(4):
            dma(p, "sp", xT[:], x1T_v[:, :, sb * 1024:(sb + 1) * 1024], [], ["xTsb"])
            dma(p, "sp", gT[0:32, :], c.s["gateT"][:, sb * 1024:(sb + 1) * 1024], [], ["gTsb"])
            for ex in range(NEXP):
                ws = ex % 2
                gu_v = c.w["w_gu"][L, ex].rearrange("(c p) n -> p c n", p=128)
                dn_v = c.w["w_dn"][L, ex].rearrange("(c p) n -> p c n", p=128)
                for ch in range(8):
                    dma(p, "pool", wgu[ws][:, ch, :], gu_v[:, ch, :], [], [("wgu", ws)])
                for ch in range(8):
                    dma(p, "pool", wdn[ws][:, ch, :], dn_v[:, ch, :], [], [("wdn", ws)])
                for tb in range(2):
                    as_ = (ex * 2 + tb) % 2
                    p.op("pe", lambda e, ex=ex, tb=tb: e.matmul(
                        psGb[:, :], identf[0:32, ex:ex + 1].to_broadcast([32, 128]),
                        gT[0:32, tb * 512:(tb + 1) * 512], start=True, stop=True),
                        ["identf", "gTsb"], ["psGb"])
                    for j in range(8):
                        k = cnt % 2
                        cnt += 1
                        mm_group(p, psg[k][:, :],
                                 [(wgu[ws][:, ch, j * 128:(j + 1) * 128], xT[:, ch, tb * 512:(tb + 1) * 512])
                                  for ch in range(8)], [("wgu", ws), "xTsb"], ["psg%d" % k])
                        mm_group(p, psu[k][:, :],
                                 [(wgu[ws][:, ch, DEXP + j * 128:DEXP + (j + 1) * 128],
                                   xT[:, ch, tb * 512:(tb + 1) * 512]) for ch in range(8)],
                                 [("wgu", ws), "xTsb"], ["psu%d" % k])
                        p.op("dve", lambda e, k=k, ex=ex, j=j: e.tensor_scalar(
                            out=gc[k][:], in0=psg[k][:, :], scalar1=bgu[:, ex, j:j + 1], scalar2=7.0,
                            op0=ALU.add, op1=ALU.min), ["psg%d" % k, "bgu"], [("gc", k)])
                        p.op("act", lambda e, k=k: e.activation(
                            out=sg[k][:], in_=gc[k][:], func=AF.Sigmoid, scale=1.702),
                            [("gc", k)], [("sg", k)])
                        p.op("dve", lambda e, k=k, ex=ex, j=j: e.tensor_scalar(
                            out=uc[k][:], in0=psu[k][:, :], scalar1=bgu[:, ex, 8 + j:9 + j], scalar2=7.0,
                            op0=ALU.add, op1=ALU.min), ["psu%d" % k, "bgu"], [("uc", k)])
                        p.op("dve", lambda e, k=k: e.tensor_scalar(
                            out=uc[k][:], in0=uc[k][:], scalar1=-7.0, scalar2=1.0,
                            op0=ALU.max, op1=ALU.add), [("uc", k)], [("uc", k)])
                        p.op("pool", lambda e, k=k: e.tensor_tensor(
                            out=t1[k][:], in0=sg[k][:], in1=gc[k][:], op=ALU.mult),
                            [("sg", k), ("gc", k)], [("t1", k)])
                        p.op("dve", lambda e, k=k: e.tensor_tensor(out=t2[k][:], in0=uc[k][:], in1=psGb[:, :],
                                                                   op=ALU.mult), [("uc", k), "psGb"], [("t2", k)])
                        p.op("pool", lambda e, k=k, j=j, as_=as_: e.tensor_tensor(
                            out=aT[as_][:, j, :], in0=t1[k][:], in1=t2[k][:], op=ALU.mult),
                            [("t1", k), ("t2", k)], [("aT", as_)])
                    for tt in range(4):
                        tile = tb * 4 + tt
                        for half in range(2):
                            yk = ycnt % 2
                            ycnt += 1
                            pairs = [(aT[as_][:, j, tt * 128:(tt + 1) * 128],
                                      wdn[ws][:, j, half * 512:(half + 1) * 512]) for j in range(8)]
                            rds = [("aT", as_), ("wdn", ws)]
                            if ex == 0:
                                pairs.append((gT[0:32, tile * 128:(tile + 1) * 128],
                                              bdn[0:32, half * 512:(half + 1) * 512]))
                                rds += ["gTsb", "bdn"]
                            mm_group(p, psy[yk][:, :], pairs, rds, ["psy%d" % yk])
                            ya = yacc[:, tile, half * 512:(half + 1) * 512]
                            if ex == 0:
                                p.op("act", lambda e, ya=ya, yk=yk: e.activation(out=ya, in_=psy[yk][:, :],
                                                                                 func=AF.Copy),
                                     ["psy%d" % yk], [("yacc", tile, half)])
                            else:
                                p.op("act", lambda e, yk=yk: e.activation(out=yev[yk][:], in_=psy[yk][:, :],
                                                                          func=AF.Copy),
                                     ["psy%d" % yk], [("yev", yk)])
                                p.op("pool", lambda e, ya=ya, yk=yk: e.tensor_tensor(out=ya, in0=ya, in1=yev[yk][:],
                                                                                     op=ALU.add),
                                     [("yev", yk), ("yacc", tile, half)], [("yacc", tile, half)])
            dma(p, "sp", ym_v[:, sb * 8:(sb + 1) * 8, :], yacc[:],
                [("yacc", t_, h_) for t_ in range(8) for h_ in range(2)], [("d_ymoe", sb)])
        p.barrier()
        p.emit()


def load_w_cast(c, dst_ap, src_ap, stg, skey, dkey, eng="pool"):
    p = c.p
    dma(p, "sp", stg, src_ap, [], [skey])
    if eng == "act":
        p.op("act", lambda e: e.activation(out=dst_ap, in_=stg, func=AF.Copy), [skey], [dkey])
    else:
        p.op(eng, lambda e: e.tensor_copy(dst_ap, stg), [skey], [dkey])


def phase_moe_sparse(c, L):
    nc, p = c.nc, c.p
    NT = CAP // 128
    groups = [(0, 512), (512, CAP)] if CAP > 512 else [(0, CAP)]
    with ExitStack() as ph:
        c.ph = ph
        wgu = [_sb(c, "wgu%d" % i, [128, 8, 2 * DEXP], BF16) for i in range(2)]
        wdn = [_sb(c, "wdn%d" % i, [128, 8, D], BF16) for i in range(2)]
        stg = [_sb(c, "stg%d" % i, [128, 2 * DEXP], F32) for i in range(3)]
        xgt = [_sb(c, "xgt%d" % i, [128, D], BF16) for i in range(NT)]
        xgT = [_sb(c, "xgT%d" % i, [128, 8, CAP], BF16) for i in range(2)]
        bgu = _sb(c, "bgu", [128, NEXP, 16], F32)
        bdr1 = _sb(c, "bdr", [128, D], F32)
        bdr = [bdr1, bdr1]
        bdb = [_sb(c, "bdb%d" % i, [128, D], BF16) for i in range(2)]
        ones1 = _sb(c, "ones1", [128, 128], BF16)
        identb = _sb(c, "identb", [128, 128], BF16)
        sg = [_sb(c, "sg%d" % i, [128, 512], F32) for i in range(2)]
        gc = [_sb(c, "gc%d" % i, [128, 512], F32) for i in range(2)]
        uc = [_sb(c, "uc%d" % i, [128, 512], F32) for i in range(2)]
        t1 = [_sb(c, "t1%d" % i, [128, 512], F32) for i in range(2)]
        aT = _sb(c, "aT", [128, 8, CAP], BF16)
        yout = [_sb(c, "yout%d" % i, [128, D], F32) for i in range(2)]
        psT2 = [_ps(c, "psT%d" % i, [128, 8, 128], BF16) for i in range(2)]
        psg = [_ps(c, "psg%d" % i, [128, 512], F32) for i in range(2)]
        psu = [_ps(c, "psu%d" % i, [128, 512], F32) for i in range(2)]
        psy = [_ps(c, "psy%d" % i, [128, 512], F32) for i in range(2)]
        dma(p, "sp", bgu[:], c.w["bgu_t"][L], [], ["bgu"])
        dma(p, "sp", identb[:], c.w["identb"][:, :], [], ["identb"])
        p.op("pool", lambda e: e.memset(ones1[:], 1.0), [], ["ones1"])
        st8 = {"scnt": 0, "cnt": 0, "ycnt": 0, "xcnt": 0}
        cast_rr = ("act", "dve", "act")

        def wload_steps(ex):
            ws = ex % 2
            gu_v = c.w["w_gu"][L, ex].rearrange("(c p) n -> p c n", p=128)
            dn_v = c.w["w_dn"][L, ex].rearrange("(c p) n -> p c n", p=128)
            steps = []
            for ch in range(12):
                si = st8["scnt"] % 3
                eng = cast_rr[st8["scnt"] % 3]
                st8["scnt"] += 1
                if ch < 8:
                    src, stv, dstv, dkey = gu_v[:, ch, :], stg[si][:], wgu[ws][:, ch, :], ("wgu", ws)
                else:
                    c2 = ch - 8
                    src = dn_v[:, 2 * c2:2 * c2 + 2, :]
                    stv = stg[si][:].rearrange("p (c n) -> p c n", c=2)
                    dstv, dkey = wdn[ws][:, 2 * c2:2 * c2 + 2, :], ("wdn", ws)

                def f_dma(src=src, stv=stv, si=si):
                    dma(p, "sp", stv, src, [], [("stg", si)])

                def f_cast(stv=stv, dstv=dstv, si=si, dkey=dkey, eng=eng):
                    if eng == "act":
                        p.op("act", lambda e: e.activation(out=dstv, in_=stv, func=AF.Copy), [("stg", si)], [dkey])
                    else:
                        p.op(eng, lambda e: e.tensor_copy(dstv, stv), [("stg", si)], [dkey])
                steps.append((f_dma, f_cast))
            steps.append((lambda ws=ws, ex=ex: dma(p, "sp", bdr[ws][0:1, :], c.w["bdn"][L, ex:ex + 1, :], [],
                                                   ["bdr"]),
                          lambda ws=ws: p.op("pool", lambda e: e.tensor_copy(bdb[ws][0:1, :], bdr[ws][0:1, :]),
                                             ["bdr"], [("bdb", ws)])))
            return steps

        def xg_dma(ex):
            for st in range(NT):
                r0 = ex * CAP + st * 128
                dma(p, "sp", xgt[st][:], c.s["xg"][r0:r0 + 128, :], [], [("xgt", st)])

        def xg_load(ex):
            xb_ = ex % 2
            for st in range(NT):
                pt = st8["xcnt"] % 2
                st8["xcnt"] += 1
                tr_group(p, [(psT2[pt][:, ch, :], xgt[st][:, ch * 128:(ch + 1) * 128]) for ch in range(8)],
                         identb[:], [("xgt", st), "identb"], ["psT%d" % pt])
                eng = "act" if st % 2 == 0 else "dve"
                if eng == "act":
                    p.op("act", lambda e, st=st, xb_=xb_, pt=pt: e.activation(
                        out=xgT[xb_][:, :, st * 128:(st + 1) * 128], in_=psT2[pt][:], func=AF.Copy),
                        ["psT%d" % pt], [("xgT", xb_)])
                else:
                    p.op("dve", lambda e, st=st, xb_=xb_, pt=pt: e.tensor_copy(
                        xgT[xb_][:, :, st * 128:(st + 1) * 128], psT2[pt][:]), ["psT%d" % pt], [("xgT", xb_)])

        pend_cast = None
        for f_dma, f_cast in wload_steps(0):
            f_dma()
            if pend_cast is not None:
                pend_cast()
            pend_cast = f_cast
        pend_cast()
        xg_dma(0)
        xg_load(0)
        for ex in range(NEXP):
            ws = ex % 2
            xb_ = ex % 2
            nxt_steps = wload_steps(ex + 1) if ex + 1 < NEXP else []
            pend_cast = None
            if ex + 1 < NEXP:
                xg_dma(ex + 1)
            for (n0, n1) in groups:
                w = n1 - n0
                for j in range(8):
                    k = st8["cnt"] % 2
                    st8["cnt"] += 1
                    if nxt_steps:
                        f_dma, f_cast = nxt_steps.pop(0)
                        f_dma()
                        if pend_cast is not None:
                            pend_cast()
                        pend_cast = f_cast
                    mm_group(p, psg[k][:, 0:w], [(wgu[ws][:, ch, j * 128:(j + 1) * 128], xgT[xb_][:, ch, n0:n1])
                                                 for ch in range(8)], [("wgu", ws), ("xgT", xb_)], ["psg%d" % k])
                    mm_group(p, psu[k][:, 0:w], [(wgu[ws][:, ch, DEXP + j * 128:DEXP + (j + 1) * 128],
                                                  xgT[xb_][:, ch, n0:n1]) for ch in range(8)],
                             [("wgu", ws), ("xgT", xb_)], ["psu%d" % k])
                    p.op("dve", lambda e, k=k, ex=ex, j=j, w=w: e.tensor_scalar(
                        out=gc[k][:, 0:w], in0=psg[k][:, 0:w], scalar1=bgu[:, ex, j:j + 1], scalar2=7.0,
                        op0=ALU.add, op1=ALU.min), ["psg%d" % k, "bgu"], [("gc", k)])
                    p.op("act", lambda e, k=k, w=w: e.activation(out=sg[k][:, 0:w], in_=gc[k][:, 0:w],
                                                                 func=AF.Sigmoid, scale=1.702),
                         [("gc", k)], [("sg", k)])
                    p.op("dve", lambda e, k=k, ex=ex, j=j, w=w: e.tensor_scalar(
                        out=uc[k][:, 0:w], in0=psu[k][:, 0:w], scalar1=bgu[:, ex, 8 + j:9 + j], scalar2=7.0,
                        op0=ALU.add, op1=ALU.min), ["psu%d" % k, "bgu"], [("uc", k)])
                    p.op("dve", lambda e, k=k, w=w: e.tensor_scalar(
                        out=uc[k][:, 0:w], in0=uc[k][:, 0:w], scalar1=-7.0, scalar2=1.0,
                        op0=ALU.max, op1=ALU.add), [("uc", k)], [("uc", k)])
                    p.op("pool", lambda e, k=k, w=w: e.tensor_tensor(out=t1[k][:, 0:w], in0=sg[k][:, 0:w],
                                                                     in1=gc[k][:, 0:w], op=ALU.mult),
                         [("sg", k), ("gc", k)], [("t1", k)])
                    p.op("pool", lambda e, k=k, j=j, n0=n0, n1=n1, w=w: e.tensor_tensor(
                        out=aT[:, j, n0:n1], in0=t1[k][:, 0:w], in1=uc[k][:, 0:w], op=ALU.mult),
                        [("t1", k), ("uc", k)], ["aT"])
            while nxt_steps:
                f_dma, f_cast = nxt_steps.pop(0)
                f_dma()
                if pend_cast is not None:
                    pend_cast()
                pend_cast = f_cast
            if pend_cast is not None:
                pend_cast()
            if ex + 1 < NEXP:
                xg_load(ex + 1)
            for st in range(NT):
                ys = st8["ycnt"] % 2
                st8["ycnt"] += 1
                for half in range(2):
                    yk = (st8["ycnt"] + half) % 2
                    pairs = [(aT[:, j, st * 128:(st + 1) * 128], wdn[ws][:, j, half * 512:(half + 1) * 512])
                             for j in range(8)]
                    pairs.append((ones1[0:1, 0:128], bdb[ws][0:1, half * 512:(half + 1) * 512]))
                    mm_group(p, psy[yk][:, :], pairs, ["aT", ("wdn", ws), ("bdb", ws), "ones1"], ["psy%d" % yk])
                    p.op("act", lambda e, ys=ys, yk=yk, half=half: e.activation(
                        out=yout[ys][:, half * 512:(half + 1) * 512], in_=psy[yk][:, :], func=AF.Copy),
                        ["psy%d" % yk], [("yout", ys)])
                r0 = ex * CAP + st * 128
                dma(p, "sp", c.s["yg"][r0:r0 + 128, :], yout[ys][:], [("yout", ys)], [("d_yg", ex, st)])
        p.barrier()
        p.emit()


def phase_ln2(c, L, dst, sparse=True):
    nc, p = c.nc, c.p
    with ExitStack() as ph:
        c.ph = ph
        lng = _sb(c, "lng2", [128, D], F32)
        lnb = _sb(c, "lnb2", [128, D], F32)
        xa = [_sb(c, "xa%d" % i, [128, D], F32) for i in range(2)]
        ya = [_sb(c, "ya%d" % i, [128, D], F32) for i in range(2)]
        yk_ = [[_sb(c, "yk%d_%d" % (i, k), [128, D], F32) for k in range(4)] for i in range(2)]
        sl = [_sb(c, "sl%d" % i, [128, 4], I32) for i in range(2)]
        gk = [_sb(c, "gk%d" % i, [128, 4], F32) for i in range(2)]
        tsb = [_sb(c, "tsb%d" % i, [128, D], F32) for i in range(2)]
        out = [_sb(c, "out%d" % i, [128, D], F32) for i in range(2)]
        junk = _sb(c, "junk2", [128, D], F32)
        sm = [_sb(c, "sm%d" % i, [128, 8], F32) for i in range(2)]
        dma(p, "sp", lng[:], c.w["ln2g"][L], [], ["lngb"])
        dma(p, "sp", lnb[:], c.w["ln2b"][L], [], ["lngb"])
        for t in range(32):
            s = t % 2
            dma(p, "sp", xa[s][:], c.s["x1"][t * 128:(t + 1) * 128, :], [], [("xa", s)])
            if sparse:
                dma(p, "sp", sl[s][:], c.s["slots"][t * 128:(t + 1) * 128, :], [], [("sl", s)])
                dma(p, "sp", gk[s][:], c.s["gks"][t * 128:(t + 1) * 128, :], [], [("gk", s)])
                for k in range(4):
                    p.op("pool", lambda e, s=s, k=k: e.indirect_dma_start(
                        out=yk_[s][k][:], out_offset=None, in_=c.s["yg"][:, :],
                        in_offset=bass.IndirectOffsetOnAxis(ap=sl[s][:, k:k + 1], axis=0),
                        bounds_check=c.breg, oob_is_err=False), [("sl", s)], [("yk", s, k)], dma=True)
                p.op("dve", lambda e, s=s: e.tensor_scalar(out=ya[s][:], in0=yk_[s][0][:], scalar1=gk[s][:, 0:1],
                                                           scalar2=None, op0=ALU.mult),
                     [("yk", s, 0), ("gk", s)], [("ya", s)])
                for k in range(1, 4):
                    p.op("dve", lambda e, s=s, k=k: e.scalar_tensor_tensor(
                        out=ya[s][:], in0=yk_[s][k][:], scalar=gk[s][:, k:k + 1], in1=ya[s][:],
                        op0=ALU.mult, op1=ALU.add), [("yk", s, k), ("gk", s), ("ya", s)], [("ya", s)])
            else:
                dma(p, "sp", ya[s][:], c.s["ymoe"][t * 128:(t + 1) * 128, :], [], [("ya", s)])
            p.op("dve", lambda e, s=s: e.scalar_tensor_tensor(
                out=tsb[s][:], in0=xa[s][:], scalar=DN_ALPHA, in1=ya[s][:], op0=ALU.mult, op1=ALU.add),
                [("xa", s), ("ya", s)], [("tsb", s)])
            layer_norm_tile(c, tsb[s], ("tsb", s), junk, "junk", sm[s], ("sm", s), lng, lnb, "lngb",
                            out[s], ("out", s))
            dma(p, "sp", dst[t * 128:(t + 1) * 128, :], out[s][:], [("out", s)], [("d_out", t)])
        p.barrier()
        p.emit()


ALL_PHASES = ("p1", "na", "gqa", "mla", "merge", "moes", "ln2")
_NC_CACHE = {}


def kernel(**inputs):
    inp = {k: np.asarray(v) for k, v in inputs.items()}
    x = np.ascontiguousarray(inp["x"].astype(np.float32))
    nb = x.shape[0]
    if "nc" not in _NC_CACHE:
        _NC_CACHE["nc"] = build()
    consts = [prep_consts(hf) for hf in range(2)]
    wts = [prep_weights(inp, [0, 1], hf) for hf in range(2)]
    in_maps = []
    for cid in range(2 * nb):
        b, hf = cid // 2, cid % 2
        m = {}
        m.update(wts[hf])
        m.update(consts[hf])
        m.update(prep_acts(x[b], hf))
        in_maps.append(m)
    res = run_bass_kernel_spmd(_NC_CACHE["nc"], in_maps, core_ids=list(range(2 * nb)))
    return np.stack([np.concatenate([np.asarray(res.results[2 * b]["y"]),
                                     np.asarray(res.results[2 * b + 1]["y"])], 0)
                     for b in range(nb)], 0).astype(np.float32)
```

```python
from contextlib import ExitStack
import numpy as np
import ml_dtypes
import concourse.bass as bass
import concourse.mybir as mybir
from concourse.bass_utils import run_bass_kernel_spmd

F32 = mybir.dt.float32
BF16 = mybir.dt.bfloat16
I32 = mybir.dt.int32
U32 = mybir.dt.uint32
AF = mybir.ActivationFunctionType
ALU = mybir.AluOpType
AX = mybir.AxisListType

D = 1024
S = 8192
HALF = 4096
DIN = 5536
NEXP = 32
DEXP = 1024
LN_EPS = 1e-5
RMS_EPS = 1e-6
DN_ALPHA = 4.0 ** 0.25
NEG = -30000.0
NA_ROWS = 72

COMPUTE = ("pe", "act", "dve", "pool")


class Prog:
    def __init__(self, nc, stack, ring=8):
        self.nc = nc
        self.e = {"pe": nc.tensor, "act": nc.scalar, "dve": nc.vector,
                  "pool": nc.gpsimd, "sp": nc.sync}
        self.ops = []
        self.done = 0
        self.sem = {k: stack.enter_context(nc.semaphore("s_" + k)) for k in COMPUTE}
        self.ticket = {k: 0 for k in COMPUTE}
        self.ring = {q: [stack.enter_context(nc.semaphore("d_%s%d" % (q, i))) for i in range(ring)]
                     for q in ("sp", "pool")}
        self.ring_cnt = {q: [0] * ring for q in ("sp", "pool")}
        self.ring_last = {q: [None] * ring for q in ("sp", "pool")}
        self.ring_pos = {q: 0 for q in ("sp", "pool")}
        self.waited = {k: {} for k in self.e}
        self.last_w = {}
        self.readers = {}
        self.sig = {}
        self.eidx = {}
        self.ecount = {k: 0 for k in self.e}
        self.last_op = {k: None for k in self.e}
        self.dma_open = []

    def op(self, eng, fn, r=(), w=(), dma=False):
        self.ops.append((eng, fn, tuple(r), tuple(w), dma))

    def barrier(self):
        self.ops.append(("BAR", None, (), (), False))

    def _wait(self, eng, sem, val):
        cur = self.waited[eng].get(sem, 0)
        if cur < val:
            self.e[eng].wait_ge(sem, val)
            self.waited[eng][sem] = val

    def emit(self):
        ops = self.ops
        n = len(ops)
        start = self.done
        deps = {}
        needed = set()
        last_w, readers = self.last_w, self.readers
        eidx, ecount = self.eidx, self.ecount
        openg = {}
        for i in range(start, n):
            eng, fn, r, w, dma = ops[i]
            if eng == "BAR":
                continue
            eidx[i] = ecount[eng]
            ecount[eng] += 1
            openg[i] = (eng, dma)
            d = set()
            raw = set()
            for k in r:
                if k in last_w:
                    d.add(last_w[k])
                    raw.add(last_w[k])
                if isinstance(k, str) and k.startswith("ps"):
                    rd = readers.get(k)
                    if rd:
                        d.update(rd[0].values())
                        d.update(rd[1])
            for k in w:
                if k in last_w:
                    d.add(last_w[k])
                rd = readers.get(k)
                if rd:
                    d.update(rd[0].values())
                    d.update(rd[1])
            for k in r:
                rd = readers.setdefault(k, ({}, []))
                if dma:
                    rd[1].append(i)
                else:
                    rd[0][eng] = i
            for k in w:
                last_w[k] = i
                readers[k] = ({}, [])
            d.discard(i)
            keep = set()
            for j in d:
                if j < start and j not in self.sig and j not in openg:
                    continue
                jeng, jdma = self._opinfo(j, openg)
                if (not dma) and (not jdma) and jeng == eng:
                    if eng == "pe":
                        continue
                    if j in raw and eidx[i] - eidx[j] <= 2:
                        keep.add(j)
                    continue
                keep.add(j)
            deps[i] = keep
            needed.update(keep)
        self._openg_all = getattr(self, "_openg_all", {})
        self._openg_all.update(openg)
        for i in range(start, n):
            eng, fn, r, w, dma = ops[i]
            if eng == "BAR":
                self._emit_barrier()
                continue
            if dma:
                q = eng
                pos = self.ring_pos[q]
                self.ring_pos[q] = (pos + 1) % len(self.ring[q])
                sem = self.ring[q][pos]
                prev = self.ring_last[q][pos]
                if prev is not None:
                    self._wait(eng, sem, prev)
            for j in sorted(deps[i]):
                if j in self.sig:
                    s, v = self.sig[j]
                    self._wait(eng, s, v)
            ins = fn(self.e[eng])
            if dma:
                self.ring_cnt[q][pos] += 16
                val = self.ring_cnt[q][pos]
                ins.then_inc(sem, 16)
                self.ring_last[q][pos] = val
                self.sig[i] = (sem, val)
                self.dma_open.append(i)
            else:
                self.last_op[eng] = i
                if i in needed:
                    self.ticket[eng] += 1
                    ins.then_inc(self.sem[eng], 1)
                    self.sig[i] = (self.sem[eng], self.ticket[eng])
        self.done = n

    def _opinfo(self, j, openg):
        if j in openg:
            return openg[j]
        return self._openg_all[j]

    def _emit_barrier(self):
        marks = []
        for eng in COMPUTE:
            self.ticket[eng] += 1
            self.e[eng].drain().then_inc(self.sem[eng], 1)
            marks.append((self.sem[eng], self.ticket[eng]))
        dmas = [self.sig[i] for i in self.dma_open]
        self.dma_open = []
        for eng in self.e:
            for s, v in marks:
                self._wait(eng, s, v)
            for s, v in dmas:
                self._wait(eng, s, v)
        self.last_w.clear()
        self.readers.clear()


class Ctx:
    pass


_UID = [0]


def _sb(c, name, shape, dt):
    _UID[0] += 1
    return c.ph.enter_context(c.nc.sbuf_tensor("sb%d_%s" % (_UID[0], name), list(shape), dt))


def _ps(c, name, shape, dt):
    _UID[0] += 1
    return c.ph.enter_context(c.nc.psum_tensor("pp%d_%s" % (_UID[0], name), list(shape), dt))


def mm_group(p, out_ap, pairs, r, w):
    n = len(pairs)

    def fn(e):
        ins = None
        for i, (l, rr) in enumerate(pairs):
            ins = e.matmul(out_ap, l, rr, start=(i == 0), stop=(i == n - 1))
        return ins
    p.op("pe", fn, r, w)


def tr_group(p, outs_ins, ident, r, w):
    def fn(e):
        ins = None
        for o, i_ in outs_ins:
            ins = e.transpose(o, i_, ident)
        return ins
    p.op("pe", fn, r, w)


def dma(p, q, out_ap, in_ap, r, w):
    p.op(q, lambda e: e.dma_start(out=out_ap, in_=in_ap), r, w, dma=True)


def load_cast_cols(p, dst, src, ncols, r, w, step=2048):
    for c0 in range(0, ncols, step):
        c1 = min(ncols, c0 + step)
        dma(p, "pool", dst[:, c0:c1], src[:, c0:c1], r, w)


def phase1(c, L):
    nc, p = c.nc, c.p
    with ExitStack() as ph:
        c.ph = ph
        winb = _sb(c, "winb", [128, 8, DIN], BF16)
        wuqb = _sb(c, "wuqb", [128, 3, 384], BF16)
        wukvb = _sb(c, "wukvb", [128, 2, 512], BF16)
        identb = _sb(c, "identb", [128, 128], BF16)
        gq = _sb(c, "gq", [128, 384], F32)
        gk = _sb(c, "gk", [128, 128], F32)
        mq = _sb(c, "mq", [128, 384], F32)
        mkv = _sb(c, "mkv", [128, 256], F32)
        xf = [_sb(c, "xf%d" % i, [128, D], F32) for i in range(2)]
        xb = [_sb(c, "xb%d" % i, [128, D], BF16) for i in range(2)]
        xT = [_sb(c, "xT%d" % i, [128, 8, 512], BF16) for i in range(2)]
        rp = [_sb(c, "rp%d" % i, [128, 192], F32) for i in range(2)]
        wk = [_sb(c, "wk%d" % i, [128, 384], F32) for i in range(6)]
        sm = [_sb(c, "sm%d" % i, [128, 8], F32) for i in range(4)]
        ob = [_sb(c, "ob%d" % i, [128, 384], BF16) for i in range(8)]
        tT = [_sb(c, "tT%d" % i, [128, 512], BF16) for i in range(2)]
        blk = {k: [_sb(c, "blk_%s%d" % (k, i), [128, 6 * 512], BF16) for i in range(2)]
               for k in ("a", "b", "c", "d")}
        vna = [_sb(c, "vna%d" % i, [128, 6, 65], BF16) for i in range(2)]
        vg = [_sb(c, "vg%d" % i, [128, 2, 65], BF16) for i in range(2)]
        vm = [_sb(c, "vm%d" % i, [128, 4, 65], BF16) for i in range(2)]
        gout = [_sb(c, "gout%d" % i, [128, 512], BF16) for i in range(3)]
        psT = _ps(c, "psT", [128, 8, 128], BF16)
        psR = _ps(c, "psR", [128, 8, 128], BF16)
        psM = [_ps(c, "psM%d" % i, [128, 512], F32) for i in range(4)]
        psG = [_ps(c, "psG%d" % i, [128, 512], F32) for i in range(2)]

        w_in = c.w["w_in"][L]
        w_in_v = w_in.rearrange("(c p) n -> p c n", p=128)
        NSTG = 2
        stgw = [_sb(c, "stgw%d" % i, [128, 692], F32) for i in range(NSTG)]
        wi = 0
        for ch in range(8):
            for pc in range(8):
                c0 = pc * 692
                si = wi % NSTG
                dma(p, "sp", stgw[si][:], w_in_v[:, ch, c0:c0 + 692], [], [("stgw", si)])
                if wi % 2 == 0:
                    p.op("dve", lambda e, si=si, ch=ch, c0=c0: e.tensor_copy(winb[:, ch, c0:c0 + 692], stgw[si][:]),
                         [("stgw", si)], ["winb"])
                else:
                    p.op("act", lambda e, si=si, ch=ch, c0=c0: e.activation(out=winb[:, ch, c0:c0 + 692],
                                                                           in_=stgw[si][:], func=AF.Copy),
                         [("stgw", si)], ["winb"])
                wi += 1
        wuq_v = c.w["w_uq"][L].rearrange("(c p) n -> p c n", p=128)
        for ch in range(3):
            load_cast_cols(p, wuqb[:, ch, :], wuq_v[:, ch, :], 384, [], ["wuqb"])
        wukv_v = c.w["w_ukv"][L].rearrange("(c p) n -> p c n", p=128)
        for ch in range(2):
            load_cast_cols(p, wukvb[:, ch, :], wukv_v[:, ch, :], 512, [], ["wukvb"])
        dma(p, "sp", identb[:], c.w["identb"][:, :], [], ["identb"])
        dma(p, "sp", gq[:], c.w["gq_rep"][L], [], ["gq"])
        dma(p, "sp", gk[:], c.w["gk_rep"][L], [], ["gk"])
        dma(p, "sp", mq[:], c.w["mq_rep"][L], [], ["mq"])
        dma(p, "sp", mkv[:], c.w["mkv_rep"][L], [], ["mkv"])
        for i in range(2):
            p.op("pool", lambda e, t=vna[i]: e.memset(t[:], 1.0), [], [("vna", i)])
            p.op("pool", lambda e, t=vg[i]: e.memset(t[:], 1.0), [], [("vg", i)])
            p.op("pool", lambda e, t=vm[i]: e.memset(t[:], 1.0), [], [("vm", i)])

        cnt = {"tile": 0, "psM": 0, "psG": 0, "wk": 0, "sm": 0, "ob": 0, "tT": 0, "gout": 0}
        pending = []

        def st(out_ap, in_ap, r, w):
            dma(p, "sp", out_ap, in_ap, r, w)

        def flush():
            for o_, i_, r_, w_ in pending:
                dma(p, "sp", o_, i_, r_, w_)
            del pending[:]

        def nxt(k, n):
            v = cnt[k] % n
            cnt[k] += 1
            return v

        def load_tile(src_ap, rope_ap, to_xT=None, mask_col=None, defer_tr=False):
            s = nxt("tile", 2)
            dma(p, "sp", xf[s][:], src_ap, [], [("xf", s)])
            if rope_ap is not None:
                dma(p, "sp", rp[s][:], rope_ap, [], [("rp", s)])
            if mask_col is not None:
                p.op("dve", lambda e: e.tensor_scalar(out=xf[s][:], in0=xf[s][:],
                                                      scalar1=c.hm[:, mask_col:mask_col + 1], scalar2=None,
                                                      op0=ALU.mult), [("xf", s), "hm"], [("xf", s)])
            p.op("dve", lambda e: e.tensor_copy(xb[s][:], xf[s][:]), [("xf", s)], [("xb", s)])
            if to_xT is None:
                dst, key = tT_full[s][:], ("tTf", s)
            else:
                dst, key = to_xT

            def fin():
                tr_group(p, [(psT[:, ch, :], xb[s][:, ch * 128:(ch + 1) * 128]) for ch in range(8)],
                         identb[:], [("xb", s), "identb"], ["psT"])
                p.op("act", lambda e: e.activation(out=dst, in_=psT[:], func=AF.Copy), ["psT"], [key])
            if defer_tr:
                return dst, key, rp[s], ("rp", s), fin
            fin()
            return dst, key, rp[s], ("rp", s)

        tT_full = [_sb(c, "tTf%d" % i, [128, 8, 128], BF16) for i in range(2)]

        def tok_mm(xTa, xTkey, c0, c1):
            b = nxt("psM", 4)
            mm_group(p, psM[b][:, 0:c1 - c0],
                     [(xTa[:, ch, :], winb[:, ch, c0:c1]) for ch in range(8)],
                     [xTkey, "winb"], ["psM%d" % b])
            return psM[b], "psM%d" % b

        def transp_out(src_bf, src_key, nh, dh, dst_ap, dst_key):
            tr_group(p, [(psR[0:dh, h, :], src_bf[:, h * dh:(h + 1) * dh]) for h in range(nh)],
                     identb[:], [src_key, "identb"], ["psR"])
            p.op("dve", lambda e: e.tensor_copy(dst_ap, psR[0:dh, 0:nh, :]), ["psR"], [dst_key])

        def rms_rope(ps_ap, pskey, nh, dh, g_ap, gkey, rope_t, rpkey, coff, out_bf, outkey):
            n = nh * dh
            hd = dh // 2
            a = nxt("wk", 6); b2 = nxt("wk", 6); c2 = nxt("wk", 6); d2 = nxt("wk", 6); e2 = nxt("wk", 6)
            s1 = nxt("sm", 4)
            A, B, C, Dd, E = wk[a], wk[b2], wk[c2], wk[d2], wk[e2]
            p.op("act", lambda e: e.activation(out=A[:, 0:n], in_=ps_ap, func=AF.Square),
                 [pskey], [("wk", a)])
            p.op("dve", lambda e: e.tensor_reduce(
                out=sm[s1][:, 0:nh], in_=A[:, 0:n].rearrange("p (h d) -> p h d", h=nh),
                axis=AX.X, op=ALU.add), [("wk", a)], [("sm", s1)])
            p.op("act", lambda e: e.activation(
                out=sm[s1][:, 0:nh], in_=sm[s1][:, 0:nh], func=AF.Sqrt, scale=1.0 / dh, bias=c.eps_rms[:, 0:1]),
                [("sm", s1)], [("sm", s1)])
            p.op("dve", lambda e: e.reciprocal(out=sm[s1][:, 0:nh], in_=sm[s1][:, 0:nh]),
                 [("sm", s1)], [("sm", s1)])
            p.op("dve", lambda e: e.tensor_tensor(
                out=B[:, 0:n].rearrange("p (h d) -> p h d", h=nh),
                in0=ps_ap.rearrange("p (h d) -> p h d", h=nh),
                in1=sm[s1][:, 0:nh].unsqueeze(2).to_broadcast([128, nh, dh]),
                op=ALU.mult), [pskey, ("sm", s1)], [("wk", b2)])
            p.op("dve", lambda e: e.tensor_tensor(out=C[:, 0:n], in0=B[:, 0:n], in1=g_ap, op=ALU.mult),
                 [("wk", b2), gkey], [("wk", c2)])
            rope(C, ("wk", c2), nh, dh, rope_t, rpkey, coff, Dd, ("wk", d2), E, ("wk", e2),
                 out_bf[:, 0:n].rearrange("p (h d) -> p h d", h=nh), outkey)

        def rope(X, xkey, nh, dh, rope_t, rpkey, coff, Dd, dkey, E, ekey, out3, outkey, xview=None):
            n = nh * dh
            hd = dh // 2
            x3 = xview if xview is not None else X[:, 0:n].rearrange("p (h d) -> p h d", h=nh)
            cs = rope_t[:, coff:coff + dh].unsqueeze(1).to_broadcast([128, nh, dh])
            sslo = rope_t[:, coff + dh:coff + dh + hd].unsqueeze(1).to_broadcast([128, nh, hd])
            sshi = rope_t[:, coff + dh + hd:coff + 2 * dh].unsqueeze(1).to_broadcast([128, nh, hd])
            d3 = Dd[:, 0:n].rearrange("p (h d) -> p h d", h=nh)
            e3 = E[:, 0:n].rearrange("p (h d) -> p h d", h=nh)
            p.op("dve", lambda e: e.tensor_tensor(out=d3, in0=x3, in1=cs, op=ALU.mult),
                 [xkey, rpkey], [dkey])
            p.op("dve", lambda e: e.tensor_tensor(out=e3[:, :, 0:hd], in0=x3[:, :, hd:dh], in1=sslo,
                                                  op=ALU.mult), [xkey, rpkey], [ekey])
            p.op("dve", lambda e: e.tensor_tensor(out=e3[:, :, hd:dh], in0=x3[:, :, 0:hd], in1=sshi,
                                                  op=ALU.mult), [xkey, rpkey, ekey], [ekey])
            p.op("pool", lambda e: e.tensor_tensor(out=out3, in0=d3, in1=e3, op=ALU.add),
                 [dkey, ekey], [outkey])

        def rms_full(ps_ap, pskey, n, g_ap, gkey, out_bf, outkey):
            a = nxt("wk", 6)
            s1 = nxt("sm", 4)
            p.op("act", lambda e: e.activation(out=wk[a][:, 0:n], in_=ps_ap, func=AF.Square,
                                               accum_out=sm[s1][:, 0:1]),
                 [pskey], [("wk", a), ("sm", s1)])
            p.op("act", lambda e: e.activation(
                out=sm[s1][:, 1:2], in_=sm[s1][:, 0:1], func=AF.Sqrt, scale=1.0 / n, bias=c.eps_rms[:, 0:1]),
                [("sm", s1)], [("sm", s1)])
            p.op("dve", lambda e: e.reciprocal(out=sm[s1][:, 2:3], in_=sm[s1][:, 1:2]),
                 [("sm", s1)], [("sm", s1)])
            p.op("dve", lambda e: e.scalar_tensor_tensor(
                out=out_bf, in0=ps_ap, scalar=sm[s1][:, 2:3], in1=g_ap,
                op0=ALU.mult, op1=ALU.mult), [pskey, ("sm", s1), gkey], [outkey])

        def na_kv(xTa, xTkey, tok_off, sub, bs, vs=None):
            ps, pk = tok_mm(xTa, xTkey, 384, 768)
            o = nxt("ob", 8)
            p.op("act", lambda e, ps=ps, o=o: e.activation(out=ob[o][:, 0:384], in_=ps[:, 0:384],
                                                           func=AF.Copy), [pk], [("ob", o)])
            kb = blk["b"][bs][0:64, :].rearrange("p (h t) -> p h t", h=6)
            transp_out(ob[o], ("ob", o), 6, 64, kb[:, :, sub * 128:(sub + 1) * 128], ("blk_b", bs))
            ps, pk = tok_mm(xTa, xTkey, 768, 1152)
            s = (cnt["tile"] - 1) % 2 if vs is None else vs
            p.op("act", lambda e, ps=ps, s=s: e.activation(
                out=vna[s][:, :, 0:64], in_=ps[:, 0:384].rearrange("p (h d) -> p h d", h=6),
                func=AF.Copy), [pk], [("vna", s)])
            st(c.s["v_na"][tok_off:tok_off + 128, :], vna[s][:].rearrange("p h d -> p (h d)"),
                [("vna", s)], [("d_v_na", tok_off)])

        def flush_blk(name, bs, dh, nh, dst, t0, nt):
            src = blk[name][bs][0:dh, 0:nh * 512].rearrange("p (h t) -> p h t", h=nh)[:, :, 0:nt]
            st(dst[:, :, t0:t0 + nt].rearrange("h d t -> d h t"), src,
               [("blk_" + name, bs)], [("d_" + name, t0)])

        def run_interleaved(gens):
            active = list(gens)
            while active:
                for g_ in list(active):
                    try:
                        next(g_)
                    except StopIteration:
                        active.remove(g_)

        def own_tile(bi, sub):
            bs = bi % 2
            t = bi * 4 + sub
            xTa, xTkey, rpt, rpk = load_tile(
                c.q_src[t * 128:(t + 1) * 128, :], c.rope_q[t * 128:(t + 1) * 128, :],
                to_xT=(xT[bs][:, :, sub * 128:(sub + 1) * 128], ("xT", bs)))
            vs = (cnt["tile"] - 1) % 2
            ps, pk = tok_mm(xTa, xTkey, 1152, 1536)
            o = nxt("ob", 8)
            rms_rope(ps[:, 0:384], pk, 6, 64, gq[:], "gq", rpt, rpk, 0, ob[o], ("ob", o))
            yield
            ps, pk = tok_mm(xTa, xTkey, 0, 384)
            oq = nxt("ob", 8)
            p.op("act", lambda e, oq=oq, ps=ps: e.activation(out=ob[oq][:, 0:384], in_=ps[:, 0:384],
                                                             func=AF.Copy), [pk], [("ob", oq)])
            qa = blk["a"][bs][0:64, :].rearrange("p (h t) -> p h t", h=6)
            transp_out(ob[oq], ("ob", oq), 6, 64, qa[:, :, sub * 128:(sub + 1) * 128], ("blk_a", bs))
            qg = blk["c"][bs][0:64, :].rearrange("p (h t) -> p h t", h=6)
            transp_out(ob[o], ("ob", o), 6, 64, qg[:, :, sub * 128:(sub + 1) * 128], ("blk_c", bs))
            yield
            ps, pk = tok_mm(xTa, xTkey, 1792, 2176)
            o = nxt("ob", 8)
            rms_full(ps[:, 0:384], pk, 384, mq[:], "mq", ob[o][:, 0:384], ("ob", o))
            yield
            na_kv(xTa, xTkey, 256 + t * 128, sub, bs, vs)
            tt = nxt("tT", 2)
            tr_group(p, [(psR[:, ch, :], ob[o][:, ch * 128:(ch + 1) * 128]) for ch in range(3)],
                     identb[:], [("ob", o), "identb"], ["psR"])
            p.op("act", lambda e, tt=tt: e.activation(
                out=tT[tt][:, 0:384].rearrange("p (c t) -> p c t", c=3), in_=psR[:, 0:3, :],
                func=AF.Copy), ["psR"], [("tT", tt)])
            g = nxt("psG", 2)
            tT3 = tT[tt][:, 0:384].rearrange("p (c t) -> p c t", c=3)
            mm_group(p, psG[g][:, 0:384], [(tT3[:, ch, :], wuqb[:, ch, :]) for ch in range(3)],
                     [("tT", tt), "wuqb"], ["psG%d" % g])
            o2 = nxt("ob", 8)
            qc3 = psG[g][:, 0:384].rearrange("p (h d) -> p h d", h=4)
            out3 = ob[o2][:, 0:384].rearrange("p (h d) -> p h d", h=4)
            p.op("act", lambda e, qc3=qc3, out3=out3: e.activation(
                out=out3[:, :, 0:64], in_=qc3[:, :, 0:64], func=AF.Copy),
                ["psG%d" % g], [("ob", o2)])
            a = nxt("wk", 6); d2 = nxt("wk", 6); e2 = nxt("wk", 6)
            xr3 = wk[a][:, 0:128].rearrange("p (h d) -> p h d", h=4)
            p.op("act", lambda e, qc3=qc3, xr3=xr3: e.activation(out=xr3, in_=qc3[:, :, 64:96],
                                                                 func=AF.Copy),
                 ["psG%d" % g], [("wk", a)])
            rope(wk[a], ("wk", a), 4, 32, rpt, rpk, 128, wk[d2], ("wk", d2), wk[e2], ("wk", e2),
                 out3[:, :, 64:96], ("ob", o2))
            yield
            qm = blk["d"][bs][0:96, 0:2048].rearrange("p (h t) -> p h t", h=4)
            transp_out(ob[o2], ("ob", o2), 4, 96, qm[:, :, sub * 128:(sub + 1) * 128], ("blk_d", bs))

        for bi in range(8):
            bs = bi % 2
            run_interleaved([own_tile(bi, 0), own_tile(bi, 1)])
            run_interleaved([own_tile(bi, 2), own_tile(bi, 3)])
            flush_blk("a", bs, 64, 6, c.s["qT_na"], bi * 512, 512)
            flush_blk("b", bs, 64, 6, c.s["kT_na"], 256 + bi * 512, 512)
            flush_blk("c", bs, 64, 6, c.s["qT_gqa"], bi * 512, 512)
            flush_blk("d", bs, 96, 4, c.s["qT_mla"], bi * 512, 512)
            flush()
            for cc in range(24):
                g = nxt("psG", 2)
                c0 = 2464 + cc * 128
                mm_group(p, psG[g][:, :], [(winb[:, ch, c0:c0 + 128], xT[bs][:, ch, :]) for ch in range(8)],
                         [("xT", bs), "winb"], ["psG%d" % g])
                go = nxt("gout", 3)
                p.op("act", lambda e, g=g, go=go: e.activation(out=gout[go][:], in_=psG[g][:, :],
                                                               func=AF.Sigmoid),
                     ["psG%d" % g], [("gout", go)])
                dma(p, "sp", c.s["gT"][cc * 128:(cc + 1) * 128, bi * 512:(bi + 1) * 512], gout[go][:],
                    [("gout", go)], [("d_gT", cc, bi)])

        for hi in range(4):
            hsrc = (c.c_src[HALF - 256 + hi * 128:HALF - 256 + (hi + 1) * 128, :] if hi < 2 else
                    c.c_src[(hi - 2) * 128:(hi - 1) * 128, :])
            xTa, xTkey, rpt, rpk = load_tile(hsrc, None, mask_col=c.hm_cols[0 if hi < 2 else 1])
            flush()
            tok_off = hi * 128 if hi < 2 else 4352 + (hi - 2) * 128
            na_kv(xTa, xTkey, tok_off, hi % 2, 0)
            if hi % 2 == 1:
                flush_blk("b", 0, 64, 6, c.s["kT_na"], 0 if hi == 1 else 4352, 256)

        def full_tile(bi, sub):
            bs = bi % 2
            t = bi * 4 + sub
            xTa, xTkey, rpt, rpk = load_tile(
                c.full_src[t * 128:(t + 1) * 128, :], c.rope_full[t * 128:(t + 1) * 128, :])
            s = (cnt["tile"] - 1) % 2
            ps, pk = tok_mm(xTa, xTkey, 1536, 1792)
            o = nxt("ob", 8)
            rms_rope(ps[:, 0:128], pk, 2, 64, gk[:], "gk", rpt, rpk, 0, ob[o], ("ob", o))
            p.op("act", lambda e, ps=ps, s=s: e.activation(
                out=vg[s][:, :, 0:64], in_=ps[:, 128:256].rearrange("p (h d) -> p h d", h=2),
                func=AF.Copy), [pk], [("vg", s)])
            st(c.s["v_gqa"][t * 128:(t + 1) * 128, :], vg[s][:].rearrange("p h d -> p (h d)"),
               [("vg", s)], [("d_v_gqa", t)])
            yield
            ps, pk = tok_mm(xTa, xTkey, 2176, 2464)
            o3 = nxt("ob", 8)
            rms_full(ps[:, 0:256], pk, 256, mkv[:], "mkv", ob[o3][:, 0:256], ("ob", o3))
            a = nxt("wk", 6)
            p.op("act", lambda e, ps=ps, a=a: e.activation(out=wk[a][:, 0:32], in_=ps[:, 256:288],
                                                           func=AF.Copy), [pk], [("wk", a)])
            d2 = nxt("wk", 6); e2 = nxt("wk", 6); f2 = nxt("wk", 6)
            kr3 = wk[f2][:, 0:32].rearrange("p (h d) -> p h d", h=1)
            rope(wk[a], ("wk", a), 1, 32, rpt, rpk, 128, wk[d2], ("wk", d2), wk[e2], ("wk", e2),
                 kr3, ("wk", f2))
            o2 = nxt("ob", 8)
            out3 = ob[o2][:, 0:384].rearrange("p (h d) -> p h d", h=4)
            p.op("pool", lambda e, out3=out3, f2=f2: e.tensor_copy(
                out3[:, :, 64:96], wk[f2][:, 0:32].unsqueeze(1).to_broadcast([128, 4, 32])),
                [("wk", f2)], [("ob", o2)])
            yield
            kg = blk["a"][bs][0:64, 0:1024].rearrange("p (h t) -> p h t", h=2)
            transp_out(ob[o], ("ob", o), 2, 64, kg[:, :, sub * 128:(sub + 1) * 128], ("blk_a", bs))
            tt = nxt("tT", 2)
            tr_group(p, [(psR[:, ch, :], ob[o3][:, ch * 128:(ch + 1) * 128]) for ch in range(2)],
                     identb[:], [("ob", o3), "identb"], ["psR"])
            tT2 = tT[tt][:, 0:256].rearrange("p (c t) -> p c t", c=2)
            p.op("act", lambda e, tT2=tT2: e.activation(out=tT2, in_=psR[:, 0:2, :], func=AF.Copy),
                 ["psR"], [("tT", tt)])
            g = nxt("psG", 2)
            mm_group(p, psG[g][:, :], [(tT2[:, ch, :], wukvb[:, ch, :]) for ch in range(2)],
                     [("tT", tt), "wukvb"], ["psG%d" % g])
            kv3 = psG[g][:, :].rearrange("p (h d) -> p h d", h=4)
            p.op("act", lambda e, kv3=kv3, out3=out3: e.activation(
                out=out3[:, :, 0:64], in_=kv3[:, :, 0:64], func=AF.Copy),
                ["psG%d" % g, ("ob", o2)], [("ob", o2)])
            p.op("dve", lambda e, kv3=kv3, s=s: e.tensor_copy(vm[s][:, :, 0:64], kv3[:, :, 64:128]),
                 ["psG%d" % g], [("vm", s)])
            st(c.s["v_mla"][t * 128:(t + 1) * 128, :], vm[s][:].rearrange("p h d -> p (h d)"),
               [("vm", s)], [("d_v_mla", t)])
            yield
            km = blk["d"][bs][0:96, 0:2048].rearrange("p (h t) -> p h t", h=4)
            transp_out(ob[o2], ("ob", o2), 4, 96, km[:, :, sub * 128:(sub + 1) * 128], ("blk_d", bs))

        flush()
        for bi in range(16 if c.do_full else 0):
            bs = bi % 2
            run_interleaved([full_tile(bi, 0), full_tile(bi, 1)])
            run_interleaved([full_tile(bi, 2), full_tile(bi, 3)])
            flush_blk("a", bs, 64, 2, c.s["kT_gqa"], bi * 512, 512)
            flush_blk("d", bs, 96, 4, c.s["kT_mla"], bi * 512, 512)
        flush()
        p.barrier()
        p.emit()


W_SHAPES = {
    "w_in": ([D, DIN], F32), "w_uq": ([384, 384], F32), "w_ukv": ([256, 512], F32),
    "w_ba": ([384, D], F32), "w_bb": ([384, D], F32), "w_bc": ([256, D], F32),
    "w_out": ([D, D], F32), "w_router": ([D, NEXP], F32),
    "w_gu": ([NEXP, D, 2 * DEXP], F32), "w_dn": ([NEXP, DEXP, D], F32),
    "gq_rep": ([128, 384], F32), "gk_rep": ([128, 128], F32),
    "mq_rep": ([128, 384], F32), "mkv_rep": ([128, 256], F32),
    "ln1g": ([128, D], F32), "ln1b": ([128, D], F32), "ln2g": ([128, D], F32), "ln2b": ([128, D], F32),
    "brouter": ([128, NEXP], F32), "bgu_t": ([128, NEXP, 16], F32), "bdn": ([NEXP, D], F32),
    "na_bint": ([64, 6, 8, 64], F32), "na_bbnd": ([7, 64, 6, 12, 64], F32),
    "na_bbnd_o": ([7, 64, 6, 12, 64], F32),
}
C_SHAPES = {
    "rope_loc": ([S, 192], F32), "hmask": ([128, 4], F32),
    "identb": ([128, 128], BF16), "identf": ([128, 128], F32),
    "triu": ([128, 128], F32), "ones128": ([128, 128], F32),
    "iota_e": ([128, NEXP], F32), "iota_cap": ([128, NEXP], F32),
}
CAP = 768
NSLOT = NEXP * CAP
S_SHAPES = {
    "qT_na": ([6, 64, HALF], BF16), "kT_na": ([6, 64, NA_ROWS * 64], BF16),
    "v_na": ([NA_ROWS * 64, 390], BF16),
    "qT_gqa": ([6, 64, HALF], BF16), "kT_gqa": ([2, 64, S], BF16), "v_gqa": ([S, 130], BF16),
    "qT_mla": ([4, 96, HALF], BF16), "kT_mla": ([4, 96, S], BF16), "v_mla": ([S, 260], BF16),
    "gT": ([3 * D, HALF], BF16),
    "oT": ([16, 64, HALF], BF16),
    "x1": ([HALF, D], F32),
    "x1T": ([D, HALF], BF16), "gateT": ([NEXP, HALF], F32), "ymoe": ([HALF, D], F32),
    "xg": ([NSLOT, D], BF16), "yg": ([NSLOT, D], F32),
    "slots": ([HALF, 4], I32), "gks": ([HALF, 4], F32),
}


def build(taps=(), passes=("A", "B", "C"), phases=None, dst_override=None):
    nc = bass.Bass("TRN2", target_bir_lowering=False)
    phases = phases or ALL_PHASES
    c = Ctx()
    c.nc = nc
    c.w = {}
    for k, (shp, dt) in W_SHAPES.items():
        c.w[k] = nc.dram_tensor(k, [2] + shp, dt, kind="ExternalInput").ap()
    for k, (shp, dt) in C_SHAPES.items():
        c.w[k] = nc.dram_tensor(k, shp, dt, kind="ExternalInput").ap()
    x_loc = nc.dram_tensor("x_loc", [S, D], F32, kind="ExternalInput").ap()
    c.s = {}
    for k, (shp, dt) in S_SHAPES.items():
        kind = "ExternalOutput" if k in taps else "Internal"
        c.s[k] = nc.dram_tensor("s_" + k, shp, dt, kind=kind).ap()
    y01 = nc.dram_tensor("y01", [S, D], F32, kind="Internal").ap()
    c.y = nc.dram_tensor("y", [HALF, D], F32, kind="ExternalOutput").ap()
    with ExitStack() as stack:
        c.p = Prog(nc, stack)
        c.breg = nc.gpsimd.to_reg(NSLOT - 1)
        c.eps_rms = stack.enter_context(nc.sbuf_tensor("eps_rms", [128, 1], F32))
        c.eps_ln = stack.enter_context(nc.sbuf_tensor("eps_ln", [128, 1], F32))
        c.hm = stack.enter_context(nc.sbuf_tensor("hmask_sb", [128, 4], F32))
        c.p.op("pool", lambda e: e.memset(c.eps_rms[:], RMS_EPS), [], ["eps"])
        c.p.op("pool", lambda e: e.memset(c.eps_ln[:], LN_EPS), [], ["eps"])
        dma(c.p, "sp", c.hm[:], c.w["hmask"][:, :], [], ["hm"])
        c.p.barrier()
        c.p.emit()
        rope = c.w["rope_loc"]
        c.rope_full = rope
        with ExitStack() as ph:
            zt = ph.enter_context(nc.sbuf_tensor("zt", [128, 8, D], BF16))
            c.p.op("pool", lambda e: e.memset(zt[:], 0.0), [], ["zt"])
            xg_v = c.s["xg"].rearrange("(n p) d -> p n d", p=128)
            for i in range(NSLOT // 1024):
                dma(c.p, "sp", xg_v[:, i * 8:(i + 1) * 8, :], zt[:], ["zt"], [("d_xg0", i)])
            c.p.barrier()
            c.p.emit()
        for ps_ in passes:
            if ps_ == "A":
                L, c.q_src, c.c_src, c.full_src = 0, x_loc[0:HALF, :], x_loc[HALF:S, :], x_loc
                c.rope_q, c.bbnd, c.hm_cols, c.do_full, dst = rope[0:HALF, :], c.w["na_bbnd"][0], (0, 1), True, y01[0:HALF, :]
            elif ps_ == "B":
                L, c.q_src, c.c_src, c.full_src = 0, x_loc[HALF:S, :], x_loc[0:HALF, :], x_loc
                c.rope_q, c.bbnd, c.hm_cols, c.do_full, dst = rope[HALF:S, :], c.w["na_bbnd_o"][0], (2, 3), False, y01[HALF:S, :]
            else:
                L, c.q_src, c.c_src, c.full_src = 1, y01[0:HALF, :], y01[HALF:S, :], y01
                c.rope_q, c.bbnd, c.hm_cols, c.do_full, dst = rope[0:HALF, :], c.w["na_bbnd"][1], (0, 1), True, c.y
            if "p1" in phases:
                phase1(c, L)
            if "na" in phases:
                phase_na(c, L)
            if "gqa" in phases:
                phase_dense_attn(c, L, "gqa")
            if "mla" in phases:
                phase_dense_attn(c, L, "mla")
            if "merge" in phases:
                phase_merge(c, L)
            if "moe" in phases:
                phase_moe(c, L)
            if "moes" in phases:
                phase_moe_sparse(c, L)
            if "ln2" in phases:
                phase_ln2(c, L, dst if dst_override is None else dst_override(c), sparse=("moes" in phases))
        c.p.barrier()
        c.p.emit()
    return nc


def rope_tables():
    t = np.arange(S)
    row = (t // 64).astype(np.float32)
    col = (t % 64).astype(np.float32)
    out = []
    for dim in (64, 32):
        quarter = dim // 4
        inv = (10000.0 ** (-np.arange(quarter, dtype=np.float32) / quarter)).astype(np.float32)
        ang = np.concatenate([row[:, None] * inv, col[:, None] * inv], -1).astype(np.float32)
        cs, sn = np.cos(ang).astype(np.float32), np.sin(ang).astype(np.float32)
        out.append(np.concatenate([cs, cs], -1))
        out.append(np.concatenate([-sn, sn], -1))
    return np.ascontiguousarray(np.concatenate(out, -1).astype(np.float32))


def na_bias_tables(rpb, hf):
    cols = np.arange(64)
    c0 = np.clip(cols - 8, 0, 48)
    in_win = (cols[None, :] >= c0[:, None]) & (cols[None, :] < c0[:, None] + 16)
    idx_c = np.clip(cols[None, :] - cols[:, None] + 15, 0, 30)

    def tab(j, rows):
        r = hf * 64 + j
        r0 = int(np.clip(r - 4, 0, 120))
        out = np.full((64, 6, len(rows), 64), NEG, np.float32)
        for ii, lr in enumerate(rows):
            gr = hf * 64 + lr - 4
            i = gr - r0
            if i < 0 or i >= 8 or gr < 0 or gr >= 128:
                continue
            ir = gr - r + 7
            b = rpb[:, ir][:, idx_c]
            b = np.where(in_win[None], b, NEG)
            out[:, :, ii, :] = b.transpose(2, 0, 1)
        return out
    interior = tab(10, list(range(10, 18)))
    bnd = []
    for j in (0, 1, 2, 3):
        bnd.append(tab(j, list(range(0, 12))))
    for j in (61, 62, 63):
        bnd.append(tab(j, list(range(60, 72))))
    return interior, np.stack(bnd, 0)


def prep_weights(inp, layers, hf):
    w = {}
    ls = list(layers)
    st = lambda f: np.ascontiguousarray(np.stack([f(l) for l in ls], 0))
    w["w_in"] = st(lambda l: inp["w_in"][l])
    w["w_uq"] = st(lambda l: inp["w_uq"][l])
    w["w_ukv"] = st(lambda l: inp["w_ukv"][l])
    w["w_ba"] = st(lambda l: inp["w_branch_a"][l])
    w["w_bb"] = st(lambda l: inp["w_branch_b"][l])
    w["w_bc"] = st(lambda l: inp["w_branch_c"][l])
    w["w_out"] = st(lambda l: inp["w_out"][l])
    w["w_router"] = st(lambda l: inp["w_router"][l])
    w["w_gu"] = st(lambda l: inp["w_gate_up"][l])
    w["w_dn"] = st(lambda l: inp["w_down"][l])
    w["gq_rep"] = st(lambda l: np.tile(inp["gqa_q_norm"][l][None, :], (128, 6)))
    w["gk_rep"] = st(lambda l: np.tile(inp["gqa_k_norm"][l][None, :], (128, 2)))
    w["mq_rep"] = st(lambda l: np.tile(inp["mla_q_norm"][l][None, :], (128, 1)))
    w["mkv_rep"] = st(lambda l: np.tile(inp["mla_kv_norm"][l][None, :], (128, 1)))
    for k, src in (("ln1g", "ln1_g"), ("ln1b", "ln1_b"), ("ln2g", "ln2_g"), ("ln2b", "ln2_b")):
        w[k] = st(lambda l: np.tile(inp[src][l][None, :], (128, 1)))
    w["brouter"] = st(lambda l: np.tile(inp["b_router"][l][None, :], (128, 1)))
    w["bgu_t"] = st(lambda l: inp["b_gate_up"][l].reshape(NEXP, 16, 128).transpose(2, 0, 1))
    w["bdn"] = st(lambda l: inp["b_down"][l])
    bi, bb = zip(*[na_bias_tables(inp["na_rpb"][l], hf) for l in ls])
    w["na_bint"] = np.ascontiguousarray(np.stack(bi, 0))
    w["na_bbnd"] = np.ascontiguousarray(np.stack(bb, 0))
    w["na_bbnd_o"] = np.ascontiguousarray(np.stack([na_bias_tables(inp["na_rpb"][l], 1 - hf)[1] for l in ls], 0))
    return {k: np.ascontiguousarray(v.astype(np.float32)) for k, v in w.items()}


def prep_consts(hf):
    rt = rope_tables()
    own, oth = rt[hf * HALF:(hf + 1) * HALF], rt[(1 - hf) * HALF:(2 - hf) * HALF]
    hm = np.zeros((128, 4), np.float32)
    hm[:, 0], hm[:, 1], hm[:, 2], hm[:, 3] = hf, 1 - hf, 1 - hf, hf
    return {
        "rope_loc": np.ascontiguousarray(np.concatenate([own, oth], 0)),
        "hmask": hm,
        "identb": np.eye(128, dtype=np.float32).astype(ml_dtypes.bfloat16),
        "identf": np.eye(128, dtype=np.float32),
        "triu": np.triu(np.ones((128, 128), np.float32), 1),
        "ones128": np.ones((128, 128), np.float32),
        "iota_e": np.tile(np.arange(NEXP, dtype=np.float32)[None, :], (128, 1)),
        "iota_cap": np.tile((np.arange(NEXP, dtype=np.float32) * CAP)[None, :], (128, 1)),
    }


def prep_acts(xb, hf):
    own, oth = xb[hf * HALF:(hf + 1) * HALF], xb[(1 - hf) * HALF:(2 - hf) * HALF]
    return {"x_loc": np.ascontiguousarray(np.concatenate([own, oth], 0))}


def _normalize(c, psO, okey, ncol, rsb, rkey, psB, ou, oukey, onesf, dst_ap, dst_key, nh=1):
    p = c.p
    p.op("dve", lambda e: e.reciprocal(out=rsb[64:65, 0:ncol], in_=psO[64:65, 0:ncol]), [okey], [rkey])
    p.op("pe", lambda e: e.matmul(psB[0:64, 0:ncol], onesf[64:65, 0:64], rsb[64:65, 0:ncol],
                                  start=True, stop=True), [rkey, "onesf"], ["psB"])
    p.op("act", lambda e: e.activation(out=ou[0:64, 0:ncol], in_=psO[0:64, 0:ncol], func=AF.Copy),
         [okey], [oukey])
    a0 = ou[0:64, 0:ncol]
    a1 = psB[0:64, 0:ncol]
    if nh > 1:
        a0 = a0.rearrange("p (h q) -> p h q", h=nh)
        a1 = a1.rearrange("p (h q) -> p h q", h=nh)
    p.op("dve", lambda e: e.tensor_tensor(out=dst_ap, in0=a0, in1=a1, op=ALU.mult),
         [oukey, "psB"], [dst_key])


def phase_dense_attn(c, L, kind):
    nc, p = c.nc, c.p
    if kind == "gqa":
        nH, dh, nK, nV, scale, obase = 6, 64, 2, 2, 64 ** -0.5, 6
        qT, kT, vS = c.s["qT_gqa"], c.s["kT_gqa"], c.s["v_gqa"]
        kmap = [0, 0, 0, 1, 1, 1]
    else:
        nH, dh, nK, nV, scale, obase = 4, 96, 4, 4, 96 ** -0.5, 12
        qT, kT, vS = c.s["qT_mla"], c.s["kT_mla"], c.s["v_mla"]
        kmap = [0, 1, 2, 3]
    with ExitStack() as ph:
        c.ph = ph
        KT = _sb(c, "KT", [128, nK, S], BF16)
        V = _sb(c, "V", [128, 64, nV * 65], BF16)
        Q = [_sb(c, "Q%d" % i, [128, nH, 512], BF16) for i in range(2)]
        pT = [_sb(c, "pT%d" % i, [128, 1024], BF16) for i in range(3)]
        rsb = _sb(c, "rsb", [128, 512], F32)
        ou = _sb(c, "ou", [128, 512], F32)
        ot = [_sb(c, "ot%d" % i, [128, 512], BF16) for i in range(2)]
        onesf = _sb(c, "onesf", [128, 64], F32)
        psS = [_ps(c, "psS%d" % i, [128, 1024], F32) for i in range(2)]
        psO = [_ps(c, "psO%d" % i, [128, 512], F32) for i in range(2)]
        psB = _ps(c, "psB", [128, 512], F32)
        p.op("pool", lambda e: e.memset(onesf[:], 1.0), [], ["onesf"])
        pack = (kind == "gqa")
        if pack:
            kT2 = kT.rearrange("g d t -> (g d) t")
            for half in range(2):
                dma(p, "sp", KT[:, 0, half * HALF:(half + 1) * HALF], kT2[:, half * HALF:(half + 1) * HALF],
                    [], ["KT"])
            for i in range(2):
                p.op("pool", lambda e, i=i: e.memset(Q[i][:], 0.0), [], [("Q", i)])
        else:
            for k in range(nK):
                for half in range(2):
                    dma(p, "sp", KT[0:dh, k, half * HALF:(half + 1) * HALF],
                        kT[k, :, half * HALF:(half + 1) * HALF], [], ["KT"])
        vv = vS.rearrange("(t p) c -> p t c", p=128)
        for q4 in range(16):
            dma(p, "sp", V[:, q4 * 4:(q4 + 1) * 4, :], vv[:, q4 * 4:(q4 + 1) * 4, :], [], ["V"])
        it = 0
        for qb in range(8):
            qs = qb % 2
            if pack:
                dma(p, "sp", Q[qs][0:64, 0:3, :], qT[0:3, :, qb * 512:(qb + 1) * 512].rearrange("h d t -> d h t"),
                    [], [("Q", qs)])
                dma(p, "sp", Q[qs][64:128, 3:6, :], qT[3:6, :, qb * 512:(qb + 1) * 512].rearrange("h d t -> d h t"),
                    [], [("Q", qs)])
            else:
                dma(p, "sp", Q[qs][0:dh, :, :], qT[:, :, qb * 512:(qb + 1) * 512].rearrange("h d t -> d h t"),
                    [], [("Q", qs)])
            for h in range(nH):
                ob_ = it % 2
                it += 1
                okey = "psO%d" % ob_
                ki = kmap[h]

                def qk2(k2, h=h, ki=ki, qs=qs):
                    b = k2 % 2

                    def fn(e):
                        ins = None
                        for u in range(2):
                            kt = 2 * k2 + u
                            if pack:
                                ins = e.matmul(psS[b][:, u * 512:(u + 1) * 512], KT[:, 0, kt * 128:(kt + 1) * 128],
                                               Q[qs][:, h, :], start=True, stop=True)
                            else:
                                ins = e.matmul(psS[b][:, u * 512:(u + 1) * 512],
                                               KT[0:dh, ki, kt * 128:(kt + 1) * 128], Q[qs][0:dh, h, :],
                                               start=True, stop=True)
                        return ins
                    p.op("pe", fn, ["KT", ("Q", qs)], ["psS%d" % b])
                qk2(0)
                for k2 in range(32):
                    b = k2 % 2
                    pb = k2 % 3
                    if k2 + 1 < 32:
                        qk2(k2 + 1)
                    p.op("act", lambda e, b=b, pb=pb: e.activation(out=pT[pb][:], in_=psS[b][:, :],
                                                                   func=AF.Exp, scale=scale),
                         ["psS%d" % b], [("pT", pb)])

                    def fpv(e, k2=k2, pb=pb, ob_=ob_, ki=ki):
                        ins = None
                        for u in range(2):
                            kt = 2 * k2 + u
                            ins = e.matmul(psO[ob_][0:65, :], V[:, kt, ki * 65:(ki + 1) * 65],
                                           pT[pb][:, u * 512:(u + 1) * 512],
                                           start=(kt == 0), stop=(kt == 63))
                        return ins
                    p.op("pe", fpv, ["V", ("pT", pb)], [okey])
                os_ = it % 2
                _normalize(c, psO[ob_], okey, 512, rsb, "rsb", psB, ou, "ou", onesf,
                           ot[os_][0:64, :], ("ot", os_))
                dma(p, "sp", c.s["oT"][obase + h, :, qb * 512:(qb + 1) * 512], ot[os_][0:64, :],
                    [("ot", os_)], [("d_oT", obase + h, qb)])
        p.barrier()
        p.emit()


def phase_na(c, L):
    nc, p = c.nc, c.p
    with ExitStack() as ph:
        c.ph = ph
        Qb = [_sb(c, "Qb%d" % i, [128, 6, 512], BF16) for i in range(2)]
        Kb = [_sb(c, "Kb%d" % i, [128, 6, 1024], BF16) for i in range(2)]
        Vb = [_sb(c, "Vb%d" % i, [128, 16, 390], BF16) for i in range(2)]
        bint = _sb(c, "bint", [128, 6, 8, 64], F32)
        bbnd = _sb(c, "bbnd", [128, 6, 12, 64], F32)
        sc = [_sb(c, "sc%d" % i, [128, 768], F32) for i in range(2)]
        pp = [_sb(c, "pp%d" % i, [128, 768], BF16) for i in range(2)]
        rsb = _sb(c, "rsb", [128, 512], F32)
        ou = _sb(c, "ou", [128, 512], F32)
        ot = [_sb(c, "ot%d" % i, [128, 6, 512], BF16) for i in range(2)]
        onesf = _sb(c, "onesf", [128, 64], F32)
        psS = [_ps(c, "psS%d" % i, [128, 1024], F32) for i in range(2)]
        psO = [_ps(c, "psO%d" % i, [128, 512], F32) for i in range(2)]
        psB = _ps(c, "psB", [128, 512], F32)
        p.op("pool", lambda e: e.memset(onesf[:], 1.0), [], ["onesf"])
        dma(p, "sp", bint[0:64], c.w["na_bint"][L], [], ["bint"])
        vrow = c.s["v_na"].rearrange("(r k) c -> k r c", k=64)
        it = 0
        for b8 in range(8):
            s = b8 % 2
            dma(p, "sp", Qb[s][0:64, :, :], c.s["qT_na"][:, :, b8 * 512:(b8 + 1) * 512].rearrange("h d t -> d h t"),
                [], [("Qb", s)])
            dma(p, "sp", Kb[s][0:64, :, :], c.s["kT_na"][:, :, b8 * 512:b8 * 512 + 1024].rearrange("h d t -> d h t"),
                [], [("Kb", s)])
            for hv in range(2):
                dma(p, "sp", Vb[s][0:64, hv * 8:(hv + 1) * 8, :], vrow[:, b8 * 8 + hv * 8:b8 * 8 + (hv + 1) * 8, :],
                    [], [("Vb", s)])
            for jj in range(8):
                j = b8 * 8 + jj
                if j < 4:
                    rows = list(range(0, 12)); bidx = j
                elif j > 60:
                    rows = list(range(60, 72)); bidx = 4 + (j - 61)
                else:
                    rows = list(range(j, j + 8)); bidx = None
                nr = len(rows)
                if bidx is not None:
                    dma(p, "sp", bbnd[0:64], c.bbnd[bidx], [], ["bbnd"])
                    btile, bkey = bbnd, "bbnd"
                else:
                    btile, bkey = bint, "bint"
                ob_ = j % 2
                okey = "psO%d" % ob_
                for h in range(6):
                    sb_ = it % 2
                    it += 1
                    skey = "psS%d" % sb_

                    def fqk(e, h=h, sb_=sb_, rows=rows, jj=jj, s=s, b8=b8):
                        ins = None
                        for i, lr in enumerate(rows):
                            ins = e.matmul(psS[sb_][0:64, i * 64:(i + 1) * 64],
                                           Kb[s][0:64, h, (lr - b8 * 8) * 64:(lr - b8 * 8 + 1) * 64],
                                           Qb[s][0:64, h, jj * 64:(jj + 1) * 64], start=True, stop=True)
                        return ins
                    p.op("pe", fqk, [("Qb", s), ("Kb", s)], [skey])
                    p.op("dve", lambda e, h=h, sb_=sb_, nr=nr, btile=btile: e.scalar_tensor_tensor(
                        out=sc[sb_][0:64, 0:nr * 64], in0=psS[sb_][0:64, 0:nr * 64], scalar=0.125,
                        in1=btile[0:64, h, 0:nr, :].rearrange("p r q -> p (r q)"),
                        op0=ALU.mult, op1=ALU.add), [skey, bkey], [("sc", sb_)])
                    p.op("act", lambda e, sb_=sb_, nr=nr: e.activation(
                        out=pp[sb_][0:64, 0:nr * 64], in_=sc[sb_][0:64, 0:nr * 64], func=AF.Exp),
                        [("sc", sb_)], [("pp", sb_)])

                    def fpv(e, h=h, sb_=sb_, rows=rows, ob_=ob_, s=s, b8=b8):
                        ins = None
                        n = len(rows)
                        for i, lr in enumerate(rows):
                            ins = e.matmul(psO[ob_][0:65, h * 64:(h + 1) * 64],
                                           Vb[s][0:64, lr - b8 * 8, h * 65:(h + 1) * 65],
                                           pp[sb_][0:64, i * 64:(i + 1) * 64],
                                           start=(i == 0), stop=(i == n - 1))
                        return ins
                    p.op("pe", fpv, [("Vb", s), ("pp", sb_)], [okey])
                _normalize(c, psO[ob_], okey, 384, rsb, "rsb", psB, ou, "ou", onesf,
                           ot[s][0:64, :, jj * 64:(jj + 1) * 64],
                           ("ot", s), nh=6)
            dma(p, "sp", c.s["oT"][0:6, :, b8 * 512:(b8 + 1) * 512].rearrange("h d t -> d h t"),
                ot[s][0:64, :, :], [("ot", s)], [("d_oT_na", b8)])
        p.barrier()
        p.emit()


def layer_norm_tile(c, tsb, tkey, junk, jkey, sm, smkey, g_t, b_t, gbkey, out_t, outkey):
    p = c.p
    p.op("act", lambda e: e.activation(out=junk[:], in_=tsb[:], func=AF.Copy, accum_out=sm[:, 0:1]),
         [tkey], [jkey, smkey])
    p.op("act", lambda e: e.activation(out=junk[:], in_=tsb[:], func=AF.Square, accum_out=sm[:, 1:2]),
         [tkey], [jkey, smkey])
    p.op("dve", lambda e: e.tensor_scalar(out=sm[:, 2:3], in0=sm[:, 0:1], scalar1=1.0 / D, scalar2=None,
                                          op0=ALU.mult), [smkey], [smkey])
    p.op("dve", lambda e: e.tensor_tensor(out=sm[:, 3:4], in0=sm[:, 2:3], in1=sm[:, 2:3], op=ALU.mult),
         [smkey], [smkey])
    p.op("dve", lambda e: e.scalar_tensor_tensor(out=sm[:, 4:5], in0=sm[:, 1:2], scalar=1.0 / D,
                                                 in1=sm[:, 3:4], op0=ALU.mult, op1=ALU.subtract),
         [smkey], [smkey])
    p.op("act", lambda e: e.activation(out=sm[:, 5:6], in_=sm[:, 4:5], func=AF.Sqrt,
                                       bias=c.eps_ln[:, 0:1]), [smkey], [smkey])
    p.op("dve", lambda e: e.reciprocal(out=sm[:, 6:7], in_=sm[:, 5:6]), [smkey], [smkey])
    p.op("dve", lambda e: e.tensor_scalar(out=junk[:], in0=tsb[:], scalar1=sm[:, 2:3], scalar2=sm[:, 6:7],
                                          op0=ALU.subtract, op1=ALU.mult), [tkey, smkey], [jkey])
    p.op("pool", lambda e: e.tensor_tensor(out=junk[:], in0=junk[:], in1=g_t[:], op=ALU.mult),
         [jkey, gbkey], [jkey])
    p.op("pool", lambda e: e.tensor_tensor(out=out_t[:], in0=junk[:], in1=b_t[:], op=ALU.add),
         [jkey, gbkey], [outkey])


def phase_merge(c, L):
    nc, p = c.nc, c.p
    with ExitStack() as ph:
        c.ph = ph
        wb = _sb(c, "wb", [128, 8, D], BF16)
        woutb = _sb(c, "woutb", [128, 8, D], BF16)
        lng = _sb(c, "lng", [128, D], F32)
        lnb = _sb(c, "lnb", [128, D], F32)
        wr = _sb(c, "wr", [128, 8, NEXP], F32)
        br = _sb(c, "br", [128, NEXP], F32)
        identb = _sb(c, "identb", [128, 128], BF16)
        identf = _sb(c, "identf", [128, 128], F32)
        oTb = [_sb(c, "oTb%d" % i, [128, 8, 512], BF16) for i in range(2)]
        gTc = [_sb(c, "gTc%d" % i, [128, 3, 512], BF16) for i in range(2)]
        mixT = [_sb(c, "mixT%d" % i, [128, 8, 512], BF16) for i in range(2)]
        mt = [_sb(c, "mt%d" % i, [128, 512], F32) for i in range(6)]
        xo = [_sb(c, "xo%d" % i, [128, D], F32) for i in range(2)]
        tsb = [_sb(c, "tsb%d" % i, [128, D], F32) for i in range(2)]
        junk = _sb(c, "junk", [128, D], F32)
        x1t = [_sb(c, "x1t%d" % i, [128, D], F32) for i in range(2)]
        x1b = [_sb(c, "x1b%d" % i, [128, D], BF16) for i in range(2)]
        x1Tb = [_sb(c, "x1Tb%d" % i, [128, 8, 128], BF16) for i in range(2)]
        x1Tf = [_sb(c, "x1Tf%d" % i, [128, 8, 128], F32) for i in range(2)]
        sm = [_sb(c, "sm%d" % i, [128, 8], F32) for i in range(2)]
        rt_ = [_sb(c, "rt%d" % i, [128, 160], F32) for i in range(2)]
        gTt = [_sb(c, "gTt%d" % i, [128, 128], F32) for i in range(2)]
        psY = [_ps(c, "psY%d" % i, [128, 512], F32) for i in range(3)]
        psOut = [_ps(c, "psOut%d" % i, [128, 512], F32) for i in range(2)]
        psT = _ps(c, "psT", [128, 8, 128], BF16)
        psTf = _ps(c, "psTf", [128, 8, 128], F32)
        triu = _sb(c, "triu", [128, 128], F32)
        ones128 = _sb(c, "ones128", [128, 128], F32)
        iota_e = _sb(c, "iota_e", [128, NEXP], F32)
        iota_cap = _sb(c, "iota_cap", [128, NEXP], F32)
        msum = _sb(c, "msum", [128, NEXP], F32)
        rx = [_sb(c, "rx%d" % i, [128, 160], F32) for i in range(2)]
        idxu = [_sb(c, "idxu%d" % i, [128, 8], U32) for i in range(2)]
        slotu = [_sb(c, "slotu%d" % i, [128, 4], I32) for i in range(2)]
        dma(p, "sp", triu[:], c.w["triu"][:, :], [], ["triu"])
        dma(p, "sp", ones128[:], c.w["ones128"][:, :], [], ["ones128"])
        dma(p, "sp", iota_e[:], c.w["iota_e"][:, :], [], ["iota_e"])
        dma(p, "sp", iota_cap[:], c.w["iota_cap"][:, :], [], ["iota_cap"])
        p.op("pool", lambda e: e.memset(msum[:], 0.0), [], ["msum"])

        stgm = [_sb(c, "stgm%d" % i, [128, D], F32) for i in range(2)]
        for pp in range(8):
            src = (c.w["w_ba"][L][pp * 128:(pp + 1) * 128, :] if pp < 3 else
                   c.w["w_bb"][L][(pp - 3) * 128:(pp - 2) * 128, :] if pp < 6 else
                   c.w["w_bc"][L][(pp - 6) * 128:(pp - 5) * 128, :])
            si = pp % 2
            dma(p, "sp", stgm[si][:], src, [], [("stgm", si)])
            p.op("dve", lambda e, pp=pp, si=si: e.tensor_copy(wb[:, pp, :], stgm[si][:]), [("stgm", si)], ["wb"])
        wo_v = c.w["w_out"][L].rearrange("(c p) n -> p c n", p=128)
        for ch in range(8):
            si = ch % 2
            dma(p, "sp", stgm[si][:], wo_v[:, ch, :], [], [("stgm", si)])
            p.op("act", lambda e, ch=ch, si=si: e.activation(out=woutb[:, ch, :], in_=stgm[si][:], func=AF.Copy),
                 [("stgm", si)], ["woutb"])
        dma(p, "sp", lng[:], c.w["ln1g"][L], [], ["lngb"])
        dma(p, "sp", lnb[:], c.w["ln1b"][L], [], ["lngb"])
        dma(p, "sp", wr[:], c.w["w_router"][L].rearrange("(c p) e -> p c e", p=128), [], ["wr"])
        dma(p, "sp", br[:], c.w["brouter"][L], [], ["br"])
        dma(p, "sp", identb[:], c.w["identb"][:, :], [], ["identb"])
        dma(p, "sp", identf[:], c.w["identf"][:, :], [], ["identf"])
        gT_v = c.s["gT"].rearrange("(i c p) t -> p i c t", i=3, c=8, p=128)
        x1T_v = c.s["x1T"].rearrange("(c p) t -> p c t", p=128)
        gcnt = 0
        for blk in range(8):
            s = blk % 2
            oT_pairs = c.s["oT"][:, :, blk * 512:(blk + 1) * 512].rearrange("(hp two) d t -> two d hp t", two=2)
            for hv in range(2):
                dma(p, "sp", oTb[s][hv * 64:(hv + 1) * 64, :, :], oT_pairs[hv], [], [("oTb", s)])
            for dc in range(8):
                gs = gcnt % 2
                gcnt += 1
                dma(p, "sp", gTc[gs][:], gT_v[:, :, dc, blk * 512:(blk + 1) * 512], [], [("gTc", gs)])
                for i, (p0, npair) in enumerate(((0, 3), (3, 3), (6, 2))):
                    mm_group(p, psY[i][:, :],
                             [(wb[:, p0 + k, dc * 128:(dc + 1) * 128], oTb[s][:, p0 + k, :])
                              for k in range(npair)], ["wb", ("oTb", s)], ["psY%d" % i])
                m3 = [(gcnt * 3 + i) % 6 for i in range(3)]
                for i in range(3):
                    p.op("dve", lambda e, i=i, gs=gs, m=m3[i]: e.tensor_tensor(
                        out=mt[m][:], in0=psY[i][:, :], in1=gTc[gs][:, i, :], op=ALU.mult),
                        ["psY%d" % i, ("gTc", gs)], [("mt", m3[i])])
                p.op("pool", lambda e, m3=m3: e.tensor_tensor(out=mt[m3[0]][:], in0=mt[m3[0]][:],
                                                              in1=mt[m3[1]][:], op=ALU.add),
                     [("mt", m3[0]), ("mt", m3[1])], [("mt", m3[0])])
                p.op("pool", lambda e, m3=m3, s=s, dc=dc: e.tensor_tensor(
                    out=mixT[s][:, dc, :], in0=mt[m3[0]][:], in1=mt[m3[2]][:], op=ALU.add),
                    [("mt", m3[0]), ("mt", m3[2])], [("mixT", s)])
            for tt in range(4):
                t = blk * 4 + tt
                ts_ = t % 2
                dma(p, "sp", xo[ts_][:], c.q_src[t * 128:(t + 1) * 128, :], [], [("xo", ts_)])
                for half in range(2):
                    mm_group(p, psOut[half][:, :],
                             [(mixT[s][:, dc, tt * 128:(tt + 1) * 128], woutb[:, dc, half * 512:(half + 1) * 512])
                              for dc in range(8)], [("mixT", s), "woutb"], ["psOut%d" % half])
                    p.op("dve", lambda e, half=half, ts_=ts_: e.scalar_tensor_tensor(
                        out=tsb[ts_][:, half * 512:(half + 1) * 512], in0=xo[ts_][:, half * 512:(half + 1) * 512],
                        scalar=DN_ALPHA, in1=psOut[half][:, :], op0=ALU.mult, op1=ALU.add),
                        [("xo", ts_), "psOut%d" % half], [("tsb", ts_)])
                layer_norm_tile(c, tsb[ts_], ("tsb", ts_), junk, "junk", sm[ts_], ("sm", ts_),
                                lng, lnb, "lngb", x1t[ts_], ("x1t", ts_))
                dma(p, "sp", c.s["x1"][t * 128:(t + 1) * 128, :], x1t[ts_][:], [("x1t", ts_)], [("d_x1", t)])
                p.op("act", lambda e, ts_=ts_: e.activation(out=x1b[ts_][:], in_=x1t[ts_][:], func=AF.Copy),
                     [("x1t", ts_)], [("x1b", ts_)])
                tr_group(p, [(psT[:, ch, :], x1b[ts_][:, ch * 128:(ch + 1) * 128]) for ch in range(8)],
                         identb[:], [("x1b", ts_), "identb"], ["psT"])
                p.op("dve", lambda e, ts_=ts_: e.tensor_copy(x1Tb[ts_][:], psT[:]), ["psT"], [("x1Tb", ts_)])
                for hv in range(2):
                    dma(p, "sp", x1T_v[:, hv * 4:(hv + 1) * 4, t * 128:(t + 1) * 128], x1Tb[ts_][:, hv * 4:(hv + 1) * 4, :],
                        [("x1Tb", ts_)], [("d_x1T", t, hv)])
                tr_group(p, [(psTf[:, ch, :], x1t[ts_][:, ch * 128:(ch + 1) * 128]) for ch in range(8)],
                         identf[:], [("x1t", ts_), "identf"], ["psTf"])
                p.op("act", lambda e, ts_=ts_: e.activation(out=x1Tf[ts_][:], in_=psTf[:], func=AF.Copy),
                     ["psTf"], [("x1Tf", ts_)])
                mm_group(p, psY[0][:, 0:NEXP], [(x1Tf[ts_][:, ch, :], wr[:, ch, :]) for ch in range(8)],
                         [("x1Tf", ts_), "wr"], ["psY0"])
                R = rt_[ts_]
                rk = ("rt", ts_)
                lg, mx, msk, ex, exm = R[:, 0:32], R[:, 32:40], R[:, 40:72], R[:, 72:104], R[:, 104:136]
                nm, ssum, rs = R[:, 136:137], R[:, 137:138], R[:, 138:139]
                p.op("dve", lambda e, lg=lg: e.tensor_tensor(out=lg, in0=psY[0][:, 0:NEXP], in1=br[:], op=ALU.add),
                     ["psY0", "br"], [rk])
                p.op("dve", lambda e, lg=lg, mx=mx: e.max(out=mx, in_=lg), [rk], [rk])
                p.op("dve", lambda e, lg=lg, mx=mx, msk=msk: e.tensor_scalar(
                    out=msk, in0=lg, scalar1=mx[:, 3:4], scalar2=None, op0=ALU.is_ge), [rk], [rk])
                p.op("dve", lambda e, mx=mx, nm=nm: e.tensor_scalar(
                    out=nm, in0=mx[:, 0:1], scalar1=-1.0, scalar2=None, op0=ALU.mult), [rk], [rk])
                p.op("act", lambda e, lg=lg, ex=ex, nm=nm: e.activation(out=ex, in_=lg, func=AF.Exp, bias=nm),
                     [rk], [rk])
                p.op("dve", lambda e, ex=ex, msk=msk, exm=exm: e.tensor_tensor(out=exm, in0=ex, in1=msk,
                                                                               op=ALU.mult), [rk], [rk])
                p.op("dve", lambda e, exm=exm, ssum=ssum: e.reduce_sum(out=ssum, in_=exm, axis=AX.X), [rk], [rk])
                p.op("dve", lambda e, ssum=ssum, rs=rs: e.reciprocal(out=rs, in_=ssum), [rk], [rk])
                p.op("dve", lambda e, exm=exm, rs=rs: e.tensor_scalar(
                    out=exm, in0=exm, scalar1=rs, scalar2=None, op0=ALU.mult), [rk], [rk])
                p.op("pe", lambda e, exm=exm: e.transpose(psY[1][0:32, 0:128], exm, identf[:]),
                     [rk, "identf"], ["psY1"])
                p.op("act", lambda e, ts_=ts_: e.activation(out=gTt[ts_][0:32, :], in_=psY[1][0:32, 0:128],
                                                            func=AF.Copy), ["psY1"], [("gTt", ts_)])
                dma(p, "sp", c.s["gateT"][:, t * 128:(t + 1) * 128], gTt[ts_][0:32, :],
                    [("gTt", ts_)], [("d_gateT", t)])
                X = rx[ts_]
                xk = ("rx", ts_)
                idxf, sv, ov, eq, tmp = X[:, 0:8], X[:, 8:40], X[:, 40:72], X[:, 72:104], X[:, 104:136]
                slotf, gkf = X[:, 136:140], X[:, 140:144]
                p.op("dve", lambda e, ts_=ts_, lg=lg, mx=mx: e.max_index(out=idxu[ts_][:], in_max=mx, in_values=lg),
                     [rk], [("idxu", ts_)])
                p.op("dve", lambda e, ts_=ts_, idxf=idxf: e.tensor_copy(idxf, idxu[ts_][:]),
                     [("idxu", ts_)], [xk])
                p.op("pe", lambda e, msk=msk: e.matmul(psY[2][:, 0:NEXP], triu[:], msk, start=True, stop=False),
                     [rk, "triu"], ["psY2"])
                p.op("pe", lambda e: e.matmul(psY[2][:, 0:NEXP], ones128[:], msum[:], start=False, stop=True),
                     ["msum", "ones128"], ["psY2"])
                p.op("dve", lambda e, ov=ov: e.tensor_scalar(out=ov, in0=psY[2][:, 0:NEXP], scalar1=float(CAP),
                                                             scalar2=1.0e6, op0=ALU.is_ge, op1=ALU.mult),
                     ["psY2"], [xk])
                p.op("dve", lambda e, sv=sv: e.tensor_tensor(out=sv, in0=psY[2][:, 0:NEXP], in1=iota_cap[:],
                                                             op=ALU.add), ["psY2", "iota_cap"], [xk])
                p.op("dve", lambda e, sv=sv, ov=ov: e.tensor_tensor(out=sv, in0=sv, in1=ov, op=ALU.add), [xk], [xk])
                p.op("pool", lambda e, msk=msk: e.tensor_tensor(out=msum[:], in0=msum[:], in1=msk, op=ALU.add),
                     [rk, "msum"], ["msum"])
                for k4 in range(4):
                    p.op("dve", lambda e, eq=eq, idxf=idxf, k4=k4: e.tensor_scalar(
                        out=eq, in0=iota_e[:], scalar1=idxf[:, k4:k4 + 1], scalar2=None, op0=ALU.is_equal),
                        [xk, "iota_e"], [xk])
                    p.op("dve", lambda e, eq=eq, sv=sv, tmp=tmp: e.tensor_tensor(out=tmp, in0=eq, in1=sv, op=ALU.mult),
                         [xk], [xk])
                    p.op("dve", lambda e, tmp=tmp, slotf=slotf, k4=k4: e.reduce_sum(
                        out=slotf[:, k4:k4 + 1], in_=tmp, axis=AX.X), [xk], [xk])
                    p.op("dve", lambda e, eq=eq, exm=exm, tmp=tmp: e.tensor_tensor(out=tmp, in0=eq, in1=exm,
                                                                                   op=ALU.mult), [xk, rk], [xk])
                    p.op("dve", lambda e, tmp=tmp, gkf=gkf, k4=k4: e.reduce_sum(
                        out=gkf[:, k4:k4 + 1], in_=tmp, axis=AX.X), [xk], [xk])
                p.op("dve", lambda e, ts_=ts_, slotf=slotf: e.tensor_copy(slotu[ts_][:], slotf),
                     [xk], [("slotu", ts_)])
                for k4 in range(4):
                    p.op("pool", lambda e, ts_=ts_, k4=k4: e.indirect_dma_start(
                        out=c.s["xg"][:, :], out_offset=bass.IndirectOffsetOnAxis(ap=slotu[ts_][:, k4:k4 + 1], axis=0),
                        in_=x1b[ts_][:], in_offset=None, bounds_check=c.breg, oob_is_err=False),
                        [("slotu", ts_), ("x1b", ts_)], [("d_xg", t, k4)], dma=True)
                dma(p, "sp", c.s["slots"][t * 128:(t + 1) * 128, :], slotu[ts_][:], [("slotu", ts_)], [("d_slots", t)])
                dma(p, "sp", c.s["gks"][t * 128:(t + 1) * 128, :], gkf, [xk], [("d_gks", t)])
        p.barrier()
        p.emit()


SIG_MAX = float(1.0 / (1.0 + np.exp(-1.702 * 7.0)))


def phase_moe(c, L):
    nc, p = c.nc, c.p
    with ExitStack() as ph:
        c.ph = ph
        wgu = [_sb(c, "wgu%d" % i, [128, 8, 2 * DEXP], BF16) for i in range(2)]
        wdn = [_sb(c, "wdn%d" % i, [128, 8, D], BF16) for i in range(2)]
        xT = _sb(c, "xTsb", [128, 8, 1024], BF16)
        gT = _sb(c, "gTsb", [128, 1024], F32)
        yacc = _sb(c, "yacc", [128, 8, D], F32)
        bgu = _sb(c, "bgu", [128, NEXP, 16], F32)
        bgs = _sb(c, "bgs", [128, NEXP, 8], F32)
        bdn = _sb(c, "bdn", [128, D], F32)
        identf = _sb(c, "identf", [128, 128], F32)
        sg = [_sb(c, "sg%d" % i, [128, 512], F32) for i in range(2)]
        gc = [_sb(c, "gc%d" % i, [128, 512], F32) for i in range(2)]
        uc = [_sb(c, "uc%d" % i, [128, 512], F32) for i in range(2)]
        t1 = [_sb(c, "t1%d" % i, [128, 512], F32) for i in range(2)]
        t2 = [_sb(c, "t2%d" % i, [128, 512], F32) for i in range(2)]
        aT = [_sb(c, "aT%d" % i, [128, 8, 512], BF16) for i in range(2)]
        yev = [_sb(c, "yev%d" % i, [128, 512], F32) for i in range(2)]
        psg = [_ps(c, "psg%d" % i, [128, 512], F32) for i in range(2)]
        psu = [_ps(c, "psu%d" % i, [128, 512], F32) for i in range(2)]
        psGb = _ps(c, "psGb", [128, 512], F32)
        psy = [_ps(c, "psy%d" % i, [128, 512], F32) for i in range(2)]

        dma(p, "sp", bgu[:], c.w["bgu_t"][L], [], ["bgu"])
        dma(p, "sp", bdn[0:32, :], c.w["bdn"][L], [], ["bdn"])
        dma(p, "sp", identf[:], c.w["identf"][:, :], [], ["identf"])
        p.op("dve", lambda e: e.tensor_scalar(out=bgs[:], in0=bgu[:, :, 0:8], scalar1=1.702, scalar2=None,
                                              op0=ALU.mult), ["bgu"], ["bgs"])
        x1T_v = c.s["x1T"].rearrange("(c p) t -> p c t", p=128)
        ym_v = c.s["ymoe"].rearrange("(t p) d -> p t d", p=128)
        cnt = 0
        ycnt = 0
        for sb in range(4):
            dma(p, "sp", xT[:], x1T_v[:, :, sb * 1024:(sb + 1) * 1024], [], ["xTsb"])
            dma(p, "sp", gT[0:32, :], c.s["gateT"][:, sb * 1024:(sb + 1) * 1024], [], ["gTsb"])
            for ex in range(NEXP):
                ws = ex % 2
                gu_v = c.w["w_gu"][L, ex].rearrange("(c p) n -> p c n", p=128)
                dn_v = c.w["w_dn"][L, ex].rearrange("(c p) n -> p c n", p=128)
                for ch in range(8):
                    dma(p, "pool", wgu[ws][:, ch, :], gu_v[:, ch, :], [], [("wgu", ws)])
                for ch in range(8):
                    dma(p, "pool", wdn[ws][:, ch, :], dn_v[:, ch, :], [], [("wdn", ws)])
                for tb in range(2):
                    as_ = (ex * 2 + tb) % 2
                    p.op("pe", lambda e, ex=ex, tb=tb: e.matmul(
                        psGb[:, :], identf[0:32, ex:ex + 1].to_broadcast([32, 128]),
                        gT[0:32, tb * 512:(tb + 1) * 512], start=True, stop=True),
                        ["identf", "gTsb"], ["psGb"])
                    for j in range(8):
                        k = cnt % 2
                        cnt += 1
                        mm_group(p, psg[k][:, :],
                                 [(wgu[ws][:, ch, j * 128:(j + 1) * 128], xT[:, ch, tb * 512:(tb + 1) * 512])
                                  for ch in range(8)], [("wgu", ws), "xTsb"], ["psg%d" % k])
                        mm_group(p, psu[k][:, :],
                                 [(wgu[ws][:, ch, DEXP + j * 128:DEXP + (j + 1) * 128],
                                   xT[:, ch, tb * 512:(tb + 1) * 512]) for ch in range(8)],
                                 [("wgu", ws), "xTsb"], ["psu%d" % k])
                        p.op("dve", lambda e, k=k, ex=ex, j=j: e.tensor_scalar(
                            out=gc[k][:], in0=psg[k][:, :], scalar1=bgu[:, ex, j:j + 1], scalar2=7.0,
                            op0=ALU.add, op1=ALU.min), ["psg%d" % k, "bgu"], [("gc", k)])
                        p.op("act", lambda e, k=k: e.activation(
                            out=sg[k][:], in_=gc[k][:], func=AF.Sigmoid, scale=1.702),
                            [("gc", k)], [("sg", k)])
                        p.op("dve", lambda e, k=k, ex=ex, j=j: e.tensor_scalar(
                            out=uc[k][:], in0=psu[k][:, :], scalar1=bgu[:, ex, 8 + j:9 + j], scalar2=7.0,
                            op0=ALU.add, op1=ALU.min), ["psu%d" % k, "bgu"], [("uc", k)])
                        p.op("dve", lambda e, k=k: e.tensor_scalar(
                            out=uc[k][:], in0=uc[k][:], scalar1=-7.0, scalar2=1.0,
                            op0=ALU.max, op1=ALU.add), [("uc", k)], [("uc", k)])
                        p.op("pool", lambda e, k=k: e.tensor_tensor(
                            out=t1[k][:], in0=sg[k][:], in1=gc[k][:], op=ALU.mult),
                            [("sg", k), ("gc", k)], [("t1", k)])
                        p.op("dve", lambda e, k=k: e.tensor_tensor(out=t2[k][:], in0=uc[k][:], in1=psGb[:, :],
                                                                   op=ALU.mult), [("uc", k), "psGb"], [("t2", k)])
                        p.op("pool", lambda e, k=k, j=j, as_=as_: e.tensor_tensor(
                            out=aT[as_][:, j, :], in0=t1[k][:], in1=t2[k][:], op=ALU.mult),
                            [("t1", k), ("t2", k)], [("aT", as_)])
                    for tt in range(4):
                        tile = tb * 4 + tt
                        for half in range(2):
                            yk = ycnt % 2
                            ycnt += 1
                            pairs = [(aT[as_][:, j, tt * 128:(tt + 1) * 128],
                                      wdn[ws][:, j, half * 512:(half + 1) * 512]) for j in range(8)]
                            rds = [("aT", as_), ("wdn", ws)]
                            if ex == 0:
                                pairs.append((gT[0:32, tile * 128:(tile + 1) * 128],
                                              bdn[0:32, half * 512:(half + 1) * 512]))
                                rds += ["gTsb", "bdn"]
                            mm_group(p, psy[yk][:, :], pairs, rds, ["psy%d" % yk])
                            ya = yacc[:, tile, half * 512:(half + 1) * 512]
                            if ex == 0:
                                p.op("act", lambda e, ya=ya, yk=yk: e.activation(out=ya, in_=psy[yk][:, :],
                                                                                 func=AF.Copy),
                                     ["psy%d" % yk], [("yacc", tile, half)])
                            else:
                                p.op("act", lambda e, yk=yk: e.activation(out=yev[yk][:], in_=psy[yk][:, :],
                                                                          func=AF.Copy),
                                     ["psy%d" % yk], [("yev", yk)])
                                p.op("pool", lambda e, ya=ya, yk=yk: e.tensor_tensor(out=ya, in0=ya, in1=yev[yk][:],
                                                                                     op=ALU.add),
                                     [("yev", yk), ("yacc", tile, half)], [("yacc", tile, half)])
            dma(p, "sp", ym_v[:, sb * 8:(sb + 1) * 8, :], yacc[:],
                [("yacc", t_, h_) for t_ in range(8) for h_ in range(2)], [("d_ymoe", sb)])
        p.barrier()
        p.emit()


def load_w_cast(c, dst_ap, src_ap, stg, skey, dkey, eng="pool"):
    p = c.p
    dma(p, "sp", stg, src_ap, [], [skey])
    if eng == "act":
        p.op("act", lambda e: e.activation(out=dst_ap, in_=stg, func=AF.Copy), [skey], [dkey])
    else:
        p.op(eng, lambda e: e.tensor_copy(dst_ap, stg), [skey], [dkey])


def phase_moe_sparse(c, L):
    nc, p = c.nc, c.p
    NT = CAP // 128
    groups = [(0, 512), (512, CAP)] if CAP > 512 else [(0, CAP)]
    with ExitStack() as ph:
        c.ph = ph
        wgu = [_sb(c, "wgu%d" % i, [128, 8, 2 * DEXP], BF16) for i in range(2)]
        wdn = [_sb(c, "wdn%d" % i, [128, 8, D], BF16) for i in range(2)]
        stg = [_sb(c, "stg%d" % i, [128, 2 * DEXP], F32) for i in range(3)]
        xgt = [_sb(c, "xgt%d" % i, [128, D], BF16) for i in range(NT)]
        xgT = [_sb(c, "xgT%d" % i, [128, 8, CAP], BF16) for i in range(2)]
        bgu = _sb(c, "bgu", [128, NEXP, 16], F32)
        bdr1 = _sb(c, "bdr", [128, D], F32)
        bdr = [bdr1, bdr1]
        bdb = [_sb(c, "bdb%d" % i, [128, D], BF16) for i in range(2)]
        ones1 = _sb(c, "ones1", [128, 128], BF16)
        identb = _sb(c, "identb", [128, 128], BF16)
        sg = [_sb(c, "sg%d" % i, [128, 512], F32) for i in range(2)]
        gc = [_sb(c, "gc%d" % i, [128, 512], F32) for i in range(2)]
        uc = [_sb(c, "uc%d" % i, [128, 512], F32) for i in range(2)]
        t1 = [_sb(c, "t1%d" % i, [128, 512], F32) for i in range(2)]
        aT = _sb(c, "aT", [128, 8, CAP], BF16)
        yout = [_sb(c, "yout%d" % i, [128, D], F32) for i in range(2)]
        psT2 = [_ps(c, "psT%d" % i, [128, 8, 128], BF16) for i in range(2)]
        psg = [_ps(c, "psg%d" % i, [128, 512], F32) for i in range(2)]
        psu = [_ps(c, "psu%d" % i, [128, 512], F32) for i in range(2)]
        psy = [_ps(c, "psy%d" % i, [128, 512], F32) for i in range(2)]
        dma(p, "sp", bgu[:], c.w["bgu_t"][L], [], ["bgu"])
        dma(p, "sp", identb[:], c.w["identb"][:, :], [], ["identb"])
        p.op("pool", lambda e: e.memset(ones1[:], 1.0), [], ["ones1"])
        st8 = {"scnt": 0, "cnt": 0, "ycnt": 0, "xcnt": 0}
        cast_rr = ("act", "dve", "act")

        def wload_steps(ex):
            ws = ex % 2
            gu_v = c.w["w_gu"][L, ex].rearrange("(c p) n -> p c n", p=128)
            dn_v = c.w["w_dn"][L, ex].rearrange("(c p) n -> p c n", p=128)
            steps = []
            for ch in range(12):
                si = st8["scnt"] % 3
                eng = cast_rr[st8["scnt"] % 3]
                st8["scnt"] += 1
                if ch < 8:
                    src, stv, dstv, dkey = gu_v[:, ch, :], stg[si][:], wgu[ws][:, ch, :], ("wgu", ws)
                else:
                    c2 = ch - 8
                    src = dn_v[:, 2 * c2:2 * c2 + 2, :]
                    stv = stg[si][:].rearrange("p (c n) -> p c n", c=2)
                    dstv, dkey = wdn[ws][:, 2 * c2:2 * c2 + 2, :], ("wdn", ws)

                def f_dma(src=src, stv=stv, si=si):
                    dma(p, "sp", stv, src, [], [("stg", si)])

                def f_cast(stv=stv, dstv=dstv, si=si, dkey=dkey, eng=eng):
                    if eng == "act":
                        p.op("act", lambda e: e.activation(out=dstv, in_=stv, func=AF.Copy), [("stg", si)], [dkey])
                    else:
                        p.op(eng, lambda e: e.tensor_copy(dstv, stv), [("stg", si)], [dkey])
                steps.append((f_dma, f_cast))
            steps.append((lambda ws=ws, ex=ex: dma(p, "sp", bdr[ws][0:1, :], c.w["bdn"][L, ex:ex + 1, :], [],
                                                   ["bdr"]),
                          lambda ws=ws: p.op("pool", lambda e: e.tensor_copy(bdb[ws][0:1, :], bdr[ws][0:1, :]),
                                             ["bdr"], [("bdb", ws)])))
            return steps

        def xg_dma(ex):
            for st in range(NT):
                r0 = ex * CAP + st * 128
                dma(p, "sp", xgt[st][:], c.s["xg"][r0:r0 + 128, :], [], [("xgt", st)])

        def xg_load(ex):
            xb_ = ex % 2
            for st in range(NT):
                pt = st8["xcnt"] % 2
                st8["xcnt"] += 1
                tr_group(p, [(psT2[pt][:, ch, :], xgt[st][:, ch * 128:(ch + 1) * 128]) for ch in range(8)],
                         identb[:], [("xgt", st), "identb"], ["psT%d" % pt])
                eng = "act" if st % 2 == 0 else "dve"
                if eng == "act":
                    p.op("act", lambda e, st=st, xb_=xb_, pt=pt: e.activation(
                        out=xgT[xb_][:, :, st * 128:(st + 1) * 128], in_=psT2[pt][:], func=AF.Copy),
                        ["psT%d" % pt], [("xgT", xb_)])
                else:
                    p.op("dve", lambda e, st=st, xb_=xb_, pt=pt: e.tensor_copy(
                        xgT[xb_][:, :, st * 128:(st + 1) * 128], psT2[pt][:]), ["psT%d" % pt], [("xgT", xb_)])

        pend_cast = None
        for f_dma, f_cast in wload_steps(0):
            f_dma()
            if pend_cast is not None:
                pend_cast()
            pend_cast = f_cast
        pend_cast()
        xg_dma(0)
        xg_load(0)
        for ex in range(NEXP):
            ws = ex % 2
            xb_ = ex % 2
            nxt_steps = wload_steps(ex + 1) if ex + 1 < NEXP else []
            pend_cast = None
            if ex + 1 < NEXP:
                xg_dma(ex + 1)
            for (n0, n1) in groups:
                w = n1 - n0
                for j in range(8):
                    k = st8["cnt"] % 2
                    st8["cnt"] += 1
                    if nxt_steps:
                        f_dma, f_cast = nxt_steps.pop(0)
                        f_dma()
                        if pend_cast is not None:
                            pend_cast()
                        pend_cast = f_cast
                    mm_group(p, psg[k][:, 0:w], [(wgu[ws][:, ch, j * 128:(j + 1) * 128], xgT[xb_][:, ch, n0:n1])
                                                 for ch in range(8)], [("wgu", ws), ("xgT", xb_)], ["psg%d" % k])
                    mm_group(p, psu[k][:, 0:w], [(wgu[ws][:, ch, DEXP + j * 128:DEXP + (j + 1) * 128],
                                                  xgT[xb_][:, ch, n0:n1]) for ch in range(8)],
                             [("wgu", ws), ("xgT", xb_)], ["psu%d" % k])
                    p.op("dve", lambda e, k=k, ex=ex, j=j, w=w: e.tensor_scalar(
                        out=gc[k][:, 0:w], in0=psg[k][:, 0:w], scalar1=bgu[:, ex, j:j + 1], scalar2=7.0,
                        op0=ALU.add, op1=ALU.min), ["psg%d" % k, "bgu"], [("gc", k)])
                    p.op("act", lambda e, k=k, w=w: e.activation(out=sg[k][:, 0:w], in_=gc[k][:, 0:w],
                                                                 func=AF.Sigmoid, scale=1.702),
                         [("gc", k)], [("sg", k)])
                    p.op("dve", lambda e, k=k, ex=ex, j=j, w=w: e.tensor_scalar(
                        out=uc[k][:, 0:w], in0=psu[k][:, 0:w], scalar1=bgu[:, ex, 8 + j:9 + j], scalar2=7.0,
                        op0=ALU.add, op1=ALU.min), ["psu%d" % k, "bgu"], [("uc", k)])
                    p.op("dve", lambda e, k=k, w=w: e.tensor_scalar(
                        out=uc[k][:, 0:w], in0=uc[k][:, 0:w], scalar1=-7.0, scalar2=1.0,
                        op0=ALU.max, op1=ALU.add), [("uc", k)], [("uc", k)])
                    p.op("pool", lambda e, k=k, w=w: e.tensor_tensor(out=t1[k][:, 0:w], in0=sg[k][:, 0:w],
                                                                     in1=gc[k][:, 0:w], op=ALU.mult),
                         [("sg", k), ("gc", k)], [("t1", k)])
                    p.op("pool", lambda e, k=k, j=j, n0=n0, n1=n1, w=w: e.tensor_tensor(
                        out=aT[:, j, n0:n1], in0=t1[k][:, 0:w], in1=uc[k][:, 0:w], op=ALU.mult),
                        [("t1", k), ("uc", k)], ["aT"])
            while nxt_steps:
                f_dma, f_cast = nxt_steps.pop(0)
                f_dma()
                if pend_cast is not None:
                    pend_cast()
                pend_cast = f_cast
            if pend_cast is not None:
                pend_cast()
            if ex + 1 < NEXP:
                xg_load(ex + 1)
            for st in range(NT):
                ys = st8["ycnt"] % 2
                st8["ycnt"] += 1
                for half in range(2):
                    yk = (st8["ycnt"] + half) % 2
                    pairs = [(aT[:, j, st * 128:(st + 1) * 128], wdn[ws][:, j, half * 512:(half + 1) * 512])
                             for j in range(8)]
                    pairs.append((ones1[0:1, 0:128], bdb[ws][0:1, half * 512:(half + 1) * 512]))
                    mm_group(p, psy[yk][:, :], pairs, ["aT", ("wdn", ws), ("bdb", ws), "ones1"], ["psy%d" % yk])
                    p.op("act", lambda e, ys=ys, yk=yk, half=half: e.activation(
                        out=yout[ys][:, half * 512:(half + 1) * 512], in_=psy[yk][:, :], func=AF.Copy),
                        ["psy%d" % yk], [("yout", ys)])
                r0 = ex * CAP + st * 128
                dma(p, "sp", c.s["yg"][r0:r0 + 128, :], yout[ys][:], [("yout", ys)], [("d_yg", ex, st)])
        p.barrier()
        p.emit()


def phase_ln2(c, L, dst, sparse=True):
    nc, p = c.nc, c.p
    with ExitStack() as ph:
        c.ph = ph
        lng = _sb(c, "lng2", [128, D], F32)
        lnb = _sb(c, "lnb2", [128, D], F32)
        xa = [_sb(c, "xa%d" % i, [128, D], F32) for i in range(2)]
        ya = [_sb(c, "ya%d" % i, [128, D], F32) for i in range(2)]
        yk_ = [[_sb(c, "yk%d_%d" % (i, k), [128, D], F32) for k in range(4)] for i in range(2)]
        sl = [_sb(c, "sl%d" % i, [128, 4], I32) for i in range(2)]
        gk = [_sb(c, "gk%d" % i, [128, 4], F32) for i in range(2)]
        tsb = [_sb(c, "tsb%d" % i, [128, D], F32) for i in range(2)]
        out = [_sb(c, "out%d" % i, [128, D], F32) for i in range(2)]
        junk = _sb(c, "junk2", [128, D], F32)
        sm = [_sb(c, "sm%d" % i, [128, 8], F32) for i in range(2)]
        dma(p, "sp", lng[:], c.w["ln2g"][L], [], ["lngb"])
        dma(p, "sp", lnb[:], c.w["ln2b"][L], [], ["lngb"])
        for t in range(32):
            s = t % 2
            dma(p, "sp", xa[s][:], c.s["x1"][t * 128:(t + 1) * 128, :], [], [("xa", s)])
            if sparse:
                dma(p, "sp", sl[s][:], c.s["slots"][t * 128:(t + 1) * 128, :], [], [("sl", s)])
                dma(p, "sp", gk[s][:], c.s["gks"][t * 128:(t + 1) * 128, :], [], [("gk", s)])
                for k in range(4):
                    p.op("pool", lambda e, s=s, k=k: e.indirect_dma_start(
                        out=yk_[s][k][:], out_offset=None, in_=c.s["yg"][:, :],
                        in_offset=bass.IndirectOffsetOnAxis(ap=sl[s][:, k:k + 1], axis=0),
                        bounds_check=c.breg, oob_is_err=False), [("sl", s)], [("yk", s, k)], dma=True)
                p.op("dve", lambda e, s=s: e.tensor_scalar(out=ya[s][:], in0=yk_[s][0][:], scalar1=gk[s][:, 0:1],
                                                           scalar2=None, op0=ALU.mult),
                     [("yk", s, 0), ("gk", s)], [("ya", s)])
                for k in range(1, 4):
                    p.op("dve", lambda e, s=s, k=k: e.scalar_tensor_tensor(
                        out=ya[s][:], in0=yk_[s][k][:], scalar=gk[s][:, k:k + 1], in1=ya[s][:],
                        op0=ALU.mult, op1=ALU.add), [("yk", s, k), ("gk", s), ("ya", s)], [("ya", s)])
            else:
                dma(p, "sp", ya[s][:], c.s["ymoe"][t * 128:(t + 1) * 128, :], [], [("ya", s)])
            p.op("dve", lambda e, s=s: e.scalar_tensor_tensor(
                out=tsb[s][:], in0=xa[s][:], scalar=DN_ALPHA, in1=ya[s][:], op0=ALU.mult, op1=ALU.add),
                [("xa", s), ("ya", s)], [("tsb", s)])
            layer_norm_tile(c, tsb[s], ("tsb", s), junk, "junk", sm[s], ("sm", s), lng, lnb, "lngb",
                            out[s], ("out", s))
            dma(p, "sp", dst[t * 128:(t + 1) * 128, :], out[s][:], [("out", s)], [("d_out", t)])
        p.barrier()
        p.emit()


ALL_PHASES = ("p1", "na", "gqa", "mla", "merge", "moes", "ln2")
_NC_CACHE = {}


def kernel(**inputs):
    inp = {k: np.asarray(v) for k, v in inputs.items()}
    x = np.ascontiguousarray(inp["x"].astype(np.float32))
    nb = x.shape[0]
    if "nc" not in _NC_CACHE:
        _NC_CACHE["nc"] = build()
    consts = [prep_consts(hf) for hf in range(2)]
    wts = [prep_weights(inp, [0, 1], hf) for hf in range(2)]
    in_maps = []
    for cid in range(2 * nb):
        b, hf = cid // 2, cid % 2
        m = {}
        m.update(wts[hf])
        m.update(consts[hf])
        m.update(prep_acts(x[b], hf))
        in_maps.append(m)
    res = run_bass_kernel_spmd(_NC_CACHE["nc"], in_maps, core_ids=list(range(2 * nb)))
    return np.stack([np.concatenate([np.asarray(res.results[2 * b]["y"]),
                                     np.asarray(res.results[2 * b + 1]["y"])], 0)
                     for b in range(nb)], 0).astype(np.float32)
```
